# Optimizing a Trainium2 kernel written in Bass

```python
import jax
import jax.numpy as jnp
from jax import lax
import numpy as np

D_MODEL = 1024
BATCH = 32
SEQ = 2048
DEPTH = 2

GRID_W = 64
CTX_LEN = 256

MIX_DIM = D_MODEL
RW_HEAD_DIM = 64
RW_DIM = MIX_DIM // 4
RW_HEADS = RW_DIM // RW_HEAD_DIM
RW_W_LORA = 64
RW_A_LORA = 64
RW_G_LORA = 128
RW_GN_EPS = 64e-5
RW_COLS = 3 * RW_DIM + 2 * RW_W_LORA + 2 * RW_A_LORA + RW_G_LORA
MLA_DIM = MIX_DIM // 2
MLA_V_DIM = 128
MLA_HEADS = MLA_DIM // MLA_V_DIM
MLA_NOPE_DIM = 128
MLA_ROPE_DIM = 64
MLA_Q_LORA = 256
MLA_KV_LORA = 128
MLA_COLS = MLA_Q_LORA + MLA_KV_LORA + MLA_ROPE_DIM
NA_DIM = MIX_DIM - RW_DIM - MLA_DIM
NA_HEAD_DIM = 64
NA_HEADS = NA_DIM // NA_HEAD_DIM
NA_WIN_R = 8
NA_WIN_C = 16
NA_COLS = 3 * NA_DIM
IN_COLS = RW_COLS + MLA_COLS + NA_COLS

ROPE_BASE = 10000.0
Q_BLOCK = 128
N_EXPERTS = 32
TOP_K = 4
D_FF = D_MODEL
SWIGLU_ALPHA = 1.702
SWIGLU_LIMIT = 7.0
MOE_BLOCK = 512
DN_ALPHA = (2 * DEPTH) ** 0.25
DN_BETA = (8 * DEPTH) ** -0.25
NEG_INF = -1e30

kernel_name = 'hybrid_rwkv7_mla_natten_moe_dit'


def split_cols(x, sizes):
    idx = [int(i) for i in np.cumsum(sizes)[:-1]]
    return jnp.split(x, idx, axis=-1)


def layer_norm(x, g=None, b=None, eps=1e-5):
    xf = x.astype(jnp.float32)
    mu = xf.mean(-1, keepdims=True)
    var = jnp.square(xf - mu).mean(-1, keepdims=True)
    y = (xf - mu) * lax.rsqrt(var + eps)
    if g is not None:
        y = y * g + b
    return y.astype(x.dtype)


def rms_norm(x, g, eps=1e-6):
    xf = x.astype(jnp.float32)
    y = xf * lax.rsqrt(jnp.mean(xf * xf, -1, keepdims=True) + eps) * g
    return y.astype(x.dtype)


def modulate(x, shift, scale):
    return x * (1 + scale) + shift


def axial_rope(n_tokens, rot_dim):
    t = jnp.arange(n_tokens)
    row = (t // GRID_W).astype(jnp.float32)
    col = (t % GRID_W).astype(jnp.float32)
    n_freq = rot_dim // 4
    inv = ROPE_BASE ** (-jnp.arange(n_freq, dtype=jnp.float32) / n_freq)
    ang = jnp.concatenate([row[:, None] * inv, col[:, None] * inv], axis=-1)
    return jnp.cos(ang), jnp.sin(ang)


def apply_rope(x, cos, sin):
    xp = x.reshape(x.shape[:-1] + (-1, 2))
    xe, xo = xp[..., 0], xp[..., 1]
    y = jnp.stack([xe * cos - xo * sin, xe * sin + xo * cos], axis=-1)
    return y.reshape(x.shape).astype(x.dtype)


def attend(q, k, v, scale):
    s = jnp.einsum('bqhd,bkhd->bhqk', q, k, preferred_element_type=jnp.float32) * scale
    p = jax.nn.softmax(s, axis=-1).astype(v.dtype)
    o = jnp.einsum('bhqk,bkhd->bqhd', p, v)
    return o.reshape(o.shape[:2] + (-1,))


def token_shift(p, mu):
    zero = jnp.zeros_like(p[:, :1])
    prev = jnp.concatenate([zero, p[:, :-1]], axis=1)
    nxt = jnp.concatenate([p[:, 1:], zero], axis=1)
    return p + mu[0] * (prev - p) + mu[1] * (nxt - p)


def rwkv_features(p, mu, w0, w2, a0, a2, k_k, k_a):
    B, T, _ = p.shape
    r, k, v, wf, wb, af, ab, g_pre = split_cols(
        token_shift(p, mu), [RW_DIM] * 3 + [RW_W_LORA] * 2 + [RW_A_LORA] * 2 + [RW_G_LORA])
    hd = lambda z: z.reshape(B, T, RW_HEADS, RW_HEAD_DIM)
    kk = hd(k * k_k).astype(jnp.float32)
    kk = kk * lax.rsqrt(jnp.sum(kk * kk, -1, keepdims=True) + 1e-12)
    dirs = []
    for d, (w_lo, a_lo) in enumerate(((wf, af), (wb, ab))):
        logw = -jax.nn.softplus(-(w0[d] + jnp.tanh(w_lo) @ w2[d]).astype(jnp.float32)) - 0.5
        decay = jnp.exp(-jnp.exp(logw))
        a = jax.nn.sigmoid((a0[d] + a_lo @ a2[d]).astype(jnp.float32))
        k_d = k * (1 + (a - 1) * k_a)
        dirs.append((hd(decay), hd(k_d), hd(a) * kk))
    return hd(r), hd(k), hd(v), g_pre, kk, dirs


def rwkv_scan(S0, decay, k, v, kk, b, r=None, reverse=False):
    emit = r is not None
    tm = lambda z: jnp.moveaxis(z.astype(jnp.float32), 1, 0)
    xs = (tm(decay), tm(k), tm(v), tm(kk), tm(b)) + ((tm(r),) if emit else ())

    def step(S, inp):
        w_t, k_t, v_t, kk_t, b_t = inp[:5]
        s_a = jnp.einsum('bhvk,bhk->bhv', S, kk_t)
        S = S * w_t[:, :, None, :] - s_a[..., None] * b_t[:, :, None, :] + v_t[..., None] * k_t[:, :, None, :]
        o = jnp.einsum('bhvk,bhk->bhv', S, inp[5]) if emit else None
        return S, o

    S, o = lax.scan(step, S0, xs, reverse=reverse)
    return S, (jnp.moveaxis(o, 0, 1) if emit else None)


def rwkv_output(o, r, k, v, g_pre, g2, r_k, gn_w, gn_b):
    B, T = o.shape[:2]
    mu = o.mean(-1, keepdims=True)
    var = jnp.square(o - mu).mean(-1, keepdims=True)
    on = ((o - mu) * lax.rsqrt(var + RW_GN_EPS)).reshape(B, T, RW_DIM) * gn_w + gn_b
    bonus = jnp.sum(r * k * r_k, -1, keepdims=True) * v
    g = jax.nn.sigmoid(g_pre) @ g2
    return ((on + bonus.reshape(B, T, RW_DIM)) * g).astype(r.dtype)


def rwkv_mixer(p_ctx, p_lat, mu, w0, w2, a0, a2, g2, k_k, k_a, r_k, gn_w, gn_b, need_ctx):
    r_c, k_c, v_c, g_c, kk_c, dirs_c = rwkv_features(p_ctx, mu, w0, w2, a0, a2, k_k, k_a)
    r_l, k_l, v_l, g_l, kk_l, dirs_l = rwkv_features(p_lat, mu, w0, w2, a0, a2, k_k, k_a)
    S0 = jnp.zeros((p_lat.shape[0], RW_HEADS, RW_HEAD_DIM, RW_HEAD_DIM), jnp.float32)
    o_lat, o_ctx = 0.0, 0.0
    for (dec_c, kd_c, b_c), (dec_l, kd_l, b_l), rev in zip(dirs_c, dirs_l, (False, True)):
        S_c, oc = rwkv_scan(S0, dec_c, kd_c, v_c, kk_c, b_c, r_c if need_ctx else None, rev)
        _, ol = rwkv_scan(S_c, dec_l, kd_l, v_l, kk_l, b_l, r_l, rev)
        o_lat = o_lat + ol
        if need_ctx:
            o_ctx = o_ctx + oc
    y_lat = rwkv_output(o_lat, r_l, k_l, v_l, g_l, g2, r_k, gn_w, gn_b)
    y_ctx = rwkv_output(o_ctx, r_c, k_c, v_c, g_c, g2, r_k, gn_w, gn_b) if need_ctx else None
    return y_lat, y_ctx


def mla_attend(qn, qr, kn, kr, v):
    scale = (MLA_NOPE_DIM + MLA_ROPE_DIM) ** -0.5
    s = (jnp.einsum('bqhd,bkhd->bhqk', qn, kn, preferred_element_type=jnp.float32)
         + jnp.einsum('bqhd,bkd->bhqk', qr, kr, preferred_element_type=jnp.float32)) * scale
    p = jax.nn.softmax(s, axis=-1).astype(v.dtype)
    o = jnp.einsum('bhqk,bkhd->bqhd', p, v)
    return o.reshape(o.shape[:2] + (-1,))


def mla_mixer(p_ctx, p_lat, q_norm, kv_norm, w_uq, w_ukv, cos, sin, need_ctx):
    def queries(p):
        q = (rms_norm(p[..., :MLA_Q_LORA], q_norm) @ w_uq).reshape(
            p.shape[:2] + (MLA_HEADS, MLA_NOPE_DIM + MLA_ROPE_DIM))
        return q[..., :MLA_NOPE_DIM], q[..., MLA_NOPE_DIM:]

    def keys_values(p):
        kv_c = p[..., MLA_Q_LORA:MLA_Q_LORA + MLA_KV_LORA]
        k_rope = p[..., MLA_Q_LORA + MLA_KV_LORA:]
        kv = (rms_norm(kv_c, kv_norm) @ w_ukv).reshape(p.shape[:2] + (MLA_HEADS, MLA_NOPE_DIM + MLA_V_DIM))
        return kv[..., :MLA_NOPE_DIM], k_rope, kv[..., MLA_NOPE_DIM:]

    B, T, _ = p_lat.shape
    qn_l, qr_l = queries(p_lat)
    qr_l = apply_rope(qr_l, cos[:, None], sin[:, None])
    kn_l, kr_l, v_l = keys_values(p_lat)
    kr_l = apply_rope(kr_l, cos, sin)
    kn_c, kr_c, v_c = keys_values(p_ctx)
    kn = jnp.concatenate([kn_l, kn_c], axis=1)
    kr = jnp.concatenate([kr_l, kr_c], axis=1)
    v = jnp.concatenate([v_l, v_c], axis=1)
    nb = T // Q_BLOCK
    blocks = lambda z: jnp.moveaxis(z.reshape((B, nb, Q_BLOCK) + z.shape[2:]), 1, 0)
    o = lax.map(lambda qs: mla_attend(qs[0], qs[1], kn, kr, v), (blocks(qn_l), blocks(qr_l)))
    y_lat = jnp.moveaxis(o, 0, 1).reshape(B, T, MLA_DIM)
    y_ctx = None
    if need_ctx:
        qn_c, qr_c = queries(p_ctx)
        y_ctx = mla_attend(qn_c, qr_c, kn_c, kr_c, v_c)
    return y_lat, y_ctx


def na_mixer(p_ctx, p_lat, rpb, need_ctx):
    B, T, _ = p_lat.shape
    rows = T // GRID_W
    kr = min(NA_WIN_R, rows)
    heads = lambda z: z.reshape(z.shape[:2] + (NA_HEADS, NA_HEAD_DIM))
    q_l, k_l, v_l = (heads(z) for z in split_cols(p_lat, [NA_DIM] * 3))
    k_c = heads(p_ctx[..., NA_DIM:2 * NA_DIM])
    v_c = heads(p_ctx[..., 2 * NA_DIM:])
    scale = NA_HEAD_DIM ** -0.5
    grid = lambda z: z.reshape(B, rows, GRID_W, NA_HEADS, NA_HEAD_DIM)
    qg, kg, vg = grid(q_l), grid(k_l), grid(v_l)
    col = jnp.arange(GRID_W)
    c_start = jnp.clip(col - NA_WIN_C // 2, 0, GRID_W - NA_WIN_C)
    in_win = (col[None, :] >= c_start[:, None]) & (col[None, :] < c_start[:, None] + NA_WIN_C)
    dc_idx = jnp.clip(col[None, :] - col[:, None] + NA_WIN_C - 1, 0, 2 * NA_WIN_C - 2)
    n_loc = kr * GRID_W

    def row_block(i):
        r_start = jnp.clip(i - kr // 2, 0, rows - kr)
        q_i = lax.dynamic_index_in_dim(qg, i, axis=1, keepdims=False)
        k_b = lax.dynamic_slice_in_dim(kg, r_start, kr, axis=1)
        v_b = lax.dynamic_slice_in_dim(vg, r_start, kr, axis=1)
        s_loc = jnp.einsum('bqhd,brkhd->bhqrk', q_i, k_b, preferred_element_type=jnp.float32) * scale
        dr_idx = r_start + jnp.arange(kr) - i + NA_WIN_R - 1
        bias = jnp.transpose(rpb[:, dr_idx][:, :, dc_idx], (0, 2, 1, 3))
        s_loc = jnp.where(in_win[:, None, :], s_loc + bias, NEG_INF)
        s_ctx = jnp.einsum('bqhd,bkhd->bhqk', q_i, k_c, preferred_element_type=jnp.float32) * scale
        s = jnp.concatenate([s_loc.reshape(B, NA_HEADS, GRID_W, n_loc), s_ctx], axis=-1)
        p = jax.nn.softmax(s, axis=-1).astype(v_c.dtype)
        p_loc = p[..., :n_loc].reshape(B, NA_HEADS, GRID_W, kr, GRID_W)
        return (jnp.einsum('bhqrk,brkhd->bqhd', p_loc, v_b)
                + jnp.einsum('bhqk,bkhd->bqhd', p[..., n_loc:], v_c))

    o = lax.map(row_block, jnp.arange(rows))
    y_lat = jnp.moveaxis(o, 0, 1).reshape(B, T, NA_DIM)
    y_ctx = attend(heads(p_ctx[..., :NA_DIM]), k_c, v_c, scale) if need_ctx else None
    return y_lat, y_ctx


def clamped_swiglu(gu):
    x_glu = jnp.minimum(gu[..., ::2], SWIGLU_LIMIT)
    x_lin = jnp.clip(gu[..., 1::2], -SWIGLU_LIMIT, SWIGLU_LIMIT)
    return x_glu * jax.nn.sigmoid(SWIGLU_ALPHA * x_glu) * (x_lin + 1)


def moe_ffn(h, router_w, router_b, w_gu, b_gu, w_dn, b_dn):
    N, D = h.shape
    logits = (h @ router_w).astype(jnp.float32) + router_b
    top_v, top_i = lax.top_k(logits, TOP_K)
    gates = jax.nn.softmax(top_v, axis=-1)
    n_assign = N * TOP_K
    flat_e = top_i.reshape(-1)
    flat_tok = (jnp.arange(n_assign) // TOP_K).astype(jnp.int32)
    order = jnp.argsort(flat_e)
    e_sorted = flat_e[order]
    counts = jnp.bincount(flat_e, length=N_EXPERTS)
    padded = (counts + MOE_BLOCK - 1) // MOE_BLOCK * MOE_BLOCK
    start = jnp.cumsum(counts) - counts
    ends_p = jnp.cumsum(padded)
    dest = (ends_p - padded)[e_sorted] + jnp.arange(n_assign) - start[e_sorted]
    n_blocks = -(-(n_assign + N_EXPERTS * (MOE_BLOCK - 1)) // MOE_BLOCK)
    n_pad = n_blocks * MOE_BLOCK
    tok_pad = jnp.full((n_pad,), N, jnp.int32).at[dest].set(flat_tok[order])
    gate_pad = jnp.zeros((n_pad,), gates.dtype).at[dest].set(gates.reshape(-1)[order])
    block_e = jnp.minimum(jnp.searchsorted(ends_p, jnp.arange(n_blocks) * MOE_BLOCK, side='right'),
                          N_EXPERTS - 1)
    h_ext = jnp.concatenate([h, jnp.zeros((1, D), h.dtype)], axis=0)

    def step(acc, blk):
        e, idx, g = blk
        gu = h_ext[idx] @ w_gu[e] + b_gu[e]
        y = clamped_swiglu(gu) @ w_dn[e] + b_dn[e]
        return acc.at[idx].add((y * g[:, None]).astype(acc.dtype)), None

    acc, _ = lax.scan(step, jnp.zeros((N + 1, D), h.dtype),
                      (block_e, tok_pad.reshape(n_blocks, MOE_BLOCK), gate_pad.reshape(n_blocks, MOE_BLOCK)))
    return acc[:N]


def setup_inputs(seed: int = 0) -> dict:
    key = jax.random.key(seed)
    ks = jax.random.split(key, 34)
    L, D = DEPTH, D_MODEL
    nrm = lambda i, shape, s: s * jax.random.normal(ks[i], shape, jnp.float32)
    return {
        'x': nrm(0, (BATCH, SEQ, D), 1.0),
        'c': nrm(1, (BATCH, D), 1.0),
        'ctx': nrm(2, (BATCH, CTX_LEN, D), 1.0),
        'c_ctx': nrm(3, (D,), 1.0),
        'ada_w': nrm(4, (L, D, 6 * D), 0.5 * D ** -0.5),
        'ada_b': nrm(5, (L, 6 * D), 0.02),
        'w_in': nrm(6, (L, D, IN_COLS), D ** -0.5),
        'rw_mu': 0.25 + nrm(7, (L, 2, RW_COLS), 0.1),
        'rw_w0': jax.random.uniform(ks[8], (L, 2, RW_DIM), jnp.float32, -5.0, -1.0),
        'rw_w2': nrm(9, (L, 2, RW_W_LORA, RW_DIM), 0.5 * RW_W_LORA ** -0.5),
        'rw_a0': nrm(10, (L, 2, RW_DIM), 0.1),
        'rw_a2': nrm(11, (L, 2, RW_A_LORA, RW_DIM), 0.5 * RW_A_LORA ** -0.5),
        'rw_g2': nrm(12, (L, RW_G_LORA, RW_DIM), RW_G_LORA ** -0.5),
        'rw_kk': 0.85 + nrm(13, (L, RW_DIM), 0.02),
        'rw_ka': 1.0 + nrm(14, (L, RW_DIM), 0.02),
        'rw_rk': nrm(15, (L, RW_HEADS, RW_HEAD_DIM), 0.1),
        'rw_gn_w': 1.0 + nrm(16, (L, RW_DIM), 0.02),
        'rw_gn_b': nrm(17, (L, RW_DIM), 0.02),
        'mla_q_norm': 1.0 + nrm(18, (L, MLA_Q_LORA), 0.02),
        'mla_kv_norm': 1.0 + nrm(19, (L, MLA_KV_LORA), 0.02),
        'mla_w_uq': nrm(20, (L, MLA_Q_LORA, MLA_HEADS * (MLA_NOPE_DIM + MLA_ROPE_DIM)), MLA_Q_LORA ** -0.5),
        'mla_w_ukv': nrm(21, (L, MLA_KV_LORA, MLA_HEADS * (MLA_NOPE_DIM + MLA_V_DIM)), MLA_KV_LORA ** -0.5),
        'na_rpb': nrm(22, (L, NA_HEADS, 2 * NA_WIN_R - 1, 2 * NA_WIN_C - 1), 0.1),
        'w_out': nrm(23, (L, MIX_DIM, D), DN_BETA * MIX_DIM ** -0.5),
        'ln1_g': 1.0 + nrm(24, (L, D), 0.02),
        'ln1_b': nrm(25, (L, D), 0.02),
        'router_w': nrm(26, (L, D, N_EXPERTS), D ** -0.5),
        'router_b': nrm(27, (L, N_EXPERTS), 0.01),
        'w_gu': nrm(28, (L, N_EXPERTS, D, 2 * D_FF), D ** -0.5),
        'b_gu': nrm(29, (L, N_EXPERTS, 2 * D_FF), 0.02),
        'w_dn': nrm(30, (L, N_EXPERTS, D_FF, D), DN_BETA * D_FF ** -0.5),
        'b_dn': nrm(31, (L, N_EXPERTS, D), 0.02),
        'ln2_g': 1.0 + nrm(32, (L, D), 0.02),
        'ln2_b': nrm(33, (L, D), 0.02),
    }


def reference(x, c, ctx, c_ctx, ada_w, ada_b, w_in, rw_mu, rw_w0, rw_w2, rw_a0, rw_a2, rw_g2,
              rw_kk, rw_ka, rw_rk, rw_gn_w, rw_gn_b, mla_q_norm, mla_kv_norm, mla_w_uq, mla_w_ukv,
              na_rpb, w_out, ln1_g, ln1_b, router_w, router_b, w_gu, b_gu, w_dn, b_dn, ln2_g, ln2_b):
    B, T, D = x.shape
    n_ctx = ctx.shape[1]
    cos, sin = axial_rope(T, MLA_ROPE_DIM)
    cond_l = jax.nn.silu(c)
    cond_c = jax.nn.silu(c_ctx)
    xl, xc = x, ctx
    for l in range(DEPTH):
        need_ctx = l < DEPTH - 1
        mod_l = cond_l @ ada_w[l] + ada_b[l]
        mod_c = cond_c @ ada_w[l] + ada_b[l]
        sa_l, ca_l, ga_l, sf_l, cf_l, gf_l = jnp.split(mod_l[:, None, :], 6, axis=-1)
        sa_c, ca_c, ga_c, sf_c, cf_c, gf_c = jnp.split(mod_c, 6, axis=-1)

        p_l = modulate(layer_norm(xl), sa_l, ca_l) @ w_in[l]
        p_c = modulate(layer_norm(xc), sa_c, ca_c) @ w_in[l]
        rw_pl, mla_pl, na_pl = split_cols(p_l, [RW_COLS, MLA_COLS, NA_COLS])
        rw_pc, mla_pc, na_pc = split_cols(p_c, [RW_COLS, MLA_COLS, NA_COLS])
        rw_yl, rw_yc = rwkv_mixer(rw_pc, rw_pl, rw_mu[l], rw_w0[l], rw_w2[l], rw_a0[l], rw_a2[l], rw_g2[l],
                                  rw_kk[l], rw_ka[l], rw_rk[l], rw_gn_w[l], rw_gn_b[l], need_ctx)
        mla_yl, mla_yc = mla_mixer(mla_pc, mla_pl, mla_q_norm[l], mla_kv_norm[l], mla_w_uq[l], mla_w_ukv[l],
                                   cos, sin, need_ctx)
        na_yl, na_yc = na_mixer(na_pc, na_pl, na_rpb[l], need_ctx)
        y_l = jnp.concatenate([rw_yl, mla_yl, na_yl], axis=-1) @ w_out[l]
        xl = layer_norm(DN_ALPHA * xl + ga_l * y_l, ln1_g[l], ln1_b[l])
        if need_ctx:
            y_c = jnp.concatenate([rw_yc, mla_yc, na_yc], axis=-1) @ w_out[l]
            xc = layer_norm(DN_ALPHA * xc + ga_c * y_c, ln1_g[l], ln1_b[l])

        h_l = modulate(layer_norm(xl), sf_l, cf_l).reshape(B * T, D)
        if need_ctx:
            h_c = modulate(layer_norm(xc), sf_c, cf_c).reshape(B * n_ctx, D)
            f = moe_ffn(jnp.concatenate([h_l, h_c], axis=0), router_w[l], router_b[l],
                        w_gu[l], b_gu[l], w_dn[l], b_dn[l])
            f_l = f[:B * T].reshape(B, T, D)
            xc = layer_norm(DN_ALPHA * xc + gf_c * f[B * T:].reshape(B, n_ctx, D), ln2_g[l], ln2_b[l])
        else:
            f_l = moe_ffn(h_l, router_w[l], router_b[l], w_gu[l], b_gu[l], w_dn[l], b_dn[l]).reshape(B, T, D)
        xl = layer_norm(DN_ALPHA * xl + gf_l * f_l, ln2_g[l], ln2_b[l])
    return xl
```

```python
import numpy as np
from contextlib import ExitStack
import concourse.bass as bass
import concourse.mybir as mybir
from concourse.bass_utils import run_bass_kernel_spmd

F32 = mybir.dt.float32; BF16 = mybir.dt.bfloat16
AF = mybir.ActivationFunctionType; ALU = mybir.AluOpType; AX = mybir.AxisListType

D = 1024; SEQ = 2048; NCTX = 256; T = SEQ + NCTX; DEPTH = 2; NTT = T // 128
RW_COLS = 1152; MLA_COLS = 448; NA_COLS = 768; IN_COLS = 2368
NE = 32
DN_ALPHA = (2 * DEPTH) ** 0.25


class Tk:
    __slots__ = ("t", "w", "r", "name", "psum")
    def __init__(s, t, name="", psum=False):
        s.t = t; s.w = None; s.r = []; s.name = name; s.psum = psum
    def __getitem__(s, idx):
        return s.t[idx]


class K:
    NDMA = 6
    def __init__(s, nc, stack):
        s.nc = nc; s.stack = stack
        s.eng = {"pe": nc.tensor, "act": nc.scalar, "dve": nc.vector, "pool": nc.gpsimd, "sp": nc.sync}
        s.sem = {}; s.cnt = {}
        for e in s.eng:
            s.sem[e] = stack.enter_context(nc.semaphore("s_" + e)); s.cnt[e] = 0
        s.seen = {e: {} for e in s.eng}
        s.dsem = [stack.enter_context(nc.semaphore("d%d" % i)) for i in range(3 * s.NDMA)]
        s.dcnt = [0] * (3 * s.NDMA); s.dnext = {"sp": 0, "act": 0, "pool": 0}; s.dq = {"sp": 0, "act": 1, "pool": 2}
        s.ninst = 0; s.uid = 0
    def sb(s, name, shape, dt, stack=None):
        s.uid += 1
        t = Tk((stack or s.stack).enter_context(s.nc.sbuf_tensor("%s_%d" % (name, s.uid), list(shape), dt)), name)
        assert s.nc.sbuf_bytes_remaining >= 28 * 1024, "SBUF budget exceeded at %s: remaining %d" % (name, s.nc.sbuf_bytes_remaining)
        return t
    def ps(s, name, shape, dt=F32, stack=None):
        s.uid += 1
        isz = 4 if dt == F32 else 2
        free = int(np.prod(shape[1:])); nb = (free * isz + 2047) // 2048
        raw = (stack or s.stack).enter_context(s.nc.psum_tensor("%s_%d" % (name, s.uid), [128, nb * 2048 // isz], dt))
        ap = raw[0:shape[0], 0:free]
        if len(shape) == 3:
            ap = ap.rearrange("p (a b) -> p a b", a=shape[1])
        return Tk(ap, name, psum=True)
    def dram(s, name, shape, dt, kind="Internal"):
        return Tk(s.nc.dram_tensor(name, list(shape), dt, kind=kind).ap(), name)
    def _wait(s, e, toks):
        seen = s.seen[e]
        for (sem, val, key) in toks:
            if seen.get(key, 0) >= val:
                continue
            s.eng[e].wait_ge(sem, val); seen[key] = val; s.ninst += 1
    def _deps(s, e, reads, writes):
        toks = []
        for t in reads:
            if t.w is not None:
                toks.append(t.w)
            if t.psum:
                toks.extend(k for k in t.r if k[2] != e)
        for t in writes:
            if t.w is not None:
                toks.append(t.w)
            toks.extend(t.r)
        if e == "pe":
            toks = [k for k in toks if k[2] != "pe"]
        return toks
    def op(s, e, fn, reads=(), writes=()):
        s._wait(e, s._deps(e, reads, writes))
        ins = fn(s.eng[e]); s.cnt[e] += 1; ins.then_inc(s.sem[e], 1); s.ninst += 1
        tok = (s.sem[e], s.cnt[e], e)
        for t in reads:
            t.r = [k for k in t.r if k[2] != e]; t.r.append(tok)
        for t in writes:
            t.w = tok; t.r = []
        return ins
    def dma(s, q, out, in_, reads=(), writes=(), **kw):
        base = s.dq[q] * s.NDMA; i = base + s.dnext[q]; s.dnext[q] = (s.dnext[q] + 1) % s.NDMA
        key = "d%d" % i
        toks = s._deps(q, reads, writes)
        if s.dcnt[i] > 0:
            toks.append((s.dsem[i], s.dcnt[i], key))
        s._wait(q, toks)
        ins = s.eng[q].dma_start(out=out, in_=in_, **kw); s.dcnt[i] += 16; ins.then_inc(s.dsem[i], 16); s.ninst += 1
        tok = (s.dsem[i], s.dcnt[i], key)
        for t in reads:
            t.r.append(tok)
        for t in writes:
            t.w = tok; t.r = []
        return ins
    def barrier(s):
        toks = [(s.sem[e], s.cnt[e], e) for e in s.eng if s.cnt[e] > 0]
        toks += [(s.dsem[i], s.dcnt[i], "d%d" % i) for i in range(len(s.dsem)) if s.dcnt[i] > 0]
        for e in s.eng:
            s._wait(e, [k for k in toks if k[2] != e])
    def finish(s, outs):
        s._wait("sp", [t.w for t in outs if t.w is not None])


def bcast_rows(ap_row, n):
    return ap_row.partition_broadcast(n)


def phase_mod(k, l, NB, condT_d, ada_w_d, ada_b_d, mod_d):
    with ExitStack() as st:
        R = NB + 1
        cond = k.sb("cond", [128, 8, R], F32, st); cond_bf = k.sb("condbf", [128, 8, R], BF16, st)
        adab = k.sb("adab", [R, 6 * D], F32, st); mod_sb = k.sb("modsb", [R, 6 * D], F32, st)
        wts = [k.sb("adaw", [128, 8, 512], BF16, st) for _ in range(2)]
        pss = [k.ps("modps", [R, 512], F32, st) for _ in range(2)]
        k.dma("sp", cond[:], condT_d[:], writes=[cond])
        k.dma("sp", adab[:], ada_b_d[l:l + 1, :].partition_broadcast(R), writes=[adab])
        k.op("act", lambda e: e.activation(cond_bf[:], cond[:], AF.Silu), reads=[cond], writes=[cond_bf])
        wv = ada_w_d.t[l].rearrange("(kc p) n -> p kc n", p=128)
        for g in range(12):
            wt = wts[g % 2]; ps = pss[g % 2]
            k.dma("pool", wt[:], wv[:, :, g * 512:(g + 1) * 512], writes=[wt])
            for kc in range(8):
                k.op("pe", lambda e: e.matmul(ps[:], lhsT=cond_bf[:, kc, :], rhs=wt[:, kc, :], start=(kc == 0), stop=(kc == 7)),
                     reads=[cond_bf, wt], writes=[ps])
            k.op("dve", lambda e: e.tensor_tensor(mod_sb[:, g * 512:(g + 1) * 512], ps[:], adab[:, g * 512:(g + 1) * 512], ALU.add),
                 reads=[ps, adab], writes=[mod_sb])
        k.dma("sp", mod_d[l], mod_sb[:], reads=[mod_sb], writes=[mod_d])
    k.barrier()


def ln_stats(k, xt, st6, mv, rstd, eps):
    for hh in range(2):
        k.op("dve", lambda e: e.bn_stats(st6[:, hh, :], xt[:, hh * 512:(hh + 1) * 512]), reads=[xt], writes=[st6])
    k.op("dve", lambda e: e.bn_aggr(mv[:], st6[:].rearrange("p a b -> p (a b)")), reads=[st6], writes=[mv])
    k.op("dve", lambda e: e.tensor_scalar(rstd[:], mv[:, 1:2], eps, None, op0=ALU.add), reads=[mv], writes=[rstd])
    k.op("act", lambda e: e.sqrt(rstd[:], rstd[:]), reads=[rstd], writes=[rstd])
    k.op("dve", lambda e: e.reciprocal(rstd[:], rstd[:]), reads=[rstd], writes=[rstd])


def phase_inproj(k, l, b, NB, x_src, x_src_tk, mod_d, w_in_bf, ident_bf, pT_rw_d, pT_na_d, p_tm_d):
    with ExitStack() as st:
        bc = {}
        for nm, row, off in (("sa_l", b, 0), ("ca_l", b, D), ("sa_c", NB, 0), ("ca_c", NB, D)):
            tle = k.sb(nm, [128, D], F32, st)
            k.dma("sp", tle[:], mod_d[l][row:row + 1, off:off + D].partition_broadcast(128), reads=[mod_d], writes=[tle])
            bc[nm] = tle
        for nm in ("ca_l", "ca_c"):
            tle = bc[nm]
            k.op("pool", lambda e: e.tensor_scalar(tle[:], tle[:], 1.0, None, op0=ALU.add), reads=[tle], writes=[tle])
        groups = [(0, 512), (512, 512), (1024, 512), (1536, 512), (2048, 256)]
        xmT = [k.sb("xmT%d" % g, [128, 8, n], BF16, st) for g, (o, n) in enumerate(groups)]
        xts = [k.sb("xt", [128, D], F32, st) for _ in range(2)]
        t1 = k.sb("t1", [128, D], F32, st); xm = k.sb("xm", [128, D], BF16, st)
        st6 = k.sb("st6", [128, 2, 6], F32, st); mv = k.sb("mv", [128, 2], F32, st); rstd = k.sb("rstd", [128, 1], F32, st)
        pTs = [k.ps("pT", [128, 8, 128], BF16, st) for _ in range(2)]
        ptm = [k.ps("ptm", [128, 512], F32, st) for _ in range(2)]
        pfm = [k.ps("pfm", [128, 512], F32, st) for _ in range(2)]
        tm_sb = [k.sb("tmsb", [128, 704], F32, st) for _ in range(2)]
        fm_sb = [k.sb("fmsb", [128, 512], F32, st) for _ in range(2)]
        fm_sbb = [k.sb("fmsbb", [128, 512], BF16, st) for _ in range(2)]
        for tt in range(NTT):
            isctx = tt < 2
            sa = bc["sa_c" if isctx else "sa_l"]; ca = bc["ca_c" if isctx else "ca_l"]
            xt = xts[tt % 2]
            k.dma("sp", xt[:], x_src(tt), reads=[x_src_tk], writes=[xt])
            ln_stats(k, xt, st6, mv, rstd, 1e-5)
            k.op("dve", lambda e: e.scalar_tensor_tensor(t1[:], xt[:], mv[:, 0:1], ca[:], op0=ALU.subtract, op1=ALU.mult),
                 reads=[xt, mv, ca], writes=[t1])
            k.op("dve", lambda e: e.scalar_tensor_tensor(xm[:], t1[:], rstd[:], sa[:], op0=ALU.mult, op1=ALU.add),
                 reads=[t1, rstd, sa], writes=[xm])
            pT = pTs[tt % 2]
            for c in range(8):
                k.op("pe", lambda e: e.transpose(pT[:, c, :], xm[:, c * 128:(c + 1) * 128], ident_bf[:]), reads=[xm, ident_bf], writes=[pT])
            g = tt // 4; o = (tt % 4) * 128
            xg = xmT[g]
            k.op("act", lambda e: e.copy(xg[:, :, o:o + 128], pT[:]), reads=[pT], writes=[xg])
            pa = ptm[0]; pb = ptm[1]
            for kc in range(8):
                k.op("pe", lambda e: e.matmul(pa[:, 0:448], lhsT=xg[:, kc, o:o + 128], rhs=w_in_bf[:, kc, 1152:1600], start=(kc == 0), stop=(kc == 7)),
                     reads=[xg, w_in_bf], writes=[pa])
            for kc in range(8):
                k.op("pe", lambda e: e.matmul(pb[:, 0:256], lhsT=xg[:, kc, o:o + 128], rhs=w_in_bf[:, kc, 2112:2368], start=(kc == 0), stop=(kc == 7)),
                     reads=[xg, w_in_bf], writes=[pb])
            ts = tm_sb[tt % 2]
            k.op("act", lambda e: e.copy(ts[:, 0:448], pa[:, 0:448]), reads=[pa], writes=[ts])
            k.op("dve", lambda e: e.tensor_copy(ts[:, 448:704], pb[:, 0:256]), reads=[pb], writes=[ts])
            k.dma("sp", p_tm_d[tt * 128:(tt + 1) * 128, :], ts[:], reads=[ts], writes=[p_tm_d])
        cnt = 0
        for g, (o, n) in enumerate(groups):
            xg = xmT[g]
            for c in range(13):
                col = c * 128 if c < 9 else 1600 + (c - 9) * 128
                ps = pfm[cnt % 2]
                for kc in range(8):
                    k.op("pe", lambda e: e.matmul(ps[:, 0:n], lhsT=w_in_bf[:, kc, col:col + 128], rhs=xg[:, kc, :], start=(kc == 0), stop=(kc == 7)),
                         reads=[w_in_bf, xg], writes=[ps])
                if c < 9:
                    fs = fm_sb[cnt % 2]
                    k.op("act" if cnt % 2 else "dve", (lambda e: e.copy(fs[:, 0:n], ps[:, 0:n])) if cnt % 2 else (lambda e: e.tensor_copy(fs[:, 0:n], ps[:, 0:n])),
                         reads=[ps], writes=[fs])
                    k.dma("sp", pT_rw_d[c * 128:(c + 1) * 128, o:o + n], fs[:, 0:n], reads=[fs], writes=[pT_rw_d])
                else:
                    fs = fm_sbb[cnt % 2]
                    k.op("act" if cnt % 2 else "dve", (lambda e: e.copy(fs[:, 0:n], ps[:, 0:n])) if cnt % 2 else (lambda e: e.tensor_copy(fs[:, 0:n], ps[:, 0:n])),
                         reads=[ps], writes=[fs])
                    k.dma("sp", pT_na_d[(c - 9) * 128:(c - 8) * 128, o:o + n], fs[:, 0:n], reads=[fs], writes=[pT_na_d])
                cnt += 1
    k.barrier()


def load_w_in(k, l, w_in_d, w_in_bf):
    wv = w_in_d.t[l].rearrange("(kc p) n -> p kc n", p=128)
    for (a, bb) in ((0, 1024), (1024, 2048), (2048, IN_COLS)):
        k.dma("pool", w_in_bf[:, :, a:bb], wv[:, :, a:bb], writes=[w_in_bf])


def load_mla_w(k, l, st, w_uq_d, w_ukv_d, qn_d, kvn_d):
    wuq = k.sb("wuq", [128, 2, 768], BF16, st); wukv = k.sb("wukv", [128, 1024], BF16, st)
    with ExitStack() as s2:
        wuq_f = k.sb("wuqf", [128, 2, 768], F32, s2); wukv_f = k.sb("wukvf", [128, 1024], F32, s2)
        qn = k.sb("qn", [128, 2], F32, s2); kvn = k.sb("kvn", [128, 1], F32, s2)
        k.dma("sp", wuq_f[:], w_uq_d.t[l].rearrange("(kc p) n -> p kc n", p=128), writes=[wuq_f])
        k.dma("sp", wukv_f[:], w_ukv_d.t[l], writes=[wukv_f])
        k.dma("sp", qn[:], qn_d.t[l].rearrange("(kc p) -> p kc", p=128), writes=[qn], allow_slow_non_contiguous=True)
        k.dma("sp", kvn[:], kvn_d.t[l].rearrange("(p o) -> p o", o=1), writes=[kvn])
        for kc in range(2):
            k.op("dve", lambda e: e.tensor_scalar(wuq[:, kc, :], wuq_f[:, kc, :], qn[:, kc:kc + 1], None, op0=ALU.mult), reads=[wuq_f, qn], writes=[wuq])
        k.op("dve", lambda e: e.tensor_scalar(wukv[:], wukv_f[:], kvn[:, 0:1], None, op0=ALU.mult), reads=[wukv_f, kvn], writes=[wukv])
        k.barrier()
    return wuq, wukv


def phase_mla(k, need_ctx, p_tm_d, wuq, wukv, cos_d, sin_d, ident_bf, ones_bf, ymla_d):
    scale = 192.0 ** -0.5
    with ExitStack() as st:
        cos_sb = k.sb("cos", [128, 16, 128], F32, st); sin_sb = k.sb("sin", [128, 16, 128], F32, st)
        k.dma("sp", cos_sb[:], cos_d.t.rearrange("(tt p) c -> p tt c", p=128), writes=[cos_sb])
        k.dma("sp", sin_sb[:], sin_d.t.rearrange("(tt p) c -> p tt c", p=128), writes=[sin_sb])
        p_all = k.sb("pall", [128, NTT, 448], F32, st)
        k.dma("sp", p_all[:], p_tm_d.t.rearrange("(tt p) c -> p tt c", p=128)[:, :, 0:448], reads=[p_tm_d], writes=[p_all])
        qlnT = k.sb("qlnT", [128, 2, T], BF16, st); kvnT = k.sb("kvnT", [128, T], BF16, st)
        qTn = k.sb("qTn", [128, 4, T], BF16, st); qTr = k.sb("qTr", [64, 4, T], BF16, st)
        knT = k.sb("knT", [128, 4, T], BF16, st); krT = k.sb("krT", [64, T], BF16, st)
        v_sb = k.sb("vsb", [128, NTT, 512], BF16, st)
        rq = k.sb("rq", [128, NTT], F32, st); rkv = k.sb("rkv", [128, NTT], F32, st)
        with ExitStack() as st1:
            sq = k.sb("sq", [128, NTT, 384], F32, st1)
            k.op("act", lambda e: e.activation(sq[:], p_all[:, :, 0:384], AF.Square), reads=[p_all], writes=[sq])
            k.op("dve", lambda e: e.reduce_sum(rq[:], sq[:, :, 0:256], axis=AX.X), reads=[sq], writes=[rq])
            k.op("dve", lambda e: e.reduce_sum(rkv[:], sq[:, :, 256:384], axis=AX.X), reads=[sq], writes=[rkv])
            k.barrier()
        k.op("dve", lambda e: e.tensor_scalar(rq[:], rq[:], 1.0 / 256, 1e-6, op0=ALU.mult, op1=ALU.add), reads=[rq], writes=[rq])
        k.op("dve", lambda e: e.tensor_scalar(rkv[:], rkv[:], 1.0 / 128, 1e-6, op0=ALU.mult, op1=ALU.add), reads=[rkv], writes=[rkv])
        for r_ in (rq, rkv):
            k.op("act", lambda e: e.sqrt(r_[:], r_[:]), reads=[r_], writes=[r_])
            k.op("dve", lambda e: e.reciprocal(r_[:], r_[:]), reads=[r_], writes=[r_])
        st2 = ExitStack()
        nrm = [k.sb("nrm", [128, 384], BF16, st2) for _ in range(2)]
        pTs = [k.ps("mlapT", [128, 3, 128], BF16, st2) for _ in range(1)]
        pq = k.ps("mlapq", [128, 4, 256], F32, st2)
        pv = k.ps("mlapv", [128, 512], F32, st2)
        q_sb = k.sb("qsb", [128, 4, 192], BF16, st2); kr_sb = k.sb("krsb", [128, 64], BF16, st2)
        ra = k.sb("ra", [128, 4, 32], F32, st2); rb = k.sb("rb", [128, 4, 32], F32, st2); qr_f = k.sb("qrf", [128, 4, 64], F32, st2)
        pqT = k.ps("mlapqT", [128, 4, 128], BF16, st2); pqTr = k.ps("mlapqTr", [64, 5, 128], BF16, st2)
        STOP = 99; SKIP = ""
        for tt in range(NTT if STOP > 1 else 0):
            isctx = tt < 2
            nt = nrm[tt % 2]; pT = pTs[0]
            k.op("dve", lambda e: e.tensor_scalar(nt[:, 0:256], p_all[:, tt, 0:256], rq[:, tt:tt + 1], None, op0=ALU.mult), reads=[p_all, rq], writes=[nt])
            k.op("dve", lambda e: e.tensor_scalar(nt[:, 256:384], p_all[:, tt, 256:384], rkv[:, tt:tt + 1], None, op0=ALU.mult), reads=[p_all, rkv], writes=[nt])
            for c in range(3):
                k.op("pe", lambda e: e.transpose(pT[:, c, :], nt[:, c * 128:(c + 1) * 128], ident_bf[:]), reads=[nt, ident_bf], writes=[pT])
            sl = slice(tt * 128, (tt + 1) * 128)
            k.op("act", lambda e: e.copy(qlnT[:, :, sl], pT[:, 0:2, :]), reads=[pT], writes=[qlnT])
            k.op("act", lambda e: e.copy(kvnT[:, sl], pT[:, 2, :]), reads=[pT], writes=[kvnT])
            if "v" not in SKIP: k.op("pe", lambda e: e.matmul(pv[:].rearrange("p (h c) -> p h c", h=4), lhsT=kvnT[:, sl],
                                          rhs=wukv[:].rearrange("p (h c) -> p h c", h=4)[:, :, 128:256], start=True, stop=True),
                 reads=[kvnT, wukv], writes=[pv])
            k.op("act", lambda e: e.copy(v_sb[:, tt, :], pv[:]), reads=[pv], writes=[v_sb])
            kre = p_all[:, tt, 384:448:2]; kro = p_all[:, tt, 385:448:2]
            if "k" in SKIP:
                pass
            elif isctx:
                k.op("dve", lambda e: e.tensor_copy(kr_sb[:, 0:32], kre), reads=[p_all], writes=[kr_sb])
                k.op("dve", lambda e: e.tensor_copy(kr_sb[:, 32:64], kro), reads=[p_all], writes=[kr_sb])
            else:
                cs = cos_sb[:, tt - 2, 0:32]; sn = sin_sb[:, tt - 2, 0:32]
                k.op("dve", lambda e: e.tensor_tensor(ra[:, 0, :], kre, cs, ALU.mult), reads=[p_all, cos_sb], writes=[ra])
                k.op("dve", lambda e: e.tensor_tensor(rb[:, 0, :], kro, sn, ALU.mult), reads=[p_all, sin_sb], writes=[rb])
                k.op("dve", lambda e: e.tensor_tensor(kr_sb[:, 0:32], ra[:, 0, :], rb[:, 0, :], ALU.subtract), reads=[ra, rb], writes=[kr_sb])
                k.op("dve", lambda e: e.tensor_tensor(ra[:, 0, :], kre, sn, ALU.mult), reads=[p_all, sin_sb], writes=[ra])
                k.op("dve", lambda e: e.tensor_tensor(rb[:, 0, :], kro, cs, ALU.mult), reads=[p_all, cos_sb], writes=[rb])
                k.op("dve", lambda e: e.tensor_tensor(kr_sb[:, 32:64], ra[:, 0, :], rb[:, 0, :], ALU.add), reads=[ra, rb], writes=[kr_sb])
            doq = ((not isctx) or need_ctx) and "q" not in SKIP
            if doq:
                for h in range(4):
                    for kc in range(2):
                        k.op("pe", lambda e: e.matmul(pq[:, h, 0:192], lhsT=qlnT[:, kc, sl], rhs=wuq[:, kc, h * 192:(h + 1) * 192], start=(kc == 0), stop=(kc == 1)),
                             reads=[qlnT, wuq], writes=[pq])
                if "a" not in SKIP: k.op("act", lambda e: e.copy(q_sb[:, :, 0:128], pq[:, :, 0:128]), reads=[pq], writes=[q_sb])
                k.op("act", lambda e: e.copy(qr_f[:], pq[:, :, 128:192]), reads=[pq], writes=[qr_f])
                qe = qr_f[:, :, 0:64:2]; qo = qr_f[:, :, 1:64:2]
                if isctx:
                    k.op("dve", lambda e: e.tensor_copy(q_sb[:, :, 128:160], qe), reads=[qr_f], writes=[q_sb])
                    k.op("dve", lambda e: e.tensor_copy(q_sb[:, :, 160:192], qo), reads=[qr_f], writes=[q_sb])
                else:
                    cs = cos_sb[:, tt - 2, :].rearrange("p (h c) -> p h c", h=4); sn = sin_sb[:, tt - 2, :].rearrange("p (h c) -> p h c", h=4)
                    k.op("dve", lambda e: e.tensor_tensor(ra[:], qe, cs, ALU.mult), reads=[qr_f, cos_sb], writes=[ra])
                    k.op("dve", lambda e: e.tensor_tensor(rb[:], qo, sn, ALU.mult), reads=[qr_f, sin_sb], writes=[rb])
                    k.op("dve", lambda e: e.tensor_tensor(q_sb[:, :, 128:160], ra[:], rb[:], ALU.subtract), reads=[ra, rb], writes=[q_sb])
                    k.op("dve", lambda e: e.tensor_tensor(ra[:], qe, sn, ALU.mult), reads=[qr_f, sin_sb], writes=[ra])
                    k.op("dve", lambda e: e.tensor_tensor(rb[:], qo, cs, ALU.mult), reads=[qr_f, cos_sb], writes=[rb])
                    k.op("dve", lambda e: e.tensor_tensor(q_sb[:, :, 160:192], ra[:], rb[:], ALU.add), reads=[ra, rb], writes=[q_sb])
                for h in range(4 if "t" not in SKIP else 0):
                    k.op("pe", lambda e: e.transpose(pqT[:, h, :], q_sb[:, h, 0:128], ident_bf[:]), reads=[q_sb, ident_bf], writes=[pqT])
                    k.op("pe", lambda e: e.transpose(pqTr[:, h, :], q_sb[:, h, 128:192], ident_bf[:]), reads=[q_sb, ident_bf], writes=[pqTr])
            if "r" not in SKIP: k.op("pe", lambda e: e.transpose(pqTr[:, 4, :], kr_sb[:], ident_bf[:]), reads=[kr_sb, ident_bf], writes=[pqTr])
            if doq:
                k.op("act", lambda e: e.copy(qTn[:, :, sl], pqT[:]), reads=[pqT], writes=[qTn])
                k.op("dve", lambda e: e.tensor_copy(qTr[:, :, sl], pqTr[:, 0:4, :]), reads=[pqTr], writes=[qTr])
            k.op("dve", lambda e: e.tensor_copy(krT[:, sl], pqTr[:, 4, :]), reads=[pqTr], writes=[krT])
        k.barrier(); st2.close()
        groups = [(0, 512), (512, 512), (1024, 512), (1536, 512), (2048, 256)]
        pk = [k.ps("mlapk", [128, 512], F32, st) for _ in range(2)]
        cnt = 0
        for h in range(4 if STOP > 2 else 0):
            for (o, n) in groups:
                ps = pk[cnt % 2]
                k.op("pe", lambda e: e.matmul(ps[:, 0:n], lhsT=wukv[:, h * 256:h * 256 + 128], rhs=kvnT[:, o:o + n], start=True, stop=True), reads=[wukv, kvnT], writes=[ps])
                k.op("act" if cnt % 2 else "dve", (lambda e: e.copy(knT[:, h, o:o + n], ps[:, 0:n])) if cnt % 2 else (lambda e: e.tensor_copy(knT[:, h, o:o + n], ps[:, 0:n])),
                     reads=[ps], writes=[knT])
                cnt += 1
        pos = [k.ps("mlapo", [128, 512], F32, st) for _ in range(2)]
        pss = [k.ps("mlapss", [128, 512], F32, st) for _ in range(2)]
        pts = [k.sb("mlapt", [128, 512], BF16, st) for _ in range(3)]
        rs = k.sb("mlars", [128, 512], F32, st); ob = [k.sb("mlaob", [128, 512], BF16, st) for _ in range(2)]
        qblocks = [(256 + i * 512, 512, list(range(NTT))) for i in range(4)]
        if need_ctx:
            qblocks.append((0, 256, [0, 1]))
        it = 0; bi = 0
        for h in range(4 if STOP > 3 else 0):
            for (qo_, nq, ktiles) in qblocks:
                po = pos[bi % 2]; psum = pss[bi % 2]
                for ji, j in enumerate(ktiles):
                    ps = pk[it % 2]; pt = pts[it % 3]
                    ks = slice(j * 128, (j + 1) * 128)
                    k.op("pe", lambda e: e.matmul(ps[:, 0:nq], lhsT=knT[:, h, ks], rhs=qTn[:, h, qo_:qo_ + nq], start=True, stop=False), reads=[knT, qTn], writes=[ps])
                    k.op("pe", lambda e: e.matmul(ps[:, 0:nq], lhsT=krT[:, ks], rhs=qTr[:, h, qo_:qo_ + nq], start=False, stop=True), reads=[krT, qTr], writes=[ps])
                    k.op("act", lambda e: e.activation(pt[:, 0:nq], ps[:, 0:nq], AF.Exp, scale=scale), reads=[ps], writes=[pt])
                    first = ji == 0; last = ji == len(ktiles) - 1
                    k.op("pe", lambda e: e.matmul(po[:, 0:nq], lhsT=v_sb[:, j, h * 128:(h + 1) * 128], rhs=pt[:, 0:nq], start=first, stop=last), reads=[v_sb, pt], writes=[po])
                    k.op("pe", lambda e: e.matmul(psum[:, 0:nq], lhsT=ones_bf[:], rhs=pt[:, 0:nq], start=first, stop=last), reads=[ones_bf, pt], writes=[psum])
                    it += 1
                o_ = ob[bi % 2]
                k.op("dve", lambda e: e.reciprocal(rs[:, 0:nq], psum[:, 0:nq]), reads=[psum], writes=[rs])
                k.op("dve", lambda e: e.tensor_tensor(o_[:, 0:nq], po[:, 0:nq], rs[:, 0:nq], ALU.mult), reads=[po, rs], writes=[o_])
                k.dma("sp", ymla_d[h][:, qo_:qo_ + nq], o_[:, 0:nq], reads=[o_], writes=[ymla_d])
                bi += 1
    k.barrier()


def rope_tables():
    t = np.arange(SEQ); row = (t // 64).astype(np.float32); col = (t % 64).astype(np.float32)
    inv = (10000.0 ** (-np.arange(16, dtype=np.float32) / 16)).astype(np.float32)
    ang = np.concatenate([row[:, None] * inv, col[:, None] * inv], -1).astype(np.float32)
    return np.tile(np.cos(ang).astype(np.float32), (1, 4)), np.tile(np.sin(ang).astype(np.float32), (1, 4))


def na_pattern(i):
    if i < 4:
        return i, 0
    if i <= 28:
        return 4, i - 4
    return i - 24, 24


def na_bias_host(rpb):
    L = rpb.shape[0]
    col = np.arange(64)
    dc = np.clip(col[None, :] - col[:, None] + 15, 0, 30)
    out = np.zeros((L, 128, 4, 8, 4, 64), np.float32)
    lk = np.arange(512); r = lk // 64; kc = lk % 64
    for i in list(range(5)) + [29, 30, 31]:
        pat, rs = na_pattern(i)
        dr = rs + r - i + 7
        g = rpb[:, :, dr[:, None], dc.T[kc, :]]
        out[:, :, :, pat] = g.reshape(L, 4, 4, 128, 64).transpose(0, 3, 1, 2, 4)
    return out


def na_mask_host():
    col = np.arange(64)
    cs = np.clip(col - 8, 0, 48)
    inw = (col[None, :] >= cs[:, None]) & (col[None, :] < cs[:, None] + 16)
    lk = np.arange(512); kc = lk % 64
    m = np.where(inw.T[kc, :], 0.0, -30000.0).astype(np.float32)
    return np.ascontiguousarray(m.reshape(4, 128, 64).transpose(1, 0, 2))


def load_na_bias(k, l, st, bias_d, mask_d):
    bm = k.sb("nabm", [128, 4, 8, 384], F32, st); mk = k.sb("namk", [128, 256], F32, st)
    k.op("pool", lambda e: e.memset(bm[:], 0.0), writes=[bm])
    k.dma("sp", mk[:], mask_d.t.rearrange("p j q -> p (j q)"), writes=[mk])
    for h in range(4):
        k.dma("sp", bm[:, h, :, 0:256], bias_d.t[l][:, h].rearrange("p a j q -> p a (j q)"), writes=[bm])
    for h in range(4):
        for pat in range(8):
            k.op("pool", lambda e: e.tensor_tensor(bm[:, h, pat, 0:256], bm[:, h, pat, 0:256], mk[:], ALU.add), reads=[bm, mk], writes=[bm])
    return bm


def phase_na(k, need_ctx, pT_na_d, p_tm_d, bm, ones_bf, yna_d):
    scale = 64.0 ** -0.5
    with ExitStack() as st:
        qT = k.sb("naqT", [64, 4, T], BF16, st); kT = k.sb("nakT", [64, 4, T], BF16, st)
        vA = k.sb("navA", [128, NTT, 256], BF16, st); vB = k.sb("navB", [128, NTT - 1, 256], BF16, st)
        yo = k.sb("nayo", [64, 4, T], BF16, st)
        k.dma("sp", qT[:], pT_na_d.t[0:256, :].rearrange("(h d) t -> d h t", h=4), reads=[pT_na_d], writes=[qT])
        k.dma("sp", kT[:], pT_na_d.t[256:512, :].rearrange("(h d) t -> d h t", h=4), reads=[pT_na_d], writes=[kT])
        k.dma("pool", vA[:], p_tm_d.t.rearrange("(tt p) c -> p tt c", p=128)[:, :, 448:704], reads=[p_tm_d], writes=[vA])
        k.dma("pool", vB[:], p_tm_d.t[64:64 + (NTT - 1) * 128, :].rearrange("(tt p) c -> p tt c", p=128)[:, :, 448:704], reads=[p_tm_d], writes=[vB])
        pSs = [k.ps("napS", [128, 6, 64], F32, st) for _ in range(2)]
        pos = [k.ps("napo", [64, 256], F32, st) for _ in range(2)]
        pms = [k.ps("napm", [64, 256], F32, st) for _ in range(2)]
        ssb = [k.sb("nassb", [128, 6, 64], F32, st) for _ in range(2)]
        pts = [k.sb("napt", [128, 6, 64], BF16, st) for _ in range(2)]
        ptc = [k.sb("naptc", [128, 2, 256], BF16, st) for _ in range(2)]
        rss = [k.sb("nars", [64, 256], F32, st) for _ in range(2)]
        it = 0
        for h in range(4):
            for i in range(32):
                pat, rs_ = na_pattern(i)
                pS = pSs[it % 2]; po = pos[it % 2]; pm = pms[it % 2]; s_sb = ssb[it % 2]; pt = pts[it % 2]; rs = rss[it % 2]
                tok0 = 256 + 64 * rs_
                qs = slice(256 + 64 * i, 256 + 64 * i + 64)
                for j in range(6):
                    ko = tok0 + 128 * j if j < 4 else (j - 4) * 128
                    k.op("pe", lambda e: e.matmul(pS[:, j, :], lhsT=kT[:, h, ko:ko + 128], rhs=qT[:, h, qs], start=True, stop=True), reads=[kT, qT], writes=[pS])
                k.op("dve", lambda e: e.scalar_tensor_tensor(s_sb[:].rearrange("p j q -> p (j q)"), pS[:].rearrange("p j q -> p (j q)"), scale, bm[:, h, pat, :], op0=ALU.mult, op1=ALU.add),
                     reads=[pS, bm], writes=[s_sb])
                k.op("act", lambda e: e.activation(pt[:], s_sb[:], AF.Exp), reads=[s_sb], writes=[pt])
                for j in range(6):
                    if j < 4:
                        vt = (vA, 2 + rs_ // 2 + j) if rs_ % 2 == 0 else (vB, (3 + rs_) // 2 + j)
                    else:
                        vt = (vA, j - 4)
                    vtk, vi = vt
                    k.op("pe", lambda e: e.matmul(po[:, 0:64], lhsT=vtk[:, vi, h * 64:(h + 1) * 64], rhs=pt[:, j, :], start=(j == 0), stop=(j == 5)), reads=[vtk, pt], writes=[po])
                for j in range(6):
                    k.op("pe", lambda e: e.matmul(pm[:, 0:64], lhsT=ones_bf[:, 0:64], rhs=pt[:, j, :], start=(j == 0), stop=(j == 5)), reads=[ones_bf, pt], writes=[pm])
                k.op("dve", lambda e: e.reciprocal(rs[:, 0:64], pm[:, 0:64]), reads=[pm], writes=[rs])
                k.op("dve", lambda e: e.tensor_tensor(yo[:, h, qs], po[:, 0:64], rs[:, 0:64], ALU.mult), reads=[po, rs], writes=[yo])
                it += 1
            if need_ctx:
                pS = pSs[it % 2]; po = pos[it % 2]; pm = pms[it % 2]; pt = ptc[it % 2]; rs = rss[it % 2]
                pSv = pS[:].rearrange("p j q -> p (j q)")
                for j in range(2):
                    k.op("pe", lambda e: e.matmul(pSv[:, 0:256], lhsT=kT[:, h, j * 128:(j + 1) * 128], rhs=qT[:, h, 0:256], start=True, stop=True), reads=[kT, qT], writes=[pS])
                    k.op("act", lambda e: e.activation(pt[:, j, :], pSv[:, 0:256], AF.Exp, scale=scale), reads=[pS], writes=[pt])
                for j in range(2):
                    k.op("pe", lambda e: e.matmul(po[:], lhsT=vA[:, j, h * 64:(h + 1) * 64], rhs=pt[:, j, :], start=(j == 0), stop=(j == 1)), reads=[vA, pt], writes=[po])
                for j in range(2):
                    k.op("pe", lambda e: e.matmul(pm[:], lhsT=ones_bf[:, 0:64], rhs=pt[:, j, :], start=(j == 0), stop=(j == 1)), reads=[ones_bf, pt], writes=[pm])
                k.op("dve", lambda e: e.reciprocal(rs[:], pm[:]), reads=[pm], writes=[rs])
                k.op("dve", lambda e: e.tensor_tensor(yo[:, h, 0:256], po[:], rs[:], ALU.mult), reads=[po, rs], writes=[yo])
                it += 1
        k.dma("sp", yna_d[:], yo[:], reads=[yo], writes=[yna_d])
    k.barrier()


NCH = T // 64
RW_STOP = 99
class _Stop(Exception):
    pass
def chk(level):
    if RW_STOP == level:
        raise _Stop()


def rw_consts_host():
    i = np.arange(128) % 64
    lt = (i[None, :] < i[:, None]).astype(np.float32); le = (i[None, :] <= i[:, None]).astype(np.float32)
    gt = (i[None, :] > i[:, None]).astype(np.float32); ge = (i[None, :] >= i[:, None]).astype(np.float32)
    blk = ((np.arange(128)[:, None] // 64) == (np.arange(128)[None, :] // 64)).astype(np.float32)
    m2 = np.stack([np.concatenate([gt, ge], 1), np.concatenate([lt, le], 1)], 0)
    m1 = np.stack([lt, gt], 0)
    isel = np.concatenate([np.eye(64, dtype=np.float32)] * 2, 0)
    return m2, m1, blk, isel


def load_rw_w(k, l, st, W, ident_f):
    o = {}
    o["w2"] = k.sb("rww2", [128, 256], BF16, st); o["a2"] = k.sb("rwa2", [128, 256], BF16, st); o["g2"] = k.sb("rwg2", [128, 256], BF16, st)
    k.dma("pool", o["w2"][:], W["rw_w2"].t[l].rearrange("d k n -> (d k) n"), writes=[o["w2"]])
    k.dma("pool", o["a2"][:], W["rw_a2"].t[l].rearrange("d k n -> (d k) n"), writes=[o["a2"]])
    k.dma("pool", o["g2"][:], W["rw_g2"].t[l], writes=[o["g2"]])
    stage = k.sb("rwstage", [32, 128], F32, st)
    k.dma("sp", stage[0:18, :], W["rw_mu"].t[l].rearrange("d (c p) -> (d c) p", p=128), writes=[stage])
    k.dma("sp", stage[18:22, :], W["rw_w0"].t[l].rearrange("d (c p) -> (d c) p", p=128), writes=[stage])
    k.dma("sp", stage[22:26, :], W["rw_a0"].t[l].rearrange("d (c p) -> (d c) p", p=128), writes=[stage])
    k.dma("sp", stage[26:28, :], W["rw_kk"].t[l].rearrange("(c p) -> c p", p=128), writes=[stage])
    k.dma("sp", stage[28:30, :], W["rw_ka"].t[l].rearrange("(c p) -> c p", p=128), writes=[stage])
    k.dma("sp", stage[30:32, :], W["rw_rk"].t[l].rearrange("(c h) n -> c (h n)", c=2), writes=[stage])
    cols = k.sb("rwcols", [128, 48], F32, st)
    with ExitStack() as s2:
        pc = k.ps("rwpc", [128, 32], F32, s2)
        k.op("pe", lambda e: e.transpose(pc[:], stage[:], ident_f[0:32, 0:32]), reads=[stage, ident_f], writes=[pc])
        k.op("dve", lambda e: e.tensor_copy(cols[:, 0:32], pc[:]), reads=[pc], writes=[cols])
        k.barrier()
    k.op("dve", lambda e: e.tensor_tensor(cols[:, 32:41], cols[:, 0:9], cols[:, 9:18], ALU.add), reads=[cols], writes=[cols])
    k.op("dve", lambda e: e.tensor_scalar(cols[:, 32:41], cols[:, 32:41], -1.0, 1.0, op0=ALU.mult, op1=ALU.add), reads=[cols], writes=[cols])
    k.op("dve", lambda e: e.tensor_scalar(cols[:, 41:43], cols[:, 28:30], -1.0, 1.0, op0=ALU.mult, op1=ALU.add), reads=[cols], writes=[cols])
    o["cols"] = cols
    gn = k.sb("rwgn", [128, 2, 2, 64], F32, st)
    for P in range(2):
        for h in range(2):
            hd = 2 * P + h
            k.dma("sp", gn[h * 64:(h + 1) * 64, P, 0, :], W["rw_gn_w"].t[l:l + 1, hd * 64:(hd + 1) * 64].partition_broadcast(64), writes=[gn])
            k.dma("sp", gn[h * 64:(h + 1) * 64, P, 1, :], W["rw_gn_b"].t[l:l + 1, hd * 64:(hd + 1) * 64].partition_broadcast(64), writes=[gn])
    o["gn"] = gn
    return o


def phase_rwkv(k, pT_rw_d, RW, C, yrw_d):
    cols = RW["cols"]
    EM05 = float(np.exp(-0.5))
    with ExitStack() as st:
        tmpA = k.sb("rwtmpA", [128, T], F32, st); tmpB = k.sb("rwtmpB", [128, T], F32, st)

        def shifted(c, out):
            k.dma("sp", tmpA[:], pT_rw_d[c * 128:(c + 1) * 128, :], reads=[pT_rw_d], writes=[tmpA])
            k.op("act", lambda e: e.activation(out[:], tmpA[:], AF.Identity, scale=cols[:, 32 + c:33 + c]), reads=[tmpA, cols], writes=[out])
            for (dst, src, mc) in (((1, 256), (0, 255), c), ((257, T), (256, T - 1), c), ((0, 255), (1, 256), 9 + c), ((256, T - 1), (257, T), 9 + c)):
                k.op("dve", lambda e: e.scalar_tensor_tensor(out[:, dst[0]:dst[1]], tmpA[:, src[0]:src[1]], cols[:, mc:mc + 1], out[:, dst[0]:dst[1]], op0=ALU.mult, op1=ALU.add),
                     reads=[tmpA, cols, out], writes=[out])

        tw = k.sb("rwtw", [128, T], BF16, st); al = k.sb("rwal", [128, T], BF16, st); sg = k.sb("rwsg", [128, T], BF16, st)
        shifted(6, tmpB); k.op("act", lambda e: e.activation(tw[:], tmpB[:], AF.Tanh), reads=[tmpB], writes=[tw])
        shifted(7, tmpB); k.op("act", lambda e: e.copy(al[:], tmpB[:]), reads=[tmpB], writes=[al])
        shifted(8, tmpB); k.op("act", lambda e: e.activation(sg[:], tmpB[:], AF.Sigmoid), reads=[tmpB], writes=[sg])
        groups = [(0, 512), (512, 512), (1024, 512), (1536, 512), (2048, 256)]
        chk(1)
        for P in range(2 if RW_STOP > 1 else 0):
            with ExitStack() as sp:
                r_f = k.sb("rwr", [128, T], F32, sp); k_f = k.sb("rwk", [128, T], F32, sp); kk_f = k.sb("rwkk", [128, T], F32, sp)
                vst = k.sb("rwvst", [128, NCH, 64], BF16, sp)
                ccol = k.sb("rwccol", [128, NCH], F32, sp)
                o_all = k.sb("rwoall", [128, NCH, 64], F32, sp)
                shifted(0 + P, r_f); shifted(2 + P, k_f)
                sv = ExitStack()
                vexp = k.sb("rwvexp", [128, NCH, 128], BF16, sv)
                shifted(4 + P, tmpB)
                k.op("pool", lambda e: e.memset(vexp[:], 0.0), writes=[vexp])
                for h in range(2):
                    hs = slice(h * 64, (h + 1) * 64)
                    k.op("dve", lambda e: e.tensor_copy(vexp[hs, :, hs], tmpB[hs, :].rearrange("p (c s) -> p c s", s=64)), reads=[tmpB], writes=[vexp])
                k.op("dve", lambda e: e.tensor_scalar(kk_f[:], k_f[:], cols[:, 26 + P:27 + P], None, op0=ALU.mult), reads=[k_f, cols], writes=[kk_f])
                k.op("act", lambda e: e.activation(tmpB[:], kk_f[:], AF.Square), reads=[kk_f], writes=[tmpB])
                with ExitStack() as s2:
                    pg = [k.ps("rwpn", [128, 512], F32, s2) for _ in range(2)]
                    for gi, (o, n) in enumerate(groups):
                        ps = pg[gi % 2]
                        k.op("pe", lambda e: e.matmul(ps[:, 0:n], lhsT=C["blk"][:], rhs=tmpB[:, o:o + n], start=True, stop=True), reads=[C["blk"], tmpB], writes=[ps])
                        k.op("dve", lambda e: e.tensor_scalar(tmpA[:, o:o + n], ps[:, 0:n], 1e-12, None, op0=ALU.add), reads=[ps], writes=[tmpA])
                    k.op("act", lambda e: e.sqrt(tmpA[:], tmpA[:]), reads=[tmpA], writes=[tmpA])
                    k.op("dve", lambda e: e.reciprocal(tmpA[:], tmpA[:]), reads=[tmpA], writes=[tmpA])
                    k.op("dve", lambda e: e.tensor_tensor(kk_f[:], kk_f[:], tmpA[:], ALU.mult), reads=[kk_f, tmpA], writes=[kk_f])
                    rkexp = k.sb("rwrkexp", [128, NCH, 128], BF16, s2)
                    k.op("pool", lambda e: e.memset(rkexp[:], 0.0), writes=[rkexp])
                    k.op("dve", lambda e: e.scalar_tensor_tensor(tmpB[:], r_f[:], cols[:, 30 + P:31 + P], k_f[:], op0=ALU.mult, op1=ALU.mult), reads=[r_f, k_f, cols], writes=[tmpB])
                    for h in range(2):
                        hs = slice(h * 64, (h + 1) * 64)
                        k.op("dve", lambda e: e.tensor_copy(rkexp[hs, :, hs], tmpB[hs, :].rearrange("p (c s) -> p c s", s=64)), reads=[tmpB], writes=[rkexp])
                    pv = [k.ps("rwpv", [128, 8, 64], F32, s2) for _ in range(2)]
                    pcc = k.ps("rwpcc", [128, NCH], F32, s2)
                    for c0 in range(0, NCH, 8):
                        nn = min(8, NCH - c0); ps = pv[(c0 // 8) % 2]
                        for j in range(nn):
                            k.op("pe", lambda e: e.matmul(ps[:, j, :], lhsT=vexp[:, c0 + j, :], rhs=C["isel"][:], start=True, stop=True), reads=[vexp, C["isel"]], writes=[ps])
                        k.op("act", lambda e: e.copy(vst[:, c0:c0 + nn, :], ps[:, 0:nn, :]), reads=[ps], writes=[vst])
                    for c in range(NCH):
                        k.op("pe", lambda e: e.matmul(pcc[:, c:c + 1], lhsT=rkexp[:, c, :], rhs=C["ones_bf"][:, 0:1], start=True, stop=True), reads=[rkexp, C["ones_bf"]], writes=[pcc])
                    k.op("dve", lambda e: e.tensor_copy(ccol[:], pcc[:]), reads=[pcc], writes=[ccol])
                    k.barrier()
                sv.close()
                if RW_STOP == 2: continue

                for d in range(2):
                    with ExitStack() as sd:
                        if RW_STOP < 7 and (P, d) != (0, 0): continue
                        rw_dir(k, sd, P, d, RW, C, cols, tw, al, r_f, k_f, kk_f, vst, o_all, tmpA, tmpB, groups, EM05)
                        k.barrier()
                if RW_STOP < 7: continue
                with ExitStack() as s2:
                    s1 = k.sb("rws1", [128, NCH], F32, s2); s2_ = k.sb("rws2", [128, NCH], F32, s2); rstd = k.sb("rwrstd", [128, NCH], F32, s2)
                    sq = k.sb("rwsq", [128, NCH, 64], F32, s2); fin = k.sb("rwfin", [128, NCH, 64], F32, s2)
                    k.op("dve", lambda e: e.reduce_sum(s1[:], o_all[:], axis=AX.X), reads=[o_all], writes=[s1])
                    k.op("act", lambda e: e.activation(sq[:], o_all[:], AF.Square), reads=[o_all], writes=[sq])
                    k.op("dve", lambda e: e.reduce_sum(s2_[:], sq[:], axis=AX.X), reads=[sq], writes=[s2_])
                    k.op("dve", lambda e: e.tensor_scalar(s1[:], s1[:], 1.0 / 64, None, op0=ALU.mult), reads=[s1], writes=[s1])
                    k.op("dve", lambda e: e.tensor_tensor(rstd[:], s1[:], s1[:], ALU.mult), reads=[s1], writes=[rstd])
                    k.op("dve", lambda e: e.scalar_tensor_tensor(rstd[:], s2_[:], 1.0 / 64, rstd[:], op0=ALU.mult, op1=ALU.subtract), reads=[s2_, rstd], writes=[rstd])
                    k.op("dve", lambda e: e.tensor_scalar(rstd[:], rstd[:], 64e-5, None, op0=ALU.add), reads=[rstd], writes=[rstd])
                    k.op("act", lambda e: e.sqrt(rstd[:], rstd[:]), reads=[rstd], writes=[rstd])
                    k.op("dve", lambda e: e.reciprocal(rstd[:], rstd[:]), reads=[rstd], writes=[rstd])
                    yfm = k.sb("rwyfm", [64, 2, T], BF16, s2)
                    gfm = k.sb("rwgfm", [64, 2, T], F32, s2); vstf = k.sb("rwvstf", [128, NCH, 64], F32, s2)
                    k.op("pool", lambda e: e.tensor_copy(vstf[:], vst[:]), reads=[vst], writes=[vstf])
                    pgg = [k.ps("rwpg", [128, 512], F32, s2) for _ in range(2)]
                    cnt = 0
                    for h in range(2):
                        hd = 2 * P + h
                        for (o, n) in groups:
                            ps = pgg[cnt % 2]
                            k.op("pe", lambda e: e.matmul(ps[0:64, 0:n], lhsT=RW["g2"][:, hd * 64:(hd + 1) * 64], rhs=sg[:, o:o + n], start=True, stop=True), reads=[RW["g2"], sg], writes=[ps])
                            k.op("act", lambda e: e.copy(gfm[:, h, o:o + n], ps[0:64, 0:n]), reads=[ps], writes=[gfm])
                            cnt += 1
                    gnw = RW["gn"][:, P, 0, :]; gnb = RW["gn"][:, P, 1, :]
                    for c in range(NCH):
                        k.op("dve", lambda e: e.scalar_tensor_tensor(sq[:, c, :], o_all[:, c, :], s1[:, c:c + 1], gnw, op0=ALU.subtract, op1=ALU.mult), reads=[o_all, s1, RW["gn"]], writes=[sq])
                        k.op("dve", lambda e: e.scalar_tensor_tensor(sq[:, c, :], sq[:, c, :], rstd[:, c:c + 1], gnb, op0=ALU.mult, op1=ALU.add), reads=[sq, rstd, RW["gn"]], writes=[sq])
                        k.op("dve", lambda e: e.scalar_tensor_tensor(fin[:, c, :], vstf[:, c, :], ccol[:, c:c + 1], sq[:, c, :], op0=ALU.mult, op1=ALU.add), reads=[vstf, ccol, sq], writes=[fin])
                    pts = [k.ps("rwpt", [64, 4, 128], F32, s2) for _ in range(2)]
                    for c0 in range(0, NCH, 4):
                        ps = pts[(c0 // 4) % 2]
                        for j in range(4):
                            k.op("pe", lambda e: e.transpose(ps[:, j, :], fin[:, c0 + j, :], C["ident_f"][:]), reads=[fin, C["ident_f"]], writes=[ps])
                        for h in range(2):
                            hd = 2 * P + h
                            k.op("dve", lambda e: e.tensor_tensor(yfm[:, h, c0 * 64:(c0 + 4) * 64].rearrange("p (c s) -> p c s", s=64), ps[:, :, h * 64:(h + 1) * 64],
                                                                  gfm[:, h, c0 * 64:(c0 + 4) * 64].rearrange("p (c s) -> p c s", s=64), ALU.mult), reads=[ps, gfm], writes=[yfm])
                    k.dma("sp", yrw_d[:, 2 * P:2 * P + 2, :], yfm[:], reads=[yfm], writes=[yrw_d])
                    k.barrier()
    k.barrier()


def rw_dir(k, sd, P, d, RW, C, cols, tw, al, r_f, k_f, kk_f, vst, o_all, tmpA, tmpB, groups, EM05):
    dsl = slice(d * 64, (d + 1) * 64)
    AR = k.sb("rwAR", [128, NCH, 2, 128], BF16, sd); Bx = k.sb("rwBx", [128, NCH, 128], BF16, sd); Kx = k.sb("rwKx", [128, NCH, 128], BF16, sd)
    gc = k.sb("rwgc", [128, NCH], F32, sd); eoff = k.sb("rweoff", [128, NCH, 2], F32, sd)
    stmp = ExitStack()
    Gp = k.sb("rwGp", [128, T + 1], F32, stmp)
    b_f = k.sb("rwb", [128, T], F32, stmp); kd_f = tmpB
    eB = k.sb("rweB", [128, T], F32, stmp); eA = k.sb("rweA", [128, T], F32, stmp); eR = tmpA
    with ExitStack() as s2:
        pg = [k.ps("rwpl", [128, 512], F32, s2) for _ in range(2)]
        cnt = 0
        for (o, n) in groups:
            ps = pg[cnt % 2]; cnt += 1
            k.op("pe", lambda e: e.matmul(ps[:, 0:n], lhsT=RW["w2"][dsl, P * 128:(P + 1) * 128], rhs=tw[dsl, o:o + n], start=True, stop=True), reads=[RW["w2"], tw], writes=[ps])
            k.op("act", lambda e: e.activation(tmpA[:, o:o + n], ps[:, 0:n], AF.Sigmoid, bias=cols[:, 18 + 2 * d + P:19 + 2 * d + P]), reads=[ps, cols], writes=[tmpA])
            ps = pg[cnt % 2]; cnt += 1
            k.op("pe", lambda e: e.matmul(ps[:, 0:n], lhsT=RW["a2"][dsl, P * 128:(P + 1) * 128], rhs=al[dsl, o:o + n], start=True, stop=True), reads=[RW["a2"], al], writes=[ps])
            k.op("act", lambda e: e.activation(tmpB[:, o:o + n], ps[:, 0:n], AF.Sigmoid, bias=cols[:, 22 + 2 * d + P:23 + 2 * d + P]), reads=[ps, cols], writes=[tmpB])
        k.barrier()
    k.op("dve", lambda e: e.tensor_tensor(b_f[:], tmpB[:], kk_f[:], ALU.mult), reads=[tmpB, kk_f], writes=[b_f])
    k.op("dve", lambda e: e.tensor_scalar(tmpB[:], tmpB[:], cols[:, 28 + P:29 + P], cols[:, 41 + P:42 + P], op0=ALU.mult, op1=ALU.add), reads=[tmpB, cols], writes=[tmpB])
    k.op("dve", lambda e: e.tensor_tensor(kd_f[:], k_f[:], tmpB[:], ALU.mult), reads=[k_f, tmpB], writes=[kd_f])
    k.op("dve", lambda e: e.tensor_scalar(tmpA[:], tmpA[:], -EM05, None, op0=ALU.mult), reads=[tmpA], writes=[tmpA])
    k.op("dve", lambda e: e.memset(Gp[:, 0:1], 0.0), writes=[Gp])
    k.op("pool", lambda e: e.memset(eA[:], 1.0), writes=[eA])
    k.op("dve", lambda e: e.tensor_tensor_scan(Gp[:, 1:T + 1], eA[:], tmpA[:], 0.0, op0=ALU.mult, op1=ALU.add), reads=[tmpA, eA, Gp], writes=[Gp])
    Gc0 = Gp[:, 0:T].rearrange("p (c s) -> p c s", s=64)[:, :, 0]; Gc1 = Gp[:, 1:T + 1].rearrange("p (c s) -> p c s", s=64)[:, :, 63]
    k.op("dve", lambda e: e.tensor_tensor(gc[:], Gc1, Gc0, ALU.subtract), reads=[Gp], writes=[gc])
    k.op("act", lambda e: e.activation(gc[:], gc[:], AF.Exp), reads=[gc], writes=[gc])
    Eref = Gc1 if d == 0 else Gc0
    k.op("dve", lambda e: e.tensor_copy(eoff[:, :, 0], Eref), reads=[Gp], writes=[eoff])
    k.op("dve", lambda e: e.tensor_scalar(eoff[:, :, 1], Eref, -1.0, None, op0=ALU.mult), reads=[Gp], writes=[eoff])
    k.barrier()
    if RW_STOP == 3:
        stmp.close(); return
    for c in range(NCH):
        cs = slice(c * 64, (c + 1) * 64); g1 = Gp[:, 1 + c * 64:1 + (c + 1) * 64]; g0 = Gp[:, c * 64:(c + 1) * 64]
        pE = eoff[:, c, 0:1]; nE = eoff[:, c, 1:2]
        if d == 0:
            k.op("act", lambda e: e.activation(eB[:, cs], g1, AF.Exp, scale=-1.0, bias=pE), reads=[Gp, eoff], writes=[eB])
            k.op("act", lambda e: e.activation(eA[:, cs], g0, AF.Exp, scale=1.0, bias=nE), reads=[Gp, eoff], writes=[eA])
            k.op("act", lambda e: e.activation(eR[:, cs], g1, AF.Exp, scale=1.0, bias=nE), reads=[Gp, eoff], writes=[eR])
        else:
            k.op("act", lambda e: e.activation(eB[:, cs], g0, AF.Exp, scale=1.0, bias=nE), reads=[Gp, eoff], writes=[eB])
            k.op("act", lambda e: e.activation(eA[:, cs], g1, AF.Exp, scale=-1.0, bias=pE), reads=[Gp, eoff], writes=[eA])
            k.op("act", lambda e: e.activation(eR[:, cs], g0, AF.Exp, scale=-1.0, bias=pE), reads=[Gp, eoff], writes=[eR])
    k.op("pool", lambda e: e.memset(AR[:], 0.0), writes=[AR]); k.op("pool", lambda e: e.memset(Bx[:], 0.0), writes=[Bx]); k.op("pool", lambda e: e.memset(Kx[:], 0.0), writes=[Kx])
    v3 = lambda tle, hs: tle[hs, :].rearrange("p (c s) -> p c s", s=64)
    for h in range(2):
        hs = slice(h * 64, (h + 1) * 64)
        k.op("dve", lambda e: e.tensor_tensor(AR[hs, :, 0, hs], v3(kk_f, hs), v3(eA, hs), ALU.mult), reads=[kk_f, eA], writes=[AR])
        k.op("dve", lambda e: e.tensor_tensor(AR[hs, :, 1, hs], v3(r_f, hs), v3(eR, hs), ALU.mult), reads=[r_f, eR], writes=[AR])
        k.op("dve", lambda e: e.tensor_tensor(Bx[hs, :, hs], v3(b_f, hs), v3(eB, hs), ALU.mult), reads=[b_f, eB], writes=[Bx])
        k.op("dve", lambda e: e.tensor_tensor(Kx[hs, :, hs], v3(kd_f, hs), v3(eB, hs), ALU.mult), reads=[kd_f, eB], writes=[Kx])
    k.barrier(); stmp.close()
    if RW_STOP == 4: return
    Rhat = k.sb("rwRhat", [128, NCH, 128], BF16, sd); Tt = k.sb("rwTt", [128, NCH, 128], BF16, sd)
    OV = k.sb("rwOV", [128, NCH, 64], F32, sd); HV = k.sb("rwHV", [128, NCH, 64], F32, sd)
    m2 = C["m2"][d]; m1 = C["m1"][d]
    with ExitStack() as s2:
        pMb = k.ps("rwpMb", [128, 256], F32, s2); pMk = k.ps("rwpMk", [128, 256], F32, s2); pMa = k.ps("rwpMa", [128, 128], F32, s2)
        pTr = k.ps("rwpTr", [128, 3, 128], BF16, s2); pY = k.ps("rwpY", [128, 192], F32, s2)
        pL = [k.ps("rwpL", [128, 256], F32, s2) for _ in range(2)]
        pX = k.ps("rwpX", [128, 512], F32, s2)
        MbS = k.sb("rwMbS", [128, 256], BF16, s2); MkS = k.sb("rwMkS", [128, 256], BF16, s2)
        Ls = [k.sb("rwLs", [128, 2, 128], F32, s2) for _ in range(2)]
        tm = k.sb("rwtm", [128, 3, 128], BF16, s2)
        Ys = [k.sb("rwY", [128, 192], F32, s2) for _ in range(2)]
        nZ = k.sb("rwnZ", [128, 192], BF16, s2)
        for c in range(NCH):
            arc = AR[:, c].rearrange("p a b -> p (a b)")
            k.op("pe", lambda e: e.matmul(pMb[:], lhsT=Bx[:, c, :], rhs=arc, start=True, stop=True), reads=[Bx, AR], writes=[pMb])
            k.op("pe", lambda e: e.matmul(pMk[:], lhsT=Kx[:, c, :], rhs=arc, start=True, stop=True), reads=[Kx, AR], writes=[pMk])
            k.op("pe", lambda e: e.matmul(pMa[:], lhsT=AR[:, c, 0, :], rhs=Bx[:, c, :], start=True, stop=True), reads=[AR, Bx], writes=[pMa])
            k.op("pe", lambda e: e.transpose(pTr[:, 0, :], Bx[:, c, :], C["ident_bf"][:]), reads=[Bx, C["ident_bf"]], writes=[pTr])
            k.op("pe", lambda e: e.transpose(pTr[:, 1, :], Kx[:, c, :], C["ident_bf"][:]), reads=[Kx, C["ident_bf"]], writes=[pTr])
            k.op("pe", lambda e: e.transpose(pTr[:, 2, :], AR[:, c, 0, :], C["ident_bf"][:]), reads=[AR, C["ident_bf"]], writes=[pTr])
            k.op("dve", lambda e: e.tensor_tensor(MbS[:], pMb[:], m2, ALU.mult), reads=[pMb, C["m2t"]], writes=[MbS])
            k.op("dve", lambda e: e.tensor_tensor(MkS[:], pMk[:], m2, ALU.mult), reads=[pMk, C["m2t"]], writes=[MkS])
            L0 = Ls[0]
            k.op("dve", lambda e: e.tensor_tensor(L0[:, 0, :], pMa[:], m1, ALU.mult), reads=[pMa, C["m1t"]], writes=[L0])
            k.op("dve", lambda e: e.tensor_tensor(L0[:, 1, :], pMb[:, 0:128], m2[:, 0:128], ALU.mult), reads=[pMb, C["m2t"]], writes=[L0])
            k.op("act", lambda e: e.copy(tm[:], pTr[:]), reads=[pTr], writes=[tm])
            if RW_STOP == 51: continue
            Y = Ys[0]
            k.op("pe", lambda e: e.matmul(pY[:, 0:64], lhsT=MkS[:, 0:128], rhs=vst[:, c, :], start=True, stop=True), reads=[MkS, vst], writes=[pY])
            k.op("act", lambda e: e.copy(Y[:, 0:128], tm[:, 2, :]), reads=[tm], writes=[Y])
            k.op("act", lambda e: e.copy(Y[:, 128:192], pY[:, 0:64]), reads=[pY], writes=[Y])
            if RW_STOP == 52: continue
            cur = 0
            for lvl in range(6):
                Lc = Ls[cur % 2]; Yc = Ys[lvl % 2]; Yn = Ys[(lvl + 1) % 2]
                k.op("pe", lambda e: e.matmul(pY[:], lhsT=Lc[:, 1, :], rhs=Yc[:], start=True, stop=True), reads=[Lc, Yc], writes=[pY])
                k.op("dve", lambda e: e.tensor_tensor(Yn[:], Yc[:], pY[:], ALU.subtract if lvl == 0 else ALU.add), reads=[Yc, pY], writes=[Yn])
                if lvl < 5:
                    Ln = Ls[(cur + 1) % 2]; pl = pL[lvl % 2]
                    k.op("pe", lambda e: e.matmul(pl[:, 128:256], lhsT=Lc[:, 0, :], rhs=Lc[:, 1, :], start=True, stop=True), reads=[Lc], writes=[pl])
                    if lvl < 4:
                        k.op("pe", lambda e: e.matmul(pl[:, 0:128], lhsT=Lc[:, 1, :], rhs=Lc[:, 0, :], start=True, stop=True), reads=[Lc], writes=[pl])
                        k.op("act", lambda e: e.copy(Ln[:].rearrange("p a b -> p (a b)"), pl[:]), reads=[pl], writes=[Ln])
                    else:
                        k.op("act", lambda e: e.copy(Ln[:, 1, :], pl[:, 128:256]), reads=[pl], writes=[Ln])
                    cur += 1
            if RW_STOP == 53: continue
            k.op("dve", lambda e: e.tensor_scalar(nZ[:], Ys[0][:], -1.0, None, op0=ALU.mult), reads=[Ys[0]], writes=[nZ])
            if RW_STOP == 54: continue
            idb = C["ident_bf"]
            k.op("pe", lambda e: e.matmul(pX[:, 0:128], lhsT=idb[:], rhs=AR[:, c, 1, :], start=True, stop=False), reads=[idb, AR], writes=[pX])
            k.op("pe", lambda e: e.matmul(pX[:, 0:128], lhsT=nZ[:, 0:128], rhs=MbS[:, 128:256], start=False, stop=True), reads=[nZ, MbS], writes=[pX])
            k.op("pe", lambda e: e.matmul(pX[:, 128:256], lhsT=idb[:], rhs=idb[:], start=True, stop=False), reads=[idb], writes=[pX])
            k.op("pe", lambda e: e.matmul(pX[:, 128:256], lhsT=nZ[:, 0:128], rhs=tm[:, 0, :], start=False, stop=True), reads=[nZ, tm], writes=[pX])
            k.op("pe", lambda e: e.matmul(pX[:, 256:320], lhsT=MkS[:, 128:256], rhs=vst[:, c, :], start=True, stop=False), reads=[MkS, vst], writes=[pX])
            k.op("pe", lambda e: e.matmul(pX[:, 256:320], lhsT=MbS[:, 128:256], rhs=nZ[:, 128:192], start=False, stop=True), reads=[MbS, nZ], writes=[pX])
            k.op("pe", lambda e: e.matmul(pX[:, 320:384], lhsT=tm[:, 1, :], rhs=vst[:, c, :], start=True, stop=False), reads=[tm, vst], writes=[pX])
            k.op("pe", lambda e: e.matmul(pX[:, 320:384], lhsT=tm[:, 0, :], rhs=nZ[:, 128:192], start=False, stop=True), reads=[tm, nZ], writes=[pX])
            if RW_STOP == 55: continue
            k.op("dve", lambda e: e.tensor_copy(Rhat[:, c, :], pX[:, 0:128]), reads=[pX], writes=[Rhat])
            if RW_STOP == 56: continue
            k.op("dve", lambda e: e.tensor_copy(Tt[:, c, :], pX[:, 128:256]), reads=[pX], writes=[Tt])
            if RW_STOP == 57: continue
            k.op("act", lambda e: e.copy(OV[:, c, :], pX[:, 256:320]), reads=[pX], writes=[OV])
            if RW_STOP == 58: continue
            k.op("act", lambda e: e.copy(HV[:, c, :], pX[:, 320:384]), reads=[pX], writes=[HV])
        k.barrier()
    if RW_STOP in (5, 51, 52, 53, 54, 55, 56, 57, 58): return
    with ExitStack() as s2:
        H = k.sb("rwH", [128, 64], F32, s2); Hs = k.sb("rwHs", [128, 64], BF16, s2)
        pO = [k.ps("rwpO", [128, 64], F32, s2) for _ in range(2)]; pH = k.ps("rwpH", [128, 64], F32, s2)
        k.op("dve", lambda e: e.memset(H[:], 0.0), writes=[H])
        order = list(range(NCH)) if d == 0 else [3, 2, 1, 0] + list(range(NCH - 1, 3, -1))
        for i, c in enumerate(order):
            po = pO[i % 2]
            k.op("dve", lambda e: e.tensor_scalar(Hs[:], H[:], gc[:, c:c + 1], None, op0=ALU.mult), reads=[H, gc], writes=[Hs])
            k.op("pe", lambda e: e.matmul(po[:], lhsT=Rhat[:, c, :], rhs=Hs[:], start=True, stop=True), reads=[Rhat, Hs], writes=[po])
            k.op("pe", lambda e: e.matmul(pH[:], lhsT=Tt[:, c, :], rhs=Hs[:], start=True, stop=True), reads=[Tt, Hs], writes=[pH])
            k.op("dve", lambda e: e.tensor_tensor(H[:], pH[:], HV[:, c, :], ALU.add), reads=[pH, HV], writes=[H])
            if d == 0:
                k.op("dve", lambda e: e.tensor_tensor(o_all[:, c, :], po[:], OV[:, c, :], ALU.add), reads=[po, OV], writes=[o_all])
            else:
                k.op("dve", lambda e: e.tensor_tensor(OV[:, c, :], po[:], OV[:, c, :], ALU.add), reads=[po, OV], writes=[OV])
                k.op("pool", lambda e: e.tensor_tensor(o_all[:, c, :], o_all[:, c, :], OV[:, c, :], ALU.add), reads=[o_all, OV], writes=[o_all])
        k.barrier()


def load_consts(k, st, CD):
    C = {}
    C["ident_f"] = k.sb("identf", [128, 128], F32, st); C["ident_bf"] = k.sb("identbf", [128, 128], BF16, st)
    C["ones_bf"] = k.sb("onesbf", [128, 128], BF16, st)
    k.dma("sp", C["ident_f"][:], CD["ident"][:], writes=[C["ident_f"]])
    k.op("dve", lambda e: e.tensor_copy(C["ident_bf"][:], C["ident_f"][:]), reads=[C["ident_f"]], writes=[C["ident_bf"]])
    k.op("dve", lambda e: e.memset(C["ones_bf"][:], 1.0), writes=[C["ones_bf"]])
    C["cos_d"] = CD["cos"]; C["sin_d"] = CD["sin"]
    m2t = k.sb("rwm2", [128, 2, 256], F32, st); m1t = k.sb("rwm1", [128, 2, 128], F32, st)
    k.dma("sp", m2t[:], CD["rw_m2"].t.rearrange("d p c -> p d c"), writes=[m2t])
    k.dma("sp", m1t[:], CD["rw_m1"].t.rearrange("d p c -> p d c"), writes=[m1t])
    C["m2t"] = m2t; C["m1t"] = m1t; C["m2"] = [m2t[:, 0, :], m2t[:, 1, :]]; C["m1"] = [m1t[:, 0, :], m1t[:, 1, :]]
    C["blk"] = k.sb("rwblk", [128, 128], F32, st); C["isel"] = k.sb("rwisel", [128, 64], BF16, st)
    k.dma("sp", C["blk"][:], CD["rw_blk"][:], writes=[C["blk"]])
    k.dma("pool", C["isel"][:], CD["rw_isel"][:], writes=[C["isel"]])
    return C


def const_arrays():
    cos, sin = rope_tables()
    m2, m1, blk, isel = rw_consts_host()
    return {"ident": np.eye(128, dtype=np.float32), "cos": cos, "sin": sin, "rw_m2": m2, "rw_m1": m1, "rw_blk": blk, "rw_isel": isel,
            "na_mask": na_mask_host()}


CONST_SHAPES = {"ident": [128, 128], "cos": [SEQ, 128], "sin": [SEQ, 128], "rw_m2": [2, 128, 256], "rw_m1": [2, 128, 128], "rw_blk": [128, 128],
                "rw_isel": [128, 64], "na_mask": [128, 4, 64]}


def phase_post(k, l, b, NB, need_ctx, x_src, x_src_tk, mod_d, yrw_d, ymla_d, yna_d, W, C, x1_d, hT_d, GT_d, col0):
    with ExitStack() as st:
        wo_rw = k.sb("worw", [64, 4, D], BF16, st); wo_mla = k.sb("womla", [128, 4, D], BF16, st); wo_na = k.sb("wona", [64, 4, D], BF16, st)
        wv = W["w_out"].t[l]
        k.dma("pool", wo_rw[:], wv[0:256, :].rearrange("(h p) n -> p h n", p=64), writes=[wo_rw])
        k.dma("pool", wo_mla[:], wv[256:768, :].rearrange("(h p) n -> p h n", p=128), writes=[wo_mla])
        k.dma("pool", wo_na[:], wv[768:1024, :].rearrange("(h p) n -> p h n", p=64), writes=[wo_na])
        rwt = k.sb("routw", [128, 8, NE], BF16, st); rbt = k.sb("routb", [128, NE], F32, st)
        k.dma("pool", rwt[:], W["router_w"].t[l].rearrange("(kc p) n -> p kc n", p=128), writes=[rwt])
        k.dma("sp", rbt[:], W["router_b"].t[l:l + 1, :].partition_broadcast(128), writes=[rbt])
        bc = {}
        for nm, src in (("g1", W["ln1_g"].t[l:l + 1, :]), ("b1", W["ln1_b"].t[l:l + 1, :]),
                        ("ga_l", mod_d[l][b:b + 1, 2 * D:3 * D]), ("sf_l", mod_d[l][b:b + 1, 3 * D:4 * D]), ("cf_l", mod_d[l][b:b + 1, 4 * D:5 * D]),
                        ("ga_c", mod_d[l][NB:NB + 1, 2 * D:3 * D]), ("sf_c", mod_d[l][NB:NB + 1, 3 * D:4 * D]), ("cf_c", mod_d[l][NB:NB + 1, 4 * D:5 * D])):
            if nm.endswith("_c") and not need_ctx:
                continue
            tle = k.sb(nm, [128, D], F32, st)
            k.dma("sp", tle[:], src.partition_broadcast(128), reads=[mod_d], writes=[tle])
            bc[nm] = tle
        for nm in ("cf_l", "cf_c"):
            if nm in bc:
                tle = bc[nm]
                k.op("pool", lambda e: e.tensor_scalar(tle[:], tle[:], 1.0, None, op0=ALU.add), reads=[tle], writes=[tle])
        yr = k.sb("yrall", [64, 4, T], BF16, st); ym = k.sb("ymall", [128, 4, T], BF16, st); yn = k.sb("ynall", [64, 4, T], BF16, st)
        k.dma("sp", yr[:], yrw_d[:], reads=[yrw_d], writes=[yr])
        k.dma("sp", ym[:], ymla_d.t.rearrange("h p t -> p h t"), reads=[ymla_d], writes=[ym])
        k.dma("sp", yn[:], yna_d[:], reads=[yna_d], writes=[yn])
        xts = [k.sb("pxt", [128, D], F32, st) for _ in range(2)]
        t1 = k.sb("pt1", [128, D], F32, st); u = k.sb("pu", [128, D], F32, st); x1 = k.sb("px1", [128, D], F32, st); hb = k.sb("phb", [128, D], BF16, st)
        st6 = k.sb("pst6", [128, 2, 6], F32, st); mv = k.sb("pmv", [128, 2], F32, st); rstd = k.sb("prstd", [128, 1], F32, st)
        hT = k.sb("phT", [128, 8, 128], BF16, st)
        lg = k.sb("plg", [128, NE], F32, st); mx8 = k.sb("pmx8", [128, 8], F32, st); nm1 = k.sb("pnm1", [128, 1], F32, st)
        msk = k.sb("pmsk", [128, NE], F32, st); ex = k.sb("pex", [128, NE], F32, st); ssum = k.sb("pssum", [128, 1], F32, st); gts = k.sb("pgts", [32, 128], F32, st)
        py = [k.ps("ppy", [128, 512], F32, st) for _ in range(2)]
        pT = k.ps("ppT", [128, 8, 128], BF16, st); pl = k.ps("ppl", [128, NE], F32, st); pg = k.ps("ppg", [32, 128], F32, st)
        for tt in range(NTT):
            isctx = tt < 2
            if isctx and not need_ctx:
                continue
            sfx = "_c" if isctx else "_l"
            sl = slice(tt * 128, (tt + 1) * 128)
            xt = xts[tt % 2]
            k.dma("sp", xt[:], x_src(tt), reads=[x_src_tk], writes=[xt])
            for half in range(2):
                ps = py[half]; cs = slice(half * 512, (half + 1) * 512)
                ops = [(yr, wo_rw, h) for h in range(4)] + [(ym, wo_mla, h) for h in range(4)] + [(yn, wo_na, h) for h in range(4)]
                for i, (ya, wa, h) in enumerate(ops):
                    k.op("pe", lambda e: e.matmul(ps[:], lhsT=ya[:, h, sl], rhs=wa[:, h, cs], start=(i == 0), stop=(i == 11)), reads=[ya, wa], writes=[ps])
                k.op("dve", lambda e: e.tensor_tensor(t1[:, cs], ps[:], bc["ga" + sfx][:, cs], ALU.mult), reads=[ps, bc["ga" + sfx]], writes=[t1])
            k.op("dve", lambda e: e.scalar_tensor_tensor(u[:], xt[:], DN_ALPHA, t1[:], op0=ALU.mult, op1=ALU.add), reads=[xt, t1], writes=[u])
            ln_stats(k, u, st6, mv, rstd, 1e-5)
            k.op("dve", lambda e: e.scalar_tensor_tensor(t1[:], u[:], mv[:, 0:1], bc["g1"][:], op0=ALU.subtract, op1=ALU.mult), reads=[u, mv, bc["g1"]], writes=[t1])
            k.op("dve", lambda e: e.scalar_tensor_tensor(x1[:], t1[:], rstd[:], bc["b1"][:], op0=ALU.mult, op1=ALU.add), reads=[t1, rstd, bc["b1"]], writes=[x1])
            k.dma("sp", x1_d[sl, :], x1[:], reads=[x1], writes=[x1_d])
            ln_stats(k, x1, st6, mv, rstd, 1e-5)
            k.op("dve", lambda e: e.scalar_tensor_tensor(t1[:], x1[:], mv[:, 0:1], bc["cf" + sfx][:], op0=ALU.subtract, op1=ALU.mult), reads=[x1, mv, bc["cf" + sfx]], writes=[t1])
            k.op("dve", lambda e: e.scalar_tensor_tensor(hb[:], t1[:], rstd[:], bc["sf" + sfx][:], op0=ALU.mult, op1=ALU.add), reads=[t1, rstd, bc["sf" + sfx]], writes=[hb])
            for c in range(8):
                k.op("pe", lambda e: e.transpose(pT[:, c, :], hb[:, c * 128:(c + 1) * 128], C["ident_bf"][:]), reads=[hb, C["ident_bf"]], writes=[pT])
            k.op("act", lambda e: e.copy(hT[:], pT[:]), reads=[pT], writes=[hT])
            k.dma("sp", hT_d.t[:, col0 + tt * 128:col0 + (tt + 1) * 128].rearrange("(kc p) t -> p kc t", p=128), hT[:], reads=[hT], writes=[hT_d])
            for kc in range(8):
                k.op("pe", lambda e: e.matmul(pl[:], lhsT=hT[:, kc, :], rhs=rwt[:, kc, :], start=(kc == 0), stop=(kc == 7)), reads=[hT, rwt], writes=[pl])
            k.op("dve", lambda e: e.tensor_tensor(lg[:], pl[:], rbt[:], ALU.add), reads=[pl, rbt], writes=[lg])
            k.op("dve", lambda e: e.max(mx8[:], lg[:]), reads=[lg], writes=[mx8])
            k.op("dve", lambda e: e.tensor_scalar(msk[:], lg[:], mx8[:, 3:4], None, op0=ALU.is_ge), reads=[lg, mx8], writes=[msk])
            k.op("dve", lambda e: e.tensor_scalar(nm1[:], mx8[:, 0:1], -1.0, None, op0=ALU.mult), reads=[mx8], writes=[nm1])
            k.op("act", lambda e: e.activation(ex[:], lg[:], AF.Exp, bias=nm1[:]), reads=[lg, nm1], writes=[ex])
            k.op("dve", lambda e: e.tensor_tensor(ex[:], ex[:], msk[:], ALU.mult), reads=[ex, msk], writes=[ex])
            k.op("dve", lambda e: e.reduce_sum(ssum[:], ex[:], axis=AX.X), reads=[ex], writes=[ssum])
            k.op("dve", lambda e: e.reciprocal(ssum[:], ssum[:]), reads=[ssum], writes=[ssum])
            k.op("dve", lambda e: e.tensor_scalar(ex[:], ex[:], ssum[:, 0:1], None, op0=ALU.mult), reads=[ex, ssum], writes=[ex])
            k.op("pe", lambda e: e.transpose(pg[:], ex[:], C["ident_f"][:]), reads=[ex, C["ident_f"]], writes=[pg])
            k.op("act", lambda e: e.copy(gts[:], pg[:]), reads=[pg], writes=[gts])
            k.dma("sp", GT_d[:, col0 + tt * 128:col0 + (tt + 1) * 128], gts[:], reads=[gts], writes=[GT_d])
    k.barrier()


def phase_moe(k, l, NB, groups, tile_io, mod_d, W, C, hT_d, GT_d):
    GP = 2
    with ExitStack() as st:
        bgl = k.sb("bgl", [128, NE, 8], F32, st); bln = k.sb("bln", [128, NE, 8], F32, st)
        bdn = k.sb("bdn", [NE, D], F32, st)
        k.dma("sp", bdn[:], W["b_dn"].t[l], writes=[bdn])
        with ExitStack() as s2:
            bgu = k.sb("bgu", [NE, 2 * D], F32, s2)
            pb = [k.ps("pbg", [128, NE], F32, s2) for _ in range(2)]
            k.dma("sp", bgu[:], W["b_gu"].t[l], writes=[bgu])
            for j in range(8):
                for par, dst in ((0, bgl), (1, bln)):
                    ps = pb[par]
                    k.op("pe", lambda e: e.transpose(ps[:], bgu[:, 256 * j + par:256 * (j + 1):2], C["ident_f"][0:NE, 0:NE]), reads=[bgu, C["ident_f"]], writes=[ps])
                    k.op("dve", lambda e: e.tensor_scalar(dst[:, :, j], ps[:], float(par), None, op0=ALU.add), reads=[ps], writes=[dst])
            k.barrier()
        gfs = [k.sb("gf%d" % i, [128, D], F32, st) for i in range(GP)]; gf_key = [None] * GP
        ln2g = k.sb("ln2g", [128, D], F32, st); ln2b = k.sb("ln2b", [128, D], F32, st)
        k.dma("sp", ln2g[:], W["ln2_g"].t[l:l + 1, :].partition_broadcast(128), writes=[ln2g])
        k.dma("sp", ln2b[:], W["ln2_b"].t[l:l + 1, :].partition_broadcast(128), writes=[ln2b])
        NU = 4
        units = [k.sb("wu%d" % i, [128, 8, 1024], BF16, st) for i in range(NU)]
        hTs = [k.sb("mhT%d" % i, [128, 8, 512], BF16, st) for i in range(GP)]
        accs = [k.sb("macc%d" % i, [128, 8, 512], F32, st) for i in range(GP)]
        gTs = [k.sb("mgT%d" % i, [NE, 512], F32, st) for i in range(GP)]
        acts = [k.sb("mact%d" % i, [128, 8, 512], BF16, st) for i in range(2)]
        gbcs = [k.sb("mgbc%d" % i, [128, 512], F32, st) for i in range(2)]
        tt_ = [k.sb("mt%d" % i, [128, 512], F32, st) for i in range(2)]
        ss_ = [k.sb("ms%d" % i, [128, 512], F32, st) for i in range(2)]
        uu_ = [k.sb("mu%d" % i, [128, 512], F32, st) for i in range(2)]
        psg = [k.ps("mpsg", [128, 512], F32, st) for _ in range(2)]
        psl = [k.ps("mpsl", [128, 512], F32, st) for _ in range(2)]
        psd = [k.ps("mpsd", [128, 512], F32, st) for _ in range(2)]
        st6 = k.sb("mst6", [128, 2, 6], F32, st); mv = k.sb("mmv", [128, 2], F32, st); rstd = k.sb("mrstd", [128, 1], F32, st)
        ucount = 0; it = 0; dcount = 0
        wgu = W["w_gu"].t[l]; wdn = W["w_dn"].t[l]
        for p0 in range(0, len(groups), GP):
            pg = groups[p0:p0 + GP]
            for gi, (col, n, bkey) in enumerate(pg):
                k.dma("sp", hTs[gi][:, :, 0:n], hT_d.t[:, col:col + n].rearrange("(kc p) t -> p kc t", p=128), reads=[hT_d], writes=[hTs[gi]])
                k.dma("sp", gTs[gi][:, 0:n], GT_d[:, col:col + n], reads=[GT_d], writes=[gTs[gi]])
                if gf_key[gi] != bkey:
                    row = NB if bkey == "c" else bkey
                    k.dma("sp", gfs[gi][:], mod_d[l][row:row + 1, 5 * D:6 * D].partition_broadcast(128), reads=[mod_d], writes=[gfs[gi]])
                    gf_key[gi] = bkey
            for e in range(NE):
                ua = units[ucount % NU]; ub = units[(ucount + 1) % NU]; ud = units[(ucount + 2) % NU]; ucount += 3
                k.dma("pool", ua[:], wgu[e][:, 0:1024].rearrange("(kc p) n -> p kc n", p=128), writes=[ua])
                k.dma("pool", ub[:], wgu[e][:, 1024:2048].rearrange("(kc p) n -> p kc n", p=128), writes=[ub])
                k.dma("pool", ud[:], wdn[e].rearrange("(kc p) n -> p kc n", p=128), writes=[ud])
                for gi, (col, n, bkey) in enumerate(pg):
                    hTg = hTs[gi]; acc = accs[gi]
                    act = acts[it % 2]; gbc = gbcs[it % 2]; it += 1
                    k.dma("act", gbc[:, 0:n], GT_d[e:e + 1, col:col + n].partition_broadcast(128), reads=[GT_d], writes=[gbc])
                    for j in range(8):
                        un = ua if j < 4 else ub; c0 = (j % 4) * 256
                        pg_ = psg[j % 2]; pl_ = psl[j % 2]; t_ = tt_[j % 2]; s_ = ss_[j % 2]; u_ = uu_[j % 2]
                        for kc in range(8):
                            k.op("pe", lambda e_: e_.matmul(pg_[:, 0:n], lhsT=un[:, kc, c0:c0 + 256:2], rhs=hTg[:, kc, 0:n], start=(kc == 0), stop=(kc == 7)), reads=[un, hTg], writes=[pg_])
                        for kc in range(8):
                            k.op("pe", lambda e_: e_.matmul(pl_[:, 0:n], lhsT=un[:, kc, c0 + 1:c0 + 256:2], rhs=hTg[:, kc, 0:n], start=(kc == 0), stop=(kc == 7)), reads=[un, hTg], writes=[pl_])
                        k.op("dve", lambda e_: e_.tensor_scalar(t_[:, 0:n], pg_[:, 0:n], bgl[:, e, j:j + 1], 7.0, op0=ALU.add, op1=ALU.min), reads=[pg_, bgl], writes=[t_])
                        k.op("act", lambda e_: e_.activation(s_[:, 0:n], t_[:, 0:n], AF.Sigmoid, scale=1.702), reads=[t_], writes=[s_])
                        k.op("dve", lambda e_: e_.tensor_scalar(u_[:, 0:n], pl_[:, 0:n], bln[:, e, j:j + 1], 8.0, op0=ALU.add, op1=ALU.min), reads=[pl_, bln], writes=[u_])
                        k.op("pool", lambda e_: e_.tensor_tensor(t_[:, 0:n], t_[:, 0:n], s_[:, 0:n], ALU.mult), reads=[t_, s_], writes=[t_])
                        k.op("pool", lambda e_: e_.tensor_scalar(u_[:, 0:n], u_[:, 0:n], -6.0, None, op0=ALU.max), reads=[u_], writes=[u_])
                        k.op("pool", lambda e_: e_.tensor_tensor(u_[:, 0:n], u_[:, 0:n], t_[:, 0:n], ALU.mult), reads=[u_, t_], writes=[u_])
                        k.op("dve", lambda e_: e_.tensor_tensor(act[:, j, 0:n], u_[:, 0:n], gbc[:, 0:n], ALU.mult), reads=[u_, gbc], writes=[act])
                    for oc in range(8):
                        pd = psd[dcount % 2]; dcount += 1
                        for j in range(8):
                            k.op("pe", lambda e_: e_.matmul(pd[:, 0:n], lhsT=ud[:, j, oc * 128:(oc + 1) * 128], rhs=act[:, j, 0:n], start=(j == 0), stop=(j == 7)), reads=[ud, act], writes=[pd])
                        if e == 0:
                            k.op("act", lambda e_: e_.copy(acc[:, oc, 0:n], pd[:, 0:n]), reads=[pd], writes=[acc])
                        else:
                            k.op("dve", lambda e_: e_.tensor_tensor(acc[:, oc, 0:n], acc[:, oc, 0:n], pd[:, 0:n], ALU.add), reads=[acc, pd], writes=[acc])
            for gi, (col, n, bkey) in enumerate(pg):
                acc = accs[gi]; gT = gTs[gi]; gf = gfs[gi]
                for oc in range(8):
                    pd = psd[dcount % 2]; dcount += 1
                    k.op("pe", lambda e_: e_.matmul(pd[:, 0:n], lhsT=bdn[:, oc * 128:(oc + 1) * 128], rhs=gT[:, 0:n], start=True, stop=True), reads=[bdn, gT], writes=[pd])
                    k.op("dve", lambda e_: e_.tensor_tensor(acc[:, oc, 0:n], acc[:, oc, 0:n], pd[:, 0:n], ALU.add), reads=[acc, pd], writes=[acc])
                for ti in range(n // 128):
                    x1_ap, x1_tk, out_ap, out_tk = tile_io(col + ti * 128)
                    for half in range(2):
                        ps = psg[half]; cs = slice(half * 512, (half + 1) * 512)
                        k.dma("sp", ss_[half][:], x1_ap[:, cs], reads=[x1_tk], writes=[ss_[half]])
                        for c in range(4):
                            oc = half * 4 + c
                            k.op("pe", lambda e_: e_.transpose(ps[:, c * 128:(c + 1) * 128], acc[:, oc, ti * 128:(ti + 1) * 128], C["ident_f"][:]), reads=[acc, C["ident_f"]], writes=[ps])
                        k.op("dve", lambda e_: e_.tensor_tensor(tt_[half][:], ps[:], gf[:, cs], ALU.mult), reads=[ps, gf], writes=[tt_[half]])
                        k.op("dve", lambda e_: e_.scalar_tensor_tensor(uu_[half][:], ss_[half][:], DN_ALPHA, tt_[half][:], op0=ALU.mult, op1=ALU.add), reads=[ss_[half], tt_[half]], writes=[uu_[half]])
                        k.op("dve", lambda e_: e_.bn_stats(st6[:, half, :], uu_[half][:]), reads=[uu_[half]], writes=[st6])
                    k.op("dve", lambda e_: e_.bn_aggr(mv[:], st6[:].rearrange("p a b -> p (a b)")), reads=[st6], writes=[mv])
                    k.op("dve", lambda e_: e_.tensor_scalar(rstd[:], mv[:, 1:2], 1e-5, None, op0=ALU.add), reads=[mv], writes=[rstd])
                    k.op("act", lambda e_: e_.sqrt(rstd[:], rstd[:]), reads=[rstd], writes=[rstd])
                    k.op("dve", lambda e_: e_.reciprocal(rstd[:], rstd[:]), reads=[rstd], writes=[rstd])
                    for half in range(2):
                        cs = slice(half * 512, (half + 1) * 512)
                        k.op("dve", lambda e_: e_.scalar_tensor_tensor(tt_[half][:], uu_[half][:], mv[:, 0:1], ln2g[:, cs], op0=ALU.subtract, op1=ALU.mult), reads=[uu_[half], mv, ln2g], writes=[tt_[half]])
                        k.op("dve", lambda e_: e_.scalar_tensor_tensor(uu_[half][:], tt_[half][:], rstd[:], ln2b[:, cs], op0=ALU.mult, op1=ALU.add), reads=[tt_[half], rstd, ln2b], writes=[uu_[half]])
                        k.dma("sp", out_ap[:, cs], uu_[half][:], reads=[uu_[half]], writes=[out_tk])
    k.barrier()


WSHAPES = {"ada_w": [2, D, 6 * D], "ada_b": [2, 6 * D], "w_in": [2, D, IN_COLS],
           "rw_mu": [2, 2, 1152], "rw_w0": [2, 2, 256], "rw_w2": [2, 2, 64, 256], "rw_a0": [2, 2, 256], "rw_a2": [2, 2, 64, 256], "rw_g2": [2, 128, 256],
           "rw_kk": [2, 256], "rw_ka": [2, 256], "rw_rk": [2, 4, 64], "rw_gn_w": [2, 256], "rw_gn_b": [2, 256],
           "mla_q_norm": [2, 256], "mla_kv_norm": [2, 128], "mla_w_uq": [2, 256, 768], "mla_w_ukv": [2, 128, 1024],
           "w_out": [2, D, D], "ln1_g": [2, D], "ln1_b": [2, D], "router_w": [2, D, NE], "router_b": [2, NE],
           "w_gu": [2, NE, D, 2 * D], "b_gu": [2, NE, 2 * D], "w_dn": [2, NE, D, D], "b_dn": [2, NE, D], "ln2_g": [2, D], "ln2_b": [2, D]}


def build_nc(NB, debug=False, layers=(0, 1), moe=True):
    nc = bass.Bass("TRN2", target_bir_lowering=False)
    with ExitStack() as st:
        k = K(nc, st)
        kin = "ExternalInput"; sk = "ExternalOutput" if debug else "Internal"
        CD = {n: k.dram("c_" + n, sh, F32, kin) for n, sh in CONST_SHAPES.items()}
        W = {n: k.dram(n, sh, F32, kin) for n, sh in WSHAPES.items()}
        xin = k.dram("xin", [NB, T, D], F32, kin)
        condT = k.dram("condT", [128, 8, NB + 1], F32, kin)
        na_bias = k.dram("na_bias", [2, 128, 4, 8, 4, 64], F32, kin)
        out_d = k.dram("out", [NB, SEQ, D], F32, "ExternalOutput")
        mod_d = k.dram("mod_d", [2, NB + 1, 6 * D], F32, sk)
        pT_rw_d = k.dram("pT_rw_d", [RW_COLS, T], F32, sk); pT_na_d = k.dram("pT_na_d", [512, T], BF16, sk); p_tm_d = k.dram("p_tm_d", [T, 704], F32, sk)
        yrw_d = k.dram("yrw_d", [64, 4, T], BF16, sk); ymla_d = k.dram("ymla_d", [4, 128, T], BF16, sk); yna_d = k.dram("yna_d", [64, 4, T], BF16, sk)
        x1_d = k.dram("x1_d", [NB * T, D], F32, sk); x2_d = k.dram("x2_d", [NB, T, D], F32, sk)
        hT_d = k.dram("hT_d", [D, NB * T], BF16, sk); GT_d = k.dram("GT_d", [NE, NB * T], F32, sk)
        C = load_consts(k, st, CD)
        k.barrier()
        for l in layers:
            need_ctx = l < DEPTH - 1
            phase_mod(k, l, NB, condT, W["ada_w"], W["ada_b"], mod_d)
            for b in range(NB):
                if l == 0:
                    x_src = (lambda tt, b=b: xin.t[b][tt * 128:(tt + 1) * 128, :]); x_tk = xin
                else:
                    x_src = (lambda tt, b=b: x2_d.t[b][tt * 128:(tt + 1) * 128, :]); x_tk = x2_d
                with ExitStack() as ps:
                    w_in_bf = k.sb("winbf", [128, 8, IN_COLS], BF16, ps)
                    load_w_in(k, l, W["w_in"], w_in_bf)
                    phase_inproj(k, l, b, NB, x_src, x_tk, mod_d, w_in_bf, C["ident_bf"], pT_rw_d, pT_na_d, p_tm_d)
                with ExitStack() as ps:
                    RW = load_rw_w(k, l, ps, W, C["ident_f"])
                    phase_rwkv(k, pT_rw_d, RW, C, yrw_d)
                with ExitStack() as ps:
                    wuq, wukv = load_mla_w(k, l, ps, W["mla_w_uq"], W["mla_w_ukv"], W["mla_q_norm"], W["mla_kv_norm"])
                    phase_mla(k, need_ctx, p_tm_d, wuq, wukv, C["cos_d"], C["sin_d"], C["ident_bf"], C["ones_bf"], ymla_d)
                with ExitStack() as ps:
                    bm = load_na_bias(k, l, ps, na_bias, CD["na_mask"])
                    phase_na(k, need_ctx, pT_na_d, p_tm_d, bm, C["ones_bf"], yna_d)
                x1_b = Tk(x1_d.t[b * T:(b + 1) * T, :], "x1b")
                phase_post(k, l, b, NB, need_ctx, x_src, x_tk, mod_d, yrw_d, ymla_d, yna_d, W, C, x1_b, hT_d, GT_d, b * T)
            groups = []
            for b in range(NB):
                if need_ctx:
                    groups.append((b * T, 256, "c"))
                for i in range(4):
                    groups.append((b * T + 256 + i * 512, 512, b))
            groups.sort(key=lambda g: -g[1])

            def tile_io(col, l=l):
                b = col // T; t = col % T
                x1_ap = x1_d.t[col:col + 128, :]
                if l == DEPTH - 1:
                    return x1_ap, x1_d, out_d.t[b][t - NCTX:t - NCTX + 128, :], out_d
                return x1_ap, x1_d, x2_d.t[b][t:t + 128, :], x2_d
            if moe:
                phase_moe(k, l, NB, groups, tile_io, mod_d, W, C, hT_d, GT_d)
        k.barrier()
        print("ninst", k.ninst, k.cnt, flush=True)
    return nc


def core_inputs(inputs, b0, NB, shared):
    cc = np.concatenate([inputs["c"][b0:b0 + NB], inputs["c_ctx"][None]], 0).astype(np.float32)
    im = dict(shared)
    im["condT"] = np.ascontiguousarray(cc.T.reshape(8, 128, NB + 1).transpose(1, 0, 2))
    im["xin"] = np.ascontiguousarray(np.concatenate([inputs["ctx"][b0:b0 + NB], inputs["x"][b0:b0 + NB]], 1))
    return im


def shared_inputs(inputs):
    sh = {n: np.ascontiguousarray(np.asarray(inputs[n], np.float32)) for n in WSHAPES}
    for n, v in const_arrays().items():
        sh["c_" + n] = np.ascontiguousarray(v.astype(np.float32))
    sh["na_bias"] = na_bias_host(np.asarray(inputs["na_rpb"], np.float32))
    return sh


def kernel(**inputs):
    NCORES = 8; B = inputs["x"].shape[0]; NB = B // NCORES
    nc = build_nc(NB)
    sh = shared_inputs(inputs)
    in_maps = [core_inputs(inputs, c * NB, NB, sh) for c in range(NCORES)]
    res = run_bass_kernel_spmd(nc, in_maps, core_ids=list(range(NCORES)))
    return np.concatenate([r["out"] for r in res.results], 0).astype(np.float32)
```

```python
import numpy as np
from contextlib import ExitStack
import concourse.bass as bass
import concourse.mybir as mybir
from concourse.bass_utils import run_bass_kernel_spmd

F32 = mybir.dt.float32; BF16 = mybir.dt.bfloat16; I32 = mybir.dt.int32
AF = mybir.ActivationFunctionType; ALU = mybir.AluOpType; AX = mybir.AxisListType

D = 1024; SEQ = 2048; NCTX = 256; T = SEQ + NCTX; DEPTH = 2; NTT = T // 128
RW_COLS = 1152; MLA_COLS = 448; NA_COLS = 768; IN_COLS = 2368
NE = 32
NBLK_MAX = 104
DN_ALPHA = (2 * DEPTH) ** 0.25


class Tk:
    __slots__ = ("t", "w", "r", "name", "psum")
    def __init__(s, t, name="", psum=False):
        s.t = t; s.w = None; s.r = []; s.name = name; s.psum = psum
    def __getitem__(s, idx):
        return s.t[idx]


class K:
    NDMA = 6
    def __init__(s, nc, stack):
        s.nc = nc; s.stack = stack
        s.eng = {"pe": nc.tensor, "act": nc.scalar, "dve": nc.vector, "pool": nc.gpsimd, "sp": nc.sync}
        s.sem = {}; s.cnt = {}
        for e in s.eng:
            s.sem[e] = stack.enter_context(nc.semaphore("s_" + e)); s.cnt[e] = 0
        s.seen = {e: {} for e in s.eng}
        s.dsem = [stack.enter_context(nc.semaphore("d%d" % i)) for i in range(3 * s.NDMA)]
        s.dcnt = [0] * (3 * s.NDMA); s.dnext = {"sp": 0, "act": 0, "pool": 0}; s.dq = {"sp": 0, "act": 1, "pool": 2}
        s.ninst = 0; s.uid = 0
    def sb(s, name, shape, dt, stack=None):
        s.uid += 1
        t = Tk((stack or s.stack).enter_context(s.nc.sbuf_tensor("%s_%d" % (name, s.uid), list(shape), dt)), name)
        assert s.nc.sbuf_bytes_remaining >= 28 * 1024, "SBUF budget exceeded at %s: remaining %d" % (name, s.nc.sbuf_bytes_remaining)
        return t
    def ps(s, name, shape, dt=F32, stack=None):
        s.uid += 1
        isz = 4 if dt == F32 else 2
        free = int(np.prod(shape[1:])); nb = (free * isz + 2047) // 2048
        raw = (stack or s.stack).enter_context(s.nc.psum_tensor("%s_%d" % (name, s.uid), [128, nb * 2048 // isz], dt))
        ap = raw[0:shape[0], 0:free]
        if len(shape) == 3:
            ap = ap.rearrange("p (a b) -> p a b", a=shape[1])
        return Tk(ap, name, psum=True)
    def dram(s, name, shape, dt, kind="Internal"):
        return Tk(s.nc.dram_tensor(name, list(shape), dt, kind=kind).ap(), name)
    def _wait(s, e, toks):
        seen = s.seen[e]
        for (sem, val, key) in toks:
            if seen.get(key, 0) >= val:
                continue
            s.eng[e].wait_ge(sem, val); seen[key] = val; s.ninst += 1
    def _deps(s, e, reads, writes):
        toks = []
        for t in reads:
            if t.w is not None:
                toks.append(t.w)
            if t.psum:
                toks.extend(k for k in t.r if k[2] != e)
        for t in writes:
            if t.w is not None:
                toks.append(t.w)
            toks.extend(t.r)
        if e == "pe":
            toks = [k for k in toks if k[2] != "pe"]
        return toks
    def op(s, e, fn, reads=(), writes=()):
        s._wait(e, s._deps(e, reads, writes))
        ins = fn(s.eng[e]); s.cnt[e] += 1; ins.then_inc(s.sem[e], 1); s.ninst += 1
        tok = (s.sem[e], s.cnt[e], e)
        for t in reads:
            t.r = [k for k in t.r if k[2] != e]; t.r.append(tok)
        for t in writes:
            t.w = tok; t.r = []
        return ins
    def dma(s, q, out, in_, reads=(), writes=(), **kw):
        base = s.dq[q] * s.NDMA; i = base + s.dnext[q]; s.dnext[q] = (s.dnext[q] + 1) % s.NDMA
        key = "d%d" % i
        toks = s._deps(q, reads, writes)
        if s.dcnt[i] > 0:
            toks.append((s.dsem[i], s.dcnt[i], key))
        s._wait(q, toks)
        ins = s.eng[q].dma_start(out=out, in_=in_, **kw); s.dcnt[i] += 16; ins.then_inc(s.dsem[i], 16); s.ninst += 1
        tok = (s.dsem[i], s.dcnt[i], key)
        for t in reads:
            t.r.append(tok)
        for t in writes:
            t.w = tok; t.r = []
        return ins
    def idma(s, out, in_, out_offset=None, in_offset=None, reads=(), writes=(), **kw):
        q = "pool"
        base = s.dq[q] * s.NDMA; i = base + s.dnext[q]; s.dnext[q] = (s.dnext[q] + 1) % s.NDMA
        key = "d%d" % i
        toks = s._deps(q, reads, writes)
        if s.dcnt[i] > 0:
            toks.append((s.dsem[i], s.dcnt[i], key))
        s._wait(q, toks)
        ins = s.eng[q].indirect_dma_start(out=out, out_offset=out_offset, in_=in_, in_offset=in_offset, **kw)
        s.dcnt[i] += 16; ins.then_inc(s.dsem[i], 16); s.ninst += 1
        tok = (s.dsem[i], s.dcnt[i], key)
        for t in reads:
            t.r.append(tok)
        for t in writes:
            t.w = tok; t.r = []
        return ins
    def barrier(s):
        toks = [(s.sem[e], s.cnt[e], e) for e in s.eng if s.cnt[e] > 0]
        toks += [(s.dsem[i], s.dcnt[i], "d%d" % i) for i in range(len(s.dsem)) if s.dcnt[i] > 0]
        for e in s.eng:
            s._wait(e, [k for k in toks if k[2] != e])
    def finish(s, outs):
        s._wait("sp", [t.w for t in outs if t.w is not None])


def bcast_rows(ap_row, n):
    return ap_row.partition_broadcast(n)


def phase_mod(k, l, NB, condT_d, ada_w_d, ada_b_d, mod_d):
    with ExitStack() as st:
        R = NB + 1
        cond = k.sb("cond", [128, 8, R], F32, st); cond_bf = k.sb("condbf", [128, 8, R], BF16, st)
        adab = k.sb("adab", [R, 6 * D], F32, st); mod_sb = k.sb("modsb", [R, 6 * D], F32, st)
        wts = [k.sb("adaw", [128, 8, 512], BF16, st) for _ in range(2)]
        pss = [k.ps("modps", [R, 512], F32, st) for _ in range(2)]
        k.dma("sp", cond[:], condT_d[:], writes=[cond])
        k.dma("sp", adab[:], ada_b_d[l:l + 1, :].partition_broadcast(R), writes=[adab])
        k.op("act", lambda e: e.activation(cond_bf[:], cond[:], AF.Silu), reads=[cond], writes=[cond_bf])
        wv = ada_w_d.t[l].rearrange("(kc p) n -> p kc n", p=128)
        for g in range(12):
            wt = wts[g % 2]; ps = pss[g % 2]
            k.dma("pool", wt[:], wv[:, :, g * 512:(g + 1) * 512], writes=[wt])
            for kc in range(8):
                k.op("pe", lambda e: e.matmul(ps[:], lhsT=cond_bf[:, kc, :], rhs=wt[:, kc, :], start=(kc == 0), stop=(kc == 7)),
                     reads=[cond_bf, wt], writes=[ps])
            k.op("dve", lambda e: e.tensor_tensor(mod_sb[:, g * 512:(g + 1) * 512], ps[:], adab[:, g * 512:(g + 1) * 512], ALU.add),
                 reads=[ps, adab], writes=[mod_sb])
        k.dma("sp", mod_d[l], mod_sb[:], reads=[mod_sb], writes=[mod_d])
    k.barrier()


def ln_stats(k, xt, st6, mv, rstd, eps):
    for hh in range(2):
        k.op("dve", lambda e: e.bn_stats(st6[:, hh, :], xt[:, hh * 512:(hh + 1) * 512]), reads=[xt], writes=[st6])
    k.op("dve", lambda e: e.bn_aggr(mv[:], st6[:].rearrange("p a b -> p (a b)")), reads=[st6], writes=[mv])
    k.op("dve", lambda e: e.tensor_scalar(rstd[:], mv[:, 1:2], eps, None, op0=ALU.add), reads=[mv], writes=[rstd])
    k.op("act", lambda e: e.sqrt(rstd[:], rstd[:]), reads=[rstd], writes=[rstd])
    k.op("dve", lambda e: e.reciprocal(rstd[:], rstd[:]), reads=[rstd], writes=[rstd])


def phase_inproj(k, l, b, NB, x_src, x_src_tk, mod_d, w_in_bf, ident_bf, pT_rw_d, pT_na_d, p_tm_d):
    with ExitStack() as st:
        bc = {}
        for nm, row, off in (("sa_l", b, 0), ("ca_l", b, D), ("sa_c", NB, 0), ("ca_c", NB, D)):
            tle = k.sb(nm, [128, D], F32, st)
            k.dma("sp", tle[:], mod_d[l][row:row + 1, off:off + D].partition_broadcast(128), reads=[mod_d], writes=[tle])
            bc[nm] = tle
        for nm in ("ca_l", "ca_c"):
            tle = bc[nm]
            k.op("pool", lambda e: e.tensor_scalar(tle[:], tle[:], 1.0, None, op0=ALU.add), reads=[tle], writes=[tle])
        groups = [(0, 512), (512, 512), (1024, 512), (1536, 512), (2048, 256)]
        xmT = [k.sb("xmT%d" % g, [128, 8, n], BF16, st) for g, (o, n) in enumerate(groups)]
        xts = [k.sb("xt", [128, D], F32, st) for _ in range(2)]
        t1 = k.sb("t1", [128, D], F32, st); xm = k.sb("xm", [128, D], BF16, st)
        st6 = k.sb("st6", [128, 2, 6], F32, st); mv = k.sb("mv", [128, 2], F32, st); rstd = k.sb("rstd", [128, 1], F32, st)
        pTs = [k.ps("pT", [128, 8, 128], BF16, st) for _ in range(2)]
        ptm = [k.ps("ptm", [128, 512], F32, st) for _ in range(2)]
        pfm = [k.ps("pfm", [128, 512], F32, st) for _ in range(2)]
        tm_sb = [k.sb("tmsb", [128, 704], F32, st) for _ in range(2)]
        fm_sb = [k.sb("fmsb", [128, 512], F32, st) for _ in range(2)]
        fm_sbb = [k.sb("fmsbb", [128, 512], BF16, st) for _ in range(2)]
        for tt in range(NTT):
            isctx = tt < 2
            sa = bc["sa_c" if isctx else "sa_l"]; ca = bc["ca_c" if isctx else "ca_l"]
            xt = xts[tt % 2]
            k.dma("sp", xt[:], x_src(tt), reads=[x_src_tk], writes=[xt])
            ln_stats(k, xt, st6, mv, rstd, 1e-5)
            k.op("dve", lambda e: e.scalar_tensor_tensor(t1[:], xt[:], mv[:, 0:1], ca[:], op0=ALU.subtract, op1=ALU.mult),
                 reads=[xt, mv, ca], writes=[t1])
            k.op("dve", lambda e: e.scalar_tensor_tensor(xm[:], t1[:], rstd[:], sa[:], op0=ALU.mult, op1=ALU.add),
                 reads=[t1, rstd, sa], writes=[xm])
            pT = pTs[tt % 2]
            for c in range(8):
                k.op("pe", lambda e: e.transpose(pT[:, c, :], xm[:, c * 128:(c + 1) * 128], ident_bf[:]), reads=[xm, ident_bf], writes=[pT])
            g = tt // 4; o = (tt % 4) * 128
            xg = xmT[g]
            k.op("act", lambda e: e.copy(xg[:, :, o:o + 128], pT[:]), reads=[pT], writes=[xg])
            pa = ptm[0]; pb = ptm[1]
            for kc in range(8):
                k.op("pe", lambda e: e.matmul(pa[:, 0:448], lhsT=xg[:, kc, o:o + 128], rhs=w_in_bf[:, kc, 1152:1600], start=(kc == 0), stop=(kc == 7)),
                     reads=[xg, w_in_bf], writes=[pa])
            for kc in range(8):
                k.op("pe", lambda e: e.matmul(pb[:, 0:256], lhsT=xg[:, kc, o:o + 128], rhs=w_in_bf[:, kc, 2112:2368], start=(kc == 0), stop=(kc == 7)),
                     reads=[xg, w_in_bf], writes=[pb])
            ts = tm_sb[tt % 2]
            k.op("act", lambda e: e.copy(ts[:, 0:448], pa[:, 0:448]), reads=[pa], writes=[ts])
            k.op("dve", lambda e: e.tensor_copy(ts[:, 448:704], pb[:, 0:256]), reads=[pb], writes=[ts])
            k.dma("sp", p_tm_d[tt * 128:(tt + 1) * 128, :], ts[:], reads=[ts], writes=[p_tm_d])
        cnt = 0
        for g, (o, n) in enumerate(groups):
            xg = xmT[g]
            for c in range(13):
                col = c * 128 if c < 9 else 1600 + (c - 9) * 128
                ps = pfm[cnt % 2]
                for kc in range(8):
                    k.op("pe", lambda e: e.matmul(ps[:, 0:n], lhsT=w_in_bf[:, kc, col:col + 128], rhs=xg[:, kc, :], start=(kc == 0), stop=(kc == 7)),
                         reads=[w_in_bf, xg], writes=[ps])
                if c < 9:
                    fs = fm_sb[cnt % 2]
                    k.op("act" if cnt % 2 else "dve", (lambda e: e.copy(fs[:, 0:n], ps[:, 0:n])) if cnt % 2 else (lambda e: e.tensor_copy(fs[:, 0:n], ps[:, 0:n])),
                         reads=[ps], writes=[fs])
                    k.dma("sp", pT_rw_d[c * 128:(c + 1) * 128, o:o + n], fs[:, 0:n], reads=[fs], writes=[pT_rw_d])
                else:
                    fs = fm_sbb[cnt % 2]
                    k.op("act" if cnt % 2 else "dve", (lambda e: e.copy(fs[:, 0:n], ps[:, 0:n])) if cnt % 2 else (lambda e: e.tensor_copy(fs[:, 0:n], ps[:, 0:n])),
                         reads=[ps], writes=[fs])
                    k.dma("sp", pT_na_d[(c - 9) * 128:(c - 8) * 128, o:o + n], fs[:, 0:n], reads=[fs], writes=[pT_na_d])
                cnt += 1
    k.barrier()


def load_w_in(k, l, w_in_d, w_in_bf):
    wv = w_in_d.t[l].rearrange("(kc p) n -> p kc n", p=128)
    for (a, bb) in ((0, 1024), (1024, 2048), (2048, IN_COLS)):
        k.dma("pool", w_in_bf[:, :, a:bb], wv[:, :, a:bb], writes=[w_in_bf])


def load_mla_w(k, l, st, w_uq_d, w_ukv_d, qn_d, kvn_d):
    wuq = k.sb("wuq", [128, 2, 768], BF16, st); wukv = k.sb("wukv", [128, 1024], BF16, st)
    with ExitStack() as s2:
        wuq_f = k.sb("wuqf", [128, 2, 768], F32, s2); wukv_f = k.sb("wukvf", [128, 1024], F32, s2)
        qn = k.sb("qn", [128, 2], F32, s2); kvn = k.sb("kvn", [128, 1], F32, s2)
        k.dma("sp", wuq_f[:], w_uq_d.t[l].rearrange("(kc p) n -> p kc n", p=128), writes=[wuq_f])
        k.dma("sp", wukv_f[:], w_ukv_d.t[l], writes=[wukv_f])
        k.dma("sp", qn[:], qn_d.t[l].rearrange("(kc p) -> p kc", p=128), writes=[qn], allow_slow_non_contiguous=True)
        k.dma("sp", kvn[:], kvn_d.t[l].rearrange("(p o) -> p o", o=1), writes=[kvn])
        for kc in range(2):
            k.op("dve", lambda e: e.tensor_scalar(wuq[:, kc, :], wuq_f[:, kc, :], qn[:, kc:kc + 1], None, op0=ALU.mult), reads=[wuq_f, qn], writes=[wuq])
        k.op("dve", lambda e: e.tensor_scalar(wukv[:], wukv_f[:], kvn[:, 0:1], None, op0=ALU.mult), reads=[wukv_f, kvn], writes=[wukv])
        k.barrier()
    return wuq, wukv


def phase_mla(k, need_ctx, p_tm_d, wuq, wukv, cos_d, sin_d, ident_bf, ones_bf, ymla_d):
    scale = 192.0 ** -0.5
    with ExitStack() as st:
        cos_sb = k.sb("cos", [128, 16, 128], F32, st); sin_sb = k.sb("sin", [128, 16, 128], F32, st)
        k.dma("sp", cos_sb[:], cos_d.t.rearrange("(tt p) c -> p tt c", p=128), writes=[cos_sb])
        k.dma("sp", sin_sb[:], sin_d.t.rearrange("(tt p) c -> p tt c", p=128), writes=[sin_sb])
        p_all = k.sb("pall", [128, NTT, 448], F32, st)
        k.dma("sp", p_all[:], p_tm_d.t.rearrange("(tt p) c -> p tt c", p=128)[:, :, 0:448], reads=[p_tm_d], writes=[p_all])
        qlnT = k.sb("qlnT", [128, 2, T], BF16, st); kvnT = k.sb("kvnT", [128, T], BF16, st)
        qTn = k.sb("qTn", [128, 4, T], BF16, st); qTr = k.sb("qTr", [64, 4, T], BF16, st)
        knT = k.sb("knT", [128, 4, T], BF16, st); krT = k.sb("krT", [64, T], BF16, st)
        v_sb = k.sb("vsb", [128, NTT, 512], BF16, st)
        rq = k.sb("rq", [128, NTT], F32, st); rkv = k.sb("rkv", [128, NTT], F32, st)
        with ExitStack() as st1:
            sq = k.sb("sq", [128, NTT, 384], F32, st1)
            k.op("act", lambda e: e.activation(sq[:], p_all[:, :, 0:384], AF.Square), reads=[p_all], writes=[sq])
            k.op("dve", lambda e: e.reduce_sum(rq[:], sq[:, :, 0:256], axis=AX.X), reads=[sq], writes=[rq])
            k.op("dve", lambda e: e.reduce_sum(rkv[:], sq[:, :, 256:384], axis=AX.X), reads=[sq], writes=[rkv])
            k.barrier()
        k.op("dve", lambda e: e.tensor_scalar(rq[:], rq[:], 1.0 / 256, 1e-6, op0=ALU.mult, op1=ALU.add), reads=[rq], writes=[rq])
        k.op("dve", lambda e: e.tensor_scalar(rkv[:], rkv[:], 1.0 / 128, 1e-6, op0=ALU.mult, op1=ALU.add), reads=[rkv], writes=[rkv])
        for r_ in (rq, rkv):
            k.op("act", lambda e: e.sqrt(r_[:], r_[:]), reads=[r_], writes=[r_])
            k.op("dve", lambda e: e.reciprocal(r_[:], r_[:]), reads=[r_], writes=[r_])
        st2 = ExitStack()
        nrm = [k.sb("nrm", [128, 384], BF16, st2) for _ in range(2)]
        pTs = [k.ps("mlapT", [128, 3, 128], BF16, st2) for _ in range(1)]
        pq = k.ps("mlapq", [128, 4, 256], F32, st2)
        pv = k.ps("mlapv", [128, 512], F32, st2)
        q_sb = k.sb("qsb", [128, 4, 192], BF16, st2); kr_sb = k.sb("krsb", [128, 64], BF16, st2)
        ra = k.sb("ra", [128, 4, 32], F32, st2); rb = k.sb("rb", [128, 4, 32], F32, st2); qr_f = k.sb("qrf", [128, 4, 64], F32, st2)
        pqT = k.ps("mlapqT", [128, 4, 128], BF16, st2); pqTr = k.ps("mlapqTr", [64, 5, 128], BF16, st2)
        STOP = 99; SKIP = ""
        for tt in range(NTT if STOP > 1 else 0):
            isctx = tt < 2
            nt = nrm[tt % 2]; pT = pTs[0]
            k.op("dve", lambda e: e.tensor_scalar(nt[:, 0:256], p_all[:, tt, 0:256], rq[:, tt:tt + 1], None, op0=ALU.mult), reads=[p_all, rq], writes=[nt])
            k.op("dve", lambda e: e.tensor_scalar(nt[:, 256:384], p_all[:, tt, 256:384], rkv[:, tt:tt + 1], None, op0=ALU.mult), reads=[p_all, rkv], writes=[nt])
            for c in range(3):
                k.op("pe", lambda e: e.transpose(pT[:, c, :], nt[:, c * 128:(c + 1) * 128], ident_bf[:]), reads=[nt, ident_bf], writes=[pT])
            sl = slice(tt * 128, (tt + 1) * 128)
            k.op("act", lambda e: e.copy(qlnT[:, :, sl], pT[:, 0:2, :]), reads=[pT], writes=[qlnT])
            k.op("act", lambda e: e.copy(kvnT[:, sl], pT[:, 2, :]), reads=[pT], writes=[kvnT])
            if "v" not in SKIP: k.op("pe", lambda e: e.matmul(pv[:].rearrange("p (h c) -> p h c", h=4), lhsT=kvnT[:, sl],
                                          rhs=wukv[:].rearrange("p (h c) -> p h c", h=4)[:, :, 128:256], start=True, stop=True),
                 reads=[kvnT, wukv], writes=[pv])
            k.op("act", lambda e: e.copy(v_sb[:, tt, :], pv[:]), reads=[pv], writes=[v_sb])
            kre = p_all[:, tt, 384:448:2]; kro = p_all[:, tt, 385:448:2]
            if "k" in SKIP:
                pass
            elif isctx:
                k.op("dve", lambda e: e.tensor_copy(kr_sb[:, 0:32], kre), reads=[p_all], writes=[kr_sb])
                k.op("dve", lambda e: e.tensor_copy(kr_sb[:, 32:64], kro), reads=[p_all], writes=[kr_sb])
            else:
                cs = cos_sb[:, tt - 2, 0:32]; sn = sin_sb[:, tt - 2, 0:32]
                k.op("dve", lambda e: e.tensor_tensor(ra[:, 0, :], kre, cs, ALU.mult), reads=[p_all, cos_sb], writes=[ra])
                k.op("dve", lambda e: e.tensor_tensor(rb[:, 0, :], kro, sn, ALU.mult), reads=[p_all, sin_sb], writes=[rb])
                k.op("dve", lambda e: e.tensor_tensor(kr_sb[:, 0:32], ra[:, 0, :], rb[:, 0, :], ALU.subtract), reads=[ra, rb], writes=[kr_sb])
                k.op("dve", lambda e: e.tensor_tensor(ra[:, 0, :], kre, sn, ALU.mult), reads=[p_all, sin_sb], writes=[ra])
                k.op("dve", lambda e: e.tensor_tensor(rb[:, 0, :], kro, cs, ALU.mult), reads=[p_all, cos_sb], writes=[rb])
                k.op("dve", lambda e: e.tensor_tensor(kr_sb[:, 32:64], ra[:, 0, :], rb[:, 0, :], ALU.add), reads=[ra, rb], writes=[kr_sb])
            doq = ((not isctx) or need_ctx) and "q" not in SKIP
            if doq:
                for h in range(4):
                    for kc in range(2):
                        k.op("pe", lambda e: e.matmul(pq[:, h, 0:192], lhsT=qlnT[:, kc, sl], rhs=wuq[:, kc, h * 192:(h + 1) * 192], start=(kc == 0), stop=(kc == 1)),
                             reads=[qlnT, wuq], writes=[pq])
                if "a" not in SKIP: k.op("act", lambda e: e.copy(q_sb[:, :, 0:128], pq[:, :, 0:128]), reads=[pq], writes=[q_sb])
                k.op("act", lambda e: e.copy(qr_f[:], pq[:, :, 128:192]), reads=[pq], writes=[qr_f])
                qe = qr_f[:, :, 0:64:2]; qo = qr_f[:, :, 1:64:2]
                if isctx:
                    k.op("dve", lambda e: e.tensor_copy(q_sb[:, :, 128:160], qe), reads=[qr_f], writes=[q_sb])
                    k.op("dve", lambda e: e.tensor_copy(q_sb[:, :, 160:192], qo), reads=[qr_f], writes=[q_sb])
                else:
                    cs = cos_sb[:, tt - 2, :].rearrange("p (h c) -> p h c", h=4); sn = sin_sb[:, tt - 2, :].rearrange("p (h c) -> p h c", h=4)
                    k.op("dve", lambda e: e.tensor_tensor(ra[:], qe, cs, ALU.mult), reads=[qr_f, cos_sb], writes=[ra])
                    k.op("dve", lambda e: e.tensor_tensor(rb[:], qo, sn, ALU.mult), reads=[qr_f, sin_sb], writes=[rb])
                    k.op("dve", lambda e: e.tensor_tensor(q_sb[:, :, 128:160], ra[:], rb[:], ALU.subtract), reads=[ra, rb], writes=[q_sb])
                    k.op("dve", lambda e: e.tensor_tensor(ra[:], qe, sn, ALU.mult), reads=[qr_f, sin_sb], writes=[ra])
                    k.op("dve", lambda e: e.tensor_tensor(rb[:], qo, cs, ALU.mult), reads=[qr_f, cos_sb], writes=[rb])
                    k.op("dve", lambda e: e.tensor_tensor(q_sb[:, :, 160:192], ra[:], rb[:], ALU.add), reads=[ra, rb], writes=[q_sb])
                for h in range(4 if "t" not in SKIP else 0):
                    k.op("pe", lambda e: e.transpose(pqT[:, h, :], q_sb[:, h, 0:128], ident_bf[:]), reads=[q_sb, ident_bf], writes=[pqT])
                    k.op("pe", lambda e: e.transpose(pqTr[:, h, :], q_sb[:, h, 128:192], ident_bf[:]), reads=[q_sb, ident_bf], writes=[pqTr])
            if "r" not in SKIP: k.op("pe", lambda e: e.transpose(pqTr[:, 4, :], kr_sb[:], ident_bf[:]), reads=[kr_sb, ident_bf], writes=[pqTr])
            if doq:
                k.op("act", lambda e: e.copy(qTn[:, :, sl], pqT[:]), reads=[pqT], writes=[qTn])
                k.op("dve", lambda e: e.tensor_copy(qTr[:, :, sl], pqTr[:, 0:4, :]), reads=[pqTr], writes=[qTr])
            k.op("dve", lambda e: e.tensor_copy(krT[:, sl], pqTr[:, 4, :]), reads=[pqTr], writes=[krT])
        k.barrier(); st2.close()
        groups = [(0, 512), (512, 512), (1024, 512), (1536, 512), (2048, 256)]
        pk = [k.ps("mlapk", [128, 512], F32, st) for _ in range(2)]
        cnt = 0
        for h in range(4 if STOP > 2 else 0):
            for (o, n) in groups:
                ps = pk[cnt % 2]
                k.op("pe", lambda e: e.matmul(ps[:, 0:n], lhsT=wukv[:, h * 256:h * 256 + 128], rhs=kvnT[:, o:o + n], start=True, stop=True), reads=[wukv, kvnT], writes=[ps])
                k.op("act" if cnt % 2 else "dve", (lambda e: e.copy(knT[:, h, o:o + n], ps[:, 0:n])) if cnt % 2 else (lambda e: e.tensor_copy(knT[:, h, o:o + n], ps[:, 0:n])),
                     reads=[ps], writes=[knT])
                cnt += 1
        pos = [k.ps("mlapo", [128, 512], F32, st) for _ in range(2)]
        pss = [k.ps("mlapss", [128, 512], F32, st) for _ in range(2)]
        pts = [k.sb("mlapt", [128, 512], BF16, st) for _ in range(3)]
        rs = k.sb("mlars", [128, 512], F32, st); ob = [k.sb("mlaob", [128, 512], BF16, st) for _ in range(2)]
        qblocks = [(256 + i * 512, 512, list(range(NTT))) for i in range(4)]
        if need_ctx:
            qblocks.append((0, 256, [0, 1]))
        it = 0; bi = 0
        for h in range(4 if STOP > 3 else 0):
            for (qo_, nq, ktiles) in qblocks:
                po = pos[bi % 2]; psum = pss[bi % 2]
                for ji, j in enumerate(ktiles):
                    ps = pk[it % 2]; pt = pts[it % 3]
                    ks = slice(j * 128, (j + 1) * 128)
                    k.op("pe", lambda e: e.matmul(ps[:, 0:nq], lhsT=knT[:, h, ks], rhs=qTn[:, h, qo_:qo_ + nq], start=True, stop=False), reads=[knT, qTn], writes=[ps])
                    k.op("pe", lambda e: e.matmul(ps[:, 0:nq], lhsT=krT[:, ks], rhs=qTr[:, h, qo_:qo_ + nq], start=False, stop=True), reads=[krT, qTr], writes=[ps])
                    k.op("act", lambda e: e.activation(pt[:, 0:nq], ps[:, 0:nq], AF.Exp, scale=scale), reads=[ps], writes=[pt])
                    first = ji == 0; last = ji == len(ktiles) - 1
                    k.op("pe", lambda e: e.matmul(po[:, 0:nq], lhsT=v_sb[:, j, h * 128:(h + 1) * 128], rhs=pt[:, 0:nq], start=first, stop=last), reads=[v_sb, pt], writes=[po])
                    k.op("pe", lambda e: e.matmul(psum[:, 0:nq], lhsT=ones_bf[:], rhs=pt[:, 0:nq], start=first, stop=last), reads=[ones_bf, pt], writes=[psum])
                    it += 1
                o_ = ob[bi % 2]
                k.op("dve", lambda e: e.reciprocal(rs[:, 0:nq], psum[:, 0:nq]), reads=[psum], writes=[rs])
                k.op("dve", lambda e: e.tensor_tensor(o_[:, 0:nq], po[:, 0:nq], rs[:, 0:nq], ALU.mult), reads=[po, rs], writes=[o_])
                k.dma("sp", ymla_d[h][:, qo_:qo_ + nq], o_[:, 0:nq], reads=[o_], writes=[ymla_d])
                bi += 1
    k.barrier()


def rope_tables():
    t = np.arange(SEQ); row = (t // 64).astype(np.float32); col = (t % 64).astype(np.float32)
    inv = (10000.0 ** (-np.arange(16, dtype=np.float32) / 16)).astype(np.float32)
    ang = np.concatenate([row[:, None] * inv, col[:, None] * inv], -1).astype(np.float32)
    return np.tile(np.cos(ang).astype(np.float32), (1, 4)), np.tile(np.sin(ang).astype(np.float32), (1, 4))


def na_pattern(i):
    if i < 4:
        return i, 0
    if i <= 28:
        return 4, i - 4
    return i - 24, 24


def na_bias_host(rpb):
    L = rpb.shape[0]
    col = np.arange(64)
    dc = np.clip(col[None, :] - col[:, None] + 15, 0, 30)
    out = np.zeros((L, 128, 4, 8, 4, 64), np.float32)
    lk = np.arange(512); r = lk // 64; kc = lk % 64
    for i in list(range(5)) + [29, 30, 31]:
        pat, rs = na_pattern(i)
        dr = rs + r - i + 7
        g = rpb[:, :, dr[:, None], dc.T[kc, :]]
        out[:, :, :, pat] = g.reshape(L, 4, 4, 128, 64).transpose(0, 3, 1, 2, 4)
    return out


def na_mask_host():
    col = np.arange(64)
    cs = np.clip(col - 8, 0, 48)
    inw = (col[None, :] >= cs[:, None]) & (col[None, :] < cs[:, None] + 16)
    lk = np.arange(512); kc = lk % 64
    m = np.where(inw.T[kc, :], 0.0, -30000.0).astype(np.float32)
    return np.ascontiguousarray(m.reshape(4, 128, 64).transpose(1, 0, 2))


def load_na_bias(k, l, st, bias_d, mask_d):
    bm = k.sb("nabm", [128, 4, 8, 384], F32, st); mk = k.sb("namk", [128, 256], F32, st)
    k.op("pool", lambda e: e.memset(bm[:], 0.0), writes=[bm])
    k.dma("sp", mk[:], mask_d.t.rearrange("p j q -> p (j q)"), writes=[mk])
    for h in range(4):
        k.dma("sp", bm[:, h, :, 0:256], bias_d.t[l][:, h].rearrange("p a j q -> p a (j q)"), writes=[bm])
    for h in range(4):
        for pat in range(8):
            k.op("pool", lambda e: e.tensor_tensor(bm[:, h, pat, 0:256], bm[:, h, pat, 0:256], mk[:], ALU.add), reads=[bm, mk], writes=[bm])
    return bm


def phase_na(k, need_ctx, pT_na_d, p_tm_d, bm, ones_bf, yna_d):
    scale = 64.0 ** -0.5
    with ExitStack() as st:
        qT = k.sb("naqT", [64, 4, T], BF16, st); kT = k.sb("nakT", [64, 4, T], BF16, st)
        vA = k.sb("navA", [128, NTT, 256], BF16, st); vB = k.sb("navB", [128, NTT - 1, 256], BF16, st)
        yo = k.sb("nayo", [64, 4, T], BF16, st)
        k.dma("sp", qT[:], pT_na_d.t[0:256, :].rearrange("(h d) t -> d h t", h=4), reads=[pT_na_d], writes=[qT])
        k.dma("sp", kT[:], pT_na_d.t[256:512, :].rearrange("(h d) t -> d h t", h=4), reads=[pT_na_d], writes=[kT])
        k.dma("pool", vA[:], p_tm_d.t.rearrange("(tt p) c -> p tt c", p=128)[:, :, 448:704], reads=[p_tm_d], writes=[vA])
        k.dma("pool", vB[:], p_tm_d.t[64:64 + (NTT - 1) * 128, :].rearrange("(tt p) c -> p tt c", p=128)[:, :, 448:704], reads=[p_tm_d], writes=[vB])
        pSs = [k.ps("napS", [128, 6, 64], F32, st) for _ in range(2)]
        pos = [k.ps("napo", [64, 256], F32, st) for _ in range(2)]
        pms = [k.ps("napm", [64, 256], F32, st) for _ in range(2)]
        ssb = [k.sb("nassb", [128, 6, 64], F32, st) for _ in range(2)]
        pts = [k.sb("napt", [128, 6, 64], BF16, st) for _ in range(2)]
        ptc = [k.sb("naptc", [128, 2, 256], BF16, st) for _ in range(2)]
        rss = [k.sb("nars", [64, 256], F32, st) for _ in range(2)]
        it = 0
        for h in range(4):
            for i in range(32):
                pat, rs_ = na_pattern(i)
                pS = pSs[it % 2]; po = pos[it % 2]; pm = pms[it % 2]; s_sb = ssb[it % 2]; pt = pts[it % 2]; rs = rss[it % 2]
                tok0 = 256 + 64 * rs_
                qs = slice(256 + 64 * i, 256 + 64 * i + 64)
                for j in range(6):
                    ko = tok0 + 128 * j if j < 4 else (j - 4) * 128
                    k.op("pe", lambda e: e.matmul(pS[:, j, :], lhsT=kT[:, h, ko:ko + 128], rhs=qT[:, h, qs], start=True, stop=True), reads=[kT, qT], writes=[pS])
                k.op("dve", lambda e: e.scalar_tensor_tensor(s_sb[:].rearrange("p j q -> p (j q)"), pS[:].rearrange("p j q -> p (j q)"), scale, bm[:, h, pat, :], op0=ALU.mult, op1=ALU.add),
                     reads=[pS, bm], writes=[s_sb])
                k.op("act", lambda e: e.activation(pt[:], s_sb[:], AF.Exp), reads=[s_sb], writes=[pt])
                for j in range(6):
                    if j < 4:
                        vt = (vA, 2 + rs_ // 2 + j) if rs_ % 2 == 0 else (vB, (3 + rs_) // 2 + j)
                    else:
                        vt = (vA, j - 4)
                    vtk, vi = vt
                    k.op("pe", lambda e: e.matmul(po[:, 0:64], lhsT=vtk[:, vi, h * 64:(h + 1) * 64], rhs=pt[:, j, :], start=(j == 0), stop=(j == 5)), reads=[vtk, pt], writes=[po])
                for j in range(6):
                    k.op("pe", lambda e: e.matmul(pm[:, 0:64], lhsT=ones_bf[:, 0:64], rhs=pt[:, j, :], start=(j == 0), stop=(j == 5)), reads=[ones_bf, pt], writes=[pm])
                k.op("dve", lambda e: e.reciprocal(rs[:, 0:64], pm[:, 0:64]), reads=[pm], writes=[rs])
                k.op("dve", lambda e: e.tensor_tensor(yo[:, h, qs], po[:, 0:64], rs[:, 0:64], ALU.mult), reads=[po, rs], writes=[yo])
                it += 1
            if need_ctx:
                pS = pSs[it % 2]; po = pos[it % 2]; pm = pms[it % 2]; pt = ptc[it % 2]; rs = rss[it % 2]
                pSv = pS[:].rearrange("p j q -> p (j q)")
                for j in range(2):
                    k.op("pe", lambda e: e.matmul(pSv[:, 0:256], lhsT=kT[:, h, j * 128:(j + 1) * 128], rhs=qT[:, h, 0:256], start=True, stop=True), reads=[kT, qT], writes=[pS])
                    k.op("act", lambda e: e.activation(pt[:, j, :], pSv[:, 0:256], AF.Exp, scale=scale), reads=[pS], writes=[pt])
                for j in range(2):
                    k.op("pe", lambda e: e.matmul(po[:], lhsT=vA[:, j, h * 64:(h + 1) * 64], rhs=pt[:, j, :], start=(j == 0), stop=(j == 1)), reads=[vA, pt], writes=[po])
                for j in range(2):
                    k.op("pe", lambda e: e.matmul(pm[:], lhsT=ones_bf[:, 0:64], rhs=pt[:, j, :], start=(j == 0), stop=(j == 1)), reads=[ones_bf, pt], writes=[pm])
                k.op("dve", lambda e: e.reciprocal(rs[:], pm[:]), reads=[pm], writes=[rs])
                k.op("dve", lambda e: e.tensor_tensor(yo[:, h, 0:256], po[:], rs[:], ALU.mult), reads=[po, rs], writes=[yo])
                it += 1
        k.dma("sp", yna_d[:], yo[:], reads=[yo], writes=[yna_d])
    k.barrier()


NCH = T // 64
RW_STOP = 99
class _Stop(Exception):
    pass
def chk(level):
    if RW_STOP == level:
        raise _Stop()


def rw_consts_host():
    i = np.arange(128) % 64
    lt = (i[None, :] < i[:, None]).astype(np.float32); le = (i[None, :] <= i[:, None]).astype(np.float32)
    gt = (i[None, :] > i[:, None]).astype(np.float32); ge = (i[None, :] >= i[:, None]).astype(np.float32)
    blk = ((np.arange(128)[:, None] // 64) == (np.arange(128)[None, :] // 64)).astype(np.float32)
    m2 = np.stack([np.concatenate([gt, ge], 1), np.concatenate([lt, le], 1)], 0)
    m1 = np.stack([lt, gt], 0)
    isel = np.concatenate([np.eye(64, dtype=np.float32)] * 2, 0)
    return m2, m1, blk, isel


def load_rw_w(k, l, st, W, ident_f):
    o = {}
    o["w2"] = k.sb("rww2", [128, 256], BF16, st); o["a2"] = k.sb("rwa2", [128, 256], BF16, st); o["g2"] = k.sb("rwg2", [128, 256], BF16, st)
    k.dma("pool", o["w2"][:], W["rw_w2"].t[l].rearrange("d k n -> (d k) n"), writes=[o["w2"]])
    k.dma("pool", o["a2"][:], W["rw_a2"].t[l].rearrange("d k n -> (d k) n"), writes=[o["a2"]])
    k.dma("pool", o["g2"][:], W["rw_g2"].t[l], writes=[o["g2"]])
    stage = k.sb("rwstage", [32, 128], F32, st)
    k.dma("sp", stage[0:18, :], W["rw_mu"].t[l].rearrange("d (c p) -> (d c) p", p=128), writes=[stage])
    k.dma("sp", stage[18:22, :], W["rw_w0"].t[l].rearrange("d (c p) -> (d c) p", p=128), writes=[stage])
    k.dma("sp", stage[22:26, :], W["rw_a0"].t[l].rearrange("d (c p) -> (d c) p", p=128), writes=[stage])
    k.dma("sp", stage[26:28, :], W["rw_kk"].t[l].rearrange("(c p) -> c p", p=128), writes=[stage])
    k.dma("sp", stage[28:30, :], W["rw_ka"].t[l].rearrange("(c p) -> c p", p=128), writes=[stage])
    k.dma("sp", stage[30:32, :], W["rw_rk"].t[l].rearrange("(c h) n -> c (h n)", c=2), writes=[stage])
    cols = k.sb("rwcols", [128, 48], F32, st)
    with ExitStack() as s2:
        pc = k.ps("rwpc", [128, 32], F32, s2)
        k.op("pe", lambda e: e.transpose(pc[:], stage[:], ident_f[0:32, 0:32]), reads=[stage, ident_f], writes=[pc])
        k.op("dve", lambda e: e.tensor_copy(cols[:, 0:32], pc[:]), reads=[pc], writes=[cols])
        k.barrier()
    k.op("dve", lambda e: e.tensor_tensor(cols[:, 32:41], cols[:, 0:9], cols[:, 9:18], ALU.add), reads=[cols], writes=[cols])
    k.op("dve", lambda e: e.tensor_scalar(cols[:, 32:41], cols[:, 32:41], -1.0, 1.0, op0=ALU.mult, op1=ALU.add), reads=[cols], writes=[cols])
    k.op("dve", lambda e: e.tensor_scalar(cols[:, 41:43], cols[:, 28:30], -1.0, 1.0, op0=ALU.mult, op1=ALU.add), reads=[cols], writes=[cols])
    o["cols"] = cols
    gn = k.sb("rwgn", [128, 2, 2, 64], F32, st)
    for P in range(2):
        for h in range(2):
            hd = 2 * P + h
            k.dma("sp", gn[h * 64:(h + 1) * 64, P, 0, :], W["rw_gn_w"].t[l:l + 1, hd * 64:(hd + 1) * 64].partition_broadcast(64), writes=[gn])
            k.dma("sp", gn[h * 64:(h + 1) * 64, P, 1, :], W["rw_gn_b"].t[l:l + 1, hd * 64:(hd + 1) * 64].partition_broadcast(64), writes=[gn])
    o["gn"] = gn
    return o


def phase_rwkv(k, pT_rw_d, RW, C, yrw_d):
    cols = RW["cols"]
    EM05 = float(np.exp(-0.5))
    with ExitStack() as st:
        tmpA = k.sb("rwtmpA", [128, T], F32, st); tmpB = k.sb("rwtmpB", [128, T], F32, st)

        def shifted(c, out):
            k.dma("sp", tmpA[:], pT_rw_d[c * 128:(c + 1) * 128, :], reads=[pT_rw_d], writes=[tmpA])
            k.op("act", lambda e: e.activation(out[:], tmpA[:], AF.Identity, scale=cols[:, 32 + c:33 + c]), reads=[tmpA, cols], writes=[out])
            for (dst, src, mc) in (((1, 256), (0, 255), c), ((257, T), (256, T - 1), c), ((0, 255), (1, 256), 9 + c), ((256, T - 1), (257, T), 9 + c)):
                k.op("dve", lambda e: e.scalar_tensor_tensor(out[:, dst[0]:dst[1]], tmpA[:, src[0]:src[1]], cols[:, mc:mc + 1], out[:, dst[0]:dst[1]], op0=ALU.mult, op1=ALU.add),
                     reads=[tmpA, cols, out], writes=[out])

        tw = k.sb("rwtw", [128, T], BF16, st); al = k.sb("rwal", [128, T], BF16, st); sg = k.sb("rwsg", [128, T], BF16, st)
        shifted(6, tmpB); k.op("act", lambda e: e.activation(tw[:], tmpB[:], AF.Tanh), reads=[tmpB], writes=[tw])
        shifted(7, tmpB); k.op("act", lambda e: e.copy(al[:], tmpB[:]), reads=[tmpB], writes=[al])
        shifted(8, tmpB); k.op("act", lambda e: e.activation(sg[:], tmpB[:], AF.Sigmoid), reads=[tmpB], writes=[sg])
        groups = [(0, 512), (512, 512), (1024, 512), (1536, 512), (2048, 256)]
        chk(1)
        for P in range(2 if RW_STOP > 1 else 0):
            with ExitStack() as sp:
                r_f = k.sb("rwr", [128, T], F32, sp); k_f = k.sb("rwk", [128, T], F32, sp); kk_f = k.sb("rwkk", [128, T], F32, sp)
                vst = k.sb("rwvst", [128, NCH, 64], BF16, sp)
                ccol = k.sb("rwccol", [128, NCH], F32, sp)
                o_all = k.sb("rwoall", [128, NCH, 64], F32, sp)
                shifted(0 + P, r_f); shifted(2 + P, k_f)
                sv = ExitStack()
                vexp = k.sb("rwvexp", [128, NCH, 128], BF16, sv)
                shifted(4 + P, tmpB)
                k.op("pool", lambda e: e.memset(vexp[:], 0.0), writes=[vexp])
                for h in range(2):
                    hs = slice(h * 64, (h + 1) * 64)
                    k.op("dve", lambda e: e.tensor_copy(vexp[hs, :, hs], tmpB[hs, :].rearrange("p (c s) -> p c s", s=64)), reads=[tmpB], writes=[vexp])
                k.op("dve", lambda e: e.tensor_scalar(kk_f[:], k_f[:], cols[:, 26 + P:27 + P], None, op0=ALU.mult), reads=[k_f, cols], writes=[kk_f])
                k.op("act", lambda e: e.activation(tmpB[:], kk_f[:], AF.Square), reads=[kk_f], writes=[tmpB])
                with ExitStack() as s2:
                    pg = [k.ps("rwpn", [128, 512], F32, s2) for _ in range(2)]
                    for gi, (o, n) in enumerate(groups):
                        ps = pg[gi % 2]
                        k.op("pe", lambda e: e.matmul(ps[:, 0:n], lhsT=C["blk"][:], rhs=tmpB[:, o:o + n], start=True, stop=True), reads=[C["blk"], tmpB], writes=[ps])
                        k.op("dve", lambda e: e.tensor_scalar(tmpA[:, o:o + n], ps[:, 0:n], 1e-12, None, op0=ALU.add), reads=[ps], writes=[tmpA])
                    k.op("act", lambda e: e.sqrt(tmpA[:], tmpA[:]), reads=[tmpA], writes=[tmpA])
                    k.op("dve", lambda e: e.reciprocal(tmpA[:], tmpA[:]), reads=[tmpA], writes=[tmpA])
                    k.op("dve", lambda e: e.tensor_tensor(kk_f[:], kk_f[:], tmpA[:], ALU.mult), reads=[kk_f, tmpA], writes=[kk_f])
                    rkexp = k.sb("rwrkexp", [128, NCH, 128], BF16, s2)
                    k.op("pool", lambda e: e.memset(rkexp[:], 0.0), writes=[rkexp])
                    k.op("dve", lambda e: e.scalar_tensor_tensor(tmpB[:], r_f[:], cols[:, 30 + P:31 + P], k_f[:], op0=ALU.mult, op1=ALU.mult), reads=[r_f, k_f, cols], writes=[tmpB])
                    for h in range(2):
                        hs = slice(h * 64, (h + 1) * 64)
                        k.op("dve", lambda e: e.tensor_copy(rkexp[hs, :, hs], tmpB[hs, :].rearrange("p (c s) -> p c s", s=64)), reads=[tmpB], writes=[rkexp])
                    pv = [k.ps("rwpv", [128, 8, 64], F32, s2) for _ in range(2)]
                    pcc = k.ps("rwpcc", [128, NCH], F32, s2)
                    for c0 in range(0, NCH, 8):
                        nn = min(8, NCH - c0); ps = pv[(c0 // 8) % 2]
                        for j in range(nn):
                            k.op("pe", lambda e: e.matmul(ps[:, j, :], lhsT=vexp[:, c0 + j, :], rhs=C["isel"][:], start=True, stop=True), reads=[vexp, C["isel"]], writes=[ps])
                        k.op("act", lambda e: e.copy(vst[:, c0:c0 + nn, :], ps[:, 0:nn, :]), reads=[ps], writes=[vst])
                    for c in range(NCH):
                        k.op("pe", lambda e: e.matmul(pcc[:, c:c + 1], lhsT=rkexp[:, c, :], rhs=C["ones_bf"][:, 0:1], start=True, stop=True), reads=[rkexp, C["ones_bf"]], writes=[pcc])
                    k.op("dve", lambda e: e.tensor_copy(ccol[:], pcc[:]), reads=[pcc], writes=[ccol])
                    k.barrier()
                sv.close()
                if RW_STOP == 2: continue

                for d in range(2):
                    with ExitStack() as sd:
                        if RW_STOP < 7 and (P, d) != (0, 0): continue
                        rw_dir(k, sd, P, d, RW, C, cols, tw, al, r_f, k_f, kk_f, vst, o_all, tmpA, tmpB, groups, EM05)
                        k.barrier()
                if RW_STOP < 7: continue
                with ExitStack() as s2:
                    s1 = k.sb("rws1", [128, NCH], F32, s2); s2_ = k.sb("rws2", [128, NCH], F32, s2); rstd = k.sb("rwrstd", [128, NCH], F32, s2)
                    sq = k.sb("rwsq", [128, NCH, 64], F32, s2); fin = k.sb("rwfin", [128, NCH, 64], F32, s2)
                    k.op("dve", lambda e: e.reduce_sum(s1[:], o_all[:], axis=AX.X), reads=[o_all], writes=[s1])
                    k.op("act", lambda e: e.activation(sq[:], o_all[:], AF.Square), reads=[o_all], writes=[sq])
                    k.op("dve", lambda e: e.reduce_sum(s2_[:], sq[:], axis=AX.X), reads=[sq], writes=[s2_])
                    k.op("dve", lambda e: e.tensor_scalar(s1[:], s1[:], 1.0 / 64, None, op0=ALU.mult), reads=[s1], writes=[s1])
                    k.op("dve", lambda e: e.tensor_tensor(rstd[:], s1[:], s1[:], ALU.mult), reads=[s1], writes=[rstd])
                    k.op("dve", lambda e: e.scalar_tensor_tensor(rstd[:], s2_[:], 1.0 / 64, rstd[:], op0=ALU.mult, op1=ALU.subtract), reads=[s2_, rstd], writes=[rstd])
                    k.op("dve", lambda e: e.tensor_scalar(rstd[:], rstd[:], 64e-5, None, op0=ALU.add), reads=[rstd], writes=[rstd])
                    k.op("act", lambda e: e.sqrt(rstd[:], rstd[:]), reads=[rstd], writes=[rstd])
                    k.op("dve", lambda e: e.reciprocal(rstd[:], rstd[:]), reads=[rstd], writes=[rstd])
                    yfm = k.sb("rwyfm", [64, 2, T], BF16, s2)
                    gfm = k.sb("rwgfm", [64, 2, T], F32, s2); vstf = k.sb("rwvstf", [128, NCH, 64], F32, s2)
                    k.op("pool", lambda e: e.tensor_copy(vstf[:], vst[:]), reads=[vst], writes=[vstf])
                    pgg = [k.ps("rwpg", [128, 512], F32, s2) for _ in range(2)]
                    cnt = 0
                    for h in range(2):
                        hd = 2 * P + h
                        for (o, n) in groups:
                            ps = pgg[cnt % 2]
                            k.op("pe", lambda e: e.matmul(ps[0:64, 0:n], lhsT=RW["g2"][:, hd * 64:(hd + 1) * 64], rhs=sg[:, o:o + n], start=True, stop=True), reads=[RW["g2"], sg], writes=[ps])
                            k.op("act", lambda e: e.copy(gfm[:, h, o:o + n], ps[0:64, 0:n]), reads=[ps], writes=[gfm])
                            cnt += 1
                    gnw = RW["gn"][:, P, 0, :]; gnb = RW["gn"][:, P, 1, :]
                    for c in range(NCH):
                        k.op("dve", lambda e: e.scalar_tensor_tensor(sq[:, c, :], o_all[:, c, :], s1[:, c:c + 1], gnw, op0=ALU.subtract, op1=ALU.mult), reads=[o_all, s1, RW["gn"]], writes=[sq])
                        k.op("dve", lambda e: e.scalar_tensor_tensor(sq[:, c, :], sq[:, c, :], rstd[:, c:c + 1], gnb, op0=ALU.mult, op1=ALU.add), reads=[sq, rstd, RW["gn"]], writes=[sq])
                        k.op("dve", lambda e: e.scalar_tensor_tensor(fin[:, c, :], vstf[:, c, :], ccol[:, c:c + 1], sq[:, c, :], op0=ALU.mult, op1=ALU.add), reads=[vstf, ccol, sq], writes=[fin])
                    pts = [k.ps("rwpt", [64, 4, 128], F32, s2) for _ in range(2)]
                    for c0 in range(0, NCH, 4):
                        ps = pts[(c0 // 4) % 2]
                        for j in range(4):
                            k.op("pe", lambda e: e.transpose(ps[:, j, :], fin[:, c0 + j, :], C["ident_f"][:]), reads=[fin, C["ident_f"]], writes=[ps])
                        for h in range(2):
                            hd = 2 * P + h
                            k.op("dve", lambda e: e.tensor_tensor(yfm[:, h, c0 * 64:(c0 + 4) * 64].rearrange("p (c s) -> p c s", s=64), ps[:, :, h * 64:(h + 1) * 64],
                                                                  gfm[:, h, c0 * 64:(c0 + 4) * 64].rearrange("p (c s) -> p c s", s=64), ALU.mult), reads=[ps, gfm], writes=[yfm])
                    k.dma("sp", yrw_d[:, 2 * P:2 * P + 2, :], yfm[:], reads=[yfm], writes=[yrw_d])
                    k.barrier()
    k.barrier()


def rw_dir(k, sd, P, d, RW, C, cols, tw, al, r_f, k_f, kk_f, vst, o_all, tmpA, tmpB, groups, EM05):
    dsl = slice(d * 64, (d + 1) * 64)
    AR = k.sb("rwAR", [128, NCH, 2, 128], BF16, sd); Bx = k.sb("rwBx", [128, NCH, 128], BF16, sd); Kx = k.sb("rwKx", [128, NCH, 128], BF16, sd)
    gc = k.sb("rwgc", [128, NCH], F32, sd); eoff = k.sb("rweoff", [128, NCH, 2], F32, sd)
    stmp = ExitStack()
    Gp = k.sb("rwGp", [128, T + 1], F32, stmp)
    b_f = k.sb("rwb", [128, T], F32, stmp); kd_f = tmpB
    eB = k.sb("rweB", [128, T], F32, stmp); eA = k.sb("rweA", [128, T], F32, stmp); eR = tmpA
    with ExitStack() as s2:
        pg = [k.ps("rwpl", [128, 512], F32, s2) for _ in range(2)]
        cnt = 0
        for (o, n) in groups:
            ps = pg[cnt % 2]; cnt += 1
            k.op("pe", lambda e: e.matmul(ps[:, 0:n], lhsT=RW["w2"][dsl, P * 128:(P + 1) * 128], rhs=tw[dsl, o:o + n], start=True, stop=True), reads=[RW["w2"], tw], writes=[ps])
            k.op("act", lambda e: e.activation(tmpA[:, o:o + n], ps[:, 0:n], AF.Sigmoid, bias=cols[:, 18 + 2 * d + P:19 + 2 * d + P]), reads=[ps, cols], writes=[tmpA])
            ps = pg[cnt % 2]; cnt += 1
            k.op("pe", lambda e: e.matmul(ps[:, 0:n], lhsT=RW["a2"][dsl, P * 128:(P + 1) * 128], rhs=al[dsl, o:o + n], start=True, stop=True), reads=[RW["a2"], al], writes=[ps])
            k.op("act", lambda e: e.activation(tmpB[:, o:o + n], ps[:, 0:n], AF.Sigmoid, bias=cols[:, 22 + 2 * d + P:23 + 2 * d + P]), reads=[ps, cols], writes=[tmpB])
        k.barrier()
    k.op("dve", lambda e: e.tensor_tensor(b_f[:], tmpB[:], kk_f[:], ALU.mult), reads=[tmpB, kk_f], writes=[b_f])
    k.op("dve", lambda e: e.tensor_scalar(tmpB[:], tmpB[:], cols[:, 28 + P:29 + P], cols[:, 41 + P:42 + P], op0=ALU.mult, op1=ALU.add), reads=[tmpB, cols], writes=[tmpB])
    k.op("dve", lambda e: e.tensor_tensor(kd_f[:], k_f[:], tmpB[:], ALU.mult), reads=[k_f, tmpB], writes=[kd_f])
    k.op("dve", lambda e: e.tensor_scalar(tmpA[:], tmpA[:], -EM05, None, op0=ALU.mult), reads=[tmpA], writes=[tmpA])
    k.op("dve", lambda e: e.memset(Gp[:, 0:1], 0.0), writes=[Gp])
    k.op("pool", lambda e: e.memset(eA[:], 1.0), writes=[eA])
    k.op("dve", lambda e: e.tensor_tensor_scan(Gp[:, 1:T + 1], eA[:], tmpA[:], 0.0, op0=ALU.mult, op1=ALU.add), reads=[tmpA, eA, Gp], writes=[Gp])
    Gc0 = Gp[:, 0:T].rearrange("p (c s) -> p c s", s=64)[:, :, 0]; Gc1 = Gp[:, 1:T + 1].rearrange("p (c s) -> p c s", s=64)[:, :, 63]
    k.op("dve", lambda e: e.tensor_tensor(gc[:], Gc1, Gc0, ALU.subtract), reads=[Gp], writes=[gc])
    k.op("act", lambda e: e.activation(gc[:], gc[:], AF.Exp), reads=[gc], writes=[gc])
    Eref = Gc1 if d == 0 else Gc0
    k.op("dve", lambda e: e.tensor_copy(eoff[:, :, 0], Eref), reads=[Gp], writes=[eoff])
    k.op("dve", lambda e: e.tensor_scalar(eoff[:, :, 1], Eref, -1.0, None, op0=ALU.mult), reads=[Gp], writes=[eoff])
    k.barrier()
    if RW_STOP == 3:
        stmp.close(); return
    for c in range(NCH):
        cs = slice(c * 64, (c + 1) * 64); g1 = Gp[:, 1 + c * 64:1 + (c + 1) * 64]; g0 = Gp[:, c * 64:(c + 1) * 64]
        pE = eoff[:, c, 0:1]; nE = eoff[:, c, 1:2]
        if d == 0:
            k.op("act", lambda e: e.activation(eB[:, cs], g1, AF.Exp, scale=-1.0, bias=pE), reads=[Gp, eoff], writes=[eB])
            k.op("act", lambda e: e.activation(eA[:, cs], g0, AF.Exp, scale=1.0, bias=nE), reads=[Gp, eoff], writes=[eA])
            k.op("act", lambda e: e.activation(eR[:, cs], g1, AF.Exp, scale=1.0, bias=nE), reads=[Gp, eoff], writes=[eR])
        else:
            k.op("act", lambda e: e.activation(eB[:, cs], g0, AF.Exp, scale=1.0, bias=nE), reads=[Gp, eoff], writes=[eB])
            k.op("act", lambda e: e.activation(eA[:, cs], g1, AF.Exp, scale=-1.0, bias=pE), reads=[Gp, eoff], writes=[eA])
            k.op("act", lambda e: e.activation(eR[:, cs], g0, AF.Exp, scale=-1.0, bias=pE), reads=[Gp, eoff], writes=[eR])
    k.op("pool", lambda e: e.memset(AR[:], 0.0), writes=[AR]); k.op("pool", lambda e: e.memset(Bx[:], 0.0), writes=[Bx]); k.op("pool", lambda e: e.memset(Kx[:], 0.0), writes=[Kx])
    v3 = lambda tle, hs: tle[hs, :].rearrange("p (c s) -> p c s", s=64)
    for h in range(2):
        hs = slice(h * 64, (h + 1) * 64)
        k.op("dve", lambda e: e.tensor_tensor(AR[hs, :, 0, hs], v3(kk_f, hs), v3(eA, hs), ALU.mult), reads=[kk_f, eA], writes=[AR])
        k.op("dve", lambda e: e.tensor_tensor(AR[hs, :, 1, hs], v3(r_f, hs), v3(eR, hs), ALU.mult), reads=[r_f, eR], writes=[AR])
        k.op("dve", lambda e: e.tensor_tensor(Bx[hs, :, hs], v3(b_f, hs), v3(eB, hs), ALU.mult), reads=[b_f, eB], writes=[Bx])
        k.op("dve", lambda e: e.tensor_tensor(Kx[hs, :, hs], v3(kd_f, hs), v3(eB, hs), ALU.mult), reads=[kd_f, eB], writes=[Kx])
    k.barrier(); stmp.close()
    if RW_STOP == 4: return
    Rhat = k.sb("rwRhat", [128, NCH, 128], BF16, sd); Tt = k.sb("rwTt", [128, NCH, 128], BF16, sd)
    OV = k.sb("rwOV", [128, NCH, 64], F32, sd); HV = k.sb("rwHV", [128, NCH, 64], F32, sd)
    m2 = C["m2"][d]; m1 = C["m1"][d]
    with ExitStack() as s2:
        pMb = k.ps("rwpMb", [128, 256], F32, s2); pMk = k.ps("rwpMk", [128, 256], F32, s2); pMa = k.ps("rwpMa", [128, 128], F32, s2)
        pTr = k.ps("rwpTr", [128, 3, 128], BF16, s2); pY = k.ps("rwpY", [128, 192], F32, s2)
        pL = [k.ps("rwpL", [128, 256], F32, s2) for _ in range(2)]
        pX = k.ps("rwpX", [128, 512], F32, s2)
        MbS = k.sb("rwMbS", [128, 256], BF16, s2); MkS = k.sb("rwMkS", [128, 256], BF16, s2)
        Ls = [k.sb("rwLs", [128, 2, 128], F32, s2) for _ in range(2)]
        tm = k.sb("rwtm", [128, 3, 128], BF16, s2)
        Ys = [k.sb("rwY", [128, 192], F32, s2) for _ in range(2)]
        nZ = k.sb("rwnZ", [128, 192], BF16, s2)
        for c in range(NCH):
            arc = AR[:, c].rearrange("p a b -> p (a b)")
            k.op("pe", lambda e: e.matmul(pMb[:], lhsT=Bx[:, c, :], rhs=arc, start=True, stop=True), reads=[Bx, AR], writes=[pMb])
            k.op("pe", lambda e: e.matmul(pMk[:], lhsT=Kx[:, c, :], rhs=arc, start=True, stop=True), reads=[Kx, AR], writes=[pMk])
            k.op("pe", lambda e: e.matmul(pMa[:], lhsT=AR[:, c, 0, :], rhs=Bx[:, c, :], start=True, stop=True), reads=[AR, Bx], writes=[pMa])
            k.op("pe", lambda e: e.transpose(pTr[:, 0, :], Bx[:, c, :], C["ident_bf"][:]), reads=[Bx, C["ident_bf"]], writes=[pTr])
            k.op("pe", lambda e: e.transpose(pTr[:, 1, :], Kx[:, c, :], C["ident_bf"][:]), reads=[Kx, C["ident_bf"]], writes=[pTr])
            k.op("pe", lambda e: e.transpose(pTr[:, 2, :], AR[:, c, 0, :], C["ident_bf"][:]), reads=[AR, C["ident_bf"]], writes=[pTr])
            k.op("dve", lambda e: e.tensor_tensor(MbS[:], pMb[:], m2, ALU.mult), reads=[pMb, C["m2t"]], writes=[MbS])
            k.op("dve", lambda e: e.tensor_tensor(MkS[:], pMk[:], m2, ALU.mult), reads=[pMk, C["m2t"]], writes=[MkS])
            L0 = Ls[0]
            k.op("dve", lambda e: e.tensor_tensor(L0[:, 0, :], pMa[:], m1, ALU.mult), reads=[pMa, C["m1t"]], writes=[L0])
            k.op("dve", lambda e: e.tensor_tensor(L0[:, 1, :], pMb[:, 0:128], m2[:, 0:128], ALU.mult), reads=[pMb, C["m2t"]], writes=[L0])
            k.op("act", lambda e: e.copy(tm[:], pTr[:]), reads=[pTr], writes=[tm])
            if RW_STOP == 51: continue
            Y = Ys[0]
            k.op("pe", lambda e: e.matmul(pY[:, 0:64], lhsT=MkS[:, 0:128], rhs=vst[:, c, :], start=True, stop=True), reads=[MkS, vst], writes=[pY])
            k.op("act", lambda e: e.copy(Y[:, 0:128], tm[:, 2, :]), reads=[tm], writes=[Y])
            k.op("act", lambda e: e.copy(Y[:, 128:192], pY[:, 0:64]), reads=[pY], writes=[Y])
            if RW_STOP == 52: continue
            cur = 0
            for lvl in range(6):
                Lc = Ls[cur % 2]; Yc = Ys[lvl % 2]; Yn = Ys[(lvl + 1) % 2]
                k.op("pe", lambda e: e.matmul(pY[:], lhsT=Lc[:, 1, :], rhs=Yc[:], start=True, stop=True), reads=[Lc, Yc], writes=[pY])
                k.op("dve", lambda e: e.tensor_tensor(Yn[:], Yc[:], pY[:], ALU.subtract if lvl == 0 else ALU.add), reads=[Yc, pY], writes=[Yn])
                if lvl < 5:
                    Ln = Ls[(cur + 1) % 2]; pl = pL[lvl % 2]
                    k.op("pe", lambda e: e.matmul(pl[:, 128:256], lhsT=Lc[:, 0, :], rhs=Lc[:, 1, :], start=True, stop=True), reads=[Lc], writes=[pl])
                    if lvl < 4:
                        k.op("pe", lambda e: e.matmul(pl[:, 0:128], lhsT=Lc[:, 1, :], rhs=Lc[:, 0, :], start=True, stop=True), reads=[Lc], writes=[pl])
                        k.op("act", lambda e: e.copy(Ln[:].rearrange("p a b -> p (a b)"), pl[:]), reads=[pl], writes=[Ln])
                    else:
                        k.op("act", lambda e: e.copy(Ln[:, 1, :], pl[:, 128:256]), reads=[pl], writes=[Ln])
                    cur += 1
            if RW_STOP == 53: continue
            k.op("dve", lambda e: e.tensor_scalar(nZ[:], Ys[0][:], -1.0, None, op0=ALU.mult), reads=[Ys[0]], writes=[nZ])
            if RW_STOP == 54: continue
            idb = C["ident_bf"]
            k.op("pe", lambda e: e.matmul(pX[:, 0:128], lhsT=idb[:], rhs=AR[:, c, 1, :], start=True, stop=False), reads=[idb, AR], writes=[pX])
            k.op("pe", lambda e: e.matmul(pX[:, 0:128], lhsT=nZ[:, 0:128], rhs=MbS[:, 128:256], start=False, stop=True), reads=[nZ, MbS], writes=[pX])
            k.op("pe", lambda e: e.matmul(pX[:, 128:256], lhsT=idb[:], rhs=idb[:], start=True, stop=False), reads=[idb], writes=[pX])
            k.op("pe", lambda e: e.matmul(pX[:, 128:256], lhsT=nZ[:, 0:128], rhs=tm[:, 0, :], start=False, stop=True), reads=[nZ, tm], writes=[pX])
            k.op("pe", lambda e: e.matmul(pX[:, 256:320], lhsT=MkS[:, 128:256], rhs=vst[:, c, :], start=True, stop=False), reads=[MkS, vst], writes=[pX])
            k.op("pe", lambda e: e.matmul(pX[:, 256:320], lhsT=MbS[:, 128:256], rhs=nZ[:, 128:192], start=False, stop=True), reads=[MbS, nZ], writes=[pX])
            k.op("pe", lambda e: e.matmul(pX[:, 320:384], lhsT=tm[:, 1, :], rhs=vst[:, c, :], start=True, stop=False), reads=[tm, vst], writes=[pX])
            k.op("pe", lambda e: e.matmul(pX[:, 320:384], lhsT=tm[:, 0, :], rhs=nZ[:, 128:192], start=False, stop=True), reads=[tm, nZ], writes=[pX])
            if RW_STOP == 55: continue
            k.op("dve", lambda e: e.tensor_copy(Rhat[:, c, :], pX[:, 0:128]), reads=[pX], writes=[Rhat])
            if RW_STOP == 56: continue
            k.op("dve", lambda e: e.tensor_copy(Tt[:, c, :], pX[:, 128:256]), reads=[pX], writes=[Tt])
            if RW_STOP == 57: continue
            k.op("act", lambda e: e.copy(OV[:, c, :], pX[:, 256:320]), reads=[pX], writes=[OV])
            if RW_STOP == 58: continue
            k.op("act", lambda e: e.copy(HV[:, c, :], pX[:, 320:384]), reads=[pX], writes=[HV])
        k.barrier()
    if RW_STOP in (5, 51, 52, 53, 54, 55, 56, 57, 58): return
    with ExitStack() as s2:
        H = k.sb("rwH", [128, 64], F32, s2); Hs = k.sb("rwHs", [128, 64], BF16, s2)
        pO = [k.ps("rwpO", [128, 64], F32, s2) for _ in range(2)]; pH = k.ps("rwpH", [128, 64], F32, s2)
        k.op("dve", lambda e: e.memset(H[:], 0.0), writes=[H])
        order = list(range(NCH)) if d == 0 else [3, 2, 1, 0] + list(range(NCH - 1, 3, -1))
        for i, c in enumerate(order):
            po = pO[i % 2]
            k.op("dve", lambda e: e.tensor_scalar(Hs[:], H[:], gc[:, c:c + 1], None, op0=ALU.mult), reads=[H, gc], writes=[Hs])
            k.op("pe", lambda e: e.matmul(po[:], lhsT=Rhat[:, c, :], rhs=Hs[:], start=True, stop=True), reads=[Rhat, Hs], writes=[po])
            k.op("pe", lambda e: e.matmul(pH[:], lhsT=Tt[:, c, :], rhs=Hs[:], start=True, stop=True), reads=[Tt, Hs], writes=[pH])
            k.op("dve", lambda e: e.tensor_tensor(H[:], pH[:], HV[:, c, :], ALU.add), reads=[pH, HV], writes=[H])
            if d == 0:
                k.op("dve", lambda e: e.tensor_tensor(o_all[:, c, :], po[:], OV[:, c, :], ALU.add), reads=[po, OV], writes=[o_all])
            else:
                k.op("dve", lambda e: e.tensor_tensor(OV[:, c, :], po[:], OV[:, c, :], ALU.add), reads=[po, OV], writes=[OV])
                k.op("pool", lambda e: e.tensor_tensor(o_all[:, c, :], o_all[:, c, :], OV[:, c, :], ALU.add), reads=[o_all, OV], writes=[o_all])
        k.barrier()


def load_consts(k, st, CD):
    C = {}
    C["ident_f"] = k.sb("identf", [128, 128], F32, st); C["ident_bf"] = k.sb("identbf", [128, 128], BF16, st)
    C["ones_bf"] = k.sb("onesbf", [128, 128], BF16, st)
    k.dma("sp", C["ident_f"][:], CD["ident"][:], writes=[C["ident_f"]])
    k.op("dve", lambda e: e.tensor_copy(C["ident_bf"][:], C["ident_f"][:]), reads=[C["ident_f"]], writes=[C["ident_bf"]])
    k.op("dve", lambda e: e.memset(C["ones_bf"][:], 1.0), writes=[C["ones_bf"]])
    C["cos_d"] = CD["cos"]; C["sin_d"] = CD["sin"]
    m2t = k.sb("rwm2", [128, 2, 256], F32, st); m1t = k.sb("rwm1", [128, 2, 128], F32, st)
    k.dma("sp", m2t[:], CD["rw_m2"].t.rearrange("d p c -> p d c"), writes=[m2t])
    k.dma("sp", m1t[:], CD["rw_m1"].t.rearrange("d p c -> p d c"), writes=[m1t])
    C["m2t"] = m2t; C["m1t"] = m1t; C["m2"] = [m2t[:, 0, :], m2t[:, 1, :]]; C["m1"] = [m1t[:, 0, :], m1t[:, 1, :]]
    C["blk"] = k.sb("rwblk", [128, 128], F32, st); C["isel"] = k.sb("rwisel", [128, 64], BF16, st)
    k.dma("sp", C["blk"][:], CD["rw_blk"][:], writes=[C["blk"]])
    k.dma("pool", C["isel"][:], CD["rw_isel"][:], writes=[C["isel"]])
    return C


def const_arrays():
    cos, sin = rope_tables()
    m2, m1, blk, isel = rw_consts_host()
    p = np.arange(128)
    return {"ident": np.eye(128, dtype=np.float32), "cos": cos, "sin": sin, "rw_m2": m2, "rw_m1": m1, "rw_blk": blk, "rw_isel": isel,
            "na_mask": na_mask_host(),
            "tri": (p[:, None] < p[None, :]).astype(np.float32), "iota64": (p % 32).astype(np.float32)[:, None],
            "rowbase": (np.arange(8)[None, :] * 128 + p[:, None]).astype(np.float32), "blk512": np.tile(512.0 * np.arange(NBLK_MAX, dtype=np.float32)[None, :], (128, 1))}


CONST_SHAPES = {"ident": [128, 128], "cos": [SEQ, 128], "sin": [SEQ, 128], "rw_m2": [2, 128, 256], "rw_m1": [2, 128, 128], "rw_blk": [128, 128],
                "rw_isel": [128, 64], "na_mask": [128, 4, 64], "tri": [128, 128], "iota64": [128, 1], "rowbase": [128, 8], "blk512": [128, NBLK_MAX]}


def phase_post(k, l, b, NB, need_ctx, x_src, x_src_tk, mod_d, yrw_d, ymla_d, yna_d, W, C, x1_d, hT_d, GT_d, col0, hTM_d=None, Gtm_d=None):
    with ExitStack() as st:
        wo_rw = k.sb("worw", [64, 4, D], BF16, st); wo_mla = k.sb("womla", [128, 4, D], BF16, st); wo_na = k.sb("wona", [64, 4, D], BF16, st)
        wv = W["w_out"].t[l]
        k.dma("pool", wo_rw[:], wv[0:256, :].rearrange("(h p) n -> p h n", p=64), writes=[wo_rw])
        k.dma("pool", wo_mla[:], wv[256:768, :].rearrange("(h p) n -> p h n", p=128), writes=[wo_mla])
        k.dma("pool", wo_na[:], wv[768:1024, :].rearrange("(h p) n -> p h n", p=64), writes=[wo_na])
        rwt = k.sb("routw", [128, 8, NE], BF16, st); rbt = k.sb("routb", [128, NE], F32, st)
        k.dma("pool", rwt[:], W["router_w"].t[l].rearrange("(kc p) n -> p kc n", p=128), writes=[rwt])
        k.dma("sp", rbt[:], W["router_b"].t[l:l + 1, :].partition_broadcast(128), writes=[rbt])
        bc = {}
        for nm, src in (("g1", W["ln1_g"].t[l:l + 1, :]), ("b1", W["ln1_b"].t[l:l + 1, :]),
                        ("ga_l", mod_d[l][b:b + 1, 2 * D:3 * D]), ("sf_l", mod_d[l][b:b + 1, 3 * D:4 * D]), ("cf_l", mod_d[l][b:b + 1, 4 * D:5 * D]),
                        ("ga_c", mod_d[l][NB:NB + 1, 2 * D:3 * D]), ("sf_c", mod_d[l][NB:NB + 1, 3 * D:4 * D]), ("cf_c", mod_d[l][NB:NB + 1, 4 * D:5 * D])):
            if nm.endswith("_c") and not need_ctx:
                continue
            tle = k.sb(nm, [128, D], F32, st)
            k.dma("sp", tle[:], src.partition_broadcast(128), reads=[mod_d], writes=[tle])
            bc[nm] = tle
        for nm in ("cf_l", "cf_c"):
            if nm in bc:
                tle = bc[nm]
                k.op("pool", lambda e: e.tensor_scalar(tle[:], tle[:], 1.0, None, op0=ALU.add), reads=[tle], writes=[tle])
        yr = k.sb("yrall", [64, 4, T], BF16, st); ym = k.sb("ymall", [128, 4, T], BF16, st); yn = k.sb("ynall", [64, 4, T], BF16, st)
        k.dma("sp", yr[:], yrw_d[:], reads=[yrw_d], writes=[yr])
        k.dma("sp", ym[:], ymla_d.t.rearrange("h p t -> p h t"), reads=[ymla_d], writes=[ym])
        k.dma("sp", yn[:], yna_d[:], reads=[yna_d], writes=[yn])
        xts = [k.sb("pxt", [128, D], F32, st) for _ in range(2)]
        t1 = k.sb("pt1", [128, D], F32, st); u = k.sb("pu", [128, D], F32, st); x1 = k.sb("px1", [128, D], F32, st); hb = k.sb("phb", [128, D], BF16, st)
        st6 = k.sb("pst6", [128, 2, 6], F32, st); mv = k.sb("pmv", [128, 2], F32, st); rstd = k.sb("prstd", [128, 1], F32, st)
        hT = k.sb("phT", [128, 8, 128], BF16, st)
        lg = k.sb("plg", [128, NE], F32, st); mx8 = k.sb("pmx8", [128, 8], F32, st); nm1 = k.sb("pnm1", [128, 1], F32, st)
        msk = k.sb("pmsk", [128, NE], F32, st); ex = k.sb("pex", [128, NE], F32, st); ssum = k.sb("pssum", [128, 1], F32, st); gts = k.sb("pgts", [32, 128], F32, st)
        py = [k.ps("ppy", [128, 512], F32, st) for _ in range(2)]
        pT = k.ps("ppT", [128, 8, 128], BF16, st); pl = k.ps("ppl", [128, NE], F32, st); pg = k.ps("ppg", [32, 128], F32, st)
        for tt in range(NTT):
            isctx = tt < 2
            if isctx and not need_ctx:
                continue
            sfx = "_c" if isctx else "_l"
            sl = slice(tt * 128, (tt + 1) * 128)
            xt = xts[tt % 2]
            k.dma("sp", xt[:], x_src(tt), reads=[x_src_tk], writes=[xt])
            for half in range(2):
                ps = py[half]; cs = slice(half * 512, (half + 1) * 512)
                ops = [(yr, wo_rw, h) for h in range(4)] + [(ym, wo_mla, h) for h in range(4)] + [(yn, wo_na, h) for h in range(4)]
                for i, (ya, wa, h) in enumerate(ops):
                    k.op("pe", lambda e: e.matmul(ps[:], lhsT=ya[:, h, sl], rhs=wa[:, h, cs], start=(i == 0), stop=(i == 11)), reads=[ya, wa], writes=[ps])
                k.op("dve", lambda e: e.tensor_tensor(t1[:, cs], ps[:], bc["ga" + sfx][:, cs], ALU.mult), reads=[ps, bc["ga" + sfx]], writes=[t1])
            k.op("dve", lambda e: e.scalar_tensor_tensor(u[:], xt[:], DN_ALPHA, t1[:], op0=ALU.mult, op1=ALU.add), reads=[xt, t1], writes=[u])
            ln_stats(k, u, st6, mv, rstd, 1e-5)
            k.op("dve", lambda e: e.scalar_tensor_tensor(t1[:], u[:], mv[:, 0:1], bc["g1"][:], op0=ALU.subtract, op1=ALU.mult), reads=[u, mv, bc["g1"]], writes=[t1])
            k.op("dve", lambda e: e.scalar_tensor_tensor(x1[:], t1[:], rstd[:], bc["b1"][:], op0=ALU.mult, op1=ALU.add), reads=[t1, rstd, bc["b1"]], writes=[x1])
            k.dma("sp", x1_d[sl, :], x1[:], reads=[x1], writes=[x1_d])
            ln_stats(k, x1, st6, mv, rstd, 1e-5)
            k.op("dve", lambda e: e.scalar_tensor_tensor(t1[:], x1[:], mv[:, 0:1], bc["cf" + sfx][:], op0=ALU.subtract, op1=ALU.mult), reads=[x1, mv, bc["cf" + sfx]], writes=[t1])
            k.op("dve", lambda e: e.scalar_tensor_tensor(hb[:], t1[:], rstd[:], bc["sf" + sfx][:], op0=ALU.mult, op1=ALU.add), reads=[t1, rstd, bc["sf" + sfx]], writes=[hb])
            for c in range(8):
                k.op("pe", lambda e: e.transpose(pT[:, c, :], hb[:, c * 128:(c + 1) * 128], C["ident_bf"][:]), reads=[hb, C["ident_bf"]], writes=[pT])
            k.op("act", lambda e: e.copy(hT[:], pT[:]), reads=[pT], writes=[hT])
            if hTM_d is not None:
                k.dma("sp", hTM_d.t[col0 + tt * 128:col0 + (tt + 1) * 128, :], hb[:], reads=[hb], writes=[hTM_d])
            else:
                k.dma("sp", hT_d.t[:, col0 + tt * 128:col0 + (tt + 1) * 128].rearrange("(kc p) t -> p kc t", p=128), hT[:], reads=[hT], writes=[hT_d])
            for kc in range(8):
                k.op("pe", lambda e: e.matmul(pl[:], lhsT=hT[:, kc, :], rhs=rwt[:, kc, :], start=(kc == 0), stop=(kc == 7)), reads=[hT, rwt], writes=[pl])
            k.op("dve", lambda e: e.tensor_tensor(lg[:], pl[:], rbt[:], ALU.add), reads=[pl, rbt], writes=[lg])
            k.op("dve", lambda e: e.max(mx8[:], lg[:]), reads=[lg], writes=[mx8])
            k.op("dve", lambda e: e.tensor_scalar(msk[:], lg[:], mx8[:, 3:4], None, op0=ALU.is_ge), reads=[lg, mx8], writes=[msk])
            k.op("dve", lambda e: e.tensor_scalar(nm1[:], mx8[:, 0:1], -1.0, None, op0=ALU.mult), reads=[mx8], writes=[nm1])
            k.op("act", lambda e: e.activation(ex[:], lg[:], AF.Exp, bias=nm1[:]), reads=[lg, nm1], writes=[ex])
            k.op("dve", lambda e: e.tensor_tensor(ex[:], ex[:], msk[:], ALU.mult), reads=[ex, msk], writes=[ex])
            k.op("dve", lambda e: e.reduce_sum(ssum[:], ex[:], axis=AX.X), reads=[ex], writes=[ssum])
            k.op("dve", lambda e: e.reciprocal(ssum[:], ssum[:]), reads=[ssum], writes=[ssum])
            k.op("dve", lambda e: e.tensor_scalar(ex[:], ex[:], ssum[:, 0:1], None, op0=ALU.mult), reads=[ex, ssum], writes=[ex])
            if Gtm_d is not None:
                k.dma("sp", Gtm_d.t[col0 + tt * 128:col0 + (tt + 1) * 128, :], ex[:], reads=[ex], writes=[Gtm_d])
            else:
                k.op("pe", lambda e: e.transpose(pg[:], ex[:], C["ident_f"][:]), reads=[ex, C["ident_f"]], writes=[pg])
                k.op("act", lambda e: e.copy(gts[:], pg[:]), reads=[pg], writes=[gts])
                k.dma("sp", GT_d[:, col0 + tt * 128:col0 + (tt + 1) * 128], gts[:], reads=[gts], writes=[GT_d])
    k.barrier()


def phase_moe(k, l, NB, groups, tile_io, mod_d, W, C, hT_d, GT_d):
    GP = 2
    with ExitStack() as st:
        bgl = k.sb("bgl", [128, NE, 8], F32, st); bln = k.sb("bln", [128, NE, 8], F32, st)
        bdn = k.sb("bdn", [NE, D], F32, st)
        k.dma("sp", bdn[:], W["b_dn"].t[l], writes=[bdn])
        with ExitStack() as s2:
            bgu = k.sb("bgu", [NE, 2 * D], F32, s2)
            pb = [k.ps("pbg", [128, NE], F32, s2) for _ in range(2)]
            k.dma("sp", bgu[:], W["b_gu"].t[l], writes=[bgu])
            for j in range(8):
                for par, dst in ((0, bgl), (1, bln)):
                    ps = pb[par]
                    k.op("pe", lambda e: e.transpose(ps[:], bgu[:, 256 * j + par:256 * (j + 1):2], C["ident_f"][0:NE, 0:NE]), reads=[bgu, C["ident_f"]], writes=[ps])
                    k.op("dve", lambda e: e.tensor_scalar(dst[:, :, j], ps[:], float(par), None, op0=ALU.add), reads=[ps], writes=[dst])
            k.barrier()
        gfs = [k.sb("gf%d" % i, [128, D], F32, st) for i in range(GP)]; gf_key = [None] * GP
        ln2g = k.sb("ln2g", [128, D], F32, st); ln2b = k.sb("ln2b", [128, D], F32, st)
        k.dma("sp", ln2g[:], W["ln2_g"].t[l:l + 1, :].partition_broadcast(128), writes=[ln2g])
        k.dma("sp", ln2b[:], W["ln2_b"].t[l:l + 1, :].partition_broadcast(128), writes=[ln2b])
        NU = 4
        units = [k.sb("wu%d" % i, [128, 8, 1024], BF16, st) for i in range(NU)]
        hTs = [k.sb("mhT%d" % i, [128, 8, 512], BF16, st) for i in range(GP)]
        accs = [k.sb("macc%d" % i, [128, 8, 512], F32, st) for i in range(GP)]
        gTs = [k.sb("mgT%d" % i, [NE, 512], F32, st) for i in range(GP)]
        acts = [k.sb("mact%d" % i, [128, 8, 512], BF16, st) for i in range(2)]
        gbcs = [k.sb("mgbc%d" % i, [128, 512], F32, st) for i in range(2)]
        tt_ = [k.sb("mt%d" % i, [128, 512], F32, st) for i in range(2)]
        ss_ = [k.sb("ms%d" % i, [128, 512], F32, st) for i in range(2)]
        uu_ = [k.sb("mu%d" % i, [128, 512], F32, st) for i in range(2)]
        psg = [k.ps("mpsg", [128, 512], F32, st) for _ in range(2)]
        psl = [k.ps("mpsl", [128, 512], F32, st) for _ in range(2)]
        psd = [k.ps("mpsd", [128, 512], F32, st) for _ in range(2)]
        st6 = k.sb("mst6", [128, 2, 6], F32, st); mv = k.sb("mmv", [128, 2], F32, st); rstd = k.sb("mrstd", [128, 1], F32, st)
        ucount = 0; it = 0; dcount = 0
        wgu = W["w_gu"].t[l]; wdn = W["w_dn"].t[l]
        for p0 in range(0, len(groups), GP):
            pg = groups[p0:p0 + GP]
            for gi, (col, n, bkey) in enumerate(pg):
                k.dma("sp", hTs[gi][:, :, 0:n], hT_d.t[:, col:col + n].rearrange("(kc p) t -> p kc t", p=128), reads=[hT_d], writes=[hTs[gi]])
                k.dma("sp", gTs[gi][:, 0:n], GT_d[:, col:col + n], reads=[GT_d], writes=[gTs[gi]])
                if gf_key[gi] != bkey:
                    row = NB if bkey == "c" else bkey
                    k.dma("sp", gfs[gi][:], mod_d[l][row:row + 1, 5 * D:6 * D].partition_broadcast(128), reads=[mod_d], writes=[gfs[gi]])
                    gf_key[gi] = bkey
            for e in range(NE):
                ua = units[ucount % NU]; ub = units[(ucount + 1) % NU]; ud = units[(ucount + 2) % NU]; ucount += 3
                k.dma("pool", ua[:], wgu[e][:, 0:1024].rearrange("(kc p) n -> p kc n", p=128), writes=[ua])
                k.dma("pool", ub[:], wgu[e][:, 1024:2048].rearrange("(kc p) n -> p kc n", p=128), writes=[ub])
                k.dma("pool", ud[:], wdn[e].rearrange("(kc p) n -> p kc n", p=128), writes=[ud])
                for gi, (col, n, bkey) in enumerate(pg):
                    hTg = hTs[gi]; acc = accs[gi]
                    act = acts[it % 2]; gbc = gbcs[it % 2]; it += 1
                    k.dma("act", gbc[:, 0:n], GT_d[e:e + 1, col:col + n].partition_broadcast(128), reads=[GT_d], writes=[gbc])
                    for j in range(8):
                        un = ua if j < 4 else ub; c0 = (j % 4) * 256
                        pg_ = psg[j % 2]; pl_ = psl[j % 2]; t_ = tt_[j % 2]; s_ = ss_[j % 2]; u_ = uu_[j % 2]
                        for kc in range(8):
                            k.op("pe", lambda e_: e_.matmul(pg_[:, 0:n], lhsT=un[:, kc, c0:c0 + 256:2], rhs=hTg[:, kc, 0:n], start=(kc == 0), stop=(kc == 7)), reads=[un, hTg], writes=[pg_])
                        for kc in range(8):
                            k.op("pe", lambda e_: e_.matmul(pl_[:, 0:n], lhsT=un[:, kc, c0 + 1:c0 + 256:2], rhs=hTg[:, kc, 0:n], start=(kc == 0), stop=(kc == 7)), reads=[un, hTg], writes=[pl_])
                        k.op("dve", lambda e_: e_.tensor_scalar(t_[:, 0:n], pg_[:, 0:n], bgl[:, e, j:j + 1], 7.0, op0=ALU.add, op1=ALU.min), reads=[pg_, bgl], writes=[t_])
                        k.op("act", lambda e_: e_.activation(s_[:, 0:n], t_[:, 0:n], AF.Sigmoid, scale=1.702), reads=[t_], writes=[s_])
                        k.op("dve", lambda e_: e_.tensor_scalar(u_[:, 0:n], pl_[:, 0:n], bln[:, e, j:j + 1], 8.0, op0=ALU.add, op1=ALU.min), reads=[pl_, bln], writes=[u_])
                        k.op("pool", lambda e_: e_.tensor_tensor(t_[:, 0:n], t_[:, 0:n], s_[:, 0:n], ALU.mult), reads=[t_, s_], writes=[t_])
                        k.op("pool", lambda e_: e_.tensor_scalar(u_[:, 0:n], u_[:, 0:n], -6.0, None, op0=ALU.max), reads=[u_], writes=[u_])
                        k.op("pool", lambda e_: e_.tensor_tensor(u_[:, 0:n], u_[:, 0:n], t_[:, 0:n], ALU.mult), reads=[u_, t_], writes=[u_])
                        k.op("dve", lambda e_: e_.tensor_tensor(act[:, j, 0:n], u_[:, 0:n], gbc[:, 0:n], ALU.mult), reads=[u_, gbc], writes=[act])
                    for oc in range(8):
                        pd = psd[dcount % 2]; dcount += 1
                        for j in range(8):
                            k.op("pe", lambda e_: e_.matmul(pd[:, 0:n], lhsT=ud[:, j, oc * 128:(oc + 1) * 128], rhs=act[:, j, 0:n], start=(j == 0), stop=(j == 7)), reads=[ud, act], writes=[pd])
                        if e == 0:
                            k.op("act", lambda e_: e_.copy(acc[:, oc, 0:n], pd[:, 0:n]), reads=[pd], writes=[acc])
                        else:
                            k.op("dve", lambda e_: e_.tensor_tensor(acc[:, oc, 0:n], acc[:, oc, 0:n], pd[:, 0:n], ALU.add), reads=[acc, pd], writes=[acc])
            for gi, (col, n, bkey) in enumerate(pg):
                acc = accs[gi]; gT = gTs[gi]; gf = gfs[gi]
                for oc in range(8):
                    pd = psd[dcount % 2]; dcount += 1
                    k.op("pe", lambda e_: e_.matmul(pd[:, 0:n], lhsT=bdn[:, oc * 128:(oc + 1) * 128], rhs=gT[:, 0:n], start=True, stop=True), reads=[bdn, gT], writes=[pd])
                    k.op("dve", lambda e_: e_.tensor_tensor(acc[:, oc, 0:n], acc[:, oc, 0:n], pd[:, 0:n], ALU.add), reads=[acc, pd], writes=[acc])
                for ti in range(n // 128):
                    x1_ap, x1_tk, out_ap, out_tk = tile_io(col + ti * 128)
                    for half in range(2):
                        ps = psg[half]; cs = slice(half * 512, (half + 1) * 512)
                        k.dma("sp", ss_[half][:], x1_ap[:, cs], reads=[x1_tk], writes=[ss_[half]])
                        for c in range(4):
                            oc = half * 4 + c
                            k.op("pe", lambda e_: e_.transpose(ps[:, c * 128:(c + 1) * 128], acc[:, oc, ti * 128:(ti + 1) * 128], C["ident_f"][:]), reads=[acc, C["ident_f"]], writes=[ps])
                        k.op("dve", lambda e_: e_.tensor_tensor(tt_[half][:], ps[:], gf[:, cs], ALU.mult), reads=[ps, gf], writes=[tt_[half]])
                        k.op("dve", lambda e_: e_.scalar_tensor_tensor(uu_[half][:], ss_[half][:], DN_ALPHA, tt_[half][:], op0=ALU.mult, op1=ALU.add), reads=[ss_[half], tt_[half]], writes=[uu_[half]])
                        k.op("dve", lambda e_: e_.bn_stats(st6[:, half, :], uu_[half][:]), reads=[uu_[half]], writes=[st6])
                    k.op("dve", lambda e_: e_.bn_aggr(mv[:], st6[:].rearrange("p a b -> p (a b)")), reads=[st6], writes=[mv])
                    k.op("dve", lambda e_: e_.tensor_scalar(rstd[:], mv[:, 1:2], 1e-5, None, op0=ALU.add), reads=[mv], writes=[rstd])
                    k.op("act", lambda e_: e_.sqrt(rstd[:], rstd[:]), reads=[rstd], writes=[rstd])
                    k.op("dve", lambda e_: e_.reciprocal(rstd[:], rstd[:]), reads=[rstd], writes=[rstd])
                    for half in range(2):
                        cs = slice(half * 512, (half + 1) * 512)
                        k.op("dve", lambda e_: e_.scalar_tensor_tensor(tt_[half][:], uu_[half][:], mv[:, 0:1], ln2g[:, cs], op0=ALU.subtract, op1=ALU.mult), reads=[uu_[half], mv, ln2g], writes=[tt_[half]])
                        k.op("dve", lambda e_: e_.scalar_tensor_tensor(uu_[half][:], tt_[half][:], rstd[:], ln2b[:, cs], op0=ALU.mult, op1=ALU.add), reads=[tt_[half], rstd, ln2b], writes=[uu_[half]])
                        k.dma("sp", out_ap[:, cs], uu_[half][:], reads=[uu_[half]], writes=[out_tk])
    k.barrier()


def phase_moe_sparse(k, l, NB, need_ctx, tile_io, mod_d, W, C, CD, hTM_d, Gtm_d, Hs_d, Ys_d):
    IOA = bass.IndirectOffsetOnAxis
    ntb = NTT if need_ctx else NTT - 2
    NT = NB * ntb; NTOK = NT * 128
    NBLK = (4 * NTOK + NE * 511) // 512
    assert NBLK <= NBLK_MAX
    BIG = 65536.0
    rows = [b * T + (0 if need_ctx else NCTX) for b in range(NB)]
    tile_row = [rows[b] + j * 128 for b in range(NB) for j in range(ntb)]
    tile_key = [("c" if (need_ctx and j < 2) else b) for b in range(NB) for j in range(ntb)]
    with ExitStack() as st:
        dest_i = k.sb("mdesti", [128, NT, 4], I32, st); gk = k.sb("mgk", [128, NT, 4], F32, st)
        widx = k.sb("mwidx", [128, NBLK, 8], I32, st); be = k.sb("mbe", [128, NBLK], F32, st)
        iota64 = k.sb("miota", [128, 1], F32, st)
        k.dma("sp", iota64[:], CD["iota64"][:], writes=[iota64])
        with ExitStack() as s2:
            rowb = k.sb("mrowb", [128, 8], F32, s2); blk512 = k.sb("mblk512", [128, NBLK], F32, s2)
            trif = k.sb("mtrif", [128, 128], F32, s2); tri = k.sb("mtri", [128, 128], BF16, s2)
            k.dma("sp", rowb[:], CD["rowbase"][:], writes=[rowb]); k.dma("sp", blk512[:], CD["blk512"][:, 0:NBLK], writes=[blk512])
            k.dma("sp", trif[:], CD["tri"][:], writes=[trif])
            k.op("dve", lambda e: e.tensor_scalar(rowb[:], rowb[:], float(l * NE * D), None, op0=ALU.add), reads=[rowb], writes=[rowb])
            k.op("dve", lambda e: e.tensor_copy(tri[:], trif[:]), reads=[trif], writes=[tri])
            G_all = k.sb("mGall", [128, NT, NE], F32, s2); maskf = k.sb("mmaskf", [128, NT, NE], F32, s2); mask = k.sb("mmask", [128, NT, NE], BF16, s2)
            rank = k.sb("mrank", [128, NT, NE], F32, s2); tot = k.sb("mtot", [128, NT, NE], F32, s2); off = k.sb("moff", [128, NT, NE], F32, s2)
            val = k.sb("mval", [128, NT, NE], F32, s2)
            cnt = k.sb("mcnt", [128, NE], F32, s2); nbe = k.sb("mnbe", [128, NE], F32, s2); start = k.sb("mstart", [128, NE + 1], F32, s2)
            mx8 = k.sb("mmx8", [128, 8], F32, s2); tmp = k.sb("mtmp", [128, NE], F32, s2); destf = k.sb("mdestf", [128, NT, 4], F32, s2)
            widxf = k.sb("mwidxf", [128, NBLK, 8], F32, s2)
            z = k.sb("mz", [128, 4 * D], BF16, s2)
            k.op("pool", lambda e: e.memset(z[:], 0.0), writes=[z])
            for blk in range(NBLK):
                k.dma("sp" if blk % 2 else "act", Hs_d.t[blk * 512:(blk + 1) * 512, :].rearrange("(p r) d -> p (r d)", p=128), z[:], reads=[z])
            for b in range(NB):
                k.dma("sp", G_all[:, b * ntb:(b + 1) * ntb, :], Gtm_d.t[rows[b]:rows[b] + ntb * 128, :].rearrange("(i p) e -> p i e", p=128), reads=[Gtm_d], writes=[G_all])
            k.op("dve", lambda e: e.tensor_scalar(maskf[:], G_all[:], 0.0, None, op0=ALU.is_gt), reads=[G_all], writes=[maskf])
            k.op("dve", lambda e: e.tensor_copy(mask[:], maskf[:]), reads=[maskf], writes=[mask])
            pP = [k.ps("mpP", [128, 512], F32, s2) for _ in range(2)]; pTt = [k.ps("mpTt", [128, 512], F32, s2) for _ in range(2)]
            mflat = mask[:].rearrange("p i e -> p (i e)"); rflat = rank[:].rearrange("p i e -> p (i e)"); tflat = tot[:].rearrange("p i e -> p (i e)")
            ncols = NT * NE
            for c in range((ncols + 511) // 512):
                c0 = c * 512; n = min(512, ncols - c0); pa = pP[c % 2]; pb = pTt[c % 2]
                k.op("pe", lambda e: e.matmul(pa[:, 0:n], lhsT=tri[:], rhs=mflat[:, c0:c0 + n], start=True, stop=True), reads=[tri, mask], writes=[pa])
                k.op("pe", lambda e: e.matmul(pb[:, 0:n], lhsT=C["ones_bf"][:], rhs=mflat[:, c0:c0 + n], start=True, stop=True), reads=[C["ones_bf"], mask], writes=[pb])
                k.op("dve", lambda e: e.tensor_copy(rflat[:, c0:c0 + n], pa[:, 0:n]), reads=[pa], writes=[rank])
                k.op("act", lambda e: e.copy(tflat[:, c0:c0 + n], pb[:, 0:n]), reads=[pb], writes=[tot])
            k.op("dve", lambda e: e.reduce_sum(cnt[:], tot[:].rearrange("p i e -> p e i"), axis=AX.X), reads=[tot], writes=[cnt])
            k.op("dve", lambda e: e.memset(nbe[:], 0.0), writes=[nbe])
            for j in range(NTOK // 512):
                k.op("dve", lambda e: e.scalar_tensor_tensor(nbe[:], cnt[:], 512.0 * j, nbe[:], op0=ALU.is_gt, op1=ALU.add), reads=[cnt, nbe], writes=[nbe])
            k.op("dve", lambda e: e.tensor_scalar(nbe[:], nbe[:], 512.0, None, op0=ALU.mult), reads=[nbe], writes=[nbe])
            k.op("dve", lambda e: e.memset(start[:, 0:1], 0.0), writes=[start])
            for ee in range(NE):
                k.op("dve", lambda e: e.tensor_tensor(start[:, ee + 1:ee + 2], start[:, ee:ee + 1], nbe[:, ee:ee + 1], ALU.add), reads=[start, nbe], writes=[start])
            k.op("dve", lambda e: e.tensor_copy(off[:, 0, :], start[:, 0:NE]), reads=[start], writes=[off])
            for i in range(1, NT):
                k.op("dve", lambda e: e.tensor_tensor(off[:, i, :], off[:, i - 1, :], tot[:, i - 1, :], ALU.add), reads=[off, tot], writes=[off])
            k.op("dve", lambda e: e.tensor_tensor(rank[:], rank[:], off[:], ALU.add), reads=[rank, off], writes=[rank])
            k.op("dve", lambda e: e.tensor_scalar(rank[:], rank[:], -1.0, BIG, op0=ALU.mult, op1=ALU.add), reads=[rank], writes=[rank])
            k.op("dve", lambda e: e.tensor_tensor(val[:], rank[:], maskf[:], ALU.mult), reads=[rank, maskf], writes=[val])
            for i in range(NT):
                k.op("dve", lambda e: e.max(mx8[:], val[:, i, :]), reads=[val], writes=[mx8])
                k.op("dve", lambda e: e.tensor_scalar(destf[:, i, :], mx8[:, 0:4], -1.0, BIG, op0=ALU.mult, op1=ALU.add), reads=[mx8], writes=[destf])
                for kk in range(4):
                    k.op("dve", lambda e: e.scalar_tensor_tensor(tmp[:], val[:, i, :], mx8[:, kk:kk + 1], G_all[:, i, :], op0=ALU.is_equal, op1=ALU.mult), reads=[val, mx8, G_all], writes=[tmp])
                    k.op("dve", lambda e: e.reduce_sum(gk[:, i, kk:kk + 1], tmp[:], axis=AX.X), reads=[tmp], writes=[gk])
            k.op("dve", lambda e: e.tensor_copy(dest_i[:], destf[:]), reads=[destf], writes=[dest_i])
            k.op("dve", lambda e: e.memset(be[:], 0.0), writes=[be])
            for ee in range(NE):
                k.op("dve", lambda e: e.scalar_tensor_tensor(be[:], blk512[:], start[:, ee + 1:ee + 2], be[:], op0=ALU.is_ge, op1=ALU.add), reads=[blk512, start, be], writes=[be])
            k.op("dve", lambda e: e.tensor_scalar(be[:], be[:], float(NE - 1), None, op0=ALU.min), reads=[be], writes=[be])
            for kc in range(8):
                k.op("dve", lambda e: e.tensor_scalar(widxf[:, :, kc], be[:], 1024.0, rowb[:, kc:kc + 1], op0=ALU.mult, op1=ALU.add), reads=[be, rowb], writes=[widxf])
            k.op("dve", lambda e: e.tensor_copy(widx[:], widxf[:]), reads=[widxf], writes=[widx])
            k.barrier()
            hts = [k.sb("mht", [128, D], BF16, s2) for _ in range(2)]
            for i in range(NT):
                ht = hts[i % 2]
                k.dma("sp", ht[:], hTM_d.t[tile_row[i]:tile_row[i] + 128, :], reads=[hTM_d], writes=[ht])
                for kk in range(4):
                    k.idma(Hs_d.t[:, :], ht[:], out_offset=IOA(ap=dest_i[:, i, kk:kk + 1], axis=0), reads=[dest_i, ht])
            k.barrier()
        with ExitStack() as s3:
            bguHL = k.sb("mbguhl", [64, 2 * D], BF16, s3); bdnHL = k.sb("mbdnhl", [64, D], BF16, s3); iotaE = k.sb("miotaE", [64, 512], F32, s3)
            with ExitStack() as s4:
                for (src, dst, n) in ((W["b_gu"].t[l], bguHL, 2 * D), (W["b_dn"].t[l], bdnHL, D)):
                    bf_ = k.sb("mbf", [64, n], F32, s4); hif = k.sb("mhif", [64, n], F32, s4)
                    k.dma("sp", bf_[0:32, :], src, writes=[bf_]); k.dma("sp", bf_[32:64, :], src, writes=[bf_])
                    k.op("dve", lambda e: e.tensor_copy(dst[:], bf_[:]), reads=[bf_], writes=[dst])
                    k.op("dve", lambda e: e.tensor_copy(hif[:], dst[:]), reads=[dst], writes=[hif])
                    k.op("dve", lambda e: e.tensor_tensor(hif[32:64, :], bf_[32:64, :], hif[32:64, :], ALU.subtract), reads=[bf_, hif], writes=[hif])
                    k.op("dve", lambda e: e.tensor_copy(dst[32:64, :], hif[32:64, :]), reads=[hif], writes=[dst])
                k.op("dve", lambda e: e.memset(iotaE[:], 0.0), writes=[iotaE])
                k.op("dve", lambda e: e.tensor_scalar(iotaE[:], iotaE[:], iota64[0:64, 0:1], None, op0=ALU.add), reads=[iotaE, iota64], writes=[iotaE])
                k.barrier()
            wgs = [[k.sb("mwg", [128, 2 * D], BF16, s3) for _ in range(8)] for _ in range(2)]
            wds = [[k.sb("mwd", [128, D], BF16, s3) for _ in range(8)] for _ in range(2)]
            hss = [k.sb("mhs", [128, 4, D], BF16, s3) for _ in range(2)]; hTs = [k.sb("mhT", [128, 8, 512], BF16, s3) for _ in range(1)]
            acts = [k.sb("mact", [128, 8, 512], BF16, s3) for _ in range(1)]; ohs = [k.sb("moh", [64, 512], BF16, s3) for _ in range(2)]
            tt_ = [k.sb("mt", [128, 512], F32, s3) for _ in range(2)]; ss_ = [k.sb("ms", [128, 512], F32, s3) for _ in range(2)]; uu_ = [k.sb("mu", [128, 512], F32, s3) for _ in range(2)]
            ysb = [k.sb("mysb", [128, D], F32, s3) for _ in range(2)]
            pT = k.ps("mpT", [128, 8, 128], BF16, s3)
            psg = [k.ps("mpsg", [128, 512], F32, s3) for _ in range(2)]; psl = [k.ps("mpsl", [128, 512], F32, s3) for _ in range(2)]; psd = [k.ps("mpsd", [128, 512], F32, s3) for _ in range(2)]
            wgu2d = W["w_gu"].t.rearrange("l e r n -> (l e r) n"); wdn2d = W["w_dn"].t.rearrange("l e r n -> (l e r) n")

            def fetch(blk):
                wg = wgs[blk % 2]; wd = wds[blk % 2]
                for kc in range(8):
                    k.idma(wg[kc][:], wgu2d, in_offset=IOA(ap=widx[:, blk, kc:kc + 1], axis=0), reads=[widx], writes=[wg[kc]])
                for kc in range(8):
                    k.idma(wd[kc][:], wdn2d, in_offset=IOA(ap=widx[:, blk, kc:kc + 1], axis=0), reads=[widx], writes=[wd[kc]])
                k.dma("sp", hss[blk % 2][:], Hs_d.t[blk * 512:(blk + 1) * 512, :].rearrange("(r p) d -> p r d", p=128), writes=[hss[blk % 2]])
            fetch(0)
            yc = 0; dc = 0
            for blk in range(NBLK):
                if blk + 1 < NBLK:
                    fetch(blk + 1)
                wg = wgs[blk % 2]; wd = wds[blk % 2]; hs = hss[blk % 2]; hT = hTs[0]; act = acts[0]; oh = ohs[blk % 2]
                for r in range(4):
                    for c in range(8):
                        k.op("pe", lambda e: e.transpose(pT[:, c, :], hs[:, r, c * 128:(c + 1) * 128], C["ident_bf"][:]), reads=[hs, C["ident_bf"]], writes=[pT])
                    if r % 2:
                        k.op("act", lambda e: e.copy(hT[:, :, r * 128:(r + 1) * 128], pT[:]), reads=[pT], writes=[hT])
                    else:
                        k.op("dve", lambda e: e.tensor_copy(hT[:, :, r * 128:(r + 1) * 128], pT[:]), reads=[pT], writes=[hT])
                k.op("dve", lambda e: e.tensor_scalar(oh[:], iotaE[:], be[0:64, blk:blk + 1], None, op0=ALU.is_equal), reads=[iotaE, be], writes=[oh])
                for j in range(8):
                    pg_ = psg[j % 2]; pl_ = psl[j % 2]; t_ = tt_[j % 2]; s_ = ss_[j % 2]; u_ = uu_[j % 2]
                    for (ps_, o_) in ((pg_, 0), (pl_, 1)):
                        for kc in range(8):
                            k.op("pe", lambda e: e.matmul(ps_[:], lhsT=wg[kc][:, 256 * j + o_:256 * (j + 1):2], rhs=hT[:, kc, :], start=(kc == 0), stop=False), reads=[wg[kc], hT], writes=[ps_])
                        k.op("pe", lambda e: e.matmul(ps_[:], lhsT=bguHL[:, 256 * j + o_:256 * (j + 1):2], rhs=oh[:], start=False, stop=True), reads=[bguHL, oh], writes=[ps_])
                    k.op("dve", lambda e: e.tensor_scalar(t_[:], pg_[:], 7.0, None, op0=ALU.min), reads=[pg_], writes=[t_])
                    k.op("act", lambda e: e.activation(s_[:], t_[:], AF.Sigmoid, scale=1.702), reads=[t_], writes=[s_])
                    k.op("dve", lambda e: e.tensor_scalar(u_[:], pl_[:], 7.0, -7.0, op0=ALU.min, op1=ALU.max), reads=[pl_], writes=[u_])
                    k.op("dve", lambda e: e.tensor_tensor(t_[:], t_[:], s_[:], ALU.mult), reads=[t_, s_], writes=[t_])
                    k.op("dve", lambda e: e.scalar_tensor_tensor(act[:, j, :], u_[:], 1.0, t_[:], op0=ALU.add, op1=ALU.mult), reads=[u_, t_], writes=[act])
                for r in range(4):
                    y = ysb[yc % 2]; yc += 1
                    for half in range(2):
                        pd = psd[dc % 2]; dc += 1; cs = slice(half * 512, (half + 1) * 512)
                        for j in range(8):
                            k.op("pe", lambda e: e.matmul(pd[:], lhsT=act[:, j, r * 128:(r + 1) * 128], rhs=wd[j][:, cs], start=(j == 0), stop=False), reads=[act, wd[j]], writes=[pd])
                        k.op("pe", lambda e: e.matmul(pd[:], lhsT=oh[:, r * 128:(r + 1) * 128], rhs=bdnHL[:, cs], start=False, stop=True), reads=[oh, bdnHL], writes=[pd])
                        if half:
                            k.op("act", lambda e: e.copy(y[:, cs], pd[:]), reads=[pd], writes=[y])
                        else:
                            k.op("dve", lambda e: e.tensor_copy(y[:, cs], pd[:]), reads=[pd], writes=[y])
                    k.dma("sp", Ys_d.t[blk * 512 + r * 128:blk * 512 + (r + 1) * 128, :], y[:], reads=[y])
            k.barrier()
        with ExitStack() as s5:
            gfs = {}
            for key in sorted(set(tile_key), key=str):
                row = NB if key == "c" else key
                gfs[key] = k.sb("mgf", [128, D], F32, s5)
                k.dma("sp", gfs[key][:], mod_d[l][row:row + 1, 5 * D:6 * D].partition_broadcast(128), reads=[mod_d], writes=[gfs[key]])
            ln2g = k.sb("ln2g", [128, D], F32, s5); ln2b = k.sb("ln2b", [128, D], F32, s5)
            k.dma("sp", ln2g[:], W["ln2_g"].t[l:l + 1, :].partition_broadcast(128), writes=[ln2g])
            k.dma("sp", ln2b[:], W["ln2_b"].t[l:l + 1, :].partition_broadcast(128), writes=[ln2b])
            yks = [[k.sb("myk", [128, D], F32, s5) for _ in range(4)] for _ in range(2)]
            x1s = [k.sb("mx1", [128, D], F32, s5) for _ in range(2)]
            acc = k.sb("macc", [128, D], F32, s5); uu = k.sb("muu", [128, D], F32, s5); oo = [k.sb("moo", [128, D], F32, s5) for _ in range(2)]
            st6 = k.sb("mst6", [128, 2, 6], F32, s5); mv = k.sb("mmv", [128, 2], F32, s5); rstd = k.sb("mrstd", [128, 1], F32, s5)
            for i in range(NT):
                yk = yks[i % 2]; x1t = x1s[i % 2]; o_ = oo[i % 2]; gf = gfs[tile_key[i]]
                x1_ap, x1_tk, out_ap, out_tk = tile_io(tile_row[i])
                for kk in range(4):
                    k.idma(yk[kk][:], Ys_d.t[:, :], in_offset=IOA(ap=dest_i[:, i, kk:kk + 1], axis=0), reads=[dest_i], writes=[yk[kk]])
                k.dma("sp", x1t[:], x1_ap, writes=[x1t])
                k.op("dve", lambda e: e.tensor_scalar(acc[:], yk[0][:], gk[:, i, 0:1], None, op0=ALU.mult), reads=[yk[0], gk], writes=[acc])
                for kk in range(1, 4):
                    k.op("dve", lambda e: e.scalar_tensor_tensor(acc[:], yk[kk][:], gk[:, i, kk:kk + 1], acc[:], op0=ALU.mult, op1=ALU.add), reads=[yk[kk], gk, acc], writes=[acc])
                k.op("pool", lambda e: e.tensor_tensor(acc[:], acc[:], gf[:], ALU.mult), reads=[acc, gf], writes=[acc])
                k.op("dve", lambda e: e.scalar_tensor_tensor(uu[:], x1t[:], DN_ALPHA, acc[:], op0=ALU.mult, op1=ALU.add), reads=[x1t, acc], writes=[uu])
                ln_stats(k, uu, st6, mv, rstd, 1e-5)
                k.op("dve", lambda e: e.scalar_tensor_tensor(acc[:], uu[:], mv[:, 0:1], ln2g[:], op0=ALU.subtract, op1=ALU.mult), reads=[uu, mv, ln2g], writes=[acc])
                k.op("dve", lambda e: e.scalar_tensor_tensor(o_[:], acc[:], rstd[:], ln2b[:], op0=ALU.mult, op1=ALU.add), reads=[acc, rstd, ln2b], writes=[o_])
                k.dma("sp", out_ap, o_[:], reads=[o_])
            k.barrier()
    k.barrier()


WSHAPES = {"ada_w": [2, D, 6 * D], "ada_b": [2, 6 * D], "w_in": [2, D, IN_COLS],
           "rw_mu": [2, 2, 1152], "rw_w0": [2, 2, 256], "rw_w2": [2, 2, 64, 256], "rw_a0": [2, 2, 256], "rw_a2": [2, 2, 64, 256], "rw_g2": [2, 128, 256],
           "rw_kk": [2, 256], "rw_ka": [2, 256], "rw_rk": [2, 4, 64], "rw_gn_w": [2, 256], "rw_gn_b": [2, 256],
           "mla_q_norm": [2, 256], "mla_kv_norm": [2, 128], "mla_w_uq": [2, 256, 768], "mla_w_ukv": [2, 128, 1024],
           "w_out": [2, D, D], "ln1_g": [2, D], "ln1_b": [2, D], "router_w": [2, D, NE], "router_b": [2, NE],
           "w_gu": [2, NE, D, 2 * D], "b_gu": [2, NE, 2 * D], "w_dn": [2, NE, D, D], "b_dn": [2, NE, D], "ln2_g": [2, D], "ln2_b": [2, D]}


def build_nc(NB, debug=False, layers=(0, 1), moe=True, sparse=True):
    nc = bass.Bass("TRN2", target_bir_lowering=False)
    with ExitStack() as st:
        k = K(nc, st)
        kin = "ExternalInput"; sk = "ExternalOutput" if debug else "Internal"
        CD = {n: k.dram("c_" + n, sh, F32, kin) for n, sh in CONST_SHAPES.items()}
        W = {n: k.dram(n, sh, F32, kin) for n, sh in WSHAPES.items()}
        xin = k.dram("xin", [NB, T, D], F32, kin)
        condT = k.dram("condT", [128, 8, NB + 1], F32, kin)
        na_bias = k.dram("na_bias", [2, 128, 4, 8, 4, 64], F32, kin)
        out_d = k.dram("out", [NB, SEQ, D], F32, "ExternalOutput")
        mod_d = k.dram("mod_d", [2, NB + 1, 6 * D], F32, sk)
        pT_rw_d = k.dram("pT_rw_d", [RW_COLS, T], F32, sk); pT_na_d = k.dram("pT_na_d", [512, T], BF16, sk); p_tm_d = k.dram("p_tm_d", [T, 704], F32, sk)
        yrw_d = k.dram("yrw_d", [64, 4, T], BF16, sk); ymla_d = k.dram("ymla_d", [4, 128, T], BF16, sk); yna_d = k.dram("yna_d", [64, 4, T], BF16, sk)
        x1_d = k.dram("x1_d", [NB * T, D], F32, sk); x2_d = k.dram("x2_d", [NB, T, D], F32, sk)
        hT_d = k.dram("hT_d", [D, NB * T], BF16, sk); GT_d = k.dram("GT_d", [NE, NB * T], F32, sk)
        hTM_d = k.dram("hTM_d", [NB * T, D], BF16, sk); Gtm_d = k.dram("Gtm_d", [NB * T, NE], F32, sk)
        nblk0 = (4 * NB * T + NE * 511) // 512
        Hs_d = k.dram("Hs_d", [nblk0 * 512, D], BF16, "Internal"); Ys_d = k.dram("Ys_d", [nblk0 * 512, D], F32, "Internal")
        C = load_consts(k, st, CD)
        k.barrier()
        for l in layers:
            need_ctx = l < DEPTH - 1
            phase_mod(k, l, NB, condT, W["ada_w"], W["ada_b"], mod_d)
            for b in range(NB):
                if l == 0:
                    x_src = (lambda tt, b=b: xin.t[b][tt * 128:(tt + 1) * 128, :]); x_tk = xin
                else:
                    x_src = (lambda tt, b=b: x2_d.t[b][tt * 128:(tt + 1) * 128, :]); x_tk = x2_d
                with ExitStack() as ps:
                    w_in_bf = k.sb("winbf", [128, 8, IN_COLS], BF16, ps)
                    load_w_in(k, l, W["w_in"], w_in_bf)
                    phase_inproj(k, l, b, NB, x_src, x_tk, mod_d, w_in_bf, C["ident_bf"], pT_rw_d, pT_na_d, p_tm_d)
                with ExitStack() as ps:
                    RW = load_rw_w(k, l, ps, W, C["ident_f"])
                    phase_rwkv(k, pT_rw_d, RW, C, yrw_d)
                with ExitStack() as ps:
                    wuq, wukv = load_mla_w(k, l, ps, W["mla_w_uq"], W["mla_w_ukv"], W["mla_q_norm"], W["mla_kv_norm"])
                    phase_mla(k, need_ctx, p_tm_d, wuq, wukv, C["cos_d"], C["sin_d"], C["ident_bf"], C["ones_bf"], ymla_d)
                with ExitStack() as ps:
                    bm = load_na_bias(k, l, ps, na_bias, CD["na_mask"])
                    phase_na(k, need_ctx, pT_na_d, p_tm_d, bm, C["ones_bf"], yna_d)
                x1_b = Tk(x1_d.t[b * T:(b + 1) * T, :], "x1b")
                if sparse:
                    phase_post(k, l, b, NB, need_ctx, x_src, x_tk, mod_d, yrw_d, ymla_d, yna_d, W, C, x1_b, hT_d, GT_d, b * T, hTM_d, Gtm_d)
                else:
                    phase_post(k, l, b, NB, need_ctx, x_src, x_tk, mod_d, yrw_d, ymla_d, yna_d, W, C, x1_b, hT_d, GT_d, b * T)
            groups = []
            for b in range(NB):
                if need_ctx:
                    groups.append((b * T, 256, "c"))
                for i in range(4):
                    groups.append((b * T + 256 + i * 512, 512, b))
            groups.sort(key=lambda g: -g[1])

            def tile_io(col, l=l):
                b = col // T; t = col % T
                x1_ap = x1_d.t[col:col + 128, :]
                if l == DEPTH - 1:
                    return x1_ap, x1_d, out_d.t[b][t - NCTX:t - NCTX + 128, :], out_d
                return x1_ap, x1_d, x2_d.t[b][t:t + 128, :], x2_d
            if moe and sparse:
                phase_moe_sparse(k, l, NB, need_ctx, tile_io, mod_d, W, C, CD, hTM_d, Gtm_d, Hs_d, Ys_d)
            elif moe:
                phase_moe(k, l, NB, groups, tile_io, mod_d, W, C, hT_d, GT_d)
        k.barrier()
        print("ninst", k.ninst, k.cnt, flush=True)
    return nc


def core_inputs(inputs, b0, NB, shared):
    cc = np.concatenate([inputs["c"][b0:b0 + NB], inputs["c_ctx"][None]], 0).astype(np.float32)
    im = dict(shared)
    im["condT"] = np.ascontiguousarray(cc.T.reshape(8, 128, NB + 1).transpose(1, 0, 2))
    im["xin"] = np.ascontiguousarray(np.concatenate([inputs["ctx"][b0:b0 + NB], inputs["x"][b0:b0 + NB]], 1))
    return im


def shared_inputs(inputs):
    sh = {n: np.ascontiguousarray(np.asarray(inputs[n], np.float32)) for n in WSHAPES}
    for n, v in const_arrays().items():
        sh["c_" + n] = np.ascontiguousarray(v.astype(np.float32))
    sh["na_bias"] = na_bias_host(np.asarray(inputs["na_rpb"], np.float32))
    return sh


def kernel(**inputs):
    NCORES = 8; B = inputs["x"].shape[0]; NB = B // NCORES
    nc = build_nc(NB)
    sh = shared_inputs(inputs)
    in_maps = [core_inputs(inputs, c * NB, NB, sh) for c in range(NCORES)]
    res = run_bass_kernel_spmd(nc, in_maps, core_ids=list(range(NCORES)))
    return np.concatenate([r["out"] for r in res.results], 0).astype(np.float32)
```

```python
import numpy as np
from contextlib import ExitStack
import concourse.bass as bass
import concourse.mybir as mybir
from concourse.bass_utils import run_bass_kernel_spmd

F32 = mybir.dt.float32; BF16 = mybir.dt.bfloat16; I32 = mybir.dt.int32
AF = mybir.ActivationFunctionType; ALU = mybir.AluOpType; AX = mybir.AxisListType

D = 1024; SEQ = 2048; NCTX = 256; T = SEQ + NCTX; DEPTH = 2; NTT = T // 128
RW_COLS = 1152; MLA_COLS = 448; NA_COLS = 768; IN_COLS = 2368
NE = 32
NBLK_MAX = 104
DN_ALPHA = (2 * DEPTH) ** 0.25


class Tk:
    __slots__ = ("t", "w", "r", "name", "psum")
    def __init__(s, t, name="", psum=False):
        s.t = t; s.w = None; s.r = []; s.name = name; s.psum = psum
    def __getitem__(s, idx):
        return s.t[idx]


class K:
    NDMA = 6
    def __init__(s, nc, stack):
        s.nc = nc; s.stack = stack
        s.eng = {"pe": nc.tensor, "act": nc.scalar, "dve": nc.vector, "pool": nc.gpsimd, "sp": nc.sync}
        s.sem = {}; s.cnt = {}
        for e in s.eng:
            s.sem[e] = stack.enter_context(nc.semaphore("s_" + e)); s.cnt[e] = 0
        s.seen = {e: {} for e in s.eng}
        s.dsem = [stack.enter_context(nc.semaphore("d%d" % i)) for i in range(3 * s.NDMA)]
        s.dcnt = [0] * (3 * s.NDMA); s.dnext = {"sp": 0, "act": 0, "pool": 0}; s.dq = {"sp": 0, "act": 1, "pool": 2}
        s.ninst = 0; s.uid = 0
    def sb(s, name, shape, dt, stack=None):
        s.uid += 1
        t = Tk((stack or s.stack).enter_context(s.nc.sbuf_tensor("%s_%d" % (name, s.uid), list(shape), dt)), name)
        assert s.nc.sbuf_bytes_remaining >= 28 * 1024, "SBUF budget exceeded at %s: remaining %d" % (name, s.nc.sbuf_bytes_remaining)
        return t
    def ps(s, name, shape, dt=F32, stack=None):
        s.uid += 1
        isz = 4 if dt == F32 else 2
        free = int(np.prod(shape[1:])); nb = (free * isz + 2047) // 2048
        raw = (stack or s.stack).enter_context(s.nc.psum_tensor("%s_%d" % (name, s.uid), [128, nb * 2048 // isz], dt))
        ap = raw[0:shape[0], 0:free]
        if len(shape) == 3:
            ap = ap.rearrange("p (a b) -> p a b", a=shape[1])
        return Tk(ap, name, psum=True)
    def dram(s, name, shape, dt, kind="Internal"):
        return Tk(s.nc.dram_tensor(name, list(shape), dt, kind=kind).ap(), name)
    def _wait(s, e, toks):
        seen = s.seen[e]
        for (sem, val, key) in toks:
            if seen.get(key, 0) >= val:
                continue
            s.eng[e].wait_ge(sem, val); seen[key] = val; s.ninst += 1
    def _deps(s, e, reads, writes):
        toks = []
        for t in reads:
            if t.w is not None:
                toks.append(t.w)
            if t.psum:
                toks.extend(k for k in t.r if k[2] != e)
        for t in writes:
            if t.w is not None:
                toks.append(t.w)
            toks.extend(t.r)
        if e == "pe":
            toks = [k for k in toks if k[2] != "pe"]
        return toks
    def op(s, e, fn, reads=(), writes=()):
        s._wait(e, s._deps(e, reads, writes))
        ins = fn(s.eng[e]); s.cnt[e] += 1; ins.then_inc(s.sem[e], 1); s.ninst += 1
        tok = (s.sem[e], s.cnt[e], e)
        for t in reads:
            t.r = [k for k in t.r if k[2] != e]; t.r.append(tok)
        for t in writes:
            t.w = tok; t.r = []
        return ins
    def dma(s, q, out, in_, reads=(), writes=(), **kw):
        base = s.dq[q] * s.NDMA; i = base + s.dnext[q]; s.dnext[q] = (s.dnext[q] + 1) % s.NDMA
        key = "d%d" % i
        toks = s._deps(q, reads, writes)
        if s.dcnt[i] > 0:
            toks.append((s.dsem[i], s.dcnt[i], key))
        s._wait(q, toks)
        ins = s.eng[q].dma_start(out=out, in_=in_, **kw); s.dcnt[i] += 16; ins.then_inc(s.dsem[i], 16); s.ninst += 1
        tok = (s.dsem[i], s.dcnt[i], key)
        for t in reads:
            t.r.append(tok)
        for t in writes:
            t.w = tok; t.r = []
        return ins
    def idma(s, out, in_, out_offset=None, in_offset=None, reads=(), writes=(), **kw):
        q = "pool"
        base = s.dq[q] * s.NDMA; i = base + s.dnext[q]; s.dnext[q] = (s.dnext[q] + 1) % s.NDMA
        key = "d%d" % i
        toks = s._deps(q, reads, writes)
        if s.dcnt[i] > 0:
            toks.append((s.dsem[i], s.dcnt[i], key))
        s._wait(q, toks)
        ins = s.eng[q].indirect_dma_start(out=out, out_offset=out_offset, in_=in_, in_offset=in_offset, **kw)
        s.dcnt[i] += 16; ins.then_inc(s.dsem[i], 16); s.ninst += 1
        tok = (s.dsem[i], s.dcnt[i], key)
        for t in reads:
            t.r.append(tok)
        for t in writes:
            t.w = tok; t.r = []
        return ins
    def barrier(s):
        toks = [(s.sem[e], s.cnt[e], e) for e in s.eng if s.cnt[e] > 0]
        toks += [(s.dsem[i], s.dcnt[i], "d%d" % i) for i in range(len(s.dsem)) if s.dcnt[i] > 0]
        for e in s.eng:
            s._wait(e, [k for k in toks if k[2] != e])
    def finish(s, outs):
        s._wait("sp", [t.w for t in outs if t.w is not None])


def bcast_rows(ap_row, n):
    return ap_row.partition_broadcast(n)


def phase_mod(k, l, NB, condT_d, ada_w_d, ada_b_d, mod_d):
    with ExitStack() as st:
        R = NB + 1
        cond = k.sb("cond", [128, 8, R], F32, st); cond_bf = k.sb("condbf", [128, 8, R], BF16, st)
        adab = k.sb("adab", [R, 6 * D], F32, st); mod_sb = k.sb("modsb", [R, 6 * D], F32, st)
        wts = [k.sb("adaw", [128, 8, 512], BF16, st) for _ in range(2)]
        pss = [k.ps("modps", [R, 512], F32, st) for _ in range(2)]
        k.dma("sp", cond[:], condT_d[:], writes=[cond])
        k.dma("sp", adab[:], ada_b_d[l:l + 1, :].partition_broadcast(R), writes=[adab])
        k.op("act", lambda e: e.activation(cond_bf[:], cond[:], AF.Silu), reads=[cond], writes=[cond_bf])
        wv = ada_w_d.t[l].rearrange("(kc p) n -> p kc n", p=128)
        for g in range(12):
            wt = wts[g % 2]; ps = pss[g % 2]
            k.dma("pool", wt[:], wv[:, :, g * 512:(g + 1) * 512], writes=[wt])
            for kc in range(8):
                k.op("pe", lambda e: e.matmul(ps[:], lhsT=cond_bf[:, kc, :], rhs=wt[:, kc, :], start=(kc == 0), stop=(kc == 7)),
                     reads=[cond_bf, wt], writes=[ps])
            k.op("dve", lambda e: e.tensor_tensor(mod_sb[:, g * 512:(g + 1) * 512], ps[:], adab[:, g * 512:(g + 1) * 512], ALU.add),
                 reads=[ps, adab], writes=[mod_sb])
        k.dma("sp", mod_d[l], mod_sb[:], reads=[mod_sb], writes=[mod_d])
    k.barrier()


def ln_stats(k, xt, st6, mv, rstd, eps):
    for hh in range(2):
        k.op("dve", lambda e: e.bn_stats(st6[:, hh, :], xt[:, hh * 512:(hh + 1) * 512]), reads=[xt], writes=[st6])
    k.op("dve", lambda e: e.bn_aggr(mv[:], st6[:].rearrange("p a b -> p (a b)")), reads=[st6], writes=[mv])
    k.op("dve", lambda e: e.tensor_scalar(rstd[:], mv[:, 1:2], eps, None, op0=ALU.add), reads=[mv], writes=[rstd])
    k.op("act", lambda e: e.sqrt(rstd[:], rstd[:]), reads=[rstd], writes=[rstd])
    k.op("dve", lambda e: e.reciprocal(rstd[:], rstd[:]), reads=[rstd], writes=[rstd])


def phase_inproj(k, l, b, NB, x_src, x_src_tk, mod_d, w_in_bf, ident_bf, pT_rw_d, pT_na_d, p_tm_d):
    with ExitStack() as st:
        bc = {}
        for nm, row, off in (("sa_l", b, 0), ("ca_l", b, D), ("sa_c", NB, 0), ("ca_c", NB, D)):
            tle = k.sb(nm, [128, D], F32, st)
            k.dma("sp", tle[:], mod_d[l][row:row + 1, off:off + D].partition_broadcast(128), reads=[mod_d], writes=[tle])
            bc[nm] = tle
        for nm in ("ca_l", "ca_c"):
            tle = bc[nm]
            k.op("pool", lambda e: e.tensor_scalar(tle[:], tle[:], 1.0, None, op0=ALU.add), reads=[tle], writes=[tle])
        groups = [(0, 512), (512, 512), (1024, 512), (1536, 512), (2048, 256)]
        xmT = [k.sb("xmT%d" % g, [128, 8, n], BF16, st) for g, (o, n) in enumerate(groups)]
        xts = [k.sb("xt", [128, D], F32, st) for _ in range(2)]
        t1 = k.sb("t1", [128, D], F32, st); xm = k.sb("xm", [128, D], BF16, st)
        st6 = k.sb("st6", [128, 2, 6], F32, st); mv = k.sb("mv", [128, 2], F32, st); rstd = k.sb("rstd", [128, 1], F32, st)
        pTs = [k.ps("pT", [128, 8, 128], BF16, st) for _ in range(2)]
        ptm = [k.ps("ptm", [128, 512], F32, st) for _ in range(2)]
        pfm = [k.ps("pfm", [128, 512], F32, st) for _ in range(2)]
        tm_sb = [k.sb("tmsb", [128, 704], F32, st) for _ in range(2)]
        fm_sb = [k.sb("fmsb", [128, 512], F32, st) for _ in range(2)]
        fm_sbb = [k.sb("fmsbb", [128, 512], BF16, st) for _ in range(2)]
        for tt in range(NTT):
            isctx = tt < 2
            sa = bc["sa_c" if isctx else "sa_l"]; ca = bc["ca_c" if isctx else "ca_l"]
            xt = xts[tt % 2]
            k.dma("sp", xt[:], x_src(tt), reads=[x_src_tk], writes=[xt])
            ln_stats(k, xt, st6, mv, rstd, 1e-5)
            k.op("dve", lambda e: e.scalar_tensor_tensor(t1[:], xt[:], mv[:, 0:1], ca[:], op0=ALU.subtract, op1=ALU.mult),
                 reads=[xt, mv, ca], writes=[t1])
            k.op("dve", lambda e: e.scalar_tensor_tensor(xm[:], t1[:], rstd[:], sa[:], op0=ALU.mult, op1=ALU.add),
                 reads=[t1, rstd, sa], writes=[xm])
            pT = pTs[tt % 2]
            for c in range(8):
                k.op("pe", lambda e: e.transpose(pT[:, c, :], xm[:, c * 128:(c + 1) * 128], ident_bf[:]), reads=[xm, ident_bf], writes=[pT])
            g = tt // 4; o = (tt % 4) * 128
            xg = xmT[g]
            k.op("act", lambda e: e.copy(xg[:, :, o:o + 128], pT[:]), reads=[pT], writes=[xg])
            pa = ptm[0]; pb = ptm[1]
            for kc in range(8):
                k.op("pe", lambda e: e.matmul(pa[:, 0:448], lhsT=xg[:, kc, o:o + 128], rhs=w_in_bf[:, kc, 1152:1600], start=(kc == 0), stop=(kc == 7)),
                     reads=[xg, w_in_bf], writes=[pa])
            for kc in range(8):
                k.op("pe", lambda e: e.matmul(pb[:, 0:256], lhsT=xg[:, kc, o:o + 128], rhs=w_in_bf[:, kc, 2112:2368], start=(kc == 0), stop=(kc == 7)),
                     reads=[xg, w_in_bf], writes=[pb])
            ts = tm_sb[tt % 2]
            k.op("act", lambda e: e.copy(ts[:, 0:448], pa[:, 0:448]), reads=[pa], writes=[ts])
            k.op("dve", lambda e: e.tensor_copy(ts[:, 448:704], pb[:, 0:256]), reads=[pb], writes=[ts])
            k.dma("sp", p_tm_d[tt * 128:(tt + 1) * 128, :], ts[:], reads=[ts], writes=[p_tm_d])
        cnt = 0
        for g, (o, n) in enumerate(groups):
            xg = xmT[g]
            for c in range(13):
                col = c * 128 if c < 9 else 1600 + (c - 9) * 128
                ps = pfm[cnt % 2]
                for kc in range(8):
                    k.op("pe", lambda e: e.matmul(ps[:, 0:n], lhsT=w_in_bf[:, kc, col:col + 128], rhs=xg[:, kc, :], start=(kc == 0), stop=(kc == 7)),
                         reads=[w_in_bf, xg], writes=[ps])
                if c < 9:
                    fs = fm_sb[cnt % 2]
                    k.op("act" if cnt % 2 else "dve", (lambda e: e.copy(fs[:, 0:n], ps[:, 0:n])) if cnt % 2 else (lambda e: e.tensor_copy(fs[:, 0:n], ps[:, 0:n])),
                         reads=[ps], writes=[fs])
                    k.dma("sp", pT_rw_d[c * 128:(c + 1) * 128, o:o + n], fs[:, 0:n], reads=[fs], writes=[pT_rw_d])
                else:
                    fs = fm_sbb[cnt % 2]
                    k.op("act" if cnt % 2 else "dve", (lambda e: e.copy(fs[:, 0:n], ps[:, 0:n])) if cnt % 2 else (lambda e: e.tensor_copy(fs[:, 0:n], ps[:, 0:n])),
                         reads=[ps], writes=[fs])
                    k.dma("sp", pT_na_d[(c - 9) * 128:(c - 8) * 128, o:o + n], fs[:, 0:n], reads=[fs], writes=[pT_na_d])
                cnt += 1
    k.barrier()


def load_w_in(k, l, w_in_d, w_in_bf):
    wv = w_in_d.t[l].rearrange("(kc p) n -> p kc n", p=128)
    for (a, bb) in ((0, 1024), (1024, 2048), (2048, IN_COLS)):
        k.dma("pool", w_in_bf[:, :, a:bb], wv[:, :, a:bb], writes=[w_in_bf])


def load_mla_w(k, l, st, w_uq_d, w_ukv_d, qn_d, kvn_d):
    wuq = k.sb("wuq", [128, 2, 768], BF16, st); wukv = k.sb("wukv", [128, 1024], BF16, st)
    with ExitStack() as s2:
        wuq_f = k.sb("wuqf", [128, 2, 768], F32, s2); wukv_f = k.sb("wukvf", [128, 1024], F32, s2)
        qn = k.sb("qn", [128, 2], F32, s2); kvn = k.sb("kvn", [128, 1], F32, s2)
        k.dma("sp", wuq_f[:], w_uq_d.t[l].rearrange("(kc p) n -> p kc n", p=128), writes=[wuq_f])
        k.dma("sp", wukv_f[:], w_ukv_d.t[l], writes=[wukv_f])
        k.dma("sp", qn[:], qn_d.t[l].rearrange("(kc p) -> p kc", p=128), writes=[qn], allow_slow_non_contiguous=True)
        k.dma("sp", kvn[:], kvn_d.t[l].rearrange("(p o) -> p o", o=1), writes=[kvn])
        for kc in range(2):
            k.op("dve", lambda e: e.tensor_scalar(wuq[:, kc, :], wuq_f[:, kc, :], qn[:, kc:kc + 1], None, op0=ALU.mult), reads=[wuq_f, qn], writes=[wuq])
        k.op("dve", lambda e: e.tensor_scalar(wukv[:], wukv_f[:], kvn[:, 0:1], None, op0=ALU.mult), reads=[wukv_f, kvn], writes=[wukv])
        k.barrier()
    return wuq, wukv


def phase_mla(k, need_ctx, p_tm_d, wuq, wukv, cos_d, sin_d, ident_bf, ones_bf, ymla_d):
    scale = 192.0 ** -0.5
    with ExitStack() as st:
        cos_sb = k.sb("cos", [128, 16, 128], F32, st); sin_sb = k.sb("sin", [128, 16, 128], F32, st)
        k.dma("sp", cos_sb[:], cos_d.t.rearrange("(tt p) c -> p tt c", p=128), writes=[cos_sb])
        k.dma("sp", sin_sb[:], sin_d.t.rearrange("(tt p) c -> p tt c", p=128), writes=[sin_sb])
        p_all = k.sb("pall", [128, NTT, 448], F32, st)
        k.dma("sp", p_all[:], p_tm_d.t.rearrange("(tt p) c -> p tt c", p=128)[:, :, 0:448], reads=[p_tm_d], writes=[p_all])
        qlnT = k.sb("qlnT", [128, 2, T], BF16, st); kvnT = k.sb("kvnT", [128, T], BF16, st)
        qTn = k.sb("qTn", [128, 4, T], BF16, st); qTr = k.sb("qTr", [64, 4, T], BF16, st)
        knT = k.sb("knT", [128, 4, T], BF16, st); krT = k.sb("krT", [64, T], BF16, st)
        v_sb = k.sb("vsb", [128, NTT, 512], BF16, st)
        rq = k.sb("rq", [128, NTT], F32, st); rkv = k.sb("rkv", [128, NTT], F32, st)
        with ExitStack() as st1:
            sq = k.sb("sq", [128, NTT, 384], F32, st1)
            k.op("act", lambda e: e.activation(sq[:], p_all[:, :, 0:384], AF.Square), reads=[p_all], writes=[sq])
            k.op("dve", lambda e: e.reduce_sum(rq[:], sq[:, :, 0:256], axis=AX.X), reads=[sq], writes=[rq])
            k.op("dve", lambda e: e.reduce_sum(rkv[:], sq[:, :, 256:384], axis=AX.X), reads=[sq], writes=[rkv])
            k.barrier()
        k.op("dve", lambda e: e.tensor_scalar(rq[:], rq[:], 1.0 / 256, 1e-6, op0=ALU.mult, op1=ALU.add), reads=[rq], writes=[rq])
        k.op("dve", lambda e: e.tensor_scalar(rkv[:], rkv[:], 1.0 / 128, 1e-6, op0=ALU.mult, op1=ALU.add), reads=[rkv], writes=[rkv])
        for r_ in (rq, rkv):
            k.op("act", lambda e: e.sqrt(r_[:], r_[:]), reads=[r_], writes=[r_])
            k.op("dve", lambda e: e.reciprocal(r_[:], r_[:]), reads=[r_], writes=[r_])
        st2 = ExitStack()
        nrm = [k.sb("nrm", [128, 384], BF16, st2) for _ in range(2)]
        pTs = [k.ps("mlapT", [128, 3, 128], BF16, st2) for _ in range(1)]
        pq = k.ps("mlapq", [128, 4, 256], F32, st2)
        pv = k.ps("mlapv", [128, 512], F32, st2)
        q_sb = k.sb("qsb", [128, 4, 192], BF16, st2); kr_sb = k.sb("krsb", [128, 64], BF16, st2)
        ra = k.sb("ra", [128, 4, 32], F32, st2); rb = k.sb("rb", [128, 4, 32], F32, st2); qr_f = k.sb("qrf", [128, 4, 64], F32, st2)
        pqT = k.ps("mlapqT", [128, 4, 128], BF16, st2); pqTr = k.ps("mlapqTr", [64, 5, 128], BF16, st2)
        STOP = 99; SKIP = ""
        for tt in range(NTT if STOP > 1 else 0):
            isctx = tt < 2
            nt = nrm[tt % 2]; pT = pTs[0]
            k.op("dve", lambda e: e.tensor_scalar(nt[:, 0:256], p_all[:, tt, 0:256], rq[:, tt:tt + 1], None, op0=ALU.mult), reads=[p_all, rq], writes=[nt])
            k.op("dve", lambda e: e.tensor_scalar(nt[:, 256:384], p_all[:, tt, 256:384], rkv[:, tt:tt + 1], None, op0=ALU.mult), reads=[p_all, rkv], writes=[nt])
            for c in range(3):
                k.op("pe", lambda e: e.transpose(pT[:, c, :], nt[:, c * 128:(c + 1) * 128], ident_bf[:]), reads=[nt, ident_bf], writes=[pT])
            sl = slice(tt * 128, (tt + 1) * 128)
            k.op("act", lambda e: e.copy(qlnT[:, :, sl], pT[:, 0:2, :]), reads=[pT], writes=[qlnT])
            k.op("act", lambda e: e.copy(kvnT[:, sl], pT[:, 2, :]), reads=[pT], writes=[kvnT])
            if "v" not in SKIP: k.op("pe", lambda e: e.matmul(pv[:].rearrange("p (h c) -> p h c", h=4), lhsT=kvnT[:, sl],
                                          rhs=wukv[:].rearrange("p (h c) -> p h c", h=4)[:, :, 128:256], start=True, stop=True),
                 reads=[kvnT, wukv], writes=[pv])
            k.op("act", lambda e: e.copy(v_sb[:, tt, :], pv[:]), reads=[pv], writes=[v_sb])
            kre = p_all[:, tt, 384:448:2]; kro = p_all[:, tt, 385:448:2]
            if "k" in SKIP:
                pass
            elif isctx:
                k.op("dve", lambda e: e.tensor_copy(kr_sb[:, 0:32], kre), reads=[p_all], writes=[kr_sb])
                k.op("dve", lambda e: e.tensor_copy(kr_sb[:, 32:64], kro), reads=[p_all], writes=[kr_sb])
            else:
                cs = cos_sb[:, tt - 2, 0:32]; sn = sin_sb[:, tt - 2, 0:32]
                k.op("dve", lambda e: e.tensor_tensor(ra[:, 0, :], kre, cs, ALU.mult), reads=[p_all, cos_sb], writes=[ra])
                k.op("dve", lambda e: e.tensor_tensor(rb[:, 0, :], kro, sn, ALU.mult), reads=[p_all, sin_sb], writes=[rb])
                k.op("dve", lambda e: e.tensor_tensor(kr_sb[:, 0:32], ra[:, 0, :], rb[:, 0, :], ALU.subtract), reads=[ra, rb], writes=[kr_sb])
                k.op("dve", lambda e: e.tensor_tensor(ra[:, 0, :], kre, sn, ALU.mult), reads=[p_all, sin_sb], writes=[ra])
                k.op("dve", lambda e: e.tensor_tensor(rb[:, 0, :], kro, cs, ALU.mult), reads=[p_all, cos_sb], writes=[rb])
                k.op("dve", lambda e: e.tensor_tensor(kr_sb[:, 32:64], ra[:, 0, :], rb[:, 0, :], ALU.add), reads=[ra, rb], writes=[kr_sb])
            doq = ((not isctx) or need_ctx) and "q" not in SKIP
            if doq:
                for h in range(4):
                    for kc in range(2):
                        k.op("pe", lambda e: e.matmul(pq[:, h, 0:192], lhsT=qlnT[:, kc, sl], rhs=wuq[:, kc, h * 192:(h + 1) * 192], start=(kc == 0), stop=(kc == 1)),
                             reads=[qlnT, wuq], writes=[pq])
                if "a" not in SKIP: k.op("act", lambda e: e.copy(q_sb[:, :, 0:128], pq[:, :, 0:128]), reads=[pq], writes=[q_sb])
                k.op("act", lambda e: e.copy(qr_f[:], pq[:, :, 128:192]), reads=[pq], writes=[qr_f])
                qe = qr_f[:, :, 0:64:2]; qo = qr_f[:, :, 1:64:2]
                if isctx:
                    k.op("dve", lambda e: e.tensor_copy(q_sb[:, :, 128:160], qe), reads=[qr_f], writes=[q_sb])
                    k.op("dve", lambda e: e.tensor_copy(q_sb[:, :, 160:192], qo), reads=[qr_f], writes=[q_sb])
                else:
                    cs = cos_sb[:, tt - 2, :].rearrange("p (h c) -> p h c", h=4); sn = sin_sb[:, tt - 2, :].rearrange("p (h c) -> p h c", h=4)
                    k.op("dve", lambda e: e.tensor_tensor(ra[:], qe, cs, ALU.mult), reads=[qr_f, cos_sb], writes=[ra])
                    k.op("dve", lambda e: e.tensor_tensor(rb[:], qo, sn, ALU.mult), reads=[qr_f, sin_sb], writes=[rb])
                    k.op("dve", lambda e: e.tensor_tensor(q_sb[:, :, 128:160], ra[:], rb[:], ALU.subtract), reads=[ra, rb], writes=[q_sb])
                    k.op("dve", lambda e: e.tensor_tensor(ra[:], qe, sn, ALU.mult), reads=[qr_f, sin_sb], writes=[ra])
                    k.op("dve", lambda e: e.tensor_tensor(rb[:], qo, cs, ALU.mult), reads=[qr_f, cos_sb], writes=[rb])
                    k.op("dve", lambda e: e.tensor_tensor(q_sb[:, :, 160:192], ra[:], rb[:], ALU.add), reads=[ra, rb], writes=[q_sb])
                for h in range(4 if "t" not in SKIP else 0):
                    k.op("pe", lambda e: e.transpose(pqT[:, h, :], q_sb[:, h, 0:128], ident_bf[:]), reads=[q_sb, ident_bf], writes=[pqT])
                    k.op("pe", lambda e: e.transpose(pqTr[:, h, :], q_sb[:, h, 128:192], ident_bf[:]), reads=[q_sb, ident_bf], writes=[pqTr])
            if "r" not in SKIP: k.op("pe", lambda e: e.transpose(pqTr[:, 4, :], kr_sb[:], ident_bf[:]), reads=[kr_sb, ident_bf], writes=[pqTr])
            if doq:
                k.op("act", lambda e: e.copy(qTn[:, :, sl], pqT[:]), reads=[pqT], writes=[qTn])
                k.op("dve", lambda e: e.tensor_copy(qTr[:, :, sl], pqTr[:, 0:4, :]), reads=[pqTr], writes=[qTr])
            k.op("dve", lambda e: e.tensor_copy(krT[:, sl], pqTr[:, 4, :]), reads=[pqTr], writes=[krT])
        k.barrier(); st2.close()
        groups = [(0, 512), (512, 512), (1024, 512), (1536, 512), (2048, 256)]
        pk = [k.ps("mlapk", [128, 512], F32, st) for _ in range(2)]
        cnt = 0
        for h in range(4 if STOP > 2 else 0):
            for (o, n) in groups:
                ps = pk[cnt % 2]
                k.op("pe", lambda e: e.matmul(ps[:, 0:n], lhsT=wukv[:, h * 256:h * 256 + 128], rhs=kvnT[:, o:o + n], start=True, stop=True), reads=[wukv, kvnT], writes=[ps])
                k.op("act" if cnt % 2 else "dve", (lambda e: e.copy(knT[:, h, o:o + n], ps[:, 0:n])) if cnt % 2 else (lambda e: e.tensor_copy(knT[:, h, o:o + n], ps[:, 0:n])),
                     reads=[ps], writes=[knT])
                cnt += 1
        pos = [k.ps("mlapo", [128, 512], F32, st) for _ in range(2)]
        pss = [k.ps("mlapss", [128, 512], F32, st) for _ in range(2)]
        pts = [k.sb("mlapt", [128, 512], BF16, st) for _ in range(3)]
        rs = k.sb("mlars", [128, 512], F32, st); ob = [k.sb("mlaob", [128, 512], BF16, st) for _ in range(2)]
        qblocks = [(256 + i * 512, 512, list(range(NTT))) for i in range(4)]
        if need_ctx:
            qblocks.append((0, 256, [0, 1]))
        it = 0; bi = 0
        for h in range(4 if STOP > 3 else 0):
            for (qo_, nq, ktiles) in qblocks:
                po = pos[bi % 2]; psum = pss[bi % 2]
                for ji, j in enumerate(ktiles):
                    ps = pk[it % 2]; pt = pts[it % 3]
                    ks = slice(j * 128, (j + 1) * 128)
                    k.op("pe", lambda e: e.matmul(ps[:, 0:nq], lhsT=knT[:, h, ks], rhs=qTn[:, h, qo_:qo_ + nq], start=True, stop=False), reads=[knT, qTn], writes=[ps])
                    k.op("pe", lambda e: e.matmul(ps[:, 0:nq], lhsT=krT[:, ks], rhs=qTr[:, h, qo_:qo_ + nq], start=False, stop=True), reads=[krT, qTr], writes=[ps])
                    k.op("act", lambda e: e.activation(pt[:, 0:nq], ps[:, 0:nq], AF.Exp, scale=scale), reads=[ps], writes=[pt])
                    first = ji == 0; last = ji == len(ktiles) - 1
                    k.op("pe", lambda e: e.matmul(po[:, 0:nq], lhsT=v_sb[:, j, h * 128:(h + 1) * 128], rhs=pt[:, 0:nq], start=first, stop=last), reads=[v_sb, pt], writes=[po])
                    k.op("pe", lambda e: e.matmul(psum[:, 0:nq], lhsT=ones_bf[:], rhs=pt[:, 0:nq], start=first, stop=last), reads=[ones_bf, pt], writes=[psum])
                    it += 1
                o_ = ob[bi % 2]
                k.op("dve", lambda e: e.reciprocal(rs[:, 0:nq], psum[:, 0:nq]), reads=[psum], writes=[rs])
                k.op("dve", lambda e: e.tensor_tensor(o_[:, 0:nq], po[:, 0:nq], rs[:, 0:nq], ALU.mult), reads=[po, rs], writes=[o_])
                k.dma("sp", ymla_d[h][:, qo_:qo_ + nq], o_[:, 0:nq], reads=[o_], writes=[ymla_d])
                bi += 1
    k.barrier()


def rope_tables():
    t = np.arange(SEQ); row = (t // 64).astype(np.float32); col = (t % 64).astype(np.float32)
    inv = (10000.0 ** (-np.arange(16, dtype=np.float32) / 16)).astype(np.float32)
    ang = np.concatenate([row[:, None] * inv, col[:, None] * inv], -1).astype(np.float32)
    return np.tile(np.cos(ang).astype(np.float32), (1, 4)), np.tile(np.sin(ang).astype(np.float32), (1, 4))


def na_pattern(i):
    if i < 4:
        return i, 0
    if i <= 28:
        return 4, i - 4
    return i - 24, 24


def na_bias_host(rpb):
    L = rpb.shape[0]
    col = np.arange(64)
    dc = np.clip(col[None, :] - col[:, None] + 15, 0, 30)
    out = np.zeros((L, 128, 4, 8, 4, 64), np.float32)
    lk = np.arange(512); r = lk // 64; kc = lk % 64
    for i in list(range(5)) + [29, 30, 31]:
        pat, rs = na_pattern(i)
        dr = rs + r - i + 7
        g = rpb[:, :, dr[:, None], dc.T[kc, :]]
        out[:, :, :, pat] = g.reshape(L, 4, 4, 128, 64).transpose(0, 3, 1, 2, 4)
    return out


def na_mask_host():
    col = np.arange(64)
    cs = np.clip(col - 8, 0, 48)
    inw = (col[None, :] >= cs[:, None]) & (col[None, :] < cs[:, None] + 16)
    lk = np.arange(512); kc = lk % 64
    m = np.where(inw.T[kc, :], 0.0, -30000.0).astype(np.float32)
    return np.ascontiguousarray(m.reshape(4, 128, 64).transpose(1, 0, 2))


def load_na_bias(k, l, st, bias_d, mask_d):
    bm = k.sb("nabm", [128, 4, 8, 384], F32, st); mk = k.sb("namk", [128, 256], F32, st)
    k.op("pool", lambda e: e.memset(bm[:], 0.0), writes=[bm])
    k.dma("sp", mk[:], mask_d.t.rearrange("p j q -> p (j q)"), writes=[mk])
    for h in range(4):
        k.dma("sp", bm[:, h, :, 0:256], bias_d.t[l][:, h].rearrange("p a j q -> p a (j q)"), writes=[bm])
    for h in range(4):
        for pat in range(8):
            k.op("pool", lambda e: e.tensor_tensor(bm[:, h, pat, 0:256], bm[:, h, pat, 0:256], mk[:], ALU.add), reads=[bm, mk], writes=[bm])
    return bm


def phase_na(k, need_ctx, pT_na_d, p_tm_d, bm, ones_bf, yna_d):
    scale = 64.0 ** -0.5
    with ExitStack() as st:
        qT = k.sb("naqT", [64, 4, T], BF16, st); kT = k.sb("nakT", [64, 4, T], BF16, st)
        vA = k.sb("navA", [128, NTT, 256], BF16, st); vB = k.sb("navB", [128, NTT - 1, 256], BF16, st)
        yo = k.sb("nayo", [64, 4, T], BF16, st)
        k.dma("sp", qT[:], pT_na_d.t[0:256, :].rearrange("(h d) t -> d h t", h=4), reads=[pT_na_d], writes=[qT])
        k.dma("sp", kT[:], pT_na_d.t[256:512, :].rearrange("(h d) t -> d h t", h=4), reads=[pT_na_d], writes=[kT])
        k.dma("pool", vA[:], p_tm_d.t.rearrange("(tt p) c -> p tt c", p=128)[:, :, 448:704], reads=[p_tm_d], writes=[vA])
        k.dma("pool", vB[:], p_tm_d.t[64:64 + (NTT - 1) * 128, :].rearrange("(tt p) c -> p tt c", p=128)[:, :, 448:704], reads=[p_tm_d], writes=[vB])
        pSs = [k.ps("napS", [128, 6, 64], F32, st) for _ in range(2)]
        pos = [k.ps("napo", [64, 256], F32, st) for _ in range(2)]
        pms = [k.ps("napm", [64, 256], F32, st) for _ in range(2)]
        ssb = [k.sb("nassb", [128, 6, 64], F32, st) for _ in range(2)]
        pts = [k.sb("napt", [128, 6, 64], BF16, st) for _ in range(2)]
        ptc = [k.sb("naptc", [128, 2, 256], BF16, st) for _ in range(2)]
        rss = [k.sb("nars", [64, 256], F32, st) for _ in range(2)]
        it = 0
        for h in range(4):
            for i in range(32):
                pat, rs_ = na_pattern(i)
                pS = pSs[it % 2]; po = pos[it % 2]; pm = pms[it % 2]; s_sb = ssb[it % 2]; pt = pts[it % 2]; rs = rss[it % 2]
                tok0 = 256 + 64 * rs_
                qs = slice(256 + 64 * i, 256 + 64 * i + 64)
                for j in range(6):
                    ko = tok0 + 128 * j if j < 4 else (j - 4) * 128
                    k.op("pe", lambda e: e.matmul(pS[:, j, :], lhsT=kT[:, h, ko:ko + 128], rhs=qT[:, h, qs], start=True, stop=True), reads=[kT, qT], writes=[pS])
                k.op("dve", lambda e: e.scalar_tensor_tensor(s_sb[:].rearrange("p j q -> p (j q)"), pS[:].rearrange("p j q -> p (j q)"), scale, bm[:, h, pat, :], op0=ALU.mult, op1=ALU.add),
                     reads=[pS, bm], writes=[s_sb])
                k.op("act", lambda e: e.activation(pt[:], s_sb[:], AF.Exp), reads=[s_sb], writes=[pt])
                for j in range(6):
                    if j < 4:
                        vt = (vA, 2 + rs_ // 2 + j) if rs_ % 2 == 0 else (vB, (3 + rs_) // 2 + j)
                    else:
                        vt = (vA, j - 4)
                    vtk, vi = vt
                    k.op("pe", lambda e: e.matmul(po[:, 0:64], lhsT=vtk[:, vi, h * 64:(h + 1) * 64], rhs=pt[:, j, :], start=(j == 0), stop=(j == 5)), reads=[vtk, pt], writes=[po])
                for j in range(6):
                    k.op("pe", lambda e: e.matmul(pm[:, 0:64], lhsT=ones_bf[:, 0:64], rhs=pt[:, j, :], start=(j == 0), stop=(j == 5)), reads=[ones_bf, pt], writes=[pm])
                k.op("dve", lambda e: e.reciprocal(rs[:, 0:64], pm[:, 0:64]), reads=[pm], writes=[rs])
                k.op("dve", lambda e: e.tensor_tensor(yo[:, h, qs], po[:, 0:64], rs[:, 0:64], ALU.mult), reads=[po, rs], writes=[yo])
                it += 1
            if need_ctx:
                pS = pSs[it % 2]; po = pos[it % 2]; pm = pms[it % 2]; pt = ptc[it % 2]; rs = rss[it % 2]
                pSv = pS[:].rearrange("p j q -> p (j q)")
                for j in range(2):
                    k.op("pe", lambda e: e.matmul(pSv[:, 0:256], lhsT=kT[:, h, j * 128:(j + 1) * 128], rhs=qT[:, h, 0:256], start=True, stop=True), reads=[kT, qT], writes=[pS])
                    k.op("act", lambda e: e.activation(pt[:, j, :], pSv[:, 0:256], AF.Exp, scale=scale), reads=[pS], writes=[pt])
                for j in range(2):
                    k.op("pe", lambda e: e.matmul(po[:], lhsT=vA[:, j, h * 64:(h + 1) * 64], rhs=pt[:, j, :], start=(j == 0), stop=(j == 1)), reads=[vA, pt], writes=[po])
                for j in range(2):
                    k.op("pe", lambda e: e.matmul(pm[:], lhsT=ones_bf[:, 0:64], rhs=pt[:, j, :], start=(j == 0), stop=(j == 1)), reads=[ones_bf, pt], writes=[pm])
                k.op("dve", lambda e: e.reciprocal(rs[:], pm[:]), reads=[pm], writes=[rs])
                k.op("dve", lambda e: e.tensor_tensor(yo[:, h, 0:256], po[:], rs[:], ALU.mult), reads=[po, rs], writes=[yo])
                it += 1
        k.dma("sp", yna_d[:], yo[:], reads=[yo], writes=[yna_d])
    k.barrier()


NCH = T // 64
RW_STOP = 99
class _Stop(Exception):
    pass
def chk(level):
    if RW_STOP == level:
        raise _Stop()


def rw_consts_host():
    i = np.arange(128) % 64
    lt = (i[None, :] < i[:, None]).astype(np.float32); le = (i[None, :] <= i[:, None]).astype(np.float32)
    gt = (i[None, :] > i[:, None]).astype(np.float32); ge = (i[None, :] >= i[:, None]).astype(np.float32)
    blk = ((np.arange(128)[:, None] // 64) == (np.arange(128)[None, :] // 64)).astype(np.float32)
    m2 = np.stack([np.concatenate([gt, ge], 1), np.concatenate([lt, le], 1)], 0)
    m1 = np.stack([lt, gt], 0)
    isel = np.concatenate([np.eye(64, dtype=np.float32)] * 2, 0)
    return m2, m1, blk, isel


def load_rw_w(k, l, st, W, ident_f):
    o = {}
    o["w2"] = k.sb("rww2", [128, 256], BF16, st); o["a2"] = k.sb("rwa2", [128, 256], BF16, st); o["g2"] = k.sb("rwg2", [128, 256], BF16, st)
    k.dma("pool", o["w2"][:], W["rw_w2"].t[l].rearrange("d k n -> (d k) n"), writes=[o["w2"]])
    k.dma("pool", o["a2"][:], W["rw_a2"].t[l].rearrange("d k n -> (d k) n"), writes=[o["a2"]])
    k.dma("pool", o["g2"][:], W["rw_g2"].t[l], writes=[o["g2"]])
    stage = k.sb("rwstage", [32, 128], F32, st)
    k.dma("sp", stage[0:18, :], W["rw_mu"].t[l].rearrange("d (c p) -> (d c) p", p=128), writes=[stage])
    k.dma("sp", stage[18:22, :], W["rw_w0"].t[l].rearrange("d (c p) -> (d c) p", p=128), writes=[stage])
    k.dma("sp", stage[22:26, :], W["rw_a0"].t[l].rearrange("d (c p) -> (d c) p", p=128), writes=[stage])
    k.dma("sp", stage[26:28, :], W["rw_kk"].t[l].rearrange("(c p) -> c p", p=128), writes=[stage])
    k.dma("sp", stage[28:30, :], W["rw_ka"].t[l].rearrange("(c p) -> c p", p=128), writes=[stage])
    k.dma("sp", stage[30:32, :], W["rw_rk"].t[l].rearrange("(c h) n -> c (h n)", c=2), writes=[stage])
    cols = k.sb("rwcols", [128, 48], F32, st)
    with ExitStack() as s2:
        pc = k.ps("rwpc", [128, 32], F32, s2)
        k.op("pe", lambda e: e.transpose(pc[:], stage[:], ident_f[0:32, 0:32]), reads=[stage, ident_f], writes=[pc])
        k.op("dve", lambda e: e.tensor_copy(cols[:, 0:32], pc[:]), reads=[pc], writes=[cols])
        k.barrier()
    k.op("dve", lambda e: e.tensor_tensor(cols[:, 32:41], cols[:, 0:9], cols[:, 9:18], ALU.add), reads=[cols], writes=[cols])
    k.op("dve", lambda e: e.tensor_scalar(cols[:, 32:41], cols[:, 32:41], -1.0, 1.0, op0=ALU.mult, op1=ALU.add), reads=[cols], writes=[cols])
    k.op("dve", lambda e: e.tensor_scalar(cols[:, 41:43], cols[:, 28:30], -1.0, 1.0, op0=ALU.mult, op1=ALU.add), reads=[cols], writes=[cols])
    o["cols"] = cols
    gn = k.sb("rwgn", [128, 2, 2, 64], F32, st)
    for P in range(2):
        for h in range(2):
            hd = 2 * P + h
            k.dma("sp", gn[h * 64:(h + 1) * 64, P, 0, :], W["rw_gn_w"].t[l:l + 1, hd * 64:(hd + 1) * 64].partition_broadcast(64), writes=[gn])
            k.dma("sp", gn[h * 64:(h + 1) * 64, P, 1, :], W["rw_gn_b"].t[l:l + 1, hd * 64:(hd + 1) * 64].partition_broadcast(64), writes=[gn])
    o["gn"] = gn
    return o


def phase_rwkv(k, pT_rw_d, RW, C, yrw_d):
    cols = RW["cols"]
    EM05 = float(np.exp(-0.5))
    with ExitStack() as st:
        tmpA = k.sb("rwtmpA", [128, T], F32, st); tmpB = k.sb("rwtmpB", [128, T], F32, st)

        def shifted(c, out):
            k.dma("sp", tmpA[:], pT_rw_d[c * 128:(c + 1) * 128, :], reads=[pT_rw_d], writes=[tmpA])
            k.op("act", lambda e: e.activation(out[:], tmpA[:], AF.Identity, scale=cols[:, 32 + c:33 + c]), reads=[tmpA, cols], writes=[out])
            for (dst, src, mc) in (((1, 256), (0, 255), c), ((257, T), (256, T - 1), c), ((0, 255), (1, 256), 9 + c), ((256, T - 1), (257, T), 9 + c)):
                k.op("dve", lambda e: e.scalar_tensor_tensor(out[:, dst[0]:dst[1]], tmpA[:, src[0]:src[1]], cols[:, mc:mc + 1], out[:, dst[0]:dst[1]], op0=ALU.mult, op1=ALU.add),
                     reads=[tmpA, cols, out], writes=[out])

        tw = k.sb("rwtw", [128, T], BF16, st); al = k.sb("rwal", [128, T], BF16, st); sg = k.sb("rwsg", [128, T], BF16, st)
        shifted(6, tmpB); k.op("act", lambda e: e.activation(tw[:], tmpB[:], AF.Tanh), reads=[tmpB], writes=[tw])
        shifted(7, tmpB); k.op("act", lambda e: e.copy(al[:], tmpB[:]), reads=[tmpB], writes=[al])
        shifted(8, tmpB); k.op("act", lambda e: e.activation(sg[:], tmpB[:], AF.Sigmoid), reads=[tmpB], writes=[sg])
        groups = [(0, 512), (512, 512), (1024, 512), (1536, 512), (2048, 256)]
        chk(1)
        for P in range(2 if RW_STOP > 1 else 0):
            with ExitStack() as sp:
                r_f = k.sb("rwr", [128, T], F32, sp); k_f = k.sb("rwk", [128, T], F32, sp); kk_f = k.sb("rwkk", [128, T], F32, sp)
                vst = k.sb("rwvst", [128, NCH, 64], BF16, sp)
                ccol = k.sb("rwccol", [128, NCH], F32, sp)
                o_all = k.sb("rwoall", [128, NCH, 64], F32, sp)
                shifted(0 + P, r_f); shifted(2 + P, k_f)
                sv = ExitStack()
                vexp = k.sb("rwvexp", [128, NCH, 128], BF16, sv)
                shifted(4 + P, tmpB)
                k.op("pool", lambda e: e.memset(vexp[:], 0.0), writes=[vexp])
                for h in range(2):
                    hs = slice(h * 64, (h + 1) * 64)
                    k.op("dve", lambda e: e.tensor_copy(vexp[hs, :, hs], tmpB[hs, :].rearrange("p (c s) -> p c s", s=64)), reads=[tmpB], writes=[vexp])
                k.op("dve", lambda e: e.tensor_scalar(kk_f[:], k_f[:], cols[:, 26 + P:27 + P], None, op0=ALU.mult), reads=[k_f, cols], writes=[kk_f])
                k.op("act", lambda e: e.activation(tmpB[:], kk_f[:], AF.Square), reads=[kk_f], writes=[tmpB])
                with ExitStack() as s2:
                    pg = [k.ps("rwpn", [128, 512], F32, s2) for _ in range(2)]
                    for gi, (o, n) in enumerate(groups):
                        ps = pg[gi % 2]
                        k.op("pe", lambda e: e.matmul(ps[:, 0:n], lhsT=C["blk"][:], rhs=tmpB[:, o:o + n], start=True, stop=True), reads=[C["blk"], tmpB], writes=[ps])
                        k.op("dve", lambda e: e.tensor_scalar(tmpA[:, o:o + n], ps[:, 0:n], 1e-12, None, op0=ALU.add), reads=[ps], writes=[tmpA])
                    k.op("act", lambda e: e.sqrt(tmpA[:], tmpA[:]), reads=[tmpA], writes=[tmpA])
                    k.op("dve", lambda e: e.reciprocal(tmpA[:], tmpA[:]), reads=[tmpA], writes=[tmpA])
                    k.op("dve", lambda e: e.tensor_tensor(kk_f[:], kk_f[:], tmpA[:], ALU.mult), reads=[kk_f, tmpA], writes=[kk_f])
                    rkexp = k.sb("rwrkexp", [128, NCH, 128], BF16, s2)
                    k.op("pool", lambda e: e.memset(rkexp[:], 0.0), writes=[rkexp])
                    k.op("dve", lambda e: e.scalar_tensor_tensor(tmpB[:], r_f[:], cols[:, 30 + P:31 + P], k_f[:], op0=ALU.mult, op1=ALU.mult), reads=[r_f, k_f, cols], writes=[tmpB])
                    for h in range(2):
                        hs = slice(h * 64, (h + 1) * 64)
                        k.op("dve", lambda e: e.tensor_copy(rkexp[hs, :, hs], tmpB[hs, :].rearrange("p (c s) -> p c s", s=64)), reads=[tmpB], writes=[rkexp])
                    pv = [k.ps("rwpv", [128, 8, 64], F32, s2) for _ in range(2)]
                    pcc = k.ps("rwpcc", [128, NCH], F32, s2)
                    for c0 in range(0, NCH, 8):
                        nn = min(8, NCH - c0); ps = pv[(c0 // 8) % 2]
                        for j in range(nn):
                            k.op("pe", lambda e: e.matmul(ps[:, j, :], lhsT=vexp[:, c0 + j, :], rhs=C["isel"][:], start=True, stop=True), reads=[vexp, C["isel"]], writes=[ps])
                        k.op("act", lambda e: e.copy(vst[:, c0:c0 + nn, :], ps[:, 0:nn, :]), reads=[ps], writes=[vst])
                    for c in range(NCH):
                        k.op("pe", lambda e: e.matmul(pcc[:, c:c + 1], lhsT=rkexp[:, c, :], rhs=C["ones_bf"][:, 0:1], start=True, stop=True), reads=[rkexp, C["ones_bf"]], writes=[pcc])
                    k.op("dve", lambda e: e.tensor_copy(ccol[:], pcc[:]), reads=[pcc], writes=[ccol])
                    k.barrier()
                sv.close()
                if RW_STOP == 2: continue

                for d in range(2):
                    with ExitStack() as sd:
                        if RW_STOP < 7 and (P, d) != (0, 0): continue
                        rw_dir(k, sd, P, d, RW, C, cols, tw, al, r_f, k_f, kk_f, vst, o_all, tmpA, tmpB, groups, EM05)
                        k.barrier()
                if RW_STOP < 7: continue
                with ExitStack() as s2:
                    s1 = k.sb("rws1", [128, NCH], F32, s2); s2_ = k.sb("rws2", [128, NCH], F32, s2); rstd = k.sb("rwrstd", [128, NCH], F32, s2)
                    sq = k.sb("rwsq", [128, NCH, 64], F32, s2); fin = k.sb("rwfin", [128, NCH, 64], F32, s2)
                    k.op("dve", lambda e: e.reduce_sum(s1[:], o_all[:], axis=AX.X), reads=[o_all], writes=[s1])
                    k.op("act", lambda e: e.activation(sq[:], o_all[:], AF.Square), reads=[o_all], writes=[sq])
                    k.op("dve", lambda e: e.reduce_sum(s2_[:], sq[:], axis=AX.X), reads=[sq], writes=[s2_])
                    k.op("dve", lambda e: e.tensor_scalar(s1[:], s1[:], 1.0 / 64, None, op0=ALU.mult), reads=[s1], writes=[s1])
                    k.op("dve", lambda e: e.tensor_tensor(rstd[:], s1[:], s1[:], ALU.mult), reads=[s1], writes=[rstd])
                    k.op("dve", lambda e: e.scalar_tensor_tensor(rstd[:], s2_[:], 1.0 / 64, rstd[:], op0=ALU.mult, op1=ALU.subtract), reads=[s2_, rstd], writes=[rstd])
                    k.op("dve", lambda e: e.tensor_scalar(rstd[:], rstd[:], 64e-5, None, op0=ALU.add), reads=[rstd], writes=[rstd])
                    k.op("act", lambda e: e.sqrt(rstd[:], rstd[:]), reads=[rstd], writes=[rstd])
                    k.op("dve", lambda e: e.reciprocal(rstd[:], rstd[:]), reads=[rstd], writes=[rstd])
                    yfm = k.sb("rwyfm", [64, 2, T], BF16, s2)
                    gfm = k.sb("rwgfm", [64, 2, T], F32, s2); vstf = k.sb("rwvstf", [128, NCH, 64], F32, s2)
                    k.op("pool", lambda e: e.tensor_copy(vstf[:], vst[:]), reads=[vst], writes=[vstf])
                    pgg = [k.ps("rwpg", [128, 512], F32, s2) for _ in range(2)]
                    cnt = 0
                    for h in range(2):
                        hd = 2 * P + h
                        for (o, n) in groups:
                            ps = pgg[cnt % 2]
                            k.op("pe", lambda e: e.matmul(ps[0:64, 0:n], lhsT=RW["g2"][:, hd * 64:(hd + 1) * 64], rhs=sg[:, o:o + n], start=True, stop=True), reads=[RW["g2"], sg], writes=[ps])
                            k.op("act", lambda e: e.copy(gfm[:, h, o:o + n], ps[0:64, 0:n]), reads=[ps], writes=[gfm])
                            cnt += 1
                    gnw = RW["gn"][:, P, 0, :]; gnb = RW["gn"][:, P, 1, :]
                    for c in range(NCH):
                        k.op("dve", lambda e: e.scalar_tensor_tensor(sq[:, c, :], o_all[:, c, :], s1[:, c:c + 1], gnw, op0=ALU.subtract, op1=ALU.mult), reads=[o_all, s1, RW["gn"]], writes=[sq])
                        k.op("dve", lambda e: e.scalar_tensor_tensor(sq[:, c, :], sq[:, c, :], rstd[:, c:c + 1], gnb, op0=ALU.mult, op1=ALU.add), reads=[sq, rstd, RW["gn"]], writes=[sq])
                        k.op("dve", lambda e: e.scalar_tensor_tensor(fin[:, c, :], vstf[:, c, :], ccol[:, c:c + 1], sq[:, c, :], op0=ALU.mult, op1=ALU.add), reads=[vstf, ccol, sq], writes=[fin])
                    pts = [k.ps("rwpt", [64, 4, 128], F32, s2) for _ in range(2)]
                    for c0 in range(0, NCH, 4):
                        ps = pts[(c0 // 4) % 2]
                        for j in range(4):
                            k.op("pe", lambda e: e.transpose(ps[:, j, :], fin[:, c0 + j, :], C["ident_f"][:]), reads=[fin, C["ident_f"]], writes=[ps])
                        for h in range(2):
                            hd = 2 * P + h
                            k.op("dve", lambda e: e.tensor_tensor(yfm[:, h, c0 * 64:(c0 + 4) * 64].rearrange("p (c s) -> p c s", s=64), ps[:, :, h * 64:(h + 1) * 64],
                                                                  gfm[:, h, c0 * 64:(c0 + 4) * 64].rearrange("p (c s) -> p c s", s=64), ALU.mult), reads=[ps, gfm], writes=[yfm])
                    k.dma("sp", yrw_d[:, 2 * P:2 * P + 2, :], yfm[:], reads=[yfm], writes=[yrw_d])
                    k.barrier()
    k.barrier()


def rw_dir(k, sd, P, d, RW, C, cols, tw, al, r_f, k_f, kk_f, vst, o_all, tmpA, tmpB, groups, EM05):
    dsl = slice(d * 64, (d + 1) * 64)
    AR = k.sb("rwAR", [128, NCH, 2, 128], BF16, sd); Bx = k.sb("rwBx", [128, NCH, 128], BF16, sd); Kx = k.sb("rwKx", [128, NCH, 128], BF16, sd)
    gc = k.sb("rwgc", [128, NCH], F32, sd); eoff = k.sb("rweoff", [128, NCH, 2], F32, sd)
    stmp = ExitStack()
    Gp = k.sb("rwGp", [128, T + 1], F32, stmp)
    b_f = k.sb("rwb", [128, T], F32, stmp); kd_f = tmpB
    eB = k.sb("rweB", [128, T], F32, stmp); eA = k.sb("rweA", [128, T], F32, stmp); eR = tmpA
    with ExitStack() as s2:
        pg = [k.ps("rwpl", [128, 512], F32, s2) for _ in range(2)]
        cnt = 0
        for (o, n) in groups:
            ps = pg[cnt % 2]; cnt += 1
            k.op("pe", lambda e: e.matmul(ps[:, 0:n], lhsT=RW["w2"][dsl, P * 128:(P + 1) * 128], rhs=tw[dsl, o:o + n], start=True, stop=True), reads=[RW["w2"], tw], writes=[ps])
            k.op("act", lambda e: e.activation(tmpA[:, o:o + n], ps[:, 0:n], AF.Sigmoid, bias=cols[:, 18 + 2 * d + P:19 + 2 * d + P]), reads=[ps, cols], writes=[tmpA])
            ps = pg[cnt % 2]; cnt += 1
            k.op("pe", lambda e: e.matmul(ps[:, 0:n], lhsT=RW["a2"][dsl, P * 128:(P + 1) * 128], rhs=al[dsl, o:o + n], start=True, stop=True), reads=[RW["a2"], al], writes=[ps])
            k.op("act", lambda e: e.activation(tmpB[:, o:o + n], ps[:, 0:n], AF.Sigmoid, bias=cols[:, 22 + 2 * d + P:23 + 2 * d + P]), reads=[ps, cols], writes=[tmpB])
        k.barrier()
    k.op("dve", lambda e: e.tensor_tensor(b_f[:], tmpB[:], kk_f[:], ALU.mult), reads=[tmpB, kk_f], writes=[b_f])
    k.op("dve", lambda e: e.tensor_scalar(tmpB[:], tmpB[:], cols[:, 28 + P:29 + P], cols[:, 41 + P:42 + P], op0=ALU.mult, op1=ALU.add), reads=[tmpB, cols], writes=[tmpB])
    k.op("dve", lambda e: e.tensor_tensor(kd_f[:], k_f[:], tmpB[:], ALU.mult), reads=[k_f, tmpB], writes=[kd_f])
    k.op("dve", lambda e: e.tensor_scalar(tmpA[:], tmpA[:], -EM05, None, op0=ALU.mult), reads=[tmpA], writes=[tmpA])
    k.op("dve", lambda e: e.memset(Gp[:, 0:1], 0.0), writes=[Gp])
    k.op("pool", lambda e: e.memset(eA[:], 1.0), writes=[eA])
    k.op("dve", lambda e: e.tensor_tensor_scan(Gp[:, 1:T + 1], eA[:], tmpA[:], 0.0, op0=ALU.mult, op1=ALU.add), reads=[tmpA, eA, Gp], writes=[Gp])
    Gc0 = Gp[:, 0:T].rearrange("p (c s) -> p c s", s=64)[:, :, 0]; Gc1 = Gp[:, 1:T + 1].rearrange("p (c s) -> p c s", s=64)[:, :, 63]
    k.op("dve", lambda e: e.tensor_tensor(gc[:], Gc1, Gc0, ALU.subtract), reads=[Gp], writes=[gc])
    k.op("act", lambda e: e.activation(gc[:], gc[:], AF.Exp), reads=[gc], writes=[gc])
    Eref = Gc1 if d == 0 else Gc0
    k.op("dve", lambda e: e.tensor_copy(eoff[:, :, 0], Eref), reads=[Gp], writes=[eoff])
    k.op("dve", lambda e: e.tensor_scalar(eoff[:, :, 1], Eref, -1.0, None, op0=ALU.mult), reads=[Gp], writes=[eoff])
    k.barrier()
    if RW_STOP == 3:
        stmp.close(); return
    for c in range(NCH):
        cs = slice(c * 64, (c + 1) * 64); g1 = Gp[:, 1 + c * 64:1 + (c + 1) * 64]; g0 = Gp[:, c * 64:(c + 1) * 64]
        pE = eoff[:, c, 0:1]; nE = eoff[:, c, 1:2]
        if d == 0:
            k.op("act", lambda e: e.activation(eB[:, cs], g1, AF.Exp, scale=-1.0, bias=pE), reads=[Gp, eoff], writes=[eB])
            k.op("act", lambda e: e.activation(eA[:, cs], g0, AF.Exp, scale=1.0, bias=nE), reads=[Gp, eoff], writes=[eA])
            k.op("act", lambda e: e.activation(eR[:, cs], g1, AF.Exp, scale=1.0, bias=nE), reads=[Gp, eoff], writes=[eR])
        else:
            k.op("act", lambda e: e.activation(eB[:, cs], g0, AF.Exp, scale=1.0, bias=nE), reads=[Gp, eoff], writes=[eB])
            k.op("act", lambda e: e.activation(eA[:, cs], g1, AF.Exp, scale=-1.0, bias=pE), reads=[Gp, eoff], writes=[eA])
            k.op("act", lambda e: e.activation(eR[:, cs], g0, AF.Exp, scale=-1.0, bias=pE), reads=[Gp, eoff], writes=[eR])
    k.op("pool", lambda e: e.memset(AR[:], 0.0), writes=[AR]); k.op("pool", lambda e: e.memset(Bx[:], 0.0), writes=[Bx]); k.op("pool", lambda e: e.memset(Kx[:], 0.0), writes=[Kx])
    v3 = lambda tle, hs: tle[hs, :].rearrange("p (c s) -> p c s", s=64)
    for h in range(2):
        hs = slice(h * 64, (h + 1) * 64)
        k.op("dve", lambda e: e.tensor_tensor(AR[hs, :, 0, hs], v3(kk_f, hs), v3(eA, hs), ALU.mult), reads=[kk_f, eA], writes=[AR])
        k.op("dve", lambda e: e.tensor_tensor(AR[hs, :, 1, hs], v3(r_f, hs), v3(eR, hs), ALU.mult), reads=[r_f, eR], writes=[AR])
        k.op("dve", lambda e: e.tensor_tensor(Bx[hs, :, hs], v3(b_f, hs), v3(eB, hs), ALU.mult), reads=[b_f, eB], writes=[Bx])
        k.op("dve", lambda e: e.tensor_tensor(Kx[hs, :, hs], v3(kd_f, hs), v3(eB, hs), ALU.mult), reads=[kd_f, eB], writes=[Kx])
    k.barrier(); stmp.close()
    if RW_STOP == 4: return
    Rhat = k.sb("rwRhat", [128, NCH, 128], BF16, sd); Tt = k.sb("rwTt", [128, NCH, 128], BF16, sd)
    OV = k.sb("rwOV", [128, NCH, 64], F32, sd); HV = k.sb("rwHV", [128, NCH, 64], F32, sd)
    m2 = C["m2"][d]; m1 = C["m1"][d]
    with ExitStack() as s2:
        NS = 2
        banks = [[k.ps("rwb" + n, [128, 512], F32, s2) for n in "ABCD"] for _ in range(NS)]
        sbs = []
        for _ in range(NS):
            sbs.append(dict(MbS=k.sb("rwMbS", [128, 256], BF16, s2), MkS=k.sb("rwMkS", [128, 256], BF16, s2),
                            Ls=[k.sb("rwLs", [128, 2, 128], F32, s2) for _ in range(2)],
                            tm=k.sb("rwtm", [128, 3, 128], BF16, s2),
                            Ys=[k.sb("rwY", [128, 192], F32, s2) for _ in range(2)], nZ=k.sb("rwnZ", [128, 192], BF16, s2)))
        idb = C["ident_bf"]; m2t = C["m2t"]; m1t = C["m1t"]

        def chunk_gen(c, sl):
            bA, bB, bC, bD = banks[sl]; S = sbs[sl]
            MbS = S["MbS"]; MkS = S["MkS"]; Ls = S["Ls"]; tm = S["tm"]; Ys = S["Ys"]; nZ = S["nZ"]
            pMb = bA[:, 0:256]; pMk = bA[:, 256:512]; pTr = bB[:, 0:384].rearrange("p (a b) -> p a b", a=3); pMa = bB[:, 384:512]
            pL = bC[:, 0:256]; pOH = bC[:, 256:384]; pY = bD[:, 0:192]; pRT = bD[:, 192:448]
            arc = AR[:, c].rearrange("p a b -> p (a b)")
            k.op("pe", lambda e: e.matmul(pMb, lhsT=Bx[:, c, :], rhs=arc, start=True, stop=True), reads=[Bx, AR], writes=[bA])
            k.op("pe", lambda e: e.matmul(pMk, lhsT=Kx[:, c, :], rhs=arc, start=True, stop=True), reads=[Kx, AR], writes=[bA])
            k.op("pe", lambda e: e.matmul(pMa, lhsT=AR[:, c, 0, :], rhs=Bx[:, c, :], start=True, stop=True), reads=[AR, Bx], writes=[bB])
            k.op("pe", lambda e: e.matmul(pTr[:, 0, :], lhsT=Bx[:, c, :], rhs=idb[:], start=True, stop=True), reads=[Bx, idb], writes=[bB])
            k.op("pe", lambda e: e.matmul(pTr[:, 1, :], lhsT=Kx[:, c, :], rhs=idb[:], start=True, stop=True), reads=[Kx, idb], writes=[bB])
            k.op("pe", lambda e: e.matmul(pTr[:, 2, :], lhsT=AR[:, c, 0, :], rhs=idb[:], start=True, stop=True), reads=[AR, idb], writes=[bB])
            L0 = Ls[0]
            k.op("dve", lambda e: e.tensor_tensor(L0[:, 1, :], pMb[:, 0:128], m2[:, 0:128], ALU.mult), reads=[bA, m2t], writes=[L0])
            k.op("dve", lambda e: e.tensor_tensor(MkS[:], pMk, m2, ALU.mult), reads=[bA, m2t], writes=[MkS])
            k.op("dve", lambda e: e.tensor_tensor(MbS[:], pMb, m2, ALU.mult), reads=[bA, m2t], writes=[MbS])
            k.op("act", lambda e: e.copy(tm[:], pTr), reads=[bB], writes=[tm])
            k.op("dve", lambda e: e.tensor_tensor(L0[:, 0, :], pMa, m1, ALU.mult), reads=[bB, m1t], writes=[L0])
            yield
            Y = Ys[0]
            k.op("pe", lambda e: e.matmul(pY[:, 0:64], lhsT=MkS[:, 0:128], rhs=vst[:, c, :], start=True, stop=True), reads=[MkS, vst], writes=[bD])
            k.op("act", lambda e: e.copy(Y[:, 0:128], tm[:, 2, :]), reads=[tm], writes=[Y])
            k.op("act", lambda e: e.copy(Y[:, 128:192], pY[:, 0:64]), reads=[bD], writes=[Y])
            yield
            cur = 0
            for lvl in range(6):
                Lc = Ls[cur % 2]; Yc = Ys[lvl % 2]; Yn = Ys[(lvl + 1) % 2]
                k.op("pe", lambda e: e.matmul(pY, lhsT=Lc[:, 1, :], rhs=Yc[:], start=True, stop=True), reads=[Lc, Yc], writes=[bD])
                if lvl < 5:
                    k.op("pe", lambda e: e.matmul(pL[:, 128:256], lhsT=Lc[:, 0, :], rhs=Lc[:, 1, :], start=True, stop=True), reads=[Lc], writes=[bC])
                    if lvl < 4:
                        k.op("pe", lambda e: e.matmul(pL[:, 0:128], lhsT=Lc[:, 1, :], rhs=Lc[:, 0, :], start=True, stop=True), reads=[Lc], writes=[bC])
                k.op("dve", lambda e: e.tensor_tensor(Yn[:], Yc[:], pY, ALU.subtract if lvl == 0 else ALU.add), reads=[Yc, bD], writes=[Yn])
                if lvl < 5:
                    Ln = Ls[(cur + 1) % 2]
                    if lvl < 4:
                        k.op("act", lambda e: e.copy(Ln[:].rearrange("p a b -> p (a b)"), pL), reads=[bC], writes=[Ln])
                    else:
                        k.op("act", lambda e: e.copy(Ln[:, 1, :], pL[:, 128:256]), reads=[bC], writes=[Ln])
                    cur += 1
                yield
            k.op("dve", lambda e: e.tensor_scalar(nZ[:], Ys[0][:], -1.0, None, op0=ALU.mult), reads=[Ys[0]], writes=[nZ])
            yield
            k.op("pe", lambda e: e.matmul(pRT[:, 0:128], lhsT=idb[:], rhs=AR[:, c, 1, :], start=True, stop=False), reads=[idb, AR], writes=[bD])
            k.op("pe", lambda e: e.matmul(pRT[:, 0:128], lhsT=nZ[:, 0:128], rhs=MbS[:, 128:256], start=False, stop=True), reads=[nZ, MbS], writes=[bD])
            k.op("pe", lambda e: e.matmul(pRT[:, 128:256], lhsT=idb[:], rhs=idb[:], start=True, stop=False), reads=[idb], writes=[bD])
            k.op("pe", lambda e: e.matmul(pRT[:, 128:256], lhsT=nZ[:, 0:128], rhs=tm[:, 0, :], start=False, stop=True), reads=[nZ, tm], writes=[bD])
            k.op("pe", lambda e: e.matmul(pOH[:, 0:64], lhsT=MkS[:, 128:256], rhs=vst[:, c, :], start=True, stop=False), reads=[MkS, vst], writes=[bC])
            k.op("pe", lambda e: e.matmul(pOH[:, 0:64], lhsT=MbS[:, 128:256], rhs=nZ[:, 128:192], start=False, stop=True), reads=[MbS, nZ], writes=[bC])
            k.op("pe", lambda e: e.matmul(pOH[:, 64:128], lhsT=tm[:, 1, :], rhs=vst[:, c, :], start=True, stop=False), reads=[tm, vst], writes=[bC])
            k.op("pe", lambda e: e.matmul(pOH[:, 64:128], lhsT=tm[:, 0, :], rhs=nZ[:, 128:192], start=False, stop=True), reads=[tm, nZ], writes=[bC])
            k.op("dve", lambda e: e.tensor_copy(Rhat[:, c, :], pRT[:, 0:128]), reads=[bD], writes=[Rhat])
            k.op("dve", lambda e: e.tensor_copy(Tt[:, c, :], pRT[:, 128:256]), reads=[bD], writes=[Tt])
            k.op("act", lambda e: e.copy(OV[:, c, :], pOH[:, 0:64]), reads=[bC], writes=[OV])
            k.op("act", lambda e: e.copy(HV[:, c, :], pOH[:, 64:128]), reads=[bC], writes=[HV])
            yield

        for c0 in range(0, NCH, NS):
            alive = [chunk_gen(c0 + i, i) for i in range(NS) if c0 + i < NCH]
            while alive:
                for g in list(alive):
                    try:
                        next(g)
                    except StopIteration:
                        alive.remove(g)
        k.barrier()
    with ExitStack() as s2:
        Hs = [k.sb("rwHs", [128, 64], BF16, s2) for _ in range(2)]; HVg = k.sb("rwHVg", [128, NCH, 64], F32, s2)
        pO = [k.ps("rwpO", [128, 64], F32, s2) for _ in range(2)]; pH = [k.ps("rwpH", [128, 64], F32, s2) for _ in range(2)]
        order = list(range(NCH)) if d == 0 else [3, 2, 1, 0] + list(range(NCH - 1, 3, -1))
        for i in range(NCH - 1):
            c = order[i]; cn = order[i + 1]
            k.op("act", lambda e: e.activation(HVg[:, c, :], HV[:, c, :], AF.Copy, scale=gc[:, cn:cn + 1]), reads=[HV, gc], writes=[HVg])
        if d == 1:
            k.op("pool", lambda e: e.tensor_tensor(OV[:], OV[:], o_all[:], ALU.add), reads=[OV, o_all], writes=[OV])
        c = order[0]
        k.op("dve", lambda e: e.tensor_copy(Hs[1][:], HVg[:, c, :]), reads=[HVg], writes=[Hs[1]])
        k.op("dve", lambda e: e.tensor_copy(o_all[:, c, :], OV[:, c, :]), reads=[OV], writes=[o_all])
        for i in range(1, NCH):
            c = order[i]; hs = Hs[i % 2]; hn = Hs[(i + 1) % 2]; po = pO[i % 2]; ph = pH[i % 2]
            k.op("pe", lambda e: e.matmul(ph[:], lhsT=Tt[:, c, :], rhs=hs[:], start=True, stop=True), reads=[Tt, hs], writes=[ph])
            k.op("pe", lambda e: e.matmul(po[:], lhsT=Rhat[:, c, :], rhs=hs[:], start=True, stop=True), reads=[Rhat, hs], writes=[po])
            if i < NCH - 1:
                cn = order[i + 1]
                k.op("dve", lambda e: e.scalar_tensor_tensor(hn[:], ph[:], gc[:, cn:cn + 1], HVg[:, c, :], op0=ALU.mult, op1=ALU.add), reads=[ph, gc, HVg], writes=[hn])
            k.op("dve", lambda e: e.tensor_tensor(o_all[:, c, :], po[:], OV[:, c, :], ALU.add), reads=[po, OV], writes=[o_all])
        k.barrier()


def load_consts(k, st, CD):
    C = {}
    C["ident_f"] = k.sb("identf", [128, 128], F32, st); C["ident_bf"] = k.sb("identbf", [128, 128], BF16, st)
    C["ones_bf"] = k.sb("onesbf", [128, 128], BF16, st)
    k.dma("sp", C["ident_f"][:], CD["ident"][:], writes=[C["ident_f"]])
    k.op("dve", lambda e: e.tensor_copy(C["ident_bf"][:], C["ident_f"][:]), reads=[C["ident_f"]], writes=[C["ident_bf"]])
    k.op("dve", lambda e: e.memset(C["ones_bf"][:], 1.0), writes=[C["ones_bf"]])
    C["cos_d"] = CD["cos"]; C["sin_d"] = CD["sin"]
    m2t = k.sb("rwm2", [128, 2, 256], F32, st); m1t = k.sb("rwm1", [128, 2, 128], F32, st)
    k.dma("sp", m2t[:], CD["rw_m2"].t.rearrange("d p c -> p d c"), writes=[m2t])
    k.dma("sp", m1t[:], CD["rw_m1"].t.rearrange("d p c -> p d c"), writes=[m1t])
    C["m2t"] = m2t; C["m1t"] = m1t; C["m2"] = [m2t[:, 0, :], m2t[:, 1, :]]; C["m1"] = [m1t[:, 0, :], m1t[:, 1, :]]
    C["blk"] = k.sb("rwblk", [128, 128], F32, st); C["isel"] = k.sb("rwisel", [128, 64], BF16, st)
    k.dma("sp", C["blk"][:], CD["rw_blk"][:], writes=[C["blk"]])
    k.dma("pool", C["isel"][:], CD["rw_isel"][:], writes=[C["isel"]])
    return C


def const_arrays():
    cos, sin = rope_tables()
    m2, m1, blk, isel = rw_consts_host()
    p = np.arange(128)
    return {"ident": np.eye(128, dtype=np.float32), "cos": cos, "sin": sin, "rw_m2": m2, "rw_m1": m1, "rw_blk": blk, "rw_isel": isel,
            "na_mask": na_mask_host(),
            "tri": (p[:, None] < p[None, :]).astype(np.float32), "iota64": (p % 32).astype(np.float32)[:, None],
            "rowbase": (np.arange(8)[None, :] * 128 + p[:, None]).astype(np.float32), "blk512": np.tile(512.0 * np.arange(NBLK_MAX, dtype=np.float32)[None, :], (128, 1))}


CONST_SHAPES = {"ident": [128, 128], "cos": [SEQ, 128], "sin": [SEQ, 128], "rw_m2": [2, 128, 256], "rw_m1": [2, 128, 128], "rw_blk": [128, 128],
                "rw_isel": [128, 64], "na_mask": [128, 4, 64], "tri": [128, 128], "iota64": [128, 1], "rowbase": [128, 8], "blk512": [128, NBLK_MAX]}


def phase_post(k, l, b, NB, need_ctx, x_src, x_src_tk, mod_d, yrw_d, ymla_d, yna_d, W, C, x1_d, hT_d, GT_d, col0, hTM_d=None, Gtm_d=None):
    with ExitStack() as st:
        wo_rw = k.sb("worw", [64, 4, D], BF16, st); wo_mla = k.sb("womla", [128, 4, D], BF16, st); wo_na = k.sb("wona", [64, 4, D], BF16, st)
        wv = W["w_out"].t[l]
        k.dma("pool", wo_rw[:], wv[0:256, :].rearrange("(h p) n -> p h n", p=64), writes=[wo_rw])
        k.dma("pool", wo_mla[:], wv[256:768, :].rearrange("(h p) n -> p h n", p=128), writes=[wo_mla])
        k.dma("pool", wo_na[:], wv[768:1024, :].rearrange("(h p) n -> p h n", p=64), writes=[wo_na])
        rwt = k.sb("routw", [128, 8, NE], BF16, st); rbt = k.sb("routb", [128, NE], F32, st)
        k.dma("pool", rwt[:], W["router_w"].t[l].rearrange("(kc p) n -> p kc n", p=128), writes=[rwt])
        k.dma("sp", rbt[:], W["router_b"].t[l:l + 1, :].partition_broadcast(128), writes=[rbt])
        bc = {}
        for nm, src in (("g1", W["ln1_g"].t[l:l + 1, :]), ("b1", W["ln1_b"].t[l:l + 1, :]),
                        ("ga_l", mod_d[l][b:b + 1, 2 * D:3 * D]), ("sf_l", mod_d[l][b:b + 1, 3 * D:4 * D]), ("cf_l", mod_d[l][b:b + 1, 4 * D:5 * D]),
                        ("ga_c", mod_d[l][NB:NB + 1, 2 * D:3 * D]), ("sf_c", mod_d[l][NB:NB + 1, 3 * D:4 * D]), ("cf_c", mod_d[l][NB:NB + 1, 4 * D:5 * D])):
            if nm.endswith("_c") and not need_ctx:
                continue
            tle = k.sb(nm, [128, D], F32, st)
            k.dma("sp", tle[:], src.partition_broadcast(128), reads=[mod_d], writes=[tle])
            bc[nm] = tle
        for nm in ("cf_l", "cf_c"):
            if nm in bc:
                tle = bc[nm]
                k.op("pool", lambda e: e.tensor_scalar(tle[:], tle[:], 1.0, None, op0=ALU.add), reads=[tle], writes=[tle])
        yr = k.sb("yrall", [64, 4, T], BF16, st); ym = k.sb("ymall", [128, 4, T], BF16, st); yn = k.sb("ynall", [64, 4, T], BF16, st)
        k.dma("sp", yr[:], yrw_d[:], reads=[yrw_d], writes=[yr])
        k.dma("sp", ym[:], ymla_d.t.rearrange("h p t -> p h t"), reads=[ymla_d], writes=[ym])
        k.dma("sp", yn[:], yna_d[:], reads=[yna_d], writes=[yn])
        xts = [k.sb("pxt", [128, D], F32, st) for _ in range(2)]
        t1 = k.sb("pt1", [128, D], F32, st); u = k.sb("pu", [128, D], F32, st); x1 = k.sb("px1", [128, D], F32, st); hb = k.sb("phb", [128, D], BF16, st)
        st6 = k.sb("pst6", [128, 2, 6], F32, st); mv = k.sb("pmv", [128, 2], F32, st); rstd = k.sb("prstd", [128, 1], F32, st)
        hT = k.sb("phT", [128, 8, 128], BF16, st)
        lg = k.sb("plg", [128, NE], F32, st); mx8 = k.sb("pmx8", [128, 8], F32, st); nm1 = k.sb("pnm1", [128, 1], F32, st)
        msk = k.sb("pmsk", [128, NE], F32, st); ex = k.sb("pex", [128, NE], F32, st); ssum = k.sb("pssum", [128, 1], F32, st); gts = k.sb("pgts", [32, 128], F32, st)
        py = [k.ps("ppy", [128, 512], F32, st) for _ in range(2)]
        pT = k.ps("ppT", [128, 8, 128], BF16, st); pl = k.ps("ppl", [128, NE], F32, st); pg = k.ps("ppg", [32, 128], F32, st)
        for tt in range(NTT):
            isctx = tt < 2
            if isctx and not need_ctx:
                continue
            sfx = "_c" if isctx else "_l"
            sl = slice(tt * 128, (tt + 1) * 128)
            xt = xts[tt % 2]
            k.dma("sp", xt[:], x_src(tt), reads=[x_src_tk], writes=[xt])
            for half in range(2):
                ps = py[half]; cs = slice(half * 512, (half + 1) * 512)
                ops = [(yr, wo_rw, h) for h in range(4)] + [(ym, wo_mla, h) for h in range(4)] + [(yn, wo_na, h) for h in range(4)]
                for i, (ya, wa, h) in enumerate(ops):
                    k.op("pe", lambda e: e.matmul(ps[:], lhsT=ya[:, h, sl], rhs=wa[:, h, cs], start=(i == 0), stop=(i == 11)), reads=[ya, wa], writes=[ps])
                k.op("dve", lambda e: e.tensor_tensor(t1[:, cs], ps[:], bc["ga" + sfx][:, cs], ALU.mult), reads=[ps, bc["ga" + sfx]], writes=[t1])
            k.op("dve", lambda e: e.scalar_tensor_tensor(u[:], xt[:], DN_ALPHA, t1[:], op0=ALU.mult, op1=ALU.add), reads=[xt, t1], writes=[u])
            ln_stats(k, u, st6, mv, rstd, 1e-5)
            k.op("dve", lambda e: e.scalar_tensor_tensor(t1[:], u[:], mv[:, 0:1], bc["g1"][:], op0=ALU.subtract, op1=ALU.mult), reads=[u, mv, bc["g1"]], writes=[t1])
            k.op("dve", lambda e: e.scalar_tensor_tensor(x1[:], t1[:], rstd[:], bc["b1"][:], op0=ALU.mult, op1=ALU.add), reads=[t1, rstd, bc["b1"]], writes=[x1])
            k.dma("sp", x1_d[sl, :], x1[:], reads=[x1], writes=[x1_d])
            ln_stats(k, x1, st6, mv, rstd, 1e-5)
            k.op("dve", lambda e: e.scalar_tensor_tensor(t1[:], x1[:], mv[:, 0:1], bc["cf" + sfx][:], op0=ALU.subtract, op1=ALU.mult), reads=[x1, mv, bc["cf" + sfx]], writes=[t1])
            k.op("dve", lambda e: e.scalar_tensor_tensor(hb[:], t1[:], rstd[:], bc["sf" + sfx][:], op0=ALU.mult, op1=ALU.add), reads=[t1, rstd, bc["sf" + sfx]], writes=[hb])
            for c in range(8):
                k.op("pe", lambda e: e.transpose(pT[:, c, :], hb[:, c * 128:(c + 1) * 128], C["ident_bf"][:]), reads=[hb, C["ident_bf"]], writes=[pT])
            k.op("act", lambda e: e.copy(hT[:], pT[:]), reads=[pT], writes=[hT])
            if hTM_d is not None:
                k.dma("sp", hTM_d.t[col0 + tt * 128:col0 + (tt + 1) * 128, :], hb[:], reads=[hb], writes=[hTM_d])
            else:
                k.dma("sp", hT_d.t[:, col0 + tt * 128:col0 + (tt + 1) * 128].rearrange("(kc p) t -> p kc t", p=128), hT[:], reads=[hT], writes=[hT_d])
            for kc in range(8):
                k.op("pe", lambda e: e.matmul(pl[:], lhsT=hT[:, kc, :], rhs=rwt[:, kc, :], start=(kc == 0), stop=(kc == 7)), reads=[hT, rwt], writes=[pl])
            k.op("dve", lambda e: e.tensor_tensor(lg[:], pl[:], rbt[:], ALU.add), reads=[pl, rbt], writes=[lg])
            k.op("dve", lambda e: e.max(mx8[:], lg[:]), reads=[lg], writes=[mx8])
            k.op("dve", lambda e: e.tensor_scalar(msk[:], lg[:], mx8[:, 3:4], None, op0=ALU.is_ge), reads=[lg, mx8], writes=[msk])
            k.op("dve", lambda e: e.tensor_scalar(nm1[:], mx8[:, 0:1], -1.0, None, op0=ALU.mult), reads=[mx8], writes=[nm1])
            k.op("act", lambda e: e.activation(ex[:], lg[:], AF.Exp, bias=nm1[:]), reads=[lg, nm1], writes=[ex])
            k.op("dve", lambda e: e.tensor_tensor(ex[:], ex[:], msk[:], ALU.mult), reads=[ex, msk], writes=[ex])
            k.op("dve", lambda e: e.reduce_sum(ssum[:], ex[:], axis=AX.X), reads=[ex], writes=[ssum])
            k.op("dve", lambda e: e.reciprocal(ssum[:], ssum[:]), reads=[ssum], writes=[ssum])
            k.op("dve", lambda e: e.tensor_scalar(ex[:], ex[:], ssum[:, 0:1], None, op0=ALU.mult), reads=[ex, ssum], writes=[ex])
            if Gtm_d is not None:
                k.dma("sp", Gtm_d.t[col0 + tt * 128:col0 + (tt + 1) * 128, :], ex[:], reads=[ex], writes=[Gtm_d])
            else:
                k.op("pe", lambda e: e.transpose(pg[:], ex[:], C["ident_f"][:]), reads=[ex, C["ident_f"]], writes=[pg])
                k.op("act", lambda e: e.copy(gts[:], pg[:]), reads=[pg], writes=[gts])
                k.dma("sp", GT_d[:, col0 + tt * 128:col0 + (tt + 1) * 128], gts[:], reads=[gts], writes=[GT_d])
    k.barrier()


def phase_moe(k, l, NB, groups, tile_io, mod_d, W, C, hT_d, GT_d):
    GP = 2
    with ExitStack() as st:
        bgl = k.sb("bgl", [128, NE, 8], F32, st); bln = k.sb("bln", [128, NE, 8], F32, st)
        bdn = k.sb("bdn", [NE, D], F32, st)
        k.dma("sp", bdn[:], W["b_dn"].t[l], writes=[bdn])
        with ExitStack() as s2:
            bgu = k.sb("bgu", [NE, 2 * D], F32, s2)
            pb = [k.ps("pbg", [128, NE], F32, s2) for _ in range(2)]
            k.dma("sp", bgu[:], W["b_gu"].t[l], writes=[bgu])
            for j in range(8):
                for par, dst in ((0, bgl), (1, bln)):
                    ps = pb[par]
                    k.op("pe", lambda e: e.transpose(ps[:], bgu[:, 256 * j + par:256 * (j + 1):2], C["ident_f"][0:NE, 0:NE]), reads=[bgu, C["ident_f"]], writes=[ps])
                    k.op("dve", lambda e: e.tensor_scalar(dst[:, :, j], ps[:], float(par), None, op0=ALU.add), reads=[ps], writes=[dst])
            k.barrier()
        gfs = [k.sb("gf%d" % i, [128, D], F32, st) for i in range(GP)]; gf_key = [None] * GP
        ln2g = k.sb("ln2g", [128, D], F32, st); ln2b = k.sb("ln2b", [128, D], F32, st)
        k.dma("sp", ln2g[:], W["ln2_g"].t[l:l + 1, :].partition_broadcast(128), writes=[ln2g])
        k.dma("sp", ln2b[:], W["ln2_b"].t[l:l + 1, :].partition_broadcast(128), writes=[ln2b])
        NU = 4
        units = [k.sb("wu%d" % i, [128, 8, 1024], BF16, st) for i in range(NU)]
        hTs = [k.sb("mhT%d" % i, [128, 8, 512], BF16, st) for i in range(GP)]
        accs = [k.sb("macc%d" % i, [128, 8, 512], F32, st) for i in range(GP)]
        gTs = [k.sb("mgT%d" % i, [NE, 512], F32, st) for i in range(GP)]
        acts = [k.sb("mact%d" % i, [128, 8, 512], BF16, st) for i in range(2)]
        gbcs = [k.sb("mgbc%d" % i, [128, 512], F32, st) for i in range(2)]
        tt_ = [k.sb("mt%d" % i, [128, 512], F32, st) for i in range(2)]
        ss_ = [k.sb("ms%d" % i, [128, 512], F32, st) for i in range(2)]
        uu_ = [k.sb("mu%d" % i, [128, 512], F32, st) for i in range(2)]
        psg = [k.ps("mpsg", [128, 512], F32, st) for _ in range(2)]
        psl = [k.ps("mpsl", [128, 512], F32, st) for _ in range(2)]
        psd = [k.ps("mpsd", [128, 512], F32, st) for _ in range(2)]
        st6 = k.sb("mst6", [128, 2, 6], F32, st); mv = k.sb("mmv", [128, 2], F32, st); rstd = k.sb("mrstd", [128, 1], F32, st)
        ucount = 0; it = 0; dcount = 0
        wgu = W["w_gu"].t[l]; wdn = W["w_dn"].t[l]
        for p0 in range(0, len(groups), GP):
            pg = groups[p0:p0 + GP]
            for gi, (col, n, bkey) in enumerate(pg):
                k.dma("sp", hTs[gi][:, :, 0:n], hT_d.t[:, col:col + n].rearrange("(kc p) t -> p kc t", p=128), reads=[hT_d], writes=[hTs[gi]])
                k.dma("sp", gTs[gi][:, 0:n], GT_d[:, col:col + n], reads=[GT_d], writes=[gTs[gi]])
                if gf_key[gi] != bkey:
                    row = NB if bkey == "c" else bkey
                    k.dma("sp", gfs[gi][:], mod_d[l][row:row + 1, 5 * D:6 * D].partition_broadcast(128), reads=[mod_d], writes=[gfs[gi]])
                    gf_key[gi] = bkey
            for e in range(NE):
                ua = units[ucount % NU]; ub = units[(ucount + 1) % NU]; ud = units[(ucount + 2) % NU]; ucount += 3
                k.dma("pool", ua[:], wgu[e][:, 0:1024].rearrange("(kc p) n -> p kc n", p=128), writes=[ua])
                k.dma("pool", ub[:], wgu[e][:, 1024:2048].rearrange("(kc p) n -> p kc n", p=128), writes=[ub])
                k.dma("pool", ud[:], wdn[e].rearrange("(kc p) n -> p kc n", p=128), writes=[ud])
                for gi, (col, n, bkey) in enumerate(pg):
                    hTg = hTs[gi]; acc = accs[gi]
                    act = acts[it % 2]; gbc = gbcs[it % 2]; it += 1
                    k.dma("act", gbc[:, 0:n], GT_d[e:e + 1, col:col + n].partition_broadcast(128), reads=[GT_d], writes=[gbc])
                    for j in range(8):
                        un = ua if j < 4 else ub; c0 = (j % 4) * 256
                        pg_ = psg[j % 2]; pl_ = psl[j % 2]; t_ = tt_[j % 2]; s_ = ss_[j % 2]; u_ = uu_[j % 2]
                        for kc in range(8):
                            k.op("pe", lambda e_: e_.matmul(pg_[:, 0:n], lhsT=un[:, kc, c0:c0 + 256:2], rhs=hTg[:, kc, 0:n], start=(kc == 0), stop=(kc == 7)), reads=[un, hTg], writes=[pg_])
                        for kc in range(8):
                            k.op("pe", lambda e_: e_.matmul(pl_[:, 0:n], lhsT=un[:, kc, c0 + 1:c0 + 256:2], rhs=hTg[:, kc, 0:n], start=(kc == 0), stop=(kc == 7)), reads=[un, hTg], writes=[pl_])
                        k.op("dve", lambda e_: e_.tensor_scalar(t_[:, 0:n], pg_[:, 0:n], bgl[:, e, j:j + 1], 7.0, op0=ALU.add, op1=ALU.min), reads=[pg_, bgl], writes=[t_])
                        k.op("act", lambda e_: e_.activation(s_[:, 0:n], t_[:, 0:n], AF.Sigmoid, scale=1.702), reads=[t_], writes=[s_])
                        k.op("dve", lambda e_: e_.tensor_scalar(u_[:, 0:n], pl_[:, 0:n], bln[:, e, j:j + 1], 8.0, op0=ALU.add, op1=ALU.min), reads=[pl_, bln], writes=[u_])
                        k.op("pool", lambda e_: e_.tensor_tensor(t_[:, 0:n], t_[:, 0:n], s_[:, 0:n], ALU.mult), reads=[t_, s_], writes=[t_])
                        k.op("pool", lambda e_: e_.tensor_scalar(u_[:, 0:n], u_[:, 0:n], -6.0, None, op0=ALU.max), reads=[u_], writes=[u_])
                        k.op("pool", lambda e_: e_.tensor_tensor(u_[:, 0:n], u_[:, 0:n], t_[:, 0:n], ALU.mult), reads=[u_, t_], writes=[u_])
                        k.op("dve", lambda e_: e_.tensor_tensor(act[:, j, 0:n], u_[:, 0:n], gbc[:, 0:n], ALU.mult), reads=[u_, gbc], writes=[act])
                    for oc in range(8):
                        pd = psd[dcount % 2]; dcount += 1
                        for j in range(8):
                            k.op("pe", lambda e_: e_.matmul(pd[:, 0:n], lhsT=ud[:, j, oc * 128:(oc + 1) * 128], rhs=act[:, j, 0:n], start=(j == 0), stop=(j == 7)), reads=[ud, act], writes=[pd])
                        if e == 0:
                            k.op("act", lambda e_: e_.copy(acc[:, oc, 0:n], pd[:, 0:n]), reads=[pd], writes=[acc])
                        else:
                            k.op("dve", lambda e_: e_.tensor_tensor(acc[:, oc, 0:n], acc[:, oc, 0:n], pd[:, 0:n], ALU.add), reads=[acc, pd], writes=[acc])
            for gi, (col, n, bkey) in enumerate(pg):
                acc = accs[gi]; gT = gTs[gi]; gf = gfs[gi]
                for oc in range(8):
                    pd = psd[dcount % 2]; dcount += 1
                    k.op("pe", lambda e_: e_.matmul(pd[:, 0:n], lhsT=bdn[:, oc * 128:(oc + 1) * 128], rhs=gT[:, 0:n], start=True, stop=True), reads=[bdn, gT], writes=[pd])
                    k.op("dve", lambda e_: e_.tensor_tensor(acc[:, oc, 0:n], acc[:, oc, 0:n], pd[:, 0:n], ALU.add), reads=[acc, pd], writes=[acc])
                for ti in range(n // 128):
                    x1_ap, x1_tk, out_ap, out_tk = tile_io(col + ti * 128)
                    for half in range(2):
                        ps = psg[half]; cs = slice(half * 512, (half + 1) * 512)
                        k.dma("sp", ss_[half][:], x1_ap[:, cs], reads=[x1_tk], writes=[ss_[half]])
                        for c in range(4):
                            oc = half * 4 + c
                            k.op("pe", lambda e_: e_.transpose(ps[:, c * 128:(c + 1) * 128], acc[:, oc, ti * 128:(ti + 1) * 128], C["ident_f"][:]), reads=[acc, C["ident_f"]], writes=[ps])
                        k.op("dve", lambda e_: e_.tensor_tensor(tt_[half][:], ps[:], gf[:, cs], ALU.mult), reads=[ps, gf], writes=[tt_[half]])
                        k.op("dve", lambda e_: e_.scalar_tensor_tensor(uu_[half][:], ss_[half][:], DN_ALPHA, tt_[half][:], op0=ALU.mult, op1=ALU.add), reads=[ss_[half], tt_[half]], writes=[uu_[half]])
                        k.op("dve", lambda e_: e_.bn_stats(st6[:, half, :], uu_[half][:]), reads=[uu_[half]], writes=[st6])
                    k.op("dve", lambda e_: e_.bn_aggr(mv[:], st6[:].rearrange("p a b -> p (a b)")), reads=[st6], writes=[mv])
                    k.op("dve", lambda e_: e_.tensor_scalar(rstd[:], mv[:, 1:2], 1e-5, None, op0=ALU.add), reads=[mv], writes=[rstd])
                    k.op("act", lambda e_: e_.sqrt(rstd[:], rstd[:]), reads=[rstd], writes=[rstd])
                    k.op("dve", lambda e_: e_.reciprocal(rstd[:], rstd[:]), reads=[rstd], writes=[rstd])
                    for half in range(2):
                        cs = slice(half * 512, (half + 1) * 512)
                        k.op("dve", lambda e_: e_.scalar_tensor_tensor(tt_[half][:], uu_[half][:], mv[:, 0:1], ln2g[:, cs], op0=ALU.subtract, op1=ALU.mult), reads=[uu_[half], mv, ln2g], writes=[tt_[half]])
                        k.op("dve", lambda e_: e_.scalar_tensor_tensor(uu_[half][:], tt_[half][:], rstd[:], ln2b[:, cs], op0=ALU.mult, op1=ALU.add), reads=[tt_[half], rstd, ln2b], writes=[uu_[half]])
                        k.dma("sp", out_ap[:, cs], uu_[half][:], reads=[uu_[half]], writes=[out_tk])
    k.barrier()


def phase_moe_sparse(k, l, NB, need_ctx, tile_io, mod_d, W, C, CD, hTM_d, Gtm_d, Hs_d, Ys_d):
    IOA = bass.IndirectOffsetOnAxis
    ntb = NTT if need_ctx else NTT - 2
    NT = NB * ntb; NTOK = NT * 128
    NBLK = (4 * NTOK + NE * 511) // 512
    assert NBLK <= NBLK_MAX
    BIG = 65536.0
    rows = [b * T + (0 if need_ctx else NCTX) for b in range(NB)]
    tile_row = [rows[b] + j * 128 for b in range(NB) for j in range(ntb)]
    tile_key = [("c" if (need_ctx and j < 2) else b) for b in range(NB) for j in range(ntb)]
    with ExitStack() as st:
        dest_i = k.sb("mdesti", [128, NT, 4], I32, st); gk = k.sb("mgk", [128, NT, 4], F32, st)
        widx = k.sb("mwidx", [128, NBLK, 8], I32, st); be = k.sb("mbe", [128, NBLK], F32, st)
        iota64 = k.sb("miota", [128, 1], F32, st)
        k.dma("sp", iota64[:], CD["iota64"][:], writes=[iota64])
        with ExitStack() as s2:
            rowb = k.sb("mrowb", [128, 8], F32, s2); blk512 = k.sb("mblk512", [128, NBLK], F32, s2)
            trif = k.sb("mtrif", [128, 128], F32, s2); tri = k.sb("mtri", [128, 128], BF16, s2)
            k.dma("sp", rowb[:], CD["rowbase"][:], writes=[rowb]); k.dma("sp", blk512[:], CD["blk512"][:, 0:NBLK], writes=[blk512])
            k.dma("sp", trif[:], CD["tri"][:], writes=[trif])
            k.op("dve", lambda e: e.tensor_scalar(rowb[:], rowb[:], float(l * NE * D), None, op0=ALU.add), reads=[rowb], writes=[rowb])
            k.op("dve", lambda e: e.tensor_copy(tri[:], trif[:]), reads=[trif], writes=[tri])
            G_all = k.sb("mGall", [128, NT, NE], F32, s2); maskf = k.sb("mmaskf", [128, NT, NE], F32, s2); mask = k.sb("mmask", [128, NT, NE], BF16, s2)
            rank = k.sb("mrank", [128, NT, NE], F32, s2); tot = k.sb("mtot", [128, NT, NE], F32, s2); off = k.sb("moff", [128, NT, NE], F32, s2)
            val = k.sb("mval", [128, NT, NE], F32, s2)
            cnt = k.sb("mcnt", [128, NE], F32, s2); nbe = k.sb("mnbe", [128, NE], F32, s2); start = k.sb("mstart", [128, NE + 1], F32, s2)
            mx8 = k.sb("mmx8", [128, 8], F32, s2); tmp = k.sb("mtmp", [128, NE], F32, s2); destf = k.sb("mdestf", [128, NT, 4], F32, s2)
            widxf = k.sb("mwidxf", [128, NBLK, 8], F32, s2)
            z = k.sb("mz", [128, 4 * D], BF16, s2)
            k.op("pool", lambda e: e.memset(z[:], 0.0), writes=[z])
            for blk in range(NBLK):
                k.dma("sp" if blk % 2 else "act", Hs_d.t[blk * 512:(blk + 1) * 512, :].rearrange("(p r) d -> p (r d)", p=128), z[:], reads=[z])
            for b in range(NB):
                k.dma("sp", G_all[:, b * ntb:(b + 1) * ntb, :], Gtm_d.t[rows[b]:rows[b] + ntb * 128, :].rearrange("(i p) e -> p i e", p=128), reads=[Gtm_d], writes=[G_all])
            k.op("dve", lambda e: e.tensor_scalar(maskf[:], G_all[:], 0.0, None, op0=ALU.is_gt), reads=[G_all], writes=[maskf])
            k.op("dve", lambda e: e.tensor_copy(mask[:], maskf[:]), reads=[maskf], writes=[mask])
            pP = [k.ps("mpP", [128, 512], F32, s2) for _ in range(2)]; pTt = [k.ps("mpTt", [128, 512], F32, s2) for _ in range(2)]
            mflat = mask[:].rearrange("p i e -> p (i e)"); rflat = rank[:].rearrange("p i e -> p (i e)"); tflat = tot[:].rearrange("p i e -> p (i e)")
            ncols = NT * NE
            for c in range((ncols + 511) // 512):
                c0 = c * 512; n = min(512, ncols - c0); pa = pP[c % 2]; pb = pTt[c % 2]
                k.op("pe", lambda e: e.matmul(pa[:, 0:n], lhsT=tri[:], rhs=mflat[:, c0:c0 + n], start=True, stop=True), reads=[tri, mask], writes=[pa])
                k.op("pe", lambda e: e.matmul(pb[:, 0:n], lhsT=C["ones_bf"][:], rhs=mflat[:, c0:c0 + n], start=True, stop=True), reads=[C["ones_bf"], mask], writes=[pb])
                k.op("dve", lambda e: e.tensor_copy(rflat[:, c0:c0 + n], pa[:, 0:n]), reads=[pa], writes=[rank])
                k.op("act", lambda e: e.copy(tflat[:, c0:c0 + n], pb[:, 0:n]), reads=[pb], writes=[tot])
            k.op("dve", lambda e: e.reduce_sum(cnt[:], tot[:].rearrange("p i e -> p e i"), axis=AX.X), reads=[tot], writes=[cnt])
            k.op("dve", lambda e: e.memset(nbe[:], 0.0), writes=[nbe])
            for j in range(NTOK // 512):
                k.op("dve", lambda e: e.scalar_tensor_tensor(nbe[:], cnt[:], 512.0 * j, nbe[:], op0=ALU.is_gt, op1=ALU.add), reads=[cnt, nbe], writes=[nbe])
            k.op("dve", lambda e: e.tensor_scalar(nbe[:], nbe[:], 512.0, None, op0=ALU.mult), reads=[nbe], writes=[nbe])
            k.op("dve", lambda e: e.memset(start[:, 0:1], 0.0), writes=[start])
            for ee in range(NE):
                k.op("dve", lambda e: e.tensor_tensor(start[:, ee + 1:ee + 2], start[:, ee:ee + 1], nbe[:, ee:ee + 1], ALU.add), reads=[start, nbe], writes=[start])
            k.op("dve", lambda e: e.tensor_copy(off[:, 0, :], start[:, 0:NE]), reads=[start], writes=[off])
            for i in range(1, NT):
                k.op("dve", lambda e: e.tensor_tensor(off[:, i, :], off[:, i - 1, :], tot[:, i - 1, :], ALU.add), reads=[off, tot], writes=[off])
            k.op("dve", lambda e: e.tensor_tensor(rank[:], rank[:], off[:], ALU.add), reads=[rank, off], writes=[rank])
            k.op("dve", lambda e: e.tensor_scalar(rank[:], rank[:], -1.0, BIG, op0=ALU.mult, op1=ALU.add), reads=[rank], writes=[rank])
            k.op("dve", lambda e: e.tensor_tensor(val[:], rank[:], maskf[:], ALU.mult), reads=[rank, maskf], writes=[val])
            for i in range(NT):
                k.op("dve", lambda e: e.max(mx8[:], val[:, i, :]), reads=[val], writes=[mx8])
                k.op("dve", lambda e: e.tensor_scalar(destf[:, i, :], mx8[:, 0:4], -1.0, BIG, op0=ALU.mult, op1=ALU.add), reads=[mx8], writes=[destf])
                for kk in range(4):
                    k.op("dve", lambda e: e.scalar_tensor_tensor(tmp[:], val[:, i, :], mx8[:, kk:kk + 1], G_all[:, i, :], op0=ALU.is_equal, op1=ALU.mult), reads=[val, mx8, G_all], writes=[tmp])
                    k.op("dve", lambda e: e.reduce_sum(gk[:, i, kk:kk + 1], tmp[:], axis=AX.X), reads=[tmp], writes=[gk])
            k.op("dve", lambda e: e.tensor_copy(dest_i[:], destf[:]), reads=[destf], writes=[dest_i])
            k.op("dve", lambda e: e.memset(be[:], 0.0), writes=[be])
            for ee in range(NE):
                k.op("dve", lambda e: e.scalar_tensor_tensor(be[:], blk512[:], start[:, ee + 1:ee + 2], be[:], op0=ALU.is_ge, op1=ALU.add), reads=[blk512, start, be], writes=[be])
            k.op("dve", lambda e: e.tensor_scalar(be[:], be[:], float(NE - 1), None, op0=ALU.min), reads=[be], writes=[be])
            for kc in range(8):
                k.op("dve", lambda e: e.tensor_scalar(widxf[:, :, kc], be[:], 1024.0, rowb[:, kc:kc + 1], op0=ALU.mult, op1=ALU.add), reads=[be, rowb], writes=[widxf])
            k.op("dve", lambda e: e.tensor_copy(widx[:], widxf[:]), reads=[widxf], writes=[widx])
            k.barrier()
            hts = [k.sb("mht", [128, D], BF16, s2) for _ in range(2)]
            for i in range(NT):
                ht = hts[i % 2]
                k.dma("sp", ht[:], hTM_d.t[tile_row[i]:tile_row[i] + 128, :], reads=[hTM_d], writes=[ht])
                for kk in range(4):
                    k.idma(Hs_d.t[:, :], ht[:], out_offset=IOA(ap=dest_i[:, i, kk:kk + 1], axis=0), reads=[dest_i, ht])
            k.barrier()
        with ExitStack() as s3:
            bguHL = k.sb("mbguhl", [64, 2 * D], BF16, s3); bdnHL = k.sb("mbdnhl", [64, D], BF16, s3); iotaE = k.sb("miotaE", [64, 512], F32, s3)
            with ExitStack() as s4:
                for (src, dst, n) in ((W["b_gu"].t[l], bguHL, 2 * D), (W["b_dn"].t[l], bdnHL, D)):
                    bf_ = k.sb("mbf", [64, n], F32, s4); hif = k.sb("mhif", [64, n], F32, s4)
                    k.dma("sp", bf_[0:32, :], src, writes=[bf_]); k.dma("sp", bf_[32:64, :], src, writes=[bf_])
                    k.op("dve", lambda e: e.tensor_copy(dst[:], bf_[:]), reads=[bf_], writes=[dst])
                    k.op("dve", lambda e: e.tensor_copy(hif[:], dst[:]), reads=[dst], writes=[hif])
                    k.op("dve", lambda e: e.tensor_tensor(hif[32:64, :], bf_[32:64, :], hif[32:64, :], ALU.subtract), reads=[bf_, hif], writes=[hif])
                    k.op("dve", lambda e: e.tensor_copy(dst[32:64, :], hif[32:64, :]), reads=[hif], writes=[dst])
                k.op("dve", lambda e: e.memset(iotaE[:], 0.0), writes=[iotaE])
                k.op("dve", lambda e: e.tensor_scalar(iotaE[:], iotaE[:], iota64[0:64, 0:1], None, op0=ALU.add), reads=[iotaE, iota64], writes=[iotaE])
                k.barrier()
            wgs = [[k.sb("mwg", [128, 2 * D], BF16, s3) for _ in range(8)] for _ in range(2)]
            wds = [[k.sb("mwd", [128, D], BF16, s3) for _ in range(8)] for _ in range(2)]
            hss = [k.sb("mhs", [128, 4, D], BF16, s3) for _ in range(2)]; hTs = [k.sb("mhT", [128, 8, 512], BF16, s3) for _ in range(1)]
            acts = [k.sb("mact", [128, 8, 512], BF16, s3) for _ in range(1)]; ohs = [k.sb("moh", [64, 512], BF16, s3) for _ in range(2)]
            tt_ = [k.sb("mt", [128, 512], F32, s3) for _ in range(2)]; ss_ = [k.sb("ms", [128, 512], F32, s3) for _ in range(2)]; uu_ = [k.sb("mu", [128, 512], F32, s3) for _ in range(2)]
            ysb = [k.sb("mysb", [128, D], F32, s3) for _ in range(2)]
            pT = k.ps("mpT", [128, 8, 128], BF16, s3)
            psg = [k.ps("mpsg", [128, 512], F32, s3) for _ in range(2)]; psl = [k.ps("mpsl", [128, 512], F32, s3) for _ in range(2)]; psd = [k.ps("mpsd", [128, 512], F32, s3) for _ in range(2)]
            wgu2d = W["w_gu"].t.rearrange("l e r n -> (l e r) n"); wdn2d = W["w_dn"].t.rearrange("l e r n -> (l e r) n")

            def fetch(blk):
                wg = wgs[blk % 2]; wd = wds[blk % 2]
                for kc in range(8):
                    k.idma(wg[kc][:], wgu2d, in_offset=IOA(ap=widx[:, blk, kc:kc + 1], axis=0), reads=[widx], writes=[wg[kc]])
                for kc in range(8):
                    k.idma(wd[kc][:], wdn2d, in_offset=IOA(ap=widx[:, blk, kc:kc + 1], axis=0), reads=[widx], writes=[wd[kc]])
                k.dma("sp", hss[blk % 2][:], Hs_d.t[blk * 512:(blk + 1) * 512, :].rearrange("(r p) d -> p r d", p=128), writes=[hss[blk % 2]])
            fetch(0)
            yc = 0; dc = 0
            for blk in range(NBLK):
                if blk + 1 < NBLK:
                    fetch(blk + 1)
                wg = wgs[blk % 2]; wd = wds[blk % 2]; hs = hss[blk % 2]; hT = hTs[0]; act = acts[0]; oh = ohs[blk % 2]
                for r in range(4):
                    for c in range(8):
                        k.op("pe", lambda e: e.transpose(pT[:, c, :], hs[:, r, c * 128:(c + 1) * 128], C["ident_bf"][:]), reads=[hs, C["ident_bf"]], writes=[pT])
                    if r % 2:
                        k.op("act", lambda e: e.copy(hT[:, :, r * 128:(r + 1) * 128], pT[:]), reads=[pT], writes=[hT])
                    else:
                        k.op("dve", lambda e: e.tensor_copy(hT[:, :, r * 128:(r + 1) * 128], pT[:]), reads=[pT], writes=[hT])
                k.op("dve", lambda e: e.tensor_scalar(oh[:], iotaE[:], be[0:64, blk:blk + 1], None, op0=ALU.is_equal), reads=[iotaE, be], writes=[oh])
                for j in range(8):
                    pg_ = psg[j % 2]; pl_ = psl[j % 2]; t_ = tt_[j % 2]; s_ = ss_[j % 2]; u_ = uu_[j % 2]
                    for (ps_, o_) in ((pg_, 0), (pl_, 1)):
                        for kc in range(8):
                            k.op("pe", lambda e: e.matmul(ps_[:], lhsT=wg[kc][:, 256 * j + o_:256 * (j + 1):2], rhs=hT[:, kc, :], start=(kc == 0), stop=False), reads=[wg[kc], hT], writes=[ps_])
                        k.op("pe", lambda e: e.matmul(ps_[:], lhsT=bguHL[:, 256 * j + o_:256 * (j + 1):2], rhs=oh[:], start=False, stop=True), reads=[bguHL, oh], writes=[ps_])
                    k.op("dve", lambda e: e.tensor_scalar(t_[:], pg_[:], 7.0, None, op0=ALU.min), reads=[pg_], writes=[t_])
                    k.op("act", lambda e: e.activation(s_[:], t_[:], AF.Sigmoid, scale=1.702), reads=[t_], writes=[s_])
                    k.op("dve", lambda e: e.tensor_scalar(u_[:], pl_[:], 7.0, -7.0, op0=ALU.min, op1=ALU.max), reads=[pl_], writes=[u_])
                    k.op("dve", lambda e: e.tensor_tensor(t_[:], t_[:], s_[:], ALU.mult), reads=[t_, s_], writes=[t_])
                    k.op("dve", lambda e: e.scalar_tensor_tensor(act[:, j, :], u_[:], 1.0, t_[:], op0=ALU.add, op1=ALU.mult), reads=[u_, t_], writes=[act])
                for r in range(4):
                    y = ysb[yc % 2]; yc += 1
                    for half in range(2):
                        pd = psd[dc % 2]; dc += 1; cs = slice(half * 512, (half + 1) * 512)
                        for j in range(8):
                            k.op("pe", lambda e: e.matmul(pd[:], lhsT=act[:, j, r * 128:(r + 1) * 128], rhs=wd[j][:, cs], start=(j == 0), stop=False), reads=[act, wd[j]], writes=[pd])
                        k.op("pe", lambda e: e.matmul(pd[:], lhsT=oh[:, r * 128:(r + 1) * 128], rhs=bdnHL[:, cs], start=False, stop=True), reads=[oh, bdnHL], writes=[pd])
                        if half:
                            k.op("act", lambda e: e.copy(y[:, cs], pd[:]), reads=[pd], writes=[y])
                        else:
                            k.op("dve", lambda e: e.tensor_copy(y[:, cs], pd[:]), reads=[pd], writes=[y])
                    k.dma("sp", Ys_d.t[blk * 512 + r * 128:blk * 512 + (r + 1) * 128, :], y[:], reads=[y])
            k.barrier()
        with ExitStack() as s5:
            gfs = {}
            for key in sorted(set(tile_key), key=str):
                row = NB if key == "c" else key
                gfs[key] = k.sb("mgf", [128, D], F32, s5)
                k.dma("sp", gfs[key][:], mod_d[l][row:row + 1, 5 * D:6 * D].partition_broadcast(128), reads=[mod_d], writes=[gfs[key]])
            ln2g = k.sb("ln2g", [128, D], F32, s5); ln2b = k.sb("ln2b", [128, D], F32, s5)
            k.dma("sp", ln2g[:], W["ln2_g"].t[l:l + 1, :].partition_broadcast(128), writes=[ln2g])
            k.dma("sp", ln2b[:], W["ln2_b"].t[l:l + 1, :].partition_broadcast(128), writes=[ln2b])
            yks = [[k.sb("myk", [128, D], F32, s5) for _ in range(4)] for _ in range(2)]
            x1s = [k.sb("mx1", [128, D], F32, s5) for _ in range(2)]
            acc = k.sb("macc", [128, D], F32, s5); uu = k.sb("muu", [128, D], F32, s5); oo = [k.sb("moo", [128, D], F32, s5) for _ in range(2)]
            st6 = k.sb("mst6", [128, 2, 6], F32, s5); mv = k.sb("mmv", [128, 2], F32, s5); rstd = k.sb("mrstd", [128, 1], F32, s5)
            for i in range(NT):
                yk = yks[i % 2]; x1t = x1s[i % 2]; o_ = oo[i % 2]; gf = gfs[tile_key[i]]
                x1_ap, x1_tk, out_ap, out_tk = tile_io(tile_row[i])
                for kk in range(4):
                    k.idma(yk[kk][:], Ys_d.t[:, :], in_offset=IOA(ap=dest_i[:, i, kk:kk + 1], axis=0), reads=[dest_i], writes=[yk[kk]])
                k.dma("sp", x1t[:], x1_ap, writes=[x1t])
                k.op("dve", lambda e: e.tensor_scalar(acc[:], yk[0][:], gk[:, i, 0:1], None, op0=ALU.mult), reads=[yk[0], gk], writes=[acc])
                for kk in range(1, 4):
                    k.op("dve", lambda e: e.scalar_tensor_tensor(acc[:], yk[kk][:], gk[:, i, kk:kk + 1], acc[:], op0=ALU.mult, op1=ALU.add), reads=[yk[kk], gk, acc], writes=[acc])
                k.op("pool", lambda e: e.tensor_tensor(acc[:], acc[:], gf[:], ALU.mult), reads=[acc, gf], writes=[acc])
                k.op("dve", lambda e: e.scalar_tensor_tensor(uu[:], x1t[:], DN_ALPHA, acc[:], op0=ALU.mult, op1=ALU.add), reads=[x1t, acc], writes=[uu])
                ln_stats(k, uu, st6, mv, rstd, 1e-5)
                k.op("dve", lambda e: e.scalar_tensor_tensor(acc[:], uu[:], mv[:, 0:1], ln2g[:], op0=ALU.subtract, op1=ALU.mult), reads=[uu, mv, ln2g], writes=[acc])
                k.op("dve", lambda e: e.scalar_tensor_tensor(o_[:], acc[:], rstd[:], ln2b[:], op0=ALU.mult, op1=ALU.add), reads=[acc, rstd, ln2b], writes=[o_])
                k.dma("sp", out_ap, o_[:], reads=[o_])
            k.barrier()
    k.barrier()


WSHAPES = {"ada_w": [2, D, 6 * D], "ada_b": [2, 6 * D], "w_in": [2, D, IN_COLS],
           "rw_mu": [2, 2, 1152], "rw_w0": [2, 2, 256], "rw_w2": [2, 2, 64, 256], "rw_a0": [2, 2, 256], "rw_a2": [2, 2, 64, 256], "rw_g2": [2, 128, 256],
           "rw_kk": [2, 256], "rw_ka": [2, 256], "rw_rk": [2, 4, 64], "rw_gn_w": [2, 256], "rw_gn_b": [2, 256],
           "mla_q_norm": [2, 256], "mla_kv_norm": [2, 128], "mla_w_uq": [2, 256, 768], "mla_w_ukv": [2, 128, 1024],
           "w_out": [2, D, D], "ln1_g": [2, D], "ln1_b": [2, D], "router_w": [2, D, NE], "router_b": [2, NE],
           "w_gu": [2, NE, D, 2 * D], "b_gu": [2, NE, 2 * D], "w_dn": [2, NE, D, D], "b_dn": [2, NE, D], "ln2_g": [2, D], "ln2_b": [2, D]}


def build_nc(NB, debug=False, layers=(0, 1), moe=True, sparse=True, phases="BCDEF"):
    nc = bass.Bass("TRN2", target_bir_lowering=False)
    with ExitStack() as st:
        k = K(nc, st)
        kin = "ExternalInput"; sk = "ExternalOutput" if debug else "Internal"
        CD = {n: k.dram("c_" + n, sh, F32, kin) for n, sh in CONST_SHAPES.items()}
        W = {n: k.dram(n, sh, F32, kin) for n, sh in WSHAPES.items()}
        xin = k.dram("xin", [NB, T, D], F32, kin)
        condT = k.dram("condT", [128, 8, NB + 1], F32, kin)
        na_bias = k.dram("na_bias", [2, 128, 4, 8, 4, 64], F32, kin)
        out_d = k.dram("out", [NB, SEQ, D], F32, "ExternalOutput")
        mod_d = k.dram("mod_d", [2, NB + 1, 6 * D], F32, sk)
        pT_rw_d = k.dram("pT_rw_d", [RW_COLS, T], F32, sk); pT_na_d = k.dram("pT_na_d", [512, T], BF16, sk); p_tm_d = k.dram("p_tm_d", [T, 704], F32, sk)
        yrw_d = k.dram("yrw_d", [64, 4, T], BF16, sk); ymla_d = k.dram("ymla_d", [4, 128, T], BF16, sk); yna_d = k.dram("yna_d", [64, 4, T], BF16, sk)
        x1_d = k.dram("x1_d", [NB * T, D], F32, sk); x2_d = k.dram("x2_d", [NB, T, D], F32, sk)
        hT_d = k.dram("hT_d", [D, NB * T], BF16, sk); GT_d = k.dram("GT_d", [NE, NB * T], F32, sk)
        hTM_d = k.dram("hTM_d", [NB * T, D], BF16, sk); Gtm_d = k.dram("Gtm_d", [NB * T, NE], F32, sk)
        nblk0 = (4 * NB * T + NE * 511) // 512
        Hs_d = k.dram("Hs_d", [nblk0 * 512, D], BF16, "Internal"); Ys_d = k.dram("Ys_d", [nblk0 * 512, D], F32, "Internal")
        C = load_consts(k, st, CD)
        k.barrier()
        for l in layers:
            need_ctx = l < DEPTH - 1
            phase_mod(k, l, NB, condT, W["ada_w"], W["ada_b"], mod_d)
            for b in range(NB):
                if l == 0:
                    x_src = (lambda tt, b=b: xin.t[b][tt * 128:(tt + 1) * 128, :]); x_tk = xin
                else:
                    x_src = (lambda tt, b=b: x2_d.t[b][tt * 128:(tt + 1) * 128, :]); x_tk = x2_d
                with ExitStack() as ps:
                    w_in_bf = k.sb("winbf", [128, 8, IN_COLS], BF16, ps)
                    load_w_in(k, l, W["w_in"], w_in_bf)
                    phase_inproj(k, l, b, NB, x_src, x_tk, mod_d, w_in_bf, C["ident_bf"], pT_rw_d, pT_na_d, p_tm_d)
                with ExitStack() as ps:
                    if "C" in phases:
                        RW = load_rw_w(k, l, ps, W, C["ident_f"])
                        phase_rwkv(k, pT_rw_d, RW, C, yrw_d)
                with ExitStack() as ps:
                    if "D" in phases:
                        wuq, wukv = load_mla_w(k, l, ps, W["mla_w_uq"], W["mla_w_ukv"], W["mla_q_norm"], W["mla_kv_norm"])
                        phase_mla(k, need_ctx, p_tm_d, wuq, wukv, C["cos_d"], C["sin_d"], C["ident_bf"], C["ones_bf"], ymla_d)
                with ExitStack() as ps:
                    if "E" in phases:
                        bm = load_na_bias(k, l, ps, na_bias, CD["na_mask"])
                        phase_na(k, need_ctx, pT_na_d, p_tm_d, bm, C["ones_bf"], yna_d)
                x1_b = Tk(x1_d.t[b * T:(b + 1) * T, :], "x1b")
                if "F" not in phases:
                    continue
                if sparse:
                    phase_post(k, l, b, NB, need_ctx, x_src, x_tk, mod_d, yrw_d, ymla_d, yna_d, W, C, x1_b, hT_d, GT_d, b * T, hTM_d, Gtm_d)
                else:
                    phase_post(k, l, b, NB, need_ctx, x_src, x_tk, mod_d, yrw_d, ymla_d, yna_d, W, C, x1_b, hT_d, GT_d, b * T)
            groups = []
            for b in range(NB):
                if need_ctx:
                    groups.append((b * T, 256, "c"))
                for i in range(4):
                    groups.append((b * T + 256 + i * 512, 512, b))
            groups.sort(key=lambda g: -g[1])

            def tile_io(col, l=l):
                b = col // T; t = col % T
                x1_ap = x1_d.t[col:col + 128, :]
                if l == DEPTH - 1:
                    return x1_ap, x1_d, out_d.t[b][t - NCTX:t - NCTX + 128, :], out_d
                return x1_ap, x1_d, x2_d.t[b][t:t + 128, :], x2_d
            if moe and sparse:
                phase_moe_sparse(k, l, NB, need_ctx, tile_io, mod_d, W, C, CD, hTM_d, Gtm_d, Hs_d, Ys_d)
            elif moe:
                phase_moe(k, l, NB, groups, tile_io, mod_d, W, C, hT_d, GT_d)
        k.barrier()
        print("ninst", k.ninst, k.cnt, flush=True)
    return nc


def core_inputs(inputs, b0, NB, shared):
    cc = np.concatenate([inputs["c"][b0:b0 + NB], inputs["c_ctx"][None]], 0).astype(np.float32)
    im = dict(shared)
    im["condT"] = np.ascontiguousarray(cc.T.reshape(8, 128, NB + 1).transpose(1, 0, 2))
    im["xin"] = np.ascontiguousarray(np.concatenate([inputs["ctx"][b0:b0 + NB], inputs["x"][b0:b0 + NB]], 1))
    return im


def shared_inputs(inputs):
    sh = {n: np.ascontiguousarray(np.asarray(inputs[n], np.float32)) for n in WSHAPES}
    for n, v in const_arrays().items():
        sh["c_" + n] = np.ascontiguousarray(v.astype(np.float32))
    sh["na_bias"] = na_bias_host(np.asarray(inputs["na_rpb"], np.float32))
    return sh


def kernel(**inputs):
    NCORES = 8; B = inputs["x"].shape[0]; NB = B // NCORES
    nc = build_nc(NB)
    sh = shared_inputs(inputs)
    in_maps = [core_inputs(inputs, c * NB, NB, sh) for c in range(NCORES)]
    res = run_bass_kernel_spmd(nc, in_maps, core_ids=list(range(NCORES)))
    return np.concatenate([r["out"] for r in res.results], 0).astype(np.float32)
```

```python
import numpy as np
from contextlib import ExitStack
import concourse.bass as bass
import concourse.mybir as mybir
from concourse.bass_utils import run_bass_kernel_spmd

F32 = mybir.dt.float32; BF16 = mybir.dt.bfloat16; I32 = mybir.dt.int32
AF = mybir.ActivationFunctionType; ALU = mybir.AluOpType; AX = mybir.AxisListType

D = 1024; SEQ = 2048; NCTX = 256; T = SEQ + NCTX; DEPTH = 2; NTT = T // 128
RW_COLS = 1152; MLA_COLS = 448; NA_COLS = 768; IN_COLS = 2368
NE = 32
NBLK_MAX = 104
DN_ALPHA = (2 * DEPTH) ** 0.25


class Tk:
    __slots__ = ("t", "w", "r", "name", "psum")
    def __init__(s, t, name="", psum=False):
        s.t = t; s.w = None; s.r = []; s.name = name; s.psum = psum
    def __getitem__(s, idx):
        return s.t[idx]


class K:
    NDMA = 6
    def __init__(s, nc, stack):
        s.nc = nc; s.stack = stack
        s.eng = {"pe": nc.tensor, "act": nc.scalar, "dve": nc.vector, "pool": nc.gpsimd, "sp": nc.sync}
        s.sem = {}; s.cnt = {}
        for e in s.eng:
            s.sem[e] = stack.enter_context(nc.semaphore("s_" + e)); s.cnt[e] = 0
        s.seen = {e: {} for e in s.eng}
        s.dsem = [stack.enter_context(nc.semaphore("d%d" % i)) for i in range(3 * s.NDMA)]
        s.dcnt = [0] * (3 * s.NDMA); s.dnext = {"sp": 0, "act": 0, "pool": 0}; s.dq = {"sp": 0, "act": 1, "pool": 2}
        s.ninst = 0; s.uid = 0
    def sb(s, name, shape, dt, stack=None):
        s.uid += 1
        t = Tk((stack or s.stack).enter_context(s.nc.sbuf_tensor("%s_%d" % (name, s.uid), list(shape), dt)), name)
        assert s.nc.sbuf_bytes_remaining >= 28 * 1024, "SBUF budget exceeded at %s: remaining %d" % (name, s.nc.sbuf_bytes_remaining)
        return t
    def ps(s, name, shape, dt=F32, stack=None):
        s.uid += 1
        isz = 4 if dt == F32 else 2
        free = int(np.prod(shape[1:])); nb = (free * isz + 2047) // 2048
        raw = (stack or s.stack).enter_context(s.nc.psum_tensor("%s_%d" % (name, s.uid), [128, nb * 2048 // isz], dt))
        ap = raw[0:shape[0], 0:free]
        if len(shape) == 3:
            ap = ap.rearrange("p (a b) -> p a b", a=shape[1])
        return Tk(ap, name, psum=True)
    def dram(s, name, shape, dt, kind="Internal"):
        return Tk(s.nc.dram_tensor(name, list(shape), dt, kind=kind).ap(), name)
    def _wait(s, e, toks):
        seen = s.seen[e]
        for (sem, val, key) in toks:
            if seen.get(key, 0) >= val:
                continue
            s.eng[e].wait_ge(sem, val); seen[key] = val; s.ninst += 1
    def _deps(s, e, reads, writes):
        toks = []
        for t in reads:
            if t.w is not None:
                toks.append(t.w)
            if t.psum:
                toks.extend(k for k in t.r if k[2] != e)
        for t in writes:
            if t.w is not None:
                toks.append(t.w)
            toks.extend(t.r)
        if e == "pe":
            toks = [k for k in toks if k[2] != "pe"]
        return toks
    def op(s, e, fn, reads=(), writes=()):
        s._wait(e, s._deps(e, reads, writes))
        ins = fn(s.eng[e]); s.cnt[e] += 1; ins.then_inc(s.sem[e], 1); s.ninst += 1
        tok = (s.sem[e], s.cnt[e], e)
        for t in reads:
            t.r = [k for k in t.r if k[2] != e]; t.r.append(tok)
        for t in writes:
            t.w = tok; t.r = []
        return ins
    def dma(s, q, out, in_, reads=(), writes=(), **kw):
        base = s.dq[q] * s.NDMA; i = base + s.dnext[q]; s.dnext[q] = (s.dnext[q] + 1) % s.NDMA
        key = "d%d" % i
        toks = s._deps(q, reads, writes)
        if s.dcnt[i] > 0:
            toks.append((s.dsem[i], s.dcnt[i], key))
        s._wait(q, toks)
        ins = s.eng[q].dma_start(out=out, in_=in_, **kw); s.dcnt[i] += 16; ins.then_inc(s.dsem[i], 16); s.ninst += 1
        tok = (s.dsem[i], s.dcnt[i], key)
        for t in reads:
            t.r.append(tok)
        for t in writes:
            t.w = tok; t.r = []
        return ins
    def idma(s, out, in_, out_offset=None, in_offset=None, reads=(), writes=(), **kw):
        q = "pool"
        base = s.dq[q] * s.NDMA; i = base + s.dnext[q]; s.dnext[q] = (s.dnext[q] + 1) % s.NDMA
        key = "d%d" % i
        toks = s._deps(q, reads, writes)
        if s.dcnt[i] > 0:
            toks.append((s.dsem[i], s.dcnt[i], key))
        s._wait(q, toks)
        ins = s.eng[q].indirect_dma_start(out=out, out_offset=out_offset, in_=in_, in_offset=in_offset, **kw)
        s.dcnt[i] += 16; ins.then_inc(s.dsem[i], 16); s.ninst += 1
        tok = (s.dsem[i], s.dcnt[i], key)
        for t in reads:
            t.r.append(tok)
        for t in writes:
            t.w = tok; t.r = []
        return ins
    def barrier(s):
        toks = [(s.sem[e], s.cnt[e], e) for e in s.eng if s.cnt[e] > 0]
        toks += [(s.dsem[i], s.dcnt[i], "d%d" % i) for i in range(len(s.dsem)) if s.dcnt[i] > 0]
        for e in s.eng:
            s._wait(e, [k for k in toks if k[2] != e])
    def finish(s, outs):
        s._wait("sp", [t.w for t in outs if t.w is not None])


def bcast_rows(ap_row, n):
    return ap_row.partition_broadcast(n)


def phase_mod(k, l, NB, condT_d, ada_w_d, ada_b_d, mod_d):
    with ExitStack() as st:
        R = NB + 1
        cond = k.sb("cond", [128, 8, R], F32, st); cond_bf = k.sb("condbf", [128, 8, R], BF16, st)
        adab = k.sb("adab", [R, 6 * D], F32, st); mod_sb = k.sb("modsb", [R, 6 * D], F32, st)
        wts = [k.sb("adaw", [128, 8, 512], BF16, st) for _ in range(2)]
        pss = [k.ps("modps", [R, 512], F32, st) for _ in range(2)]
        k.dma("sp", cond[:], condT_d[:], writes=[cond])
        k.dma("sp", adab[:], ada_b_d[l:l + 1, :].partition_broadcast(R), writes=[adab])
        k.op("act", lambda e: e.activation(cond_bf[:], cond[:], AF.Silu), reads=[cond], writes=[cond_bf])
        wv = ada_w_d.t[l].rearrange("(kc p) n -> p kc n", p=128)
        for g in range(12):
            wt = wts[g % 2]; ps = pss[g % 2]
            k.dma("pool", wt[:], wv[:, :, g * 512:(g + 1) * 512], writes=[wt])
            for kc in range(8):
                k.op("pe", lambda e: e.matmul(ps[:], lhsT=cond_bf[:, kc, :], rhs=wt[:, kc, :], start=(kc == 0), stop=(kc == 7)),
                     reads=[cond_bf, wt], writes=[ps])
            k.op("dve", lambda e: e.tensor_tensor(mod_sb[:, g * 512:(g + 1) * 512], ps[:], adab[:, g * 512:(g + 1) * 512], ALU.add),
                 reads=[ps, adab], writes=[mod_sb])
        k.dma("sp", mod_d[l], mod_sb[:], reads=[mod_sb], writes=[mod_d])
    k.barrier()


def ln_stats(k, xt, st6, mv, rstd, eps):
    for hh in range(2):
        k.op("dve", lambda e: e.bn_stats(st6[:, hh, :], xt[:, hh * 512:(hh + 1) * 512]), reads=[xt], writes=[st6])
    k.op("dve", lambda e: e.bn_aggr(mv[:], st6[:].rearrange("p a b -> p (a b)")), reads=[st6], writes=[mv])
    k.op("dve", lambda e: e.tensor_scalar(rstd[:], mv[:, 1:2], eps, None, op0=ALU.add), reads=[mv], writes=[rstd])
    k.op("act", lambda e: e.sqrt(rstd[:], rstd[:]), reads=[rstd], writes=[rstd])
    k.op("dve", lambda e: e.reciprocal(rstd[:], rstd[:]), reads=[rstd], writes=[rstd])


def phase_inproj(k, l, b, NB, x_src, x_src_tk, mod_d, w_in_bf, ident_bf, pT_rw_d, pT_na_d, p_tm_d):
    with ExitStack() as st:
        bc = {}
        for nm, row, off in (("sa_l", b, 0), ("ca_l", b, D), ("sa_c", NB, 0), ("ca_c", NB, D)):
            tle = k.sb(nm, [128, D], F32, st)
            k.dma("sp", tle[:], mod_d[l][row:row + 1, off:off + D].partition_broadcast(128), reads=[mod_d], writes=[tle])
            bc[nm] = tle
        for nm in ("ca_l", "ca_c"):
            tle = bc[nm]
            k.op("pool", lambda e: e.tensor_scalar(tle[:], tle[:], 1.0, None, op0=ALU.add), reads=[tle], writes=[tle])
        groups = [(0, 512), (512, 512), (1024, 512), (1536, 512), (2048, 256)]
        xmT = [k.sb("xmT%d" % g, [128, 8, n], BF16, st) for g, (o, n) in enumerate(groups)]
        xts = [k.sb("xt", [128, D], F32, st) for _ in range(2)]
        t1 = k.sb("t1", [128, D], F32, st); xm = k.sb("xm", [128, D], BF16, st)
        st6 = k.sb("st6", [128, 2, 6], F32, st); mv = k.sb("mv", [128, 2], F32, st); rstd = k.sb("rstd", [128, 1], F32, st)
        pTs = [k.ps("pT", [128, 8, 128], BF16, st) for _ in range(2)]
        ptm = [k.ps("ptm", [128, 512], F32, st) for _ in range(2)]
        pfm = [k.ps("pfm", [128, 512], F32, st) for _ in range(2)]
        tm_sb = [k.sb("tmsb", [128, 704], F32, st) for _ in range(2)]
        fm_sb = [k.sb("fmsb", [128, 512], F32, st) for _ in range(2)]
        fm_sbb = [k.sb("fmsbb", [128, 512], BF16, st) for _ in range(2)]
        for tt in range(NTT):
            isctx = tt < 2
            sa = bc["sa_c" if isctx else "sa_l"]; ca = bc["ca_c" if isctx else "ca_l"]
            xt = xts[tt % 2]
            k.dma("sp", xt[:], x_src(tt), reads=[x_src_tk], writes=[xt])
            ln_stats(k, xt, st6, mv, rstd, 1e-5)
            k.op("dve", lambda e: e.scalar_tensor_tensor(t1[:], xt[:], mv[:, 0:1], ca[:], op0=ALU.subtract, op1=ALU.mult),
                 reads=[xt, mv, ca], writes=[t1])
            k.op("dve", lambda e: e.scalar_tensor_tensor(xm[:], t1[:], rstd[:], sa[:], op0=ALU.mult, op1=ALU.add),
                 reads=[t1, rstd, sa], writes=[xm])
            pT = pTs[tt % 2]
            for c in range(8):
                k.op("pe", lambda e: e.transpose(pT[:, c, :], xm[:, c * 128:(c + 1) * 128], ident_bf[:]), reads=[xm, ident_bf], writes=[pT])
            g = tt // 4; o = (tt % 4) * 128
            xg = xmT[g]
            k.op("act", lambda e: e.copy(xg[:, :, o:o + 128], pT[:]), reads=[pT], writes=[xg])
            pa = ptm[0]; pb = ptm[1]
            for kc in range(8):
                k.op("pe", lambda e: e.matmul(pa[:, 0:448], lhsT=xg[:, kc, o:o + 128], rhs=w_in_bf[:, kc, 1152:1600], start=(kc == 0), stop=(kc == 7)),
                     reads=[xg, w_in_bf], writes=[pa])
            for kc in range(8):
                k.op("pe", lambda e: e.matmul(pb[:, 0:256], lhsT=xg[:, kc, o:o + 128], rhs=w_in_bf[:, kc, 2112:2368], start=(kc == 0), stop=(kc == 7)),
                     reads=[xg, w_in_bf], writes=[pb])
            ts = tm_sb[tt % 2]
            k.op("act", lambda e: e.copy(ts[:, 0:448], pa[:, 0:448]), reads=[pa], writes=[ts])
            k.op("dve", lambda e: e.tensor_copy(ts[:, 448:704], pb[:, 0:256]), reads=[pb], writes=[ts])
            k.dma("sp", p_tm_d[tt * 128:(tt + 1) * 128, :], ts[:], reads=[ts], writes=[p_tm_d])
        cnt = 0
        for g, (o, n) in enumerate(groups):
            xg = xmT[g]
            for c in range(13):
                col = c * 128 if c < 9 else 1600 + (c - 9) * 128
                ps = pfm[cnt % 2]
                for kc in range(8):
                    k.op("pe", lambda e: e.matmul(ps[:, 0:n], lhsT=w_in_bf[:, kc, col:col + 128], rhs=xg[:, kc, :], start=(kc == 0), stop=(kc == 7)),
                         reads=[w_in_bf, xg], writes=[ps])
                if c < 9:
                    fs = fm_sb[cnt % 2]
                    k.op("act" if cnt % 2 else "dve", (lambda e: e.copy(fs[:, 0:n], ps[:, 0:n])) if cnt % 2 else (lambda e: e.tensor_copy(fs[:, 0:n], ps[:, 0:n])),
                         reads=[ps], writes=[fs])
                    k.dma("sp", pT_rw_d[c * 128:(c + 1) * 128, o:o + n], fs[:, 0:n], reads=[fs], writes=[pT_rw_d])
                else:
                    fs = fm_sbb[cnt % 2]
                    k.op("act" if cnt % 2 else "dve", (lambda e: e.copy(fs[:, 0:n], ps[:, 0:n])) if cnt % 2 else (lambda e: e.tensor_copy(fs[:, 0:n], ps[:, 0:n])),
                         reads=[ps], writes=[fs])
                    k.dma("sp", pT_na_d[(c - 9) * 128:(c - 8) * 128, o:o + n], fs[:, 0:n], reads=[fs], writes=[pT_na_d])
                cnt += 1
    k.barrier()


def load_w_in(k, l, w_in_d, w_in_bf):
    wv = w_in_d.t[l].rearrange("(kc p) n -> p kc n", p=128)
    for (a, bb) in ((0, 1024), (1024, 2048), (2048, IN_COLS)):
        k.dma("pool", w_in_bf[:, :, a:bb], wv[:, :, a:bb], writes=[w_in_bf])


def load_mla_w(k, l, st, w_uq_d, w_ukv_d, qn_d, kvn_d):
    wuq = k.sb("wuq", [128, 2, 768], BF16, st); wukv = k.sb("wukv", [128, 1024], BF16, st)
    with ExitStack() as s2:
        wuq_f = k.sb("wuqf", [128, 2, 768], F32, s2); wukv_f = k.sb("wukvf", [128, 1024], F32, s2)
        qn = k.sb("qn", [128, 2], F32, s2); kvn = k.sb("kvn", [128, 1], F32, s2)
        k.dma("sp", wuq_f[:], w_uq_d.t[l].rearrange("(kc p) n -> p kc n", p=128), writes=[wuq_f])
        k.dma("sp", wukv_f[:], w_ukv_d.t[l], writes=[wukv_f])
        k.dma("sp", qn[:], qn_d.t[l].rearrange("(kc p) -> p kc", p=128), writes=[qn], allow_slow_non_contiguous=True)
        k.dma("sp", kvn[:], kvn_d.t[l].rearrange("(p o) -> p o", o=1), writes=[kvn])
        for kc in range(2):
            k.op("dve", lambda e: e.tensor_scalar(wuq[:, kc, :], wuq_f[:, kc, :], qn[:, kc:kc + 1], None, op0=ALU.mult), reads=[wuq_f, qn], writes=[wuq])
        k.op("dve", lambda e: e.tensor_scalar(wukv[:], wukv_f[:], kvn[:, 0:1], None, op0=ALU.mult), reads=[wukv_f, kvn], writes=[wukv])
        k.barrier()
    return wuq, wukv


def phase_mla(k, need_ctx, p_tm_d, wuq, wukv, cos_d, sin_d, ident_bf, ones_bf, ymla_d):
    scale = 192.0 ** -0.5
    with ExitStack() as st:
        cos_sb = k.sb("cos", [128, 16, 128], F32, st); sin_sb = k.sb("sin", [128, 16, 128], F32, st)
        k.dma("sp", cos_sb[:], cos_d.t.rearrange("(tt p) c -> p tt c", p=128), writes=[cos_sb])
        k.dma("sp", sin_sb[:], sin_d.t.rearrange("(tt p) c -> p tt c", p=128), writes=[sin_sb])
        p_all = k.sb("pall", [128, NTT, 448], F32, st)
        k.dma("sp", p_all[:], p_tm_d.t.rearrange("(tt p) c -> p tt c", p=128)[:, :, 0:448], reads=[p_tm_d], writes=[p_all])
        qlnT = k.sb("qlnT", [128, 2, T], BF16, st); kvnT = k.sb("kvnT", [128, T], BF16, st)
        qTn = k.sb("qTn", [128, 4, T], BF16, st); qTr = k.sb("qTr", [64, 4, T], BF16, st)
        knT = k.sb("knT", [128, 4, T], BF16, st); krT = k.sb("krT", [64, T], BF16, st)
        v_sb = k.sb("vsb", [128, NTT, 512], BF16, st)
        rq = k.sb("rq", [128, NTT], F32, st); rkv = k.sb("rkv", [128, NTT], F32, st)
        with ExitStack() as st1:
            sq = k.sb("sq", [128, NTT, 384], F32, st1)
            k.op("act", lambda e: e.activation(sq[:], p_all[:, :, 0:384], AF.Square), reads=[p_all], writes=[sq])
            k.op("dve", lambda e: e.reduce_sum(rq[:], sq[:, :, 0:256], axis=AX.X), reads=[sq], writes=[rq])
            k.op("dve", lambda e: e.reduce_sum(rkv[:], sq[:, :, 256:384], axis=AX.X), reads=[sq], writes=[rkv])
            k.barrier()
        k.op("dve", lambda e: e.tensor_scalar(rq[:], rq[:], 1.0 / 256, 1e-6, op0=ALU.mult, op1=ALU.add), reads=[rq], writes=[rq])
        k.op("dve", lambda e: e.tensor_scalar(rkv[:], rkv[:], 1.0 / 128, 1e-6, op0=ALU.mult, op1=ALU.add), reads=[rkv], writes=[rkv])
        for r_ in (rq, rkv):
            k.op("act", lambda e: e.sqrt(r_[:], r_[:]), reads=[r_], writes=[r_])
            k.op("dve", lambda e: e.reciprocal(r_[:], r_[:]), reads=[r_], writes=[r_])
        st2 = ExitStack()
        nrm = [k.sb("nrm", [128, 384], BF16, st2) for _ in range(2)]
        pTs = k.ps("mlapT", [128, 3, 128], BF16, st2)
        pqs = [k.ps("mlapq", [128, 4, 256], F32, st2) for _ in range(2)]
        pv = k.ps("mlapv", [128, 512], F32, st2)
        q_sbs = [k.sb("qsb", [128, 4, 192], BF16, st2) for _ in range(2)]; kr_sbs = [k.sb("krsb", [128, 64], BF16, st2) for _ in range(2)]
        qr_fs = [k.sb("qrf", [128, 4, 64], F32, st2) for _ in range(2)]
        ra = k.sb("ra", [128, 4, 32], F32, st2); rb = k.sb("rb", [128, 4, 32], F32, st2)
        pqT = k.ps("mlapqT", [128, 4, 128], BF16, st2); pqTr = k.ps("mlapqTr", [64, 5, 128], BF16, st2)

        def stage_x(tt):
            isctx = tt < 2
            nt = nrm[tt % 2]; pT = pTs; pq = pqs[tt % 2]; q_sb = q_sbs[tt % 2]; kr_sb = kr_sbs[tt % 2]; qr_f = qr_fs[tt % 2]
            k.op("dve", lambda e: e.tensor_scalar(nt[:, 0:256], p_all[:, tt, 0:256], rq[:, tt:tt + 1], None, op0=ALU.mult), reads=[p_all, rq], writes=[nt])
            k.op("dve", lambda e: e.tensor_scalar(nt[:, 256:384], p_all[:, tt, 256:384], rkv[:, tt:tt + 1], None, op0=ALU.mult), reads=[p_all, rkv], writes=[nt])
            for c in range(3):
                k.op("pe", lambda e: e.transpose(pT[:, c, :], nt[:, c * 128:(c + 1) * 128], ident_bf[:]), reads=[nt, ident_bf], writes=[pT])
            sl = slice(tt * 128, (tt + 1) * 128)
            k.op("act", lambda e: e.copy(qlnT[:, :, sl], pT[:, 0:2, :]), reads=[pT], writes=[qlnT])
            k.op("act", lambda e: e.copy(kvnT[:, sl], pT[:, 2, :]), reads=[pT], writes=[kvnT])
            k.op("pe", lambda e: e.matmul(pv[:].rearrange("p (h c) -> p h c", h=4), lhsT=kvnT[:, sl],
                                          rhs=wukv[:].rearrange("p (h c) -> p h c", h=4)[:, :, 128:256], start=True, stop=True),
                 reads=[kvnT, wukv], writes=[pv])
            doq = (not isctx) or need_ctx
            if doq:
                for h in range(4):
                    for kc in range(2):
                        k.op("pe", lambda e: e.matmul(pq[:, h, 0:192], lhsT=qlnT[:, kc, sl], rhs=wuq[:, kc, h * 192:(h + 1) * 192], start=(kc == 0), stop=(kc == 1)),
                             reads=[qlnT, wuq], writes=[pq])
            k.op("act", lambda e: e.copy(v_sb[:, tt, :], pv[:]), reads=[pv], writes=[v_sb])
            kre = p_all[:, tt, 384:448:2]; kro = p_all[:, tt, 385:448:2]
            if isctx:
                k.op("dve", lambda e: e.tensor_copy(kr_sb[:, 0:32], kre), reads=[p_all], writes=[kr_sb])
                k.op("dve", lambda e: e.tensor_copy(kr_sb[:, 32:64], kro), reads=[p_all], writes=[kr_sb])
            else:
                cs = cos_sb[:, tt - 2, 0:32]; sn = sin_sb[:, tt - 2, 0:32]
                k.op("dve", lambda e: e.tensor_tensor(ra[:, 0, :], kre, cs, ALU.mult), reads=[p_all, cos_sb], writes=[ra])
                k.op("dve", lambda e: e.tensor_tensor(rb[:, 0, :], kro, sn, ALU.mult), reads=[p_all, sin_sb], writes=[rb])
                k.op("dve", lambda e: e.tensor_tensor(kr_sb[:, 0:32], ra[:, 0, :], rb[:, 0, :], ALU.subtract), reads=[ra, rb], writes=[kr_sb])
                k.op("dve", lambda e: e.tensor_tensor(ra[:, 0, :], kre, sn, ALU.mult), reads=[p_all, sin_sb], writes=[ra])
                k.op("dve", lambda e: e.tensor_tensor(rb[:, 0, :], kro, cs, ALU.mult), reads=[p_all, cos_sb], writes=[rb])
                k.op("dve", lambda e: e.tensor_tensor(kr_sb[:, 32:64], ra[:, 0, :], rb[:, 0, :], ALU.add), reads=[ra, rb], writes=[kr_sb])
            if doq:
                k.op("act", lambda e: e.copy(q_sb[:, :, 0:128], pq[:, :, 0:128]), reads=[pq], writes=[q_sb])
                k.op("act", lambda e: e.copy(qr_f[:], pq[:, :, 128:192]), reads=[pq], writes=[qr_f])
                qe = qr_f[:, :, 0:64:2]; qo = qr_f[:, :, 1:64:2]
                if isctx:
                    k.op("dve", lambda e: e.tensor_copy(q_sb[:, :, 128:160], qe), reads=[qr_f], writes=[q_sb])
                    k.op("dve", lambda e: e.tensor_copy(q_sb[:, :, 160:192], qo), reads=[qr_f], writes=[q_sb])
                else:
                    cs = cos_sb[:, tt - 2, :].rearrange("p (h c) -> p h c", h=4); sn = sin_sb[:, tt - 2, :].rearrange("p (h c) -> p h c", h=4)
                    k.op("dve", lambda e: e.tensor_tensor(ra[:], qe, cs, ALU.mult), reads=[qr_f, cos_sb], writes=[ra])
                    k.op("dve", lambda e: e.tensor_tensor(rb[:], qo, sn, ALU.mult), reads=[qr_f, sin_sb], writes=[rb])
                    k.op("dve", lambda e: e.tensor_tensor(q_sb[:, :, 128:160], ra[:], rb[:], ALU.subtract), reads=[ra, rb], writes=[q_sb])
                    k.op("dve", lambda e: e.tensor_tensor(ra[:], qe, sn, ALU.mult), reads=[qr_f, sin_sb], writes=[ra])
                    k.op("dve", lambda e: e.tensor_tensor(rb[:], qo, cs, ALU.mult), reads=[qr_f, cos_sb], writes=[rb])
                    k.op("dve", lambda e: e.tensor_tensor(q_sb[:, :, 160:192], ra[:], rb[:], ALU.add), reads=[ra, rb], writes=[q_sb])

        def stage_y(tt):
            isctx = tt < 2
            q_sb = q_sbs[tt % 2]; kr_sb = kr_sbs[tt % 2]
            sl = slice(tt * 128, (tt + 1) * 128)
            doq = (not isctx) or need_ctx
            if doq:
                for h in range(4):
                    k.op("pe", lambda e: e.transpose(pqT[:, h, :], q_sb[:, h, 0:128], ident_bf[:]), reads=[q_sb, ident_bf], writes=[pqT])
                    k.op("pe", lambda e: e.transpose(pqTr[:, h, :], q_sb[:, h, 128:192], ident_bf[:]), reads=[q_sb, ident_bf], writes=[pqTr])
            k.op("pe", lambda e: e.transpose(pqTr[:, 4, :], kr_sb[:], ident_bf[:]), reads=[kr_sb, ident_bf], writes=[pqTr])
            if doq:
                k.op("act", lambda e: e.copy(qTn[:, :, sl], pqT[:]), reads=[pqT], writes=[qTn])
                k.op("dve", lambda e: e.tensor_copy(qTr[:, :, sl], pqTr[:, 0:4, :]), reads=[pqTr], writes=[qTr])
            k.op("dve", lambda e: e.tensor_copy(krT[:, sl], pqTr[:, 4, :]), reads=[pqTr], writes=[krT])

        STOP = 99
        for tt in range(NTT):
            stage_x(tt)
            if tt > 0:
                stage_y(tt - 1)
        stage_y(NTT - 1)
        k.barrier(); st2.close()
        groups = [(0, 512), (512, 512), (1024, 512), (1536, 512), (2048, 256)]
        pk = [k.ps("mlapk", [128, 512], F32, st) for _ in range(2)]
        cnt = 0
        for h in range(4 if STOP > 2 else 0):
            for (o, n) in groups:
                ps = pk[cnt % 2]
                k.op("pe", lambda e: e.matmul(ps[:, 0:n], lhsT=wukv[:, h * 256:h * 256 + 128], rhs=kvnT[:, o:o + n], start=True, stop=True), reads=[wukv, kvnT], writes=[ps])
                k.op("act" if cnt % 2 else "dve", (lambda e: e.copy(knT[:, h, o:o + n], ps[:, 0:n])) if cnt % 2 else (lambda e: e.tensor_copy(knT[:, h, o:o + n], ps[:, 0:n])),
                     reads=[ps], writes=[knT])
                cnt += 1
        pos = [k.ps("mlapo", [128, 512], F32, st) for _ in range(2)]
        pss = [k.ps("mlapss", [128, 512], F32, st) for _ in range(2)]
        pts = [k.sb("mlapt", [128, 512], BF16, st) for _ in range(3)]
        rs = k.sb("mlars", [128, 512], F32, st); ob = [k.sb("mlaob", [128, 512], BF16, st) for _ in range(2)]
        qblocks = [(256 + i * 512, 512, list(range(NTT))) for i in range(4)]
        if need_ctx:
            qblocks.append((0, 256, [0, 1]))
        it = 0; bi = 0
        for h in range(4 if STOP > 3 else 0):
            for (qo_, nq, ktiles) in qblocks:
                po = pos[bi % 2]; psum = pss[bi % 2]
                for ji, j in enumerate(ktiles):
                    ps = pk[it % 2]; pt = pts[it % 3]
                    ks = slice(j * 128, (j + 1) * 128)
                    k.op("pe", lambda e: e.matmul(ps[:, 0:nq], lhsT=knT[:, h, ks], rhs=qTn[:, h, qo_:qo_ + nq], start=True, stop=False), reads=[knT, qTn], writes=[ps])
                    k.op("pe", lambda e: e.matmul(ps[:, 0:nq], lhsT=krT[:, ks], rhs=qTr[:, h, qo_:qo_ + nq], start=False, stop=True), reads=[krT, qTr], writes=[ps])
                    k.op("act", lambda e: e.activation(pt[:, 0:nq], ps[:, 0:nq], AF.Exp, scale=scale), reads=[ps], writes=[pt])
                    first = ji == 0; last = ji == len(ktiles) - 1
                    k.op("pe", lambda e: e.matmul(po[:, 0:nq], lhsT=v_sb[:, j, h * 128:(h + 1) * 128], rhs=pt[:, 0:nq], start=first, stop=last), reads=[v_sb, pt], writes=[po])
                    k.op("pe", lambda e: e.matmul(psum[:, 0:nq], lhsT=ones_bf[:], rhs=pt[:, 0:nq], start=first, stop=last), reads=[ones_bf, pt], writes=[psum])
                    it += 1
                o_ = ob[bi % 2]
                k.op("dve", lambda e: e.reciprocal(rs[:, 0:nq], psum[:, 0:nq]), reads=[psum], writes=[rs])
                k.op("dve", lambda e: e.tensor_tensor(o_[:, 0:nq], po[:, 0:nq], rs[:, 0:nq], ALU.mult), reads=[po, rs], writes=[o_])
                k.dma("sp", ymla_d[h][:, qo_:qo_ + nq], o_[:, 0:nq], reads=[o_], writes=[ymla_d])
                bi += 1
    k.barrier()


def rope_tables():
    t = np.arange(SEQ); row = (t // 64).astype(np.float32); col = (t % 64).astype(np.float32)
    inv = (10000.0 ** (-np.arange(16, dtype=np.float32) / 16)).astype(np.float32)
    ang = np.concatenate([row[:, None] * inv, col[:, None] * inv], -1).astype(np.float32)
    return np.tile(np.cos(ang).astype(np.float32), (1, 4)), np.tile(np.sin(ang).astype(np.float32), (1, 4))


def na_pattern(i):
    if i < 4:
        return i, 0
    if i <= 28:
        return 4, i - 4
    return i - 24, 24


def na_bias_host(rpb):
    L = rpb.shape[0]
    col = np.arange(64)
    dc = np.clip(col[None, :] - col[:, None] + 15, 0, 30)
    out = np.zeros((L, 128, 4, 8, 4, 64), np.float32)
    lk = np.arange(512); r = lk // 64; kc = lk % 64
    for i in list(range(5)) + [29, 30, 31]:
        pat, rs = na_pattern(i)
        dr = rs + r - i + 7
        g = rpb[:, :, dr[:, None], dc.T[kc, :]]
        out[:, :, :, pat] = g.reshape(L, 4, 4, 128, 64).transpose(0, 3, 1, 2, 4)
    return out


def na_mask_host():
    col = np.arange(64)
    cs = np.clip(col - 8, 0, 48)
    inw = (col[None, :] >= cs[:, None]) & (col[None, :] < cs[:, None] + 16)
    lk = np.arange(512); kc = lk % 64
    m = np.where(inw.T[kc, :], 0.0, -30000.0).astype(np.float32)
    return np.ascontiguousarray(m.reshape(4, 128, 64).transpose(1, 0, 2))


def load_na_bias(k, l, st, bias_d, mask_d):
    bm = k.sb("nabm", [128, 4, 8, 384], F32, st); mk = k.sb("namk", [128, 256], F32, st)
    k.op("pool", lambda e: e.memset(bm[:], 0.0), writes=[bm])
    k.dma("sp", mk[:], mask_d.t.rearrange("p j q -> p (j q)"), writes=[mk])
    for h in range(4):
        k.dma("sp", bm[:, h, :, 0:256], bias_d.t[l][:, h].rearrange("p a j q -> p a (j q)"), writes=[bm])
    for h in range(4):
        for pat in range(8):
            k.op("pool", lambda e: e.tensor_tensor(bm[:, h, pat, 0:256], bm[:, h, pat, 0:256], mk[:], ALU.add), reads=[bm, mk], writes=[bm])
    return bm


def phase_na(k, need_ctx, pT_na_d, p_tm_d, bm, ones_bf, yna_d):
    scale = 64.0 ** -0.5
    with ExitStack() as st:
        qT = k.sb("naqT", [64, 4, T], BF16, st); kT = k.sb("nakT", [64, 4, T], BF16, st)
        vA = k.sb("navA", [128, NTT, 256], BF16, st); vB = k.sb("navB", [128, NTT - 1, 256], BF16, st)
        yo = k.sb("nayo", [64, 4, T], BF16, st)
        k.dma("sp", qT[:], pT_na_d.t[0:256, :].rearrange("(h d) t -> d h t", h=4), reads=[pT_na_d], writes=[qT])
        k.dma("sp", kT[:], pT_na_d.t[256:512, :].rearrange("(h d) t -> d h t", h=4), reads=[pT_na_d], writes=[kT])
        k.dma("pool", vA[:], p_tm_d.t.rearrange("(tt p) c -> p tt c", p=128)[:, :, 448:704], reads=[p_tm_d], writes=[vA])
        k.dma("pool", vB[:], p_tm_d.t[64:64 + (NTT - 1) * 128, :].rearrange("(tt p) c -> p tt c", p=128)[:, :, 448:704], reads=[p_tm_d], writes=[vB])
        NSL = 4
        pSs = [k.ps("napS", [128, 6, 64], F32, st) for _ in range(NSL)]
        poms = [k.ps("napom", [64, 512], F32, st) for _ in range(NSL)]
        ssb = [k.sb("nassb", [128, 6, 64], F32, st) for _ in range(NSL)]
        pts = [k.sb("napt", [128, 6, 64], BF16, st) for _ in range(NSL)]
        ptc = [k.sb("naptc", [128, 2, 256], BF16, st) for _ in range(NSL)]
        rss = [k.sb("nars", [64, 256], F32, st) for _ in range(NSL)]
        it = 0
        for h in range(4):
            for i in range(32):
                pat, rs_ = na_pattern(i)
                pS = pSs[it % NSL]; pom = poms[it % NSL]; s_sb = ssb[it % NSL]; pt = pts[it % NSL]; rs = rss[it % NSL]
                tok0 = 256 + 64 * rs_
                qs = slice(256 + 64 * i, 256 + 64 * i + 64)
                for j in range(6):
                    ko = tok0 + 128 * j if j < 4 else (j - 4) * 128
                    k.op("pe", lambda e: e.matmul(pS[:, j, :], lhsT=kT[:, h, ko:ko + 128], rhs=qT[:, h, qs], start=True, stop=True), reads=[kT, qT], writes=[pS])
                k.op("dve", lambda e: e.scalar_tensor_tensor(s_sb[:].rearrange("p j q -> p (j q)"), pS[:].rearrange("p j q -> p (j q)"), scale, bm[:, h, pat, :], op0=ALU.mult, op1=ALU.add),
                     reads=[pS, bm], writes=[s_sb])
                k.op("act", lambda e: e.activation(pt[:], s_sb[:], AF.Exp), reads=[s_sb], writes=[pt])
                for j in range(6):
                    if j < 4:
                        vt = (vA, 2 + rs_ // 2 + j) if rs_ % 2 == 0 else (vB, (3 + rs_) // 2 + j)
                    else:
                        vt = (vA, j - 4)
                    vtk, vi = vt
                    k.op("pe", lambda e: e.matmul(pom[:, 0:64], lhsT=vtk[:, vi, h * 64:(h + 1) * 64], rhs=pt[:, j, :], start=(j == 0), stop=(j == 5)), reads=[vtk, pt], writes=[pom])
                for j in range(6):
                    k.op("pe", lambda e: e.matmul(pom[:, 256:320], lhsT=ones_bf[:, 0:64], rhs=pt[:, j, :], start=(j == 0), stop=(j == 5)), reads=[ones_bf, pt], writes=[pom])
                k.op("dve", lambda e: e.reciprocal(rs[:, 0:64], pom[:, 256:320]), reads=[pom], writes=[rs])
                k.op("dve", lambda e: e.tensor_tensor(yo[:, h, qs], pom[:, 0:64], rs[:, 0:64], ALU.mult), reads=[pom, rs], writes=[yo])
                it += 1
            if need_ctx:
                pS = pSs[it % NSL]; pom = poms[it % NSL]; pt = ptc[it % NSL]; rs = rss[it % NSL]
                pSv = pS[:].rearrange("p j q -> p (j q)")
                for j in range(2):
                    k.op("pe", lambda e: e.matmul(pSv[:, 0:256], lhsT=kT[:, h, j * 128:(j + 1) * 128], rhs=qT[:, h, 0:256], start=True, stop=True), reads=[kT, qT], writes=[pS])
                    k.op("act", lambda e: e.activation(pt[:, j, :], pSv[:, 0:256], AF.Exp, scale=scale), reads=[pS], writes=[pt])
                for j in range(2):
                    k.op("pe", lambda e: e.matmul(pom[:, 0:256], lhsT=vA[:, j, h * 64:(h + 1) * 64], rhs=pt[:, j, :], start=(j == 0), stop=(j == 1)), reads=[vA, pt], writes=[pom])
                for j in range(2):
                    k.op("pe", lambda e: e.matmul(pom[:, 256:512], lhsT=ones_bf[:, 0:64], rhs=pt[:, j, :], start=(j == 0), stop=(j == 1)), reads=[ones_bf, pt], writes=[pom])
                k.op("dve", lambda e: e.reciprocal(rs[:], pom[:, 256:512]), reads=[pom], writes=[rs])
                k.op("dve", lambda e: e.tensor_tensor(yo[:, h, 0:256], pom[:, 0:256], rs[:], ALU.mult), reads=[pom, rs], writes=[yo])
                it += 1
        k.dma("sp", yna_d[:], yo[:], reads=[yo], writes=[yna_d])
    k.barrier()


NCH = T // 64
RW_STOP = 99
class _Stop(Exception):
    pass
def chk(level):
    if RW_STOP == level:
        raise _Stop()


def rw_consts_host():
    i = np.arange(128) % 64
    lt = (i[None, :] < i[:, None]).astype(np.float32); le = (i[None, :] <= i[:, None]).astype(np.float32)
    gt = (i[None, :] > i[:, None]).astype(np.float32); ge = (i[None, :] >= i[:, None]).astype(np.float32)
    blk = ((np.arange(128)[:, None] // 64) == (np.arange(128)[None, :] // 64)).astype(np.float32)
    m2 = np.stack([np.concatenate([gt, ge], 1), np.concatenate([lt, le], 1)], 0)
    m1 = np.stack([lt, gt], 0)
    isel = np.concatenate([np.eye(64, dtype=np.float32)] * 2, 0)
    return m2, m1, blk, isel


def load_rw_w(k, l, st, W, ident_f):
    o = {}
    o["w2"] = k.sb("rww2", [128, 256], BF16, st); o["a2"] = k.sb("rwa2", [128, 256], BF16, st); o["g2"] = k.sb("rwg2", [128, 256], BF16, st)
    k.dma("pool", o["w2"][:], W["rw_w2"].t[l].rearrange("d k n -> (d k) n"), writes=[o["w2"]])
    k.dma("pool", o["a2"][:], W["rw_a2"].t[l].rearrange("d k n -> (d k) n"), writes=[o["a2"]])
    k.dma("pool", o["g2"][:], W["rw_g2"].t[l], writes=[o["g2"]])
    stage = k.sb("rwstage", [32, 128], F32, st)
    k.dma("sp", stage[0:18, :], W["rw_mu"].t[l].rearrange("d (c p) -> (d c) p", p=128), writes=[stage])
    k.dma("sp", stage[18:22, :], W["rw_w0"].t[l].rearrange("d (c p) -> (d c) p", p=128), writes=[stage])
    k.dma("sp", stage[22:26, :], W["rw_a0"].t[l].rearrange("d (c p) -> (d c) p", p=128), writes=[stage])
    k.dma("sp", stage[26:28, :], W["rw_kk"].t[l].rearrange("(c p) -> c p", p=128), writes=[stage])
    k.dma("sp", stage[28:30, :], W["rw_ka"].t[l].rearrange("(c p) -> c p", p=128), writes=[stage])
    k.dma("sp", stage[30:32, :], W["rw_rk"].t[l].rearrange("(c h) n -> c (h n)", c=2), writes=[stage])
    cols = k.sb("rwcols", [128, 48], F32, st)
    with ExitStack() as s2:
        pc = k.ps("rwpc", [128, 32], F32, s2)
        k.op("pe", lambda e: e.transpose(pc[:], stage[:], ident_f[0:32, 0:32]), reads=[stage, ident_f], writes=[pc])
        k.op("dve", lambda e: e.tensor_copy(cols[:, 0:32], pc[:]), reads=[pc], writes=[cols])
        k.barrier()
    k.op("dve", lambda e: e.tensor_tensor(cols[:, 32:41], cols[:, 0:9], cols[:, 9:18], ALU.add), reads=[cols], writes=[cols])
    k.op("dve", lambda e: e.tensor_scalar(cols[:, 32:41], cols[:, 32:41], -1.0, 1.0, op0=ALU.mult, op1=ALU.add), reads=[cols], writes=[cols])
    k.op("dve", lambda e: e.tensor_scalar(cols[:, 41:43], cols[:, 28:30], -1.0, 1.0, op0=ALU.mult, op1=ALU.add), reads=[cols], writes=[cols])
    o["cols"] = cols
    gn = k.sb("rwgn", [128, 2, 2, 64], F32, st)
    for P in range(2):
        for h in range(2):
            hd = 2 * P + h
            k.dma("sp", gn[h * 64:(h + 1) * 64, P, 0, :], W["rw_gn_w"].t[l:l + 1, hd * 64:(hd + 1) * 64].partition_broadcast(64), writes=[gn])
            k.dma("sp", gn[h * 64:(h + 1) * 64, P, 1, :], W["rw_gn_b"].t[l:l + 1, hd * 64:(hd + 1) * 64].partition_broadcast(64), writes=[gn])
    o["gn"] = gn
    return o


def phase_rwkv(k, pT_rw_d, RW, C, yrw_d):
    cols = RW["cols"]
    EM05 = float(np.exp(-0.5))
    with ExitStack() as st:
        tmpA = k.sb("rwtmpA", [128, T], F32, st); tmpB = k.sb("rwtmpB", [128, T], F32, st)

        def shifted(c, out):
            k.dma("sp", tmpA[:], pT_rw_d[c * 128:(c + 1) * 128, :], reads=[pT_rw_d], writes=[tmpA])
            k.op("act", lambda e: e.activation(out[:], tmpA[:], AF.Identity, scale=cols[:, 32 + c:33 + c]), reads=[tmpA, cols], writes=[out])
            for (dst, src, mc) in (((1, 256), (0, 255), c), ((257, T), (256, T - 1), c), ((0, 255), (1, 256), 9 + c), ((256, T - 1), (257, T), 9 + c)):
                k.op("dve", lambda e: e.scalar_tensor_tensor(out[:, dst[0]:dst[1]], tmpA[:, src[0]:src[1]], cols[:, mc:mc + 1], out[:, dst[0]:dst[1]], op0=ALU.mult, op1=ALU.add),
                     reads=[tmpA, cols, out], writes=[out])

        tw = k.sb("rwtw", [128, T], BF16, st); al = k.sb("rwal", [128, T], BF16, st); sg = k.sb("rwsg", [128, T], BF16, st)
        shifted(6, tmpB); k.op("act", lambda e: e.activation(tw[:], tmpB[:], AF.Tanh), reads=[tmpB], writes=[tw])
        shifted(7, tmpB); k.op("act", lambda e: e.copy(al[:], tmpB[:]), reads=[tmpB], writes=[al])
        shifted(8, tmpB); k.op("act", lambda e: e.activation(sg[:], tmpB[:], AF.Sigmoid), reads=[tmpB], writes=[sg])
        groups = [(0, 512), (512, 512), (1024, 512), (1536, 512), (2048, 256)]
        chk(1)
        for P in range(2 if RW_STOP > 1 else 0):
            with ExitStack() as sp:
                r_f = k.sb("rwr", [128, T], F32, sp); k_f = k.sb("rwk", [128, T], F32, sp); kk_f = k.sb("rwkk", [128, T], F32, sp)
                vst = k.sb("rwvst", [128, NCH, 64], BF16, sp)
                ccol = k.sb("rwccol", [128, NCH], F32, sp)
                o_all = k.sb("rwoall", [128, NCH, 64], F32, sp)
                shifted(0 + P, r_f); shifted(2 + P, k_f)
                sv = ExitStack()
                vexp = k.sb("rwvexp", [128, NCH, 128], BF16, sv)
                shifted(4 + P, tmpB)
                k.op("pool", lambda e: e.memset(vexp[:], 0.0), writes=[vexp])
                for h in range(2):
                    hs = slice(h * 64, (h + 1) * 64)
                    k.op("dve", lambda e: e.tensor_copy(vexp[hs, :, hs], tmpB[hs, :].rearrange("p (c s) -> p c s", s=64)), reads=[tmpB], writes=[vexp])
                k.op("dve", lambda e: e.tensor_scalar(kk_f[:], k_f[:], cols[:, 26 + P:27 + P], None, op0=ALU.mult), reads=[k_f, cols], writes=[kk_f])
                k.op("act", lambda e: e.activation(tmpB[:], kk_f[:], AF.Square), reads=[kk_f], writes=[tmpB])
                with ExitStack() as s2:
                    pg = [k.ps("rwpn", [128, 512], F32, s2) for _ in range(2)]
                    for gi, (o, n) in enumerate(groups):
                        ps = pg[gi % 2]
                        k.op("pe", lambda e: e.matmul(ps[:, 0:n], lhsT=C["blk"][:], rhs=tmpB[:, o:o + n], start=True, stop=True), reads=[C["blk"], tmpB], writes=[ps])
                        k.op("dve", lambda e: e.tensor_scalar(tmpA[:, o:o + n], ps[:, 0:n], 1e-12, None, op0=ALU.add), reads=[ps], writes=[tmpA])
                    k.op("act", lambda e: e.sqrt(tmpA[:], tmpA[:]), reads=[tmpA], writes=[tmpA])
                    k.op("dve", lambda e: e.reciprocal(tmpA[:], tmpA[:]), reads=[tmpA], writes=[tmpA])
                    k.op("dve", lambda e: e.tensor_tensor(kk_f[:], kk_f[:], tmpA[:], ALU.mult), reads=[kk_f, tmpA], writes=[kk_f])
                    rkexp = k.sb("rwrkexp", [128, NCH, 128], BF16, s2)
                    k.op("pool", lambda e: e.memset(rkexp[:], 0.0), writes=[rkexp])
                    k.op("dve", lambda e: e.scalar_tensor_tensor(tmpB[:], r_f[:], cols[:, 30 + P:31 + P], k_f[:], op0=ALU.mult, op1=ALU.mult), reads=[r_f, k_f, cols], writes=[tmpB])
                    for h in range(2):
                        hs = slice(h * 64, (h + 1) * 64)
                        k.op("dve", lambda e: e.tensor_copy(rkexp[hs, :, hs], tmpB[hs, :].rearrange("p (c s) -> p c s", s=64)), reads=[tmpB], writes=[rkexp])
                    pv = [k.ps("rwpv", [128, 8, 64], F32, s2) for _ in range(2)]
                    pcc = k.ps("rwpcc", [128, NCH], F32, s2)
                    for c0 in range(0, NCH, 8):
                        nn = min(8, NCH - c0); ps = pv[(c0 // 8) % 2]
                        for j in range(nn):
                            k.op("pe", lambda e: e.matmul(ps[:, j, :], lhsT=vexp[:, c0 + j, :], rhs=C["isel"][:], start=True, stop=True), reads=[vexp, C["isel"]], writes=[ps])
                        k.op("act", lambda e: e.copy(vst[:, c0:c0 + nn, :], ps[:, 0:nn, :]), reads=[ps], writes=[vst])
                    for c in range(NCH):
                        k.op("pe", lambda e: e.matmul(pcc[:, c:c + 1], lhsT=rkexp[:, c, :], rhs=C["ones_bf"][:, 0:1], start=True, stop=True), reads=[rkexp, C["ones_bf"]], writes=[pcc])
                    k.op("dve", lambda e: e.tensor_copy(ccol[:], pcc[:]), reads=[pcc], writes=[ccol])
                    k.barrier()
                sv.close()
                if RW_STOP == 2: continue

                for d in range(2):
                    with ExitStack() as sd:
                        if RW_STOP < 7 and (P, d) != (0, 0): continue
                        rw_dir(k, sd, P, d, RW, C, cols, tw, al, r_f, k_f, kk_f, vst, o_all, tmpA, tmpB, groups, EM05)
                        k.barrier()
                if RW_STOP < 7: continue
                with ExitStack() as s2:
                    s1 = k.sb("rws1", [128, NCH], F32, s2); s2_ = k.sb("rws2", [128, NCH], F32, s2); rstd = k.sb("rwrstd", [128, NCH], F32, s2)
                    sq = k.sb("rwsq", [128, NCH, 64], F32, s2); fin = k.sb("rwfin", [128, NCH, 64], F32, s2)
                    k.op("dve", lambda e: e.reduce_sum(s1[:], o_all[:], axis=AX.X), reads=[o_all], writes=[s1])
                    k.op("act", lambda e: e.activation(sq[:], o_all[:], AF.Square), reads=[o_all], writes=[sq])
                    k.op("dve", lambda e: e.reduce_sum(s2_[:], sq[:], axis=AX.X), reads=[sq], writes=[s2_])
                    k.op("dve", lambda e: e.tensor_scalar(s1[:], s1[:], 1.0 / 64, None, op0=ALU.mult), reads=[s1], writes=[s1])
                    k.op("dve", lambda e: e.tensor_tensor(rstd[:], s1[:], s1[:], ALU.mult), reads=[s1], writes=[rstd])
                    k.op("dve", lambda e: e.scalar_tensor_tensor(rstd[:], s2_[:], 1.0 / 64, rstd[:], op0=ALU.mult, op1=ALU.subtract), reads=[s2_, rstd], writes=[rstd])
                    k.op("dve", lambda e: e.tensor_scalar(rstd[:], rstd[:], 64e-5, None, op0=ALU.add), reads=[rstd], writes=[rstd])
                    k.op("act", lambda e: e.sqrt(rstd[:], rstd[:]), reads=[rstd], writes=[rstd])
                    k.op("dve", lambda e: e.reciprocal(rstd[:], rstd[:]), reads=[rstd], writes=[rstd])
                    yfm = k.sb("rwyfm", [64, 2, T], BF16, s2)
                    gfm = k.sb("rwgfm", [64, 2, T], F32, s2); vstf = k.sb("rwvstf", [128, NCH, 64], F32, s2)
                    k.op("pool", lambda e: e.tensor_copy(vstf[:], vst[:]), reads=[vst], writes=[vstf])
                    pgg = [k.ps("rwpg", [128, 512], F32, s2) for _ in range(2)]
                    cnt = 0
                    for h in range(2):
                        hd = 2 * P + h
                        for (o, n) in groups:
                            ps = pgg[cnt % 2]
                            k.op("pe", lambda e: e.matmul(ps[0:64, 0:n], lhsT=RW["g2"][:, hd * 64:(hd + 1) * 64], rhs=sg[:, o:o + n], start=True, stop=True), reads=[RW["g2"], sg], writes=[ps])
                            k.op("act", lambda e: e.copy(gfm[:, h, o:o + n], ps[0:64, 0:n]), reads=[ps], writes=[gfm])
                            cnt += 1
                    gnw = RW["gn"][:, P, 0, :]; gnb = RW["gn"][:, P, 1, :]
                    for c in range(NCH):
                        k.op("dve", lambda e: e.scalar_tensor_tensor(sq[:, c, :], o_all[:, c, :], s1[:, c:c + 1], gnw, op0=ALU.subtract, op1=ALU.mult), reads=[o_all, s1, RW["gn"]], writes=[sq])
                        k.op("dve", lambda e: e.scalar_tensor_tensor(sq[:, c, :], sq[:, c, :], rstd[:, c:c + 1], gnb, op0=ALU.mult, op1=ALU.add), reads=[sq, rstd, RW["gn"]], writes=[sq])
                        k.op("dve", lambda e: e.scalar_tensor_tensor(fin[:, c, :], vstf[:, c, :], ccol[:, c:c + 1], sq[:, c, :], op0=ALU.mult, op1=ALU.add), reads=[vstf, ccol, sq], writes=[fin])
                    pts = [k.ps("rwpt", [64, 4, 128], F32, s2) for _ in range(2)]
                    for c0 in range(0, NCH, 4):
                        ps = pts[(c0 // 4) % 2]
                        for j in range(4):
                            k.op("pe", lambda e: e.transpose(ps[:, j, :], fin[:, c0 + j, :], C["ident_f"][:]), reads=[fin, C["ident_f"]], writes=[ps])
                        for h in range(2):
                            hd = 2 * P + h
                            k.op("dve", lambda e: e.tensor_tensor(yfm[:, h, c0 * 64:(c0 + 4) * 64].rearrange("p (c s) -> p c s", s=64), ps[:, :, h * 64:(h + 1) * 64],
                                                                  gfm[:, h, c0 * 64:(c0 + 4) * 64].rearrange("p (c s) -> p c s", s=64), ALU.mult), reads=[ps, gfm], writes=[yfm])
                    k.dma("sp", yrw_d[:, 2 * P:2 * P + 2, :], yfm[:], reads=[yfm], writes=[yrw_d])
                    k.barrier()
    k.barrier()


def rw_dir(k, sd, P, d, RW, C, cols, tw, al, r_f, k_f, kk_f, vst, o_all, tmpA, tmpB, groups, EM05):
    dsl = slice(d * 64, (d + 1) * 64)
    AR = k.sb("rwAR", [128, NCH, 2, 128], BF16, sd); Bx = k.sb("rwBx", [128, NCH, 128], BF16, sd); Kx = k.sb("rwKx", [128, NCH, 128], BF16, sd)
    gc = k.sb("rwgc", [128, NCH], F32, sd); eoff = k.sb("rweoff", [128, NCH, 2], F32, sd)
    stmp = ExitStack()
    Gp = k.sb("rwGp", [128, T + 1], F32, stmp)
    b_f = k.sb("rwb", [128, T], F32, stmp); kd_f = tmpB
    eB = k.sb("rweB", [128, T], F32, stmp); eA = k.sb("rweA", [128, T], F32, stmp); eR = tmpA
    with ExitStack() as s2:
        pg = [k.ps("rwpl", [128, 512], F32, s2) for _ in range(2)]
        cnt = 0
        for (o, n) in groups:
            ps = pg[cnt % 2]; cnt += 1
            k.op("pe", lambda e: e.matmul(ps[:, 0:n], lhsT=RW["w2"][dsl, P * 128:(P + 1) * 128], rhs=tw[dsl, o:o + n], start=True, stop=True), reads=[RW["w2"], tw], writes=[ps])
            k.op("act", lambda e: e.activation(tmpA[:, o:o + n], ps[:, 0:n], AF.Sigmoid, bias=cols[:, 18 + 2 * d + P:19 + 2 * d + P]), reads=[ps, cols], writes=[tmpA])
            ps = pg[cnt % 2]; cnt += 1
            k.op("pe", lambda e: e.matmul(ps[:, 0:n], lhsT=RW["a2"][dsl, P * 128:(P + 1) * 128], rhs=al[dsl, o:o + n], start=True, stop=True), reads=[RW["a2"], al], writes=[ps])
            k.op("act", lambda e: e.activation(tmpB[:, o:o + n], ps[:, 0:n], AF.Sigmoid, bias=cols[:, 22 + 2 * d + P:23 + 2 * d + P]), reads=[ps, cols], writes=[tmpB])
        k.barrier()
    k.op("dve", lambda e: e.tensor_tensor(b_f[:], tmpB[:], kk_f[:], ALU.mult), reads=[tmpB, kk_f], writes=[b_f])
    k.op("dve", lambda e: e.tensor_scalar(tmpB[:], tmpB[:], cols[:, 28 + P:29 + P], cols[:, 41 + P:42 + P], op0=ALU.mult, op1=ALU.add), reads=[tmpB, cols], writes=[tmpB])
    k.op("dve", lambda e: e.tensor_tensor(kd_f[:], k_f[:], tmpB[:], ALU.mult), reads=[k_f, tmpB], writes=[kd_f])
    k.op("dve", lambda e: e.tensor_scalar(tmpA[:], tmpA[:], -EM05, None, op0=ALU.mult), reads=[tmpA], writes=[tmpA])
    k.op("dve", lambda e: e.memset(Gp[:, 0:1], 0.0), writes=[Gp])
    k.op("pool", lambda e: e.memset(eA[:], 1.0), writes=[eA])
    k.op("dve", lambda e: e.tensor_tensor_scan(Gp[:, 1:T + 1], eA[:], tmpA[:], 0.0, op0=ALU.mult, op1=ALU.add), reads=[tmpA, eA, Gp], writes=[Gp])
    Gc0 = Gp[:, 0:T].rearrange("p (c s) -> p c s", s=64)[:, :, 0]; Gc1 = Gp[:, 1:T + 1].rearrange("p (c s) -> p c s", s=64)[:, :, 63]
    k.op("dve", lambda e: e.tensor_tensor(gc[:], Gc1, Gc0, ALU.subtract), reads=[Gp], writes=[gc])
    k.op("act", lambda e: e.activation(gc[:], gc[:], AF.Exp), reads=[gc], writes=[gc])
    Eref = Gc1 if d == 0 else Gc0
    k.op("dve", lambda e: e.tensor_copy(eoff[:, :, 0], Eref), reads=[Gp], writes=[eoff])
    k.op("dve", lambda e: e.tensor_scalar(eoff[:, :, 1], Eref, -1.0, None, op0=ALU.mult), reads=[Gp], writes=[eoff])
    k.barrier()
    if RW_STOP == 3:
        stmp.close(); return
    for c in range(NCH):
        cs = slice(c * 64, (c + 1) * 64); g1 = Gp[:, 1 + c * 64:1 + (c + 1) * 64]; g0 = Gp[:, c * 64:(c + 1) * 64]
        pE = eoff[:, c, 0:1]; nE = eoff[:, c, 1:2]
        if d == 0:
            k.op("act", lambda e: e.activation(eB[:, cs], g1, AF.Exp, scale=-1.0, bias=pE), reads=[Gp, eoff], writes=[eB])
            k.op("act", lambda e: e.activation(eA[:, cs], g0, AF.Exp, scale=1.0, bias=nE), reads=[Gp, eoff], writes=[eA])
            k.op("act", lambda e: e.activation(eR[:, cs], g1, AF.Exp, scale=1.0, bias=nE), reads=[Gp, eoff], writes=[eR])
        else:
            k.op("act", lambda e: e.activation(eB[:, cs], g0, AF.Exp, scale=1.0, bias=nE), reads=[Gp, eoff], writes=[eB])
            k.op("act", lambda e: e.activation(eA[:, cs], g1, AF.Exp, scale=-1.0, bias=pE), reads=[Gp, eoff], writes=[eA])
            k.op("act", lambda e: e.activation(eR[:, cs], g0, AF.Exp, scale=-1.0, bias=pE), reads=[Gp, eoff], writes=[eR])
    k.op("pool", lambda e: e.memset(AR[:], 0.0), writes=[AR]); k.op("pool", lambda e: e.memset(Bx[:], 0.0), writes=[Bx]); k.op("pool", lambda e: e.memset(Kx[:], 0.0), writes=[Kx])
    v3 = lambda tle, hs: tle[hs, :].rearrange("p (c s) -> p c s", s=64)
    for h in range(2):
        hs = slice(h * 64, (h + 1) * 64)
        k.op("dve", lambda e: e.tensor_tensor(AR[hs, :, 0, hs], v3(kk_f, hs), v3(eA, hs), ALU.mult), reads=[kk_f, eA], writes=[AR])
        k.op("dve", lambda e: e.tensor_tensor(AR[hs, :, 1, hs], v3(r_f, hs), v3(eR, hs), ALU.mult), reads=[r_f, eR], writes=[AR])
        k.op("dve", lambda e: e.tensor_tensor(Bx[hs, :, hs], v3(b_f, hs), v3(eB, hs), ALU.mult), reads=[b_f, eB], writes=[Bx])
        k.op("dve", lambda e: e.tensor_tensor(Kx[hs, :, hs], v3(kd_f, hs), v3(eB, hs), ALU.mult), reads=[kd_f, eB], writes=[Kx])
    k.barrier(); stmp.close()
    if RW_STOP == 4: return
    Rhat = k.sb("rwRhat", [128, NCH, 128], BF16, sd); Tt = k.sb("rwTt", [128, NCH, 128], BF16, sd)
    OV = k.sb("rwOV", [128, NCH, 64], F32, sd); HV = k.sb("rwHV", [128, NCH, 64], F32, sd)
    m2 = C["m2"][d]; m1 = C["m1"][d]
    with ExitStack() as s2:
        NS = 2
        banks = [[k.ps("rwb" + n, [128, 512], F32, s2) for n in "ABCD"] for _ in range(NS)]
        sbs = []
        for _ in range(NS):
            sbs.append(dict(MbS=k.sb("rwMbS", [128, 256], BF16, s2), MkS=k.sb("rwMkS", [128, 256], BF16, s2),
                            Ls=[k.sb("rwLs", [128, 2, 128], F32, s2) for _ in range(2)],
                            tm=k.sb("rwtm", [128, 3, 128], BF16, s2),
                            Ys=[k.sb("rwY", [128, 192], F32, s2) for _ in range(2)], nZ=k.sb("rwnZ", [128, 192], BF16, s2)))
        idb = C["ident_bf"]; m2t = C["m2t"]; m1t = C["m1t"]

        def chunk_gen(c, sl):
            bA, bB, bC, bD = banks[sl]; S = sbs[sl]
            MbS = S["MbS"]; MkS = S["MkS"]; Ls = S["Ls"]; tm = S["tm"]; Ys = S["Ys"]; nZ = S["nZ"]
            pMb = bA[:, 0:256]; pMk = bA[:, 256:512]; pTr = bB[:, 0:384].rearrange("p (a b) -> p a b", a=3); pMa = bB[:, 384:512]
            pL = bC[:, 0:256]; pOH = bC[:, 256:384]; pY = bD[:, 0:192]; pRT = bD[:, 192:448]
            arc = AR[:, c].rearrange("p a b -> p (a b)")
            k.op("pe", lambda e: e.matmul(pMb, lhsT=Bx[:, c, :], rhs=arc, start=True, stop=True), reads=[Bx, AR], writes=[bA])
            k.op("pe", lambda e: e.matmul(pMk, lhsT=Kx[:, c, :], rhs=arc, start=True, stop=True), reads=[Kx, AR], writes=[bA])
            k.op("pe", lambda e: e.matmul(pMa, lhsT=AR[:, c, 0, :], rhs=Bx[:, c, :], start=True, stop=True), reads=[AR, Bx], writes=[bB])
            k.op("pe", lambda e: e.matmul(pTr[:, 0, :], lhsT=Bx[:, c, :], rhs=idb[:], start=True, stop=True), reads=[Bx, idb], writes=[bB])
            k.op("pe", lambda e: e.matmul(pTr[:, 1, :], lhsT=Kx[:, c, :], rhs=idb[:], start=True, stop=True), reads=[Kx, idb], writes=[bB])
            k.op("pe", lambda e: e.matmul(pTr[:, 2, :], lhsT=AR[:, c, 0, :], rhs=idb[:], start=True, stop=True), reads=[AR, idb], writes=[bB])
            L0 = Ls[0]
            k.op("dve", lambda e: e.tensor_tensor(L0[:, 1, :], pMb[:, 0:128], m2[:, 0:128], ALU.mult), reads=[bA, m2t], writes=[L0])
            k.op("dve", lambda e: e.tensor_tensor(MkS[:], pMk, m2, ALU.mult), reads=[bA, m2t], writes=[MkS])
            k.op("dve", lambda e: e.tensor_tensor(MbS[:], pMb, m2, ALU.mult), reads=[bA, m2t], writes=[MbS])
            k.op("act", lambda e: e.copy(tm[:], pTr), reads=[bB], writes=[tm])
            k.op("dve", lambda e: e.tensor_tensor(L0[:, 0, :], pMa, m1, ALU.mult), reads=[bB, m1t], writes=[L0])
            yield
            Y = Ys[0]
            k.op("pe", lambda e: e.matmul(pY[:, 0:64], lhsT=MkS[:, 0:128], rhs=vst[:, c, :], start=True, stop=True), reads=[MkS, vst], writes=[bD])
            k.op("act", lambda e: e.copy(Y[:, 0:128], tm[:, 2, :]), reads=[tm], writes=[Y])
            k.op("act", lambda e: e.copy(Y[:, 128:192], pY[:, 0:64]), reads=[bD], writes=[Y])
            yield
            cur = 0
            for lvl in range(6):
                Lc = Ls[cur % 2]; Yc = Ys[lvl % 2]; Yn = Ys[(lvl + 1) % 2]
                k.op("pe", lambda e: e.matmul(pY, lhsT=Lc[:, 1, :], rhs=Yc[:], start=True, stop=True), reads=[Lc, Yc], writes=[bD])
                if lvl < 5:
                    k.op("pe", lambda e: e.matmul(pL[:, 128:256], lhsT=Lc[:, 0, :], rhs=Lc[:, 1, :], start=True, stop=True), reads=[Lc], writes=[bC])
                    if lvl < 4:
                        k.op("pe", lambda e: e.matmul(pL[:, 0:128], lhsT=Lc[:, 1, :], rhs=Lc[:, 0, :], start=True, stop=True), reads=[Lc], writes=[bC])
                k.op("dve", lambda e: e.tensor_tensor(Yn[:], Yc[:], pY, ALU.subtract if lvl == 0 else ALU.add), reads=[Yc, bD], writes=[Yn])
                if lvl < 5:
                    Ln = Ls[(cur + 1) % 2]
                    if lvl < 4:
                        k.op("act", lambda e: e.copy(Ln[:].rearrange("p a b -> p (a b)"), pL), reads=[bC], writes=[Ln])
                    else:
                        k.op("act", lambda e: e.copy(Ln[:, 1, :], pL[:, 128:256]), reads=[bC], writes=[Ln])
                    cur += 1
                yield
            k.op("dve", lambda e: e.tensor_scalar(nZ[:], Ys[0][:], -1.0, None, op0=ALU.mult), reads=[Ys[0]], writes=[nZ])
            yield
            k.op("pe", lambda e: e.matmul(pRT[:, 0:128], lhsT=idb[:], rhs=AR[:, c, 1, :], start=True, stop=False), reads=[idb, AR], writes=[bD])
            k.op("pe", lambda e: e.matmul(pRT[:, 0:128], lhsT=nZ[:, 0:128], rhs=MbS[:, 128:256], start=False, stop=True), reads=[nZ, MbS], writes=[bD])
            k.op("pe", lambda e: e.matmul(pRT[:, 128:256], lhsT=idb[:], rhs=idb[:], start=True, stop=False), reads=[idb], writes=[bD])
            k.op("pe", lambda e: e.matmul(pRT[:, 128:256], lhsT=nZ[:, 0:128], rhs=tm[:, 0, :], start=False, stop=True), reads=[nZ, tm], writes=[bD])
            k.op("pe", lambda e: e.matmul(pOH[:, 0:64], lhsT=MkS[:, 128:256], rhs=vst[:, c, :], start=True, stop=False), reads=[MkS, vst], writes=[bC])
            k.op("pe", lambda e: e.matmul(pOH[:, 0:64], lhsT=MbS[:, 128:256], rhs=nZ[:, 128:192], start=False, stop=True), reads=[MbS, nZ], writes=[bC])
            k.op("pe", lambda e: e.matmul(pOH[:, 64:128], lhsT=tm[:, 1, :], rhs=vst[:, c, :], start=True, stop=False), reads=[tm, vst], writes=[bC])
            k.op("pe", lambda e: e.matmul(pOH[:, 64:128], lhsT=tm[:, 0, :], rhs=nZ[:, 128:192], start=False, stop=True), reads=[tm, nZ], writes=[bC])
            k.op("dve", lambda e: e.tensor_copy(Rhat[:, c, :], pRT[:, 0:128]), reads=[bD], writes=[Rhat])
            k.op("dve", lambda e: e.tensor_copy(Tt[:, c, :], pRT[:, 128:256]), reads=[bD], writes=[Tt])
            k.op("act", lambda e: e.copy(OV[:, c, :], pOH[:, 0:64]), reads=[bC], writes=[OV])
            k.op("act", lambda e: e.copy(HV[:, c, :], pOH[:, 64:128]), reads=[bC], writes=[HV])
            yield

        for c0 in range(0, NCH, NS):
            alive = [chunk_gen(c0 + i, i) for i in range(NS) if c0 + i < NCH]
            while alive:
                for g in list(alive):
                    try:
                        next(g)
                    except StopIteration:
                        alive.remove(g)
        k.barrier()
    with ExitStack() as s2:
        Hs = [k.sb("rwHs", [128, 64], BF16, s2) for _ in range(2)]; HVg = k.sb("rwHVg", [128, NCH, 64], F32, s2)
        pO = [k.ps("rwpO", [128, 64], F32, s2) for _ in range(2)]; pH = [k.ps("rwpH", [128, 64], F32, s2) for _ in range(2)]
        order = list(range(NCH)) if d == 0 else [3, 2, 1, 0] + list(range(NCH - 1, 3, -1))
        for i in range(NCH - 1):
            c = order[i]; cn = order[i + 1]
            k.op("act", lambda e: e.activation(HVg[:, c, :], HV[:, c, :], AF.Copy, scale=gc[:, cn:cn + 1]), reads=[HV, gc], writes=[HVg])
        if d == 1:
            k.op("pool", lambda e: e.tensor_tensor(OV[:], OV[:], o_all[:], ALU.add), reads=[OV, o_all], writes=[OV])
        c = order[0]
        k.op("dve", lambda e: e.tensor_copy(Hs[1][:], HVg[:, c, :]), reads=[HVg], writes=[Hs[1]])
        k.op("dve", lambda e: e.tensor_copy(o_all[:, c, :], OV[:, c, :]), reads=[OV], writes=[o_all])
        for i in range(1, NCH):
            c = order[i]; hs = Hs[i % 2]; hn = Hs[(i + 1) % 2]; po = pO[i % 2]; ph = pH[i % 2]
            k.op("pe", lambda e: e.matmul(ph[:], lhsT=Tt[:, c, :], rhs=hs[:], start=True, stop=True), reads=[Tt, hs], writes=[ph])
            k.op("pe", lambda e: e.matmul(po[:], lhsT=Rhat[:, c, :], rhs=hs[:], start=True, stop=True), reads=[Rhat, hs], writes=[po])
            if i < NCH - 1:
                cn = order[i + 1]
                k.op("dve", lambda e: e.scalar_tensor_tensor(hn[:], ph[:], gc[:, cn:cn + 1], HVg[:, c, :], op0=ALU.mult, op1=ALU.add), reads=[ph, gc, HVg], writes=[hn])
            k.op("dve", lambda e: e.tensor_tensor(o_all[:, c, :], po[:], OV[:, c, :], ALU.add), reads=[po, OV], writes=[o_all])
        k.barrier()


def load_consts(k, st, CD):
    C = {}
    C["ident_f"] = k.sb("identf", [128, 128], F32, st); C["ident_bf"] = k.sb("identbf", [128, 128], BF16, st)
    C["ones_bf"] = k.sb("onesbf", [128, 128], BF16, st)
    k.dma("sp", C["ident_f"][:], CD["ident"][:], writes=[C["ident_f"]])
    k.op("dve", lambda e: e.tensor_copy(C["ident_bf"][:], C["ident_f"][:]), reads=[C["ident_f"]], writes=[C["ident_bf"]])
    k.op("dve", lambda e: e.memset(C["ones_bf"][:], 1.0), writes=[C["ones_bf"]])
    C["cos_d"] = CD["cos"]; C["sin_d"] = CD["sin"]
    m2t = k.sb("rwm2", [128, 2, 256], F32, st); m1t = k.sb("rwm1", [128, 2, 128], F32, st)
    k.dma("sp", m2t[:], CD["rw_m2"].t.rearrange("d p c -> p d c"), writes=[m2t])
    k.dma("sp", m1t[:], CD["rw_m1"].t.rearrange("d p c -> p d c"), writes=[m1t])
    C["m2t"] = m2t; C["m1t"] = m1t; C["m2"] = [m2t[:, 0, :], m2t[:, 1, :]]; C["m1"] = [m1t[:, 0, :], m1t[:, 1, :]]
    C["blk"] = k.sb("rwblk", [128, 128], F32, st); C["isel"] = k.sb("rwisel", [128, 64], BF16, st)
    k.dma("sp", C["blk"][:], CD["rw_blk"][:], writes=[C["blk"]])
    k.dma("pool", C["isel"][:], CD["rw_isel"][:], writes=[C["isel"]])
    return C


def const_arrays():
    cos, sin = rope_tables()
    m2, m1, blk, isel = rw_consts_host()
    p = np.arange(128)
    return {"ident": np.eye(128, dtype=np.float32), "cos": cos, "sin": sin, "rw_m2": m2, "rw_m1": m1, "rw_blk": blk, "rw_isel": isel,
            "na_mask": na_mask_host(),
            "tri": (p[:, None] < p[None, :]).astype(np.float32), "iota64": (p % 32).astype(np.float32)[:, None],
            "rowbase": (np.arange(8)[None, :] * 128 + p[:, None]).astype(np.float32), "blk512": np.tile(512.0 * np.arange(NBLK_MAX, dtype=np.float32)[None, :], (128, 1))}


CONST_SHAPES = {"ident": [128, 128], "cos": [SEQ, 128], "sin": [SEQ, 128], "rw_m2": [2, 128, 256], "rw_m1": [2, 128, 128], "rw_blk": [128, 128],
                "rw_isel": [128, 64], "na_mask": [128, 4, 64], "tri": [128, 128], "iota64": [128, 1], "rowbase": [128, 8], "blk512": [128, NBLK_MAX]}


def phase_post(k, l, b, NB, need_ctx, x_src, x_src_tk, mod_d, yrw_d, ymla_d, yna_d, W, C, x1_d, hT_d, GT_d, col0, hTM_d=None, Gtm_d=None):
    with ExitStack() as st:
        wo_rw = k.sb("worw", [64, 4, D], BF16, st); wo_mla = k.sb("womla", [128, 4, D], BF16, st); wo_na = k.sb("wona", [64, 4, D], BF16, st)
        wv = W["w_out"].t[l]
        k.dma("pool", wo_rw[:], wv[0:256, :].rearrange("(h p) n -> p h n", p=64), writes=[wo_rw])
        k.dma("pool", wo_mla[:], wv[256:768, :].rearrange("(h p) n -> p h n", p=128), writes=[wo_mla])
        k.dma("pool", wo_na[:], wv[768:1024, :].rearrange("(h p) n -> p h n", p=64), writes=[wo_na])
        rwt = k.sb("routw", [128, 8, NE], BF16, st); rbt = k.sb("routb", [128, NE], F32, st)
        k.dma("pool", rwt[:], W["router_w"].t[l].rearrange("(kc p) n -> p kc n", p=128), writes=[rwt])
        k.dma("sp", rbt[:], W["router_b"].t[l:l + 1, :].partition_broadcast(128), writes=[rbt])
        bc = {}
        for nm, src in (("g1", W["ln1_g"].t[l:l + 1, :]), ("b1", W["ln1_b"].t[l:l + 1, :]),
                        ("ga_l", mod_d[l][b:b + 1, 2 * D:3 * D]), ("sf_l", mod_d[l][b:b + 1, 3 * D:4 * D]), ("cf_l", mod_d[l][b:b + 1, 4 * D:5 * D]),
                        ("ga_c", mod_d[l][NB:NB + 1, 2 * D:3 * D]), ("sf_c", mod_d[l][NB:NB + 1, 3 * D:4 * D]), ("cf_c", mod_d[l][NB:NB + 1, 4 * D:5 * D])):
            if nm.endswith("_c") and not need_ctx:
                continue
            tle = k.sb(nm, [128, D], F32, st)
            k.dma("sp", tle[:], src.partition_broadcast(128), reads=[mod_d], writes=[tle])
            bc[nm] = tle
        for nm in ("cf_l", "cf_c"):
            if nm in bc:
                tle = bc[nm]
                k.op("pool", lambda e: e.tensor_scalar(tle[:], tle[:], 1.0, None, op0=ALU.add), reads=[tle], writes=[tle])
        yr = k.sb("yrall", [64, 4, T], BF16, st); ym = k.sb("ymall", [128, 4, T], BF16, st); yn = k.sb("ynall", [64, 4, T], BF16, st)
        k.dma("sp", yr[:], yrw_d[:], reads=[yrw_d], writes=[yr])
        k.dma("sp", ym[:], ymla_d.t.rearrange("h p t -> p h t"), reads=[ymla_d], writes=[ym])
        k.dma("sp", yn[:], yna_d[:], reads=[yna_d], writes=[yn])
        xts = [k.sb("pxt", [128, D], F32, st) for _ in range(2)]
        t1 = k.sb("pt1", [128, D], F32, st); u = k.sb("pu", [128, D], F32, st); x1 = k.sb("px1", [128, D], F32, st); hb = k.sb("phb", [128, D], BF16, st)
        st6 = k.sb("pst6", [128, 2, 6], F32, st); mv = k.sb("pmv", [128, 2], F32, st); rstd = k.sb("prstd", [128, 1], F32, st)
        hT = k.sb("phT", [128, 8, 128], BF16, st)
        lg = k.sb("plg", [128, NE], F32, st); mx8 = k.sb("pmx8", [128, 8], F32, st); nm1 = k.sb("pnm1", [128, 1], F32, st)
        msk = k.sb("pmsk", [128, NE], F32, st); ex = k.sb("pex", [128, NE], F32, st); ssum = k.sb("pssum", [128, 1], F32, st); gts = k.sb("pgts", [32, 128], F32, st)
        py = [k.ps("ppy", [128, 512], F32, st) for _ in range(2)]
        pT = k.ps("ppT", [128, 8, 128], BF16, st); pl = k.ps("ppl", [128, NE], F32, st); pg = k.ps("ppg", [32, 128], F32, st)
        for tt in range(NTT):
            isctx = tt < 2
            if isctx and not need_ctx:
                continue
            sfx = "_c" if isctx else "_l"
            sl = slice(tt * 128, (tt + 1) * 128)
            xt = xts[tt % 2]
            k.dma("sp", xt[:], x_src(tt), reads=[x_src_tk], writes=[xt])
            for half in range(2):
                ps = py[half]; cs = slice(half * 512, (half + 1) * 512)
                ops = [(yr, wo_rw, h) for h in range(4)] + [(ym, wo_mla, h) for h in range(4)] + [(yn, wo_na, h) for h in range(4)]
                for i, (ya, wa, h) in enumerate(ops):
                    k.op("pe", lambda e: e.matmul(ps[:], lhsT=ya[:, h, sl], rhs=wa[:, h, cs], start=(i == 0), stop=(i == 11)), reads=[ya, wa], writes=[ps])
                k.op("dve", lambda e: e.tensor_tensor(t1[:, cs], ps[:], bc["ga" + sfx][:, cs], ALU.mult), reads=[ps, bc["ga" + sfx]], writes=[t1])
            k.op("dve", lambda e: e.scalar_tensor_tensor(u[:], xt[:], DN_ALPHA, t1[:], op0=ALU.mult, op1=ALU.add), reads=[xt, t1], writes=[u])
            ln_stats(k, u, st6, mv, rstd, 1e-5)
            k.op("dve", lambda e: e.scalar_tensor_tensor(t1[:], u[:], mv[:, 0:1], bc["g1"][:], op0=ALU.subtract, op1=ALU.mult), reads=[u, mv, bc["g1"]], writes=[t1])
            k.op("dve", lambda e: e.scalar_tensor_tensor(x1[:], t1[:], rstd[:], bc["b1"][:], op0=ALU.mult, op1=ALU.add), reads=[t1, rstd, bc["b1"]], writes=[x1])
            k.dma("sp", x1_d[sl, :], x1[:], reads=[x1], writes=[x1_d])
            ln_stats(k, x1, st6, mv, rstd, 1e-5)
            k.op("dve", lambda e: e.scalar_tensor_tensor(t1[:], x1[:], mv[:, 0:1], bc["cf" + sfx][:], op0=ALU.subtract, op1=ALU.mult), reads=[x1, mv, bc["cf" + sfx]], writes=[t1])
            k.op("dve", lambda e: e.scalar_tensor_tensor(hb[:], t1[:], rstd[:], bc["sf" + sfx][:], op0=ALU.mult, op1=ALU.add), reads=[t1, rstd, bc["sf" + sfx]], writes=[hb])
            for c in range(8):
                k.op("pe", lambda e: e.transpose(pT[:, c, :], hb[:, c * 128:(c + 1) * 128], C["ident_bf"][:]), reads=[hb, C["ident_bf"]], writes=[pT])
            k.op("act", lambda e: e.copy(hT[:], pT[:]), reads=[pT], writes=[hT])
            if hTM_d is not None:
                k.dma("sp", hTM_d.t[col0 + tt * 128:col0 + (tt + 1) * 128, :], hb[:], reads=[hb], writes=[hTM_d])
            else:
                k.dma("sp", hT_d.t[:, col0 + tt * 128:col0 + (tt + 1) * 128].rearrange("(kc p) t -> p kc t", p=128), hT[:], reads=[hT], writes=[hT_d])
            for kc in range(8):
                k.op("pe", lambda e: e.matmul(pl[:], lhsT=hT[:, kc, :], rhs=rwt[:, kc, :], start=(kc == 0), stop=(kc == 7)), reads=[hT, rwt], writes=[pl])
            k.op("dve", lambda e: e.tensor_tensor(lg[:], pl[:], rbt[:], ALU.add), reads=[pl, rbt], writes=[lg])
            k.op("dve", lambda e: e.max(mx8[:], lg[:]), reads=[lg], writes=[mx8])
            k.op("dve", lambda e: e.tensor_scalar(msk[:], lg[:], mx8[:, 3:4], None, op0=ALU.is_ge), reads=[lg, mx8], writes=[msk])
            k.op("dve", lambda e: e.tensor_scalar(nm1[:], mx8[:, 0:1], -1.0, None, op0=ALU.mult), reads=[mx8], writes=[nm1])
            k.op("act", lambda e: e.activation(ex[:], lg[:], AF.Exp, bias=nm1[:]), reads=[lg, nm1], writes=[ex])
            k.op("dve", lambda e: e.tensor_tensor(ex[:], ex[:], msk[:], ALU.mult), reads=[ex, msk], writes=[ex])
            k.op("dve", lambda e: e.reduce_sum(ssum[:], ex[:], axis=AX.X), reads=[ex], writes=[ssum])
            k.op("dve", lambda e: e.reciprocal(ssum[:], ssum[:]), reads=[ssum], writes=[ssum])
            k.op("dve", lambda e: e.tensor_scalar(ex[:], ex[:], ssum[:, 0:1], None, op0=ALU.mult), reads=[ex, ssum], writes=[ex])
            if Gtm_d is not None:
                k.dma("sp", Gtm_d.t[col0 + tt * 128:col0 + (tt + 1) * 128, :], ex[:], reads=[ex], writes=[Gtm_d])
            else:
                k.op("pe", lambda e: e.transpose(pg[:], ex[:], C["ident_f"][:]), reads=[ex, C["ident_f"]], writes=[pg])
                k.op("act", lambda e: e.copy(gts[:], pg[:]), reads=[pg], writes=[gts])
                k.dma("sp", GT_d[:, col0 + tt * 128:col0 + (tt + 1) * 128], gts[:], reads=[gts], writes=[GT_d])
    k.barrier()


def phase_moe(k, l, NB, groups, tile_io, mod_d, W, C, hT_d, GT_d):
    GP = 2
    with ExitStack() as st:
        bgl = k.sb("bgl", [128, NE, 8], F32, st); bln = k.sb("bln", [128, NE, 8], F32, st)
        bdn = k.sb("bdn", [NE, D], F32, st)
        k.dma("sp", bdn[:], W["b_dn"].t[l], writes=[bdn])
        with ExitStack() as s2:
            bgu = k.sb("bgu", [NE, 2 * D], F32, s2)
            pb = [k.ps("pbg", [128, NE], F32, s2) for _ in range(2)]
            k.dma("sp", bgu[:], W["b_gu"].t[l], writes=[bgu])
            for j in range(8):
                for par, dst in ((0, bgl), (1, bln)):
                    ps = pb[par]
                    k.op("pe", lambda e: e.transpose(ps[:], bgu[:, 256 * j + par:256 * (j + 1):2], C["ident_f"][0:NE, 0:NE]), reads=[bgu, C["ident_f"]], writes=[ps])
                    k.op("dve", lambda e: e.tensor_scalar(dst[:, :, j], ps[:], float(par), None, op0=ALU.add), reads=[ps], writes=[dst])
            k.barrier()
        gfs = [k.sb("gf%d" % i, [128, D], F32, st) for i in range(GP)]; gf_key = [None] * GP
        ln2g = k.sb("ln2g", [128, D], F32, st); ln2b = k.sb("ln2b", [128, D], F32, st)
        k.dma("sp", ln2g[:], W["ln2_g"].t[l:l + 1, :].partition_broadcast(128), writes=[ln2g])
        k.dma("sp", ln2b[:], W["ln2_b"].t[l:l + 1, :].partition_broadcast(128), writes=[ln2b])
        NU = 4
        units = [k.sb("wu%d" % i, [128, 8, 1024], BF16, st) for i in range(NU)]
        hTs = [k.sb("mhT%d" % i, [128, 8, 512], BF16, st) for i in range(GP)]
        accs = [k.sb("macc%d" % i, [128, 8, 512], F32, st) for i in range(GP)]
        gTs = [k.sb("mgT%d" % i, [NE, 512], F32, st) for i in range(GP)]
        acts = [k.sb("mact%d" % i, [128, 8, 512], BF16, st) for i in range(2)]
        gbcs = [k.sb("mgbc%d" % i, [128, 512], F32, st) for i in range(2)]
        tt_ = [k.sb("mt%d" % i, [128, 512], F32, st) for i in range(2)]
        ss_ = [k.sb("ms%d" % i, [128, 512], F32, st) for i in range(2)]
        uu_ = [k.sb("mu%d" % i, [128, 512], F32, st) for i in range(2)]
        psg = [k.ps("mpsg", [128, 512], F32, st) for _ in range(2)]
        psl = [k.ps("mpsl", [128, 512], F32, st) for _ in range(2)]
        psd = [k.ps("mpsd", [128, 512], F32, st) for _ in range(2)]
        st6 = k.sb("mst6", [128, 2, 6], F32, st); mv = k.sb("mmv", [128, 2], F32, st); rstd = k.sb("mrstd", [128, 1], F32, st)
        ucount = 0; it = 0; dcount = 0
        wgu = W["w_gu"].t[l]; wdn = W["w_dn"].t[l]
        for p0 in range(0, len(groups), GP):
            pg = groups[p0:p0 + GP]
            for gi, (col, n, bkey) in enumerate(pg):
                k.dma("sp", hTs[gi][:, :, 0:n], hT_d.t[:, col:col + n].rearrange("(kc p) t -> p kc t", p=128), reads=[hT_d], writes=[hTs[gi]])
                k.dma("sp", gTs[gi][:, 0:n], GT_d[:, col:col + n], reads=[GT_d], writes=[gTs[gi]])
                if gf_key[gi] != bkey:
                    row = NB if bkey == "c" else bkey
                    k.dma("sp", gfs[gi][:], mod_d[l][row:row + 1, 5 * D:6 * D].partition_broadcast(128), reads=[mod_d], writes=[gfs[gi]])
                    gf_key[gi] = bkey
            for e in range(NE):
                ua = units[ucount % NU]; ub = units[(ucount + 1) % NU]; ud = units[(ucount + 2) % NU]; ucount += 3
                k.dma("pool", ua[:], wgu[e][:, 0:1024].rearrange("(kc p) n -> p kc n", p=128), writes=[ua])
                k.dma("pool", ub[:], wgu[e][:, 1024:2048].rearrange("(kc p) n -> p kc n", p=128), writes=[ub])
                k.dma("pool", ud[:], wdn[e].rearrange("(kc p) n -> p kc n", p=128), writes=[ud])
                for gi, (col, n, bkey) in enumerate(pg):
                    hTg = hTs[gi]; acc = accs[gi]
                    act = acts[it % 2]; gbc = gbcs[it % 2]; it += 1
                    k.dma("act", gbc[:, 0:n], GT_d[e:e + 1, col:col + n].partition_broadcast(128), reads=[GT_d], writes=[gbc])
                    for j in range(8):
                        un = ua if j < 4 else ub; c0 = (j % 4) * 256
                        pg_ = psg[j % 2]; pl_ = psl[j % 2]; t_ = tt_[j % 2]; s_ = ss_[j % 2]; u_ = uu_[j % 2]
                        for kc in range(8):
                            k.op("pe", lambda e_: e_.matmul(pg_[:, 0:n], lhsT=un[:, kc, c0:c0 + 256:2], rhs=hTg[:, kc, 0:n], start=(kc == 0), stop=(kc == 7)), reads=[un, hTg], writes=[pg_])
                        for kc in range(8):
                            k.op("pe", lambda e_: e_.matmul(pl_[:, 0:n], lhsT=un[:, kc, c0 + 1:c0 + 256:2], rhs=hTg[:, kc, 0:n], start=(kc == 0), stop=(kc == 7)), reads=[un, hTg], writes=[pl_])
                        k.op("dve", lambda e_: e_.tensor_scalar(t_[:, 0:n], pg_[:, 0:n], bgl[:, e, j:j + 1], 7.0, op0=ALU.add, op1=ALU.min), reads=[pg_, bgl], writes=[t_])
                        k.op("act", lambda e_: e_.activation(s_[:, 0:n], t_[:, 0:n], AF.Sigmoid, scale=1.702), reads=[t_], writes=[s_])
                        k.op("dve", lambda e_: e_.tensor_scalar(u_[:, 0:n], pl_[:, 0:n], bln[:, e, j:j + 1], 8.0, op0=ALU.add, op1=ALU.min), reads=[pl_, bln], writes=[u_])
                        k.op("pool", lambda e_: e_.tensor_tensor(t_[:, 0:n], t_[:, 0:n], s_[:, 0:n], ALU.mult), reads=[t_, s_], writes=[t_])
                        k.op("pool", lambda e_: e_.tensor_scalar(u_[:, 0:n], u_[:, 0:n], -6.0, None, op0=ALU.max), reads=[u_], writes=[u_])
                        k.op("pool", lambda e_: e_.tensor_tensor(u_[:, 0:n], u_[:, 0:n], t_[:, 0:n], ALU.mult), reads=[u_, t_], writes=[u_])
                        k.op("dve", lambda e_: e_.tensor_tensor(act[:, j, 0:n], u_[:, 0:n], gbc[:, 0:n], ALU.mult), reads=[u_, gbc], writes=[act])
                    for oc in range(8):
                        pd = psd[dcount % 2]; dcount += 1
                        for j in range(8):
                            k.op("pe", lambda e_: e_.matmul(pd[:, 0:n], lhsT=ud[:, j, oc * 128:(oc + 1) * 128], rhs=act[:, j, 0:n], start=(j == 0), stop=(j == 7)), reads=[ud, act], writes=[pd])
                        if e == 0:
                            k.op("act", lambda e_: e_.copy(acc[:, oc, 0:n], pd[:, 0:n]), reads=[pd], writes=[acc])
                        else:
                            k.op("dve", lambda e_: e_.tensor_tensor(acc[:, oc, 0:n], acc[:, oc, 0:n], pd[:, 0:n], ALU.add), reads=[acc, pd], writes=[acc])
            for gi, (col, n, bkey) in enumerate(pg):
                acc = accs[gi]; gT = gTs[gi]; gf = gfs[gi]
                for oc in range(8):
                    pd = psd[dcount % 2]; dcount += 1
                    k.op("pe", lambda e_: e_.matmul(pd[:, 0:n], lhsT=bdn[:, oc * 128:(oc + 1) * 128], rhs=gT[:, 0:n], start=True, stop=True), reads=[bdn, gT], writes=[pd])
                    k.op("dve", lambda e_: e_.tensor_tensor(acc[:, oc, 0:n], acc[:, oc, 0:n], pd[:, 0:n], ALU.add), reads=[acc, pd], writes=[acc])
                for ti in range(n // 128):
                    x1_ap, x1_tk, out_ap, out_tk = tile_io(col + ti * 128)
                    for half in range(2):
                        ps = psg[half]; cs = slice(half * 512, (half + 1) * 512)
                        k.dma("sp", ss_[half][:], x1_ap[:, cs], reads=[x1_tk], writes=[ss_[half]])
                        for c in range(4):
                            oc = half * 4 + c
                            k.op("pe", lambda e_: e_.transpose(ps[:, c * 128:(c + 1) * 128], acc[:, oc, ti * 128:(ti + 1) * 128], C["ident_f"][:]), reads=[acc, C["ident_f"]], writes=[ps])
                        k.op("dve", lambda e_: e_.tensor_tensor(tt_[half][:], ps[:], gf[:, cs], ALU.mult), reads=[ps, gf], writes=[tt_[half]])
                        k.op("dve", lambda e_: e_.scalar_tensor_tensor(uu_[half][:], ss_[half][:], DN_ALPHA, tt_[half][:], op0=ALU.mult, op1=ALU.add), reads=[ss_[half], tt_[half]], writes=[uu_[half]])
                        k.op("dve", lambda e_: e_.bn_stats(st6[:, half, :], uu_[half][:]), reads=[uu_[half]], writes=[st6])
                    k.op("dve", lambda e_: e_.bn_aggr(mv[:], st6[:].rearrange("p a b -> p (a b)")), reads=[st6], writes=[mv])
                    k.op("dve", lambda e_: e_.tensor_scalar(rstd[:], mv[:, 1:2], 1e-5, None, op0=ALU.add), reads=[mv], writes=[rstd])
                    k.op("act", lambda e_: e_.sqrt(rstd[:], rstd[:]), reads=[rstd], writes=[rstd])
                    k.op("dve", lambda e_: e_.reciprocal(rstd[:], rstd[:]), reads=[rstd], writes=[rstd])
                    for half in range(2):
                        cs = slice(half * 512, (half + 1) * 512)
                        k.op("dve", lambda e_: e_.scalar_tensor_tensor(tt_[half][:], uu_[half][:], mv[:, 0:1], ln2g[:, cs], op0=ALU.subtract, op1=ALU.mult), reads=[uu_[half], mv, ln2g], writes=[tt_[half]])
                        k.op("dve", lambda e_: e_.scalar_tensor_tensor(uu_[half][:], tt_[half][:], rstd[:], ln2b[:, cs], op0=ALU.mult, op1=ALU.add), reads=[tt_[half], rstd, ln2b], writes=[uu_[half]])
                        k.dma("sp", out_ap[:, cs], uu_[half][:], reads=[uu_[half]], writes=[out_tk])
    k.barrier()


def phase_moe_sparse(k, l, NB, need_ctx, tile_io, mod_d, W, C, CD, hTM_d, Gtm_d, Hs_d, Ys_d):
    IOA = bass.IndirectOffsetOnAxis
    ntb = NTT if need_ctx else NTT - 2
    NT = NB * ntb; NTOK = NT * 128
    NBLK = (4 * NTOK + NE * 511) // 512
    assert NBLK <= NBLK_MAX
    BIG = 65536.0
    rows = [b * T + (0 if need_ctx else NCTX) for b in range(NB)]
    tile_row = [rows[b] + j * 128 for b in range(NB) for j in range(ntb)]
    tile_key = [("c" if (need_ctx and j < 2) else b) for b in range(NB) for j in range(ntb)]
    with ExitStack() as st:
        dest_i = k.sb("mdesti", [128, NT, 4], I32, st); gk = k.sb("mgk", [128, NT, 4], F32, st)
        widx = k.sb("mwidx", [128, NBLK, 8], I32, st); be = k.sb("mbe", [128, NBLK], F32, st)
        iota64 = k.sb("miota", [128, 1], F32, st)
        k.dma("sp", iota64[:], CD["iota64"][:], writes=[iota64])
        with ExitStack() as s2:
            rowb = k.sb("mrowb", [128, 8], F32, s2); blk512 = k.sb("mblk512", [128, NBLK], F32, s2)
            trif = k.sb("mtrif", [128, 128], F32, s2); tri = k.sb("mtri", [128, 128], BF16, s2)
            k.dma("sp", rowb[:], CD["rowbase"][:], writes=[rowb]); k.dma("sp", blk512[:], CD["blk512"][:, 0:NBLK], writes=[blk512])
            k.dma("sp", trif[:], CD["tri"][:], writes=[trif])
            k.op("dve", lambda e: e.tensor_scalar(rowb[:], rowb[:], float(l * NE * D), None, op0=ALU.add), reads=[rowb], writes=[rowb])
            k.op("dve", lambda e: e.tensor_copy(tri[:], trif[:]), reads=[trif], writes=[tri])
            G_all = k.sb("mGall", [128, NT, NE], F32, s2); maskf = k.sb("mmaskf", [128, NT, NE], F32, s2); mask = k.sb("mmask", [128, NT, NE], BF16, s2)
            rank = k.sb("mrank", [128, NT, NE], F32, s2); tot = k.sb("mtot", [128, NT, NE], F32, s2); off = k.sb("moff", [128, NT, NE], F32, s2)
            val = k.sb("mval", [128, NT, NE], F32, s2)
            cnt = k.sb("mcnt", [128, NE], F32, s2); nbe = k.sb("mnbe", [128, NE], F32, s2); start = k.sb("mstart", [128, NE + 1], F32, s2)
            mx8 = k.sb("mmx8", [128, 8], F32, s2); tmp = k.sb("mtmp", [128, NE], F32, s2); destf = k.sb("mdestf", [128, NT, 4], F32, s2)
            widxf = k.sb("mwidxf", [128, NBLK, 8], F32, s2)
            z = k.sb("mz", [128, 4 * D], BF16, s2)
            k.op("pool", lambda e: e.memset(z[:], 0.0), writes=[z])
            for blk in range(NBLK):
                k.dma("sp" if blk % 2 else "act", Hs_d.t[blk * 512:(blk + 1) * 512, :].rearrange("(p r) d -> p (r d)", p=128), z[:], reads=[z])
            for b in range(NB):
                k.dma("sp", G_all[:, b * ntb:(b + 1) * ntb, :], Gtm_d.t[rows[b]:rows[b] + ntb * 128, :].rearrange("(i p) e -> p i e", p=128), reads=[Gtm_d], writes=[G_all])
            k.op("dve", lambda e: e.tensor_scalar(maskf[:], G_all[:], 0.0, None, op0=ALU.is_gt), reads=[G_all], writes=[maskf])
            k.op("dve", lambda e: e.tensor_copy(mask[:], maskf[:]), reads=[maskf], writes=[mask])
            pP = [k.ps("mpP", [128, 512], F32, s2) for _ in range(2)]; pTt = [k.ps("mpTt", [128, 512], F32, s2) for _ in range(2)]
            mflat = mask[:].rearrange("p i e -> p (i e)"); rflat = rank[:].rearrange("p i e -> p (i e)"); tflat = tot[:].rearrange("p i e -> p (i e)")
            ncols = NT * NE
            for c in range((ncols + 511) // 512):
                c0 = c * 512; n = min(512, ncols - c0); pa = pP[c % 2]; pb = pTt[c % 2]
                k.op("pe", lambda e: e.matmul(pa[:, 0:n], lhsT=tri[:], rhs=mflat[:, c0:c0 + n], start=True, stop=True), reads=[tri, mask], writes=[pa])
                k.op("pe", lambda e: e.matmul(pb[:, 0:n], lhsT=C["ones_bf"][:], rhs=mflat[:, c0:c0 + n], start=True, stop=True), reads=[C["ones_bf"], mask], writes=[pb])
                k.op("dve", lambda e: e.tensor_copy(rflat[:, c0:c0 + n], pa[:, 0:n]), reads=[pa], writes=[rank])
                k.op("act", lambda e: e.copy(tflat[:, c0:c0 + n], pb[:, 0:n]), reads=[pb], writes=[tot])
            k.op("dve", lambda e: e.reduce_sum(cnt[:], tot[:].rearrange("p i e -> p e i"), axis=AX.X), reads=[tot], writes=[cnt])
            k.op("dve", lambda e: e.memset(nbe[:], 0.0), writes=[nbe])
            for j in range(NTOK // 512):
                k.op("dve", lambda e: e.scalar_tensor_tensor(nbe[:], cnt[:], 512.0 * j, nbe[:], op0=ALU.is_gt, op1=ALU.add), reads=[cnt, nbe], writes=[nbe])
            k.op("dve", lambda e: e.tensor_scalar(nbe[:], nbe[:], 512.0, None, op0=ALU.mult), reads=[nbe], writes=[nbe])
            k.op("dve", lambda e: e.memset(start[:, 0:1], 0.0), writes=[start])
            for ee in range(NE):
                k.op("dve", lambda e: e.tensor_tensor(start[:, ee + 1:ee + 2], start[:, ee:ee + 1], nbe[:, ee:ee + 1], ALU.add), reads=[start, nbe], writes=[start])
            k.op("dve", lambda e: e.tensor_copy(off[:, 0, :], start[:, 0:NE]), reads=[start], writes=[off])
            for i in range(1, NT):
                k.op("dve", lambda e: e.tensor_tensor(off[:, i, :], off[:, i - 1, :], tot[:, i - 1, :], ALU.add), reads=[off, tot], writes=[off])
            k.op("dve", lambda e: e.tensor_tensor(rank[:], rank[:], off[:], ALU.add), reads=[rank, off], writes=[rank])
            k.op("dve", lambda e: e.tensor_scalar(rank[:], rank[:], -1.0, BIG, op0=ALU.mult, op1=ALU.add), reads=[rank], writes=[rank])
            k.op("dve", lambda e: e.tensor_tensor(val[:], rank[:], maskf[:], ALU.mult), reads=[rank, maskf], writes=[val])
            for i in range(NT):
                k.op("dve", lambda e: e.max(mx8[:], val[:, i, :]), reads=[val], writes=[mx8])
                k.op("dve", lambda e: e.tensor_scalar(destf[:, i, :], mx8[:, 0:4], -1.0, BIG, op0=ALU.mult, op1=ALU.add), reads=[mx8], writes=[destf])
                for kk in range(4):
                    k.op("dve", lambda e: e.scalar_tensor_tensor(tmp[:], val[:, i, :], mx8[:, kk:kk + 1], G_all[:, i, :], op0=ALU.is_equal, op1=ALU.mult), reads=[val, mx8, G_all], writes=[tmp])
                    k.op("dve", lambda e: e.reduce_sum(gk[:, i, kk:kk + 1], tmp[:], axis=AX.X), reads=[tmp], writes=[gk])
            k.op("dve", lambda e: e.tensor_copy(dest_i[:], destf[:]), reads=[destf], writes=[dest_i])
            k.op("dve", lambda e: e.memset(be[:], 0.0), writes=[be])
            for ee in range(NE):
                k.op("dve", lambda e: e.scalar_tensor_tensor(be[:], blk512[:], start[:, ee + 1:ee + 2], be[:], op0=ALU.is_ge, op1=ALU.add), reads=[blk512, start, be], writes=[be])
            k.op("dve", lambda e: e.tensor_scalar(be[:], be[:], float(NE - 1), None, op0=ALU.min), reads=[be], writes=[be])
            for kc in range(8):
                k.op("dve", lambda e: e.tensor_scalar(widxf[:, :, kc], be[:], 1024.0, rowb[:, kc:kc + 1], op0=ALU.mult, op1=ALU.add), reads=[be, rowb], writes=[widxf])
            k.op("dve", lambda e: e.tensor_copy(widx[:], widxf[:]), reads=[widxf], writes=[widx])
            k.barrier()
            hts = [k.sb("mht", [128, D], BF16, s2) for _ in range(2)]
            for i in range(NT):
                ht = hts[i % 2]
                k.dma("sp", ht[:], hTM_d.t[tile_row[i]:tile_row[i] + 128, :], reads=[hTM_d], writes=[ht])
                for kk in range(4):
                    k.idma(Hs_d.t[:, :], ht[:], out_offset=IOA(ap=dest_i[:, i, kk:kk + 1], axis=0), reads=[dest_i, ht])
            k.barrier()
        with ExitStack() as s3:
            bguHL = k.sb("mbguhl", [64, 2 * D], BF16, s3); bdnHL = k.sb("mbdnhl", [64, D], BF16, s3); iotaE = k.sb("miotaE", [64, 512], F32, s3)
            with ExitStack() as s4:
                for (src, dst, n) in ((W["b_gu"].t[l], bguHL, 2 * D), (W["b_dn"].t[l], bdnHL, D)):
                    bf_ = k.sb("mbf", [64, n], F32, s4); hif = k.sb("mhif", [64, n], F32, s4)
                    k.dma("sp", bf_[0:32, :], src, writes=[bf_]); k.dma("sp", bf_[32:64, :], src, writes=[bf_])
                    k.op("dve", lambda e: e.tensor_copy(dst[:], bf_[:]), reads=[bf_], writes=[dst])
                    k.op("dve", lambda e: e.tensor_copy(hif[:], dst[:]), reads=[dst], writes=[hif])
                    k.op("dve", lambda e: e.tensor_tensor(hif[32:64, :], bf_[32:64, :], hif[32:64, :], ALU.subtract), reads=[bf_, hif], writes=[hif])
                    k.op("dve", lambda e: e.tensor_copy(dst[32:64, :], hif[32:64, :]), reads=[hif], writes=[dst])
                k.op("dve", lambda e: e.memset(iotaE[:], 0.0), writes=[iotaE])
                k.op("dve", lambda e: e.tensor_scalar(iotaE[:], iotaE[:], iota64[0:64, 0:1], None, op0=ALU.add), reads=[iotaE, iota64], writes=[iotaE])
                k.barrier()
            wgs = [[k.sb("mwg", [128, 2 * D], BF16, s3) for _ in range(8)] for _ in range(2)]
            wds = [[k.sb("mwd", [128, D], BF16, s3) for _ in range(8)] for _ in range(2)]
            hss = [k.sb("mhs", [128, 4, D], BF16, s3) for _ in range(2)]; hTs = [k.sb("mhT", [128, 8, 512], BF16, s3) for _ in range(1)]
            acts = [k.sb("mact", [128, 8, 512], BF16, s3) for _ in range(1)]; ohs = [k.sb("moh", [64, 512], BF16, s3) for _ in range(2)]
            tt_ = [k.sb("mt", [128, 512], F32, s3) for _ in range(2)]; ss_ = [k.sb("ms", [128, 512], F32, s3) for _ in range(2)]; uu_ = [k.sb("mu", [128, 512], F32, s3) for _ in range(2)]
            ysb = [k.sb("mysb", [128, D], F32, s3) for _ in range(2)]
            pT = k.ps("mpT", [128, 8, 128], BF16, s3)
            psg = [k.ps("mpsg", [128, 512], F32, s3) for _ in range(2)]; psl = [k.ps("mpsl", [128, 512], F32, s3) for _ in range(2)]; psd = [k.ps("mpsd", [128, 512], F32, s3) for _ in range(2)]
            wgu2d = W["w_gu"].t.rearrange("l e r n -> (l e r) n"); wdn2d = W["w_dn"].t.rearrange("l e r n -> (l e r) n")

            def fetch(blk):
                wg = wgs[blk % 2]; wd = wds[blk % 2]
                for kc in range(8):
                    k.idma(wg[kc][:], wgu2d, in_offset=IOA(ap=widx[:, blk, kc:kc + 1], axis=0), reads=[widx], writes=[wg[kc]])
                for kc in range(8):
                    k.idma(wd[kc][:], wdn2d, in_offset=IOA(ap=widx[:, blk, kc:kc + 1], axis=0), reads=[widx], writes=[wd[kc]])
                k.dma("sp", hss[blk % 2][:], Hs_d.t[blk * 512:(blk + 1) * 512, :].rearrange("(r p) d -> p r d", p=128), writes=[hss[blk % 2]])
            fetch(0)
            yc = 0; dc = 0
            for blk in range(NBLK):
                if blk + 1 < NBLK:
                    fetch(blk + 1)
                wg = wgs[blk % 2]; wd = wds[blk % 2]; hs = hss[blk % 2]; hT = hTs[0]; act = acts[0]; oh = ohs[blk % 2]
                for r in range(4):
                    for c in range(8):
                        k.op("pe", lambda e: e.transpose(pT[:, c, :], hs[:, r, c * 128:(c + 1) * 128], C["ident_bf"][:]), reads=[hs, C["ident_bf"]], writes=[pT])
                    if r % 2:
                        k.op("act", lambda e: e.copy(hT[:, :, r * 128:(r + 1) * 128], pT[:]), reads=[pT], writes=[hT])
                    else:
                        k.op("dve", lambda e: e.tensor_copy(hT[:, :, r * 128:(r + 1) * 128], pT[:]), reads=[pT], writes=[hT])
                k.op("dve", lambda e: e.tensor_scalar(oh[:], iotaE[:], be[0:64, blk:blk + 1], None, op0=ALU.is_equal), reads=[iotaE, be], writes=[oh])
                for j in range(8):
                    pg_ = psg[j % 2]; pl_ = psl[j % 2]; t_ = tt_[j % 2]; s_ = ss_[j % 2]; u_ = uu_[j % 2]
                    for (ps_, o_) in ((pg_, 0), (pl_, 1)):
                        for kc in range(8):
                            k.op("pe", lambda e: e.matmul(ps_[:], lhsT=wg[kc][:, 256 * j + o_:256 * (j + 1):2], rhs=hT[:, kc, :], start=(kc == 0), stop=False), reads=[wg[kc], hT], writes=[ps_])
                        k.op("pe", lambda e: e.matmul(ps_[:], lhsT=bguHL[:, 256 * j + o_:256 * (j + 1):2], rhs=oh[:], start=False, stop=True), reads=[bguHL, oh], writes=[ps_])
                    k.op("dve", lambda e: e.tensor_scalar(t_[:], pg_[:], 7.0, None, op0=ALU.min), reads=[pg_], writes=[t_])
                    k.op("act", lambda e: e.activation(s_[:], t_[:], AF.Sigmoid, scale=1.702), reads=[t_], writes=[s_])
                    k.op("dve", lambda e: e.tensor_scalar(u_[:], pl_[:], 7.0, -7.0, op0=ALU.min, op1=ALU.max), reads=[pl_], writes=[u_])
                    k.op("dve", lambda e: e.tensor_tensor(t_[:], t_[:], s_[:], ALU.mult), reads=[t_, s_], writes=[t_])
                    k.op("dve", lambda e: e.scalar_tensor_tensor(act[:, j, :], u_[:], 1.0, t_[:], op0=ALU.add, op1=ALU.mult), reads=[u_, t_], writes=[act])
                for r in range(4):
                    y = ysb[yc % 2]; yc += 1
                    for half in range(2):
                        pd = psd[dc % 2]; dc += 1; cs = slice(half * 512, (half + 1) * 512)
                        for j in range(8):
                            k.op("pe", lambda e: e.matmul(pd[:], lhsT=act[:, j, r * 128:(r + 1) * 128], rhs=wd[j][:, cs], start=(j == 0), stop=False), reads=[act, wd[j]], writes=[pd])
                        k.op("pe", lambda e: e.matmul(pd[:], lhsT=oh[:, r * 128:(r + 1) * 128], rhs=bdnHL[:, cs], start=False, stop=True), reads=[oh, bdnHL], writes=[pd])
                        if half:
                            k.op("act", lambda e: e.copy(y[:, cs], pd[:]), reads=[pd], writes=[y])
                        else:
                            k.op("dve", lambda e: e.tensor_copy(y[:, cs], pd[:]), reads=[pd], writes=[y])
                    k.dma("sp", Ys_d.t[blk * 512 + r * 128:blk * 512 + (r + 1) * 128, :], y[:], reads=[y])
            k.barrier()
        with ExitStack() as s5:
            gfs = {}
            for key in sorted(set(tile_key), key=str):
                row = NB if key == "c" else key
                gfs[key] = k.sb("mgf", [128, D], F32, s5)
                k.dma("sp", gfs[key][:], mod_d[l][row:row + 1, 5 * D:6 * D].partition_broadcast(128), reads=[mod_d], writes=[gfs[key]])
            ln2g = k.sb("ln2g", [128, D], F32, s5); ln2b = k.sb("ln2b", [128, D], F32, s5)
            k.dma("sp", ln2g[:], W["ln2_g"].t[l:l + 1, :].partition_broadcast(128), writes=[ln2g])
            k.dma("sp", ln2b[:], W["ln2_b"].t[l:l + 1, :].partition_broadcast(128), writes=[ln2b])
            yks = [[k.sb("myk", [128, D], F32, s5) for _ in range(4)] for _ in range(2)]
            x1s = [k.sb("mx1", [128, D], F32, s5) for _ in range(2)]
            acc = k.sb("macc", [128, D], F32, s5); uu = k.sb("muu", [128, D], F32, s5); oo = [k.sb("moo", [128, D], F32, s5) for _ in range(2)]
            st6 = k.sb("mst6", [128, 2, 6], F32, s5); mv = k.sb("mmv", [128, 2], F32, s5); rstd = k.sb("mrstd", [128, 1], F32, s5)
            for i in range(NT):
                yk = yks[i % 2]; x1t = x1s[i % 2]; o_ = oo[i % 2]; gf = gfs[tile_key[i]]
                x1_ap, x1_tk, out_ap, out_tk = tile_io(tile_row[i])
                for kk in range(4):
                    k.idma(yk[kk][:], Ys_d.t[:, :], in_offset=IOA(ap=dest_i[:, i, kk:kk + 1], axis=0), reads=[dest_i], writes=[yk[kk]])
                k.dma("sp", x1t[:], x1_ap, writes=[x1t])
                k.op("dve", lambda e: e.tensor_scalar(acc[:], yk[0][:], gk[:, i, 0:1], None, op0=ALU.mult), reads=[yk[0], gk], writes=[acc])
                for kk in range(1, 4):
                    k.op("dve", lambda e: e.scalar_tensor_tensor(acc[:], yk[kk][:], gk[:, i, kk:kk + 1], acc[:], op0=ALU.mult, op1=ALU.add), reads=[yk[kk], gk, acc], writes=[acc])
                k.op("pool", lambda e: e.tensor_tensor(acc[:], acc[:], gf[:], ALU.mult), reads=[acc, gf], writes=[acc])
                k.op("dve", lambda e: e.scalar_tensor_tensor(uu[:], x1t[:], DN_ALPHA, acc[:], op0=ALU.mult, op1=ALU.add), reads=[x1t, acc], writes=[uu])
                ln_stats(k, uu, st6, mv, rstd, 1e-5)
                k.op("dve", lambda e: e.scalar_tensor_tensor(acc[:], uu[:], mv[:, 0:1], ln2g[:], op0=ALU.subtract, op1=ALU.mult), reads=[uu, mv, ln2g], writes=[acc])
                k.op("dve", lambda e: e.scalar_tensor_tensor(o_[:], acc[:], rstd[:], ln2b[:], op0=ALU.mult, op1=ALU.add), reads=[acc, rstd, ln2b], writes=[o_])
                k.dma("sp", out_ap, o_[:], reads=[o_])
            k.barrier()
    k.barrier()


WSHAPES = {"ada_w": [2, D, 6 * D], "ada_b": [2, 6 * D], "w_in": [2, D, IN_COLS],
           "rw_mu": [2, 2, 1152], "rw_w0": [2, 2, 256], "rw_w2": [2, 2, 64, 256], "rw_a0": [2, 2, 256], "rw_a2": [2, 2, 64, 256], "rw_g2": [2, 128, 256],
           "rw_kk": [2, 256], "rw_ka": [2, 256], "rw_rk": [2, 4, 64], "rw_gn_w": [2, 256], "rw_gn_b": [2, 256],
           "mla_q_norm": [2, 256], "mla_kv_norm": [2, 128], "mla_w_uq": [2, 256, 768], "mla_w_ukv": [2, 128, 1024],
           "w_out": [2, D, D], "ln1_g": [2, D], "ln1_b": [2, D], "router_w": [2, D, NE], "router_b": [2, NE],
           "w_gu": [2, NE, D, 2 * D], "b_gu": [2, NE, 2 * D], "w_dn": [2, NE, D, D], "b_dn": [2, NE, D], "ln2_g": [2, D], "ln2_b": [2, D]}


def build_nc(NB, debug=False, layers=(0, 1), moe=True, sparse=True, phases="BCDEF"):
    nc = bass.Bass("TRN2", target_bir_lowering=False)
    with ExitStack() as st:
        k = K(nc, st)
        kin = "ExternalInput"; sk = "ExternalOutput" if debug else "Internal"
        CD = {n: k.dram("c_" + n, sh, F32, kin) for n, sh in CONST_SHAPES.items()}
        W = {n: k.dram(n, sh, F32, kin) for n, sh in WSHAPES.items()}
        xin = k.dram("xin", [NB, T, D], F32, kin)
        condT = k.dram("condT", [128, 8, NB + 1], F32, kin)
        na_bias = k.dram("na_bias", [2, 128, 4, 8, 4, 64], F32, kin)
        out_d = k.dram("out", [NB, SEQ, D], F32, "ExternalOutput")
        mod_d = k.dram("mod_d", [2, NB + 1, 6 * D], F32, sk)
        pT_rw_d = k.dram("pT_rw_d", [RW_COLS, T], F32, sk); pT_na_d = k.dram("pT_na_d", [512, T], BF16, sk); p_tm_d = k.dram("p_tm_d", [T, 704], F32, sk)
        yrw_d = k.dram("yrw_d", [64, 4, T], BF16, sk); ymla_d = k.dram("ymla_d", [4, 128, T], BF16, sk); yna_d = k.dram("yna_d", [64, 4, T], BF16, sk)
        x1_d = k.dram("x1_d", [NB * T, D], F32, sk); x2_d = k.dram("x2_d", [NB, T, D], F32, sk)
        hT_d = k.dram("hT_d", [D, NB * T], BF16, sk); GT_d = k.dram("GT_d", [NE, NB * T], F32, sk)
        hTM_d = k.dram("hTM_d", [NB * T, D], BF16, sk); Gtm_d = k.dram("Gtm_d", [NB * T, NE], F32, sk)
        nblk0 = (4 * NB * T + NE * 511) // 512
        Hs_d = k.dram("Hs_d", [nblk0 * 512, D], BF16, "Internal"); Ys_d = k.dram("Ys_d", [nblk0 * 512, D], F32, "Internal")
        C = load_consts(k, st, CD)
        k.barrier()
        for l in layers:
            need_ctx = l < DEPTH - 1
            phase_mod(k, l, NB, condT, W["ada_w"], W["ada_b"], mod_d)
            for b in range(NB):
                if l == 0:
                    x_src = (lambda tt, b=b: xin.t[b][tt * 128:(tt + 1) * 128, :]); x_tk = xin
                else:
                    x_src = (lambda tt, b=b: x2_d.t[b][tt * 128:(tt + 1) * 128, :]); x_tk = x2_d
                with ExitStack() as ps:
                    w_in_bf = k.sb("winbf", [128, 8, IN_COLS], BF16, ps)
                    load_w_in(k, l, W["w_in"], w_in_bf)
                    phase_inproj(k, l, b, NB, x_src, x_tk, mod_d, w_in_bf, C["ident_bf"], pT_rw_d, pT_na_d, p_tm_d)
                with ExitStack() as ps:
                    if "C" in phases:
                        RW = load_rw_w(k, l, ps, W, C["ident_f"])
                        phase_rwkv(k, pT_rw_d, RW, C, yrw_d)
                with ExitStack() as ps:
                    if "D" in phases:
                        wuq, wukv = load_mla_w(k, l, ps, W["mla_w_uq"], W["mla_w_ukv"], W["mla_q_norm"], W["mla_kv_norm"])
                        phase_mla(k, need_ctx, p_tm_d, wuq, wukv, C["cos_d"], C["sin_d"], C["ident_bf"], C["ones_bf"], ymla_d)
                with ExitStack() as ps:
                    if "E" in phases:
                        bm = load_na_bias(k, l, ps, na_bias, CD["na_mask"])
                        phase_na(k, need_ctx, pT_na_d, p_tm_d, bm, C["ones_bf"], yna_d)
                x1_b = Tk(x1_d.t[b * T:(b + 1) * T, :], "x1b")
                if "F" not in phases:
                    continue
                if sparse:
                    phase_post(k, l, b, NB, need_ctx, x_src, x_tk, mod_d, yrw_d, ymla_d, yna_d, W, C, x1_b, hT_d, GT_d, b * T, hTM_d, Gtm_d)
                else:
                    phase_post(k, l, b, NB, need_ctx, x_src, x_tk, mod_d, yrw_d, ymla_d, yna_d, W, C, x1_b, hT_d, GT_d, b * T)
            groups = []
            for b in range(NB):
                if need_ctx:
                    groups.append((b * T, 256, "c"))
                for i in range(4):
                    groups.append((b * T + 256 + i * 512, 512, b))
            groups.sort(key=lambda g: -g[1])

            def tile_io(col, l=l):
                b = col // T; t = col % T
                x1_ap = x1_d.t[col:col + 128, :]
                if l == DEPTH - 1:
                    return x1_ap, x1_d, out_d.t[b][t - NCTX:t - NCTX + 128, :], out_d
                return x1_ap, x1_d, x2_d.t[b][t:t + 128, :], x2_d
            if moe and sparse:
                phase_moe_sparse(k, l, NB, need_ctx, tile_io, mod_d, W, C, CD, hTM_d, Gtm_d, Hs_d, Ys_d)
            elif moe:
                phase_moe(k, l, NB, groups, tile_io, mod_d, W, C, hT_d, GT_d)
        k.barrier()
        print("ninst", k.ninst, k.cnt, flush=True)
    return nc


def core_inputs(inputs, b0, NB, shared):
    cc = np.concatenate([inputs["c"][b0:b0 + NB], inputs["c_ctx"][None]], 0).astype(np.float32)
    im = dict(shared)
    im["condT"] = np.ascontiguousarray(cc.T.reshape(8, 128, NB + 1).transpose(1, 0, 2))
    im["xin"] = np.ascontiguousarray(np.concatenate([inputs["ctx"][b0:b0 + NB], inputs["x"][b0:b0 + NB]], 1))
    return im


def shared_inputs(inputs):
    sh = {n: np.ascontiguousarray(np.asarray(inputs[n], np.float32)) for n in WSHAPES}
    for n, v in const_arrays().items():
        sh["c_" + n] = np.ascontiguousarray(v.astype(np.float32))
    sh["na_bias"] = na_bias_host(np.asarray(inputs["na_rpb"], np.float32))
    return sh


def kernel(**inputs):
    NCORES = 8; B = inputs["x"].shape[0]; NB = B // NCORES
    nc = build_nc(NB)
    sh = shared_inputs(inputs)
    in_maps = [core_inputs(inputs, c * NB, NB, sh) for c in range(NCORES)]
    res = run_bass_kernel_spmd(nc, in_maps, core_ids=list(range(NCORES)))
    return np.concatenate([r["out"] for r in res.results], 0).astype(np.float32)
```

```python
import numpy as np
from contextlib import ExitStack
import concourse.bass as bass
import concourse.mybir as mybir
from concourse.bass_utils import run_bass_kernel_spmd

F32 = mybir.dt.float32; BF16 = mybir.dt.bfloat16; I32 = mybir.dt.int32
AF = mybir.ActivationFunctionType; ALU = mybir.AluOpType; AX = mybir.AxisListType

D = 1024; SEQ = 2048; NCTX = 256; T = SEQ + NCTX; DEPTH = 2; NTT = T // 128
RW_COLS = 1152; MLA_COLS = 448; NA_COLS = 768; IN_COLS = 2368
NE = 32
NBLK_MAX = 104
DN_ALPHA = (2 * DEPTH) ** 0.25


class Tk:
    __slots__ = ("t", "w", "r", "name", "psum")
    def __init__(s, t, name="", psum=False):
        s.t = t; s.w = None; s.r = []; s.name = name; s.psum = psum
    def __getitem__(s, idx):
        return s.t[idx]


class K:
    NDMA = 6
    def __init__(s, nc, stack):
        s.nc = nc; s.stack = stack
        s.eng = {"pe": nc.tensor, "act": nc.scalar, "dve": nc.vector, "pool": nc.gpsimd, "sp": nc.sync}
        s.sem = {}; s.cnt = {}
        for e in s.eng:
            s.sem[e] = stack.enter_context(nc.semaphore("s_" + e)); s.cnt[e] = 0
        s.seen = {e: {} for e in s.eng}
        s.dsem = [stack.enter_context(nc.semaphore("d%d" % i)) for i in range(3 * s.NDMA)]
        s.dcnt = [0] * (3 * s.NDMA); s.dnext = {"sp": 0, "act": 0, "pool": 0}; s.dq = {"sp": 0, "act": 1, "pool": 2}
        s.ninst = 0; s.uid = 0
    def sb(s, name, shape, dt, stack=None):
        s.uid += 1
        t = Tk((stack or s.stack).enter_context(s.nc.sbuf_tensor("%s_%d" % (name, s.uid), list(shape), dt)), name)
        assert s.nc.sbuf_bytes_remaining >= 28 * 1024, "SBUF budget exceeded at %s: remaining %d" % (name, s.nc.sbuf_bytes_remaining)
        return t
    def ps(s, name, shape, dt=F32, stack=None):
        s.uid += 1
        isz = 4 if dt == F32 else 2
        free = int(np.prod(shape[1:])); nb = (free * isz + 2047) // 2048
        raw = (stack or s.stack).enter_context(s.nc.psum_tensor("%s_%d" % (name, s.uid), [128, nb * 2048 // isz], dt))
        ap = raw[0:shape[0], 0:free]
        if len(shape) == 3:
            ap = ap.rearrange("p (a b) -> p a b", a=shape[1])
        return Tk(ap, name, psum=True)
    def dram(s, name, shape, dt, kind="Internal"):
        return Tk(s.nc.dram_tensor(name, list(shape), dt, kind=kind).ap(), name)
    def _wait(s, e, toks):
        seen = s.seen[e]
        for (sem, val, key) in toks:
            if seen.get(key, 0) >= val:
                continue
            s.eng[e].wait_ge(sem, val); seen[key] = val; s.ninst += 1
    def _deps(s, e, reads, writes):
        toks = []
        for t in reads:
            if t.w is not None:
                toks.append(t.w)
            if t.psum:
                toks.extend(k for k in t.r if k[2] != e)
        for t in writes:
            if t.w is not None:
                toks.append(t.w)
            toks.extend(t.r)
        if e == "pe":
            toks = [k for k in toks if k[2] != "pe"]
        return toks
    def op(s, e, fn, reads=(), writes=()):
        s._wait(e, s._deps(e, reads, writes))
        ins = fn(s.eng[e]); s.cnt[e] += 1; ins.then_inc(s.sem[e], 1); s.ninst += 1
        tok = (s.sem[e], s.cnt[e], e)
        for t in reads:
            t.r = [k for k in t.r if k[2] != e]; t.r.append(tok)
        for t in writes:
            t.w = tok; t.r = []
        return ins
    def dma(s, q, out, in_, reads=(), writes=(), **kw):
        base = s.dq[q] * s.NDMA; i = base + s.dnext[q]; s.dnext[q] = (s.dnext[q] + 1) % s.NDMA
        key = "d%d" % i
        toks = s._deps(q, reads, writes)
        if s.dcnt[i] > 0:
            toks.append((s.dsem[i], s.dcnt[i], key))
        s._wait(q, toks)
        ins = s.eng[q].dma_start(out=out, in_=in_, **kw); s.dcnt[i] += 16; ins.then_inc(s.dsem[i], 16); s.ninst += 1
        tok = (s.dsem[i], s.dcnt[i], key)
        for t in reads:
            t.r.append(tok)
        for t in writes:
            t.w = tok; t.r = []
        return ins
    def idma(s, out, in_, out_offset=None, in_offset=None, reads=(), writes=(), **kw):
        q = "pool"
        base = s.dq[q] * s.NDMA; i = base + s.dnext[q]; s.dnext[q] = (s.dnext[q] + 1) % s.NDMA
        key = "d%d" % i
        toks = s._deps(q, reads, writes)
        if s.dcnt[i] > 0:
            toks.append((s.dsem[i], s.dcnt[i], key))
        s._wait(q, toks)
        ins = s.eng[q].indirect_dma_start(out=out, out_offset=out_offset, in_=in_, in_offset=in_offset, **kw)
        s.dcnt[i] += 16; ins.then_inc(s.dsem[i], 16); s.ninst += 1
        tok = (s.dsem[i], s.dcnt[i], key)
        for t in reads:
            t.r.append(tok)
        for t in writes:
            t.w = tok; t.r = []
        return ins
    def barrier(s):
        toks = [(s.sem[e], s.cnt[e], e) for e in s.eng if s.cnt[e] > 0]
        toks += [(s.dsem[i], s.dcnt[i], "d%d" % i) for i in range(len(s.dsem)) if s.dcnt[i] > 0]
        for e in s.eng:
            s._wait(e, [k for k in toks if k[2] != e])
    def finish(s, outs):
        s._wait("sp", [t.w for t in outs if t.w is not None])


def bcast_rows(ap_row, n):
    return ap_row.partition_broadcast(n)


def phase_mod(k, l, NB, condT_d, ada_w_d, ada_b_d, mod_d):
    with ExitStack() as st:
        R = NB + 1
        cond = k.sb("cond", [128, 8, R], F32, st); cond_bf = k.sb("condbf", [128, 8, R], BF16, st)
        adab = k.sb("adab", [R, 6 * D], F32, st); mod_sb = k.sb("modsb", [R, 6 * D], F32, st)
        wts = [k.sb("adaw", [128, 8, 512], BF16, st) for _ in range(2)]
        pss = [k.ps("modps", [R, 512], F32, st) for _ in range(2)]
        k.dma("sp", cond[:], condT_d[:], writes=[cond])
        k.dma("sp", adab[:], ada_b_d[l:l + 1, :].partition_broadcast(R), writes=[adab])
        k.op("act", lambda e: e.activation(cond_bf[:], cond[:], AF.Silu), reads=[cond], writes=[cond_bf])
        wv = ada_w_d.t[l].rearrange("(kc p) n -> p kc n", p=128)
        for g in range(12):
            wt = wts[g % 2]; ps = pss[g % 2]
            k.dma("pool", wt[:], wv[:, :, g * 512:(g + 1) * 512], writes=[wt])
            for kc in range(8):
                k.op("pe", lambda e: e.matmul(ps[:], lhsT=cond_bf[:, kc, :], rhs=wt[:, kc, :], start=(kc == 0), stop=(kc == 7)),
                     reads=[cond_bf, wt], writes=[ps])
            k.op("dve", lambda e: e.tensor_tensor(mod_sb[:, g * 512:(g + 1) * 512], ps[:], adab[:, g * 512:(g + 1) * 512], ALU.add),
                 reads=[ps, adab], writes=[mod_sb])
        k.dma("sp", mod_d[l], mod_sb[:], reads=[mod_sb], writes=[mod_d])
    k.barrier()


def ln_stats(k, xt, st6, mv, rstd, eps):
    for hh in range(2):
        k.op("dve", lambda e: e.bn_stats(st6[:, hh, :], xt[:, hh * 512:(hh + 1) * 512]), reads=[xt], writes=[st6])
    k.op("dve", lambda e: e.bn_aggr(mv[:], st6[:].rearrange("p a b -> p (a b)")), reads=[st6], writes=[mv])
    k.op("dve", lambda e: e.tensor_scalar(rstd[:], mv[:, 1:2], eps, None, op0=ALU.add), reads=[mv], writes=[rstd])
    k.op("act", lambda e: e.sqrt(rstd[:], rstd[:]), reads=[rstd], writes=[rstd])
    k.op("dve", lambda e: e.reciprocal(rstd[:], rstd[:]), reads=[rstd], writes=[rstd])


def phase_inproj(k, l, b, NB, x_src, x_src_tk, mod_d, w_in_bf, ident_bf, pT_rw_d, pT_na_d, p_tm_d):
    with ExitStack() as st:
        bc = {}
        for nm, row, off in (("sa_l", b, 0), ("ca_l", b, D), ("sa_c", NB, 0), ("ca_c", NB, D)):
            tle = k.sb(nm, [128, D], F32, st)
            k.dma("sp", tle[:], mod_d[l][row:row + 1, off:off + D].partition_broadcast(128), reads=[mod_d], writes=[tle])
            bc[nm] = tle
        for nm in ("ca_l", "ca_c"):
            tle = bc[nm]
            k.op("pool", lambda e: e.tensor_scalar(tle[:], tle[:], 1.0, None, op0=ALU.add), reads=[tle], writes=[tle])
        groups = [(0, 512), (512, 512), (1024, 512), (1536, 512), (2048, 256)]
        xmT = [k.sb("xmT%d" % g, [128, 8, n], BF16, st) for g, (o, n) in enumerate(groups)]
        xts = [k.sb("xt", [128, D], F32, st) for _ in range(2)]
        t1 = k.sb("t1", [128, D], F32, st); xm = k.sb("xm", [128, D], BF16, st)
        st6 = k.sb("st6", [128, 2, 6], F32, st); mv = k.sb("mv", [128, 2], F32, st); rstd = k.sb("rstd", [128, 1], F32, st)
        pTs = [k.ps("pT", [128, 8, 128], BF16, st) for _ in range(2)]
        ptm = [k.ps("ptm", [128, 512], F32, st) for _ in range(2)]
        pfm = [k.ps("pfm", [128, 512], F32, st) for _ in range(2)]
        tm_sb = [k.sb("tmsb", [128, 704], F32, st) for _ in range(2)]
        fm_sb = [k.sb("fmsb", [128, 512], F32, st) for _ in range(2)]
        fm_sbb = [k.sb("fmsbb", [128, 512], BF16, st) for _ in range(2)]
        for tt in range(NTT):
            isctx = tt < 2
            sa = bc["sa_c" if isctx else "sa_l"]; ca = bc["ca_c" if isctx else "ca_l"]
            xt = xts[tt % 2]
            k.dma("sp", xt[:], x_src(tt), reads=[x_src_tk], writes=[xt])
            ln_stats(k, xt, st6, mv, rstd, 1e-5)
            k.op("dve", lambda e: e.scalar_tensor_tensor(t1[:], xt[:], mv[:, 0:1], ca[:], op0=ALU.subtract, op1=ALU.mult),
                 reads=[xt, mv, ca], writes=[t1])
            k.op("dve", lambda e: e.scalar_tensor_tensor(xm[:], t1[:], rstd[:], sa[:], op0=ALU.mult, op1=ALU.add),
                 reads=[t1, rstd, sa], writes=[xm])
            pT = pTs[tt % 2]
            for c in range(8):
                k.op("pe", lambda e: e.transpose(pT[:, c, :], xm[:, c * 128:(c + 1) * 128], ident_bf[:]), reads=[xm, ident_bf], writes=[pT])
            g = tt // 4; o = (tt % 4) * 128
            xg = xmT[g]
            k.op("act", lambda e: e.copy(xg[:, :, o:o + 128], pT[:]), reads=[pT], writes=[xg])
            pa = ptm[0]; pb = ptm[1]
            for kc in range(8):
                k.op("pe", lambda e: e.matmul(pa[:, 0:448], lhsT=xg[:, kc, o:o + 128], rhs=w_in_bf[:, kc, 1152:1600], start=(kc == 0), stop=(kc == 7)),
                     reads=[xg, w_in_bf], writes=[pa])
            for kc in range(8):
                k.op("pe", lambda e: e.matmul(pb[:, 0:256], lhsT=xg[:, kc, o:o + 128], rhs=w_in_bf[:, kc, 2112:2368], start=(kc == 0), stop=(kc == 7)),
                     reads=[xg, w_in_bf], writes=[pb])
            ts = tm_sb[tt % 2]
            k.op("act", lambda e: e.copy(ts[:, 0:448], pa[:, 0:448]), reads=[pa], writes=[ts])
            k.op("dve", lambda e: e.tensor_copy(ts[:, 448:704], pb[:, 0:256]), reads=[pb], writes=[ts])
            k.dma("sp", p_tm_d[tt * 128:(tt + 1) * 128, :], ts[:], reads=[ts], writes=[p_tm_d])
        cnt = 0
        for g, (o, n) in enumerate(groups):
            xg = xmT[g]
            for c in range(13):
                col = c * 128 if c < 9 else 1600 + (c - 9) * 128
                ps = pfm[cnt % 2]
                for kc in range(8):
                    k.op("pe", lambda e: e.matmul(ps[:, 0:n], lhsT=w_in_bf[:, kc, col:col + 128], rhs=xg[:, kc, :], start=(kc == 0), stop=(kc == 7)),
                         reads=[w_in_bf, xg], writes=[ps])
                if c < 9:
                    fs = fm_sb[cnt % 2]
                    k.op("act" if cnt % 2 else "dve", (lambda e: e.copy(fs[:, 0:n], ps[:, 0:n])) if cnt % 2 else (lambda e: e.tensor_copy(fs[:, 0:n], ps[:, 0:n])),
                         reads=[ps], writes=[fs])
                    k.dma("sp", pT_rw_d[c * 128:(c + 1) * 128, o:o + n], fs[:, 0:n], reads=[fs], writes=[pT_rw_d])
                else:
                    fs = fm_sbb[cnt % 2]
                    k.op("act" if cnt % 2 else "dve", (lambda e: e.copy(fs[:, 0:n], ps[:, 0:n])) if cnt % 2 else (lambda e: e.tensor_copy(fs[:, 0:n], ps[:, 0:n])),
                         reads=[ps], writes=[fs])
                    k.dma("sp", pT_na_d[(c - 9) * 128:(c - 8) * 128, o:o + n], fs[:, 0:n], reads=[fs], writes=[pT_na_d])
                cnt += 1
    k.barrier()


def load_w_in(k, l, w_in_d, w_in_bf):
    wv = w_in_d.t[l].rearrange("(kc p) n -> p kc n", p=128)
    for (a, bb) in ((0, 1024), (1024, 2048), (2048, IN_COLS)):
        k.dma("pool", w_in_bf[:, :, a:bb], wv[:, :, a:bb], writes=[w_in_bf])


def load_mla_w(k, l, st, w_uq_d, w_ukv_d, qn_d, kvn_d):
    wuq = k.sb("wuq", [128, 2, 768], BF16, st); wukv = k.sb("wukv", [128, 1024], BF16, st)
    with ExitStack() as s2:
        wuq_f = k.sb("wuqf", [128, 2, 768], F32, s2); wukv_f = k.sb("wukvf", [128, 1024], F32, s2)
        qn = k.sb("qn", [128, 2], F32, s2); kvn = k.sb("kvn", [128, 1], F32, s2)
        k.dma("sp", wuq_f[:], w_uq_d.t[l].rearrange("(kc p) n -> p kc n", p=128), writes=[wuq_f])
        k.dma("sp", wukv_f[:], w_ukv_d.t[l], writes=[wukv_f])
        k.dma("sp", qn[:], qn_d.t[l].rearrange("(kc p) -> p kc", p=128), writes=[qn], allow_slow_non_contiguous=True)
        k.dma("sp", kvn[:], kvn_d.t[l].rearrange("(p o) -> p o", o=1), writes=[kvn])
        for kc in range(2):
            k.op("dve", lambda e: e.tensor_scalar(wuq[:, kc, :], wuq_f[:, kc, :], qn[:, kc:kc + 1], None, op0=ALU.mult), reads=[wuq_f, qn], writes=[wuq])
        k.op("dve", lambda e: e.tensor_scalar(wukv[:], wukv_f[:], kvn[:, 0:1], None, op0=ALU.mult), reads=[wukv_f, kvn], writes=[wukv])
        k.barrier()
    return wuq, wukv


def phase_mla(k, need_ctx, p_tm_d, wuq, wukv, cos_d, sin_d, ident_bf, ones_bf, ymla_d):
    scale = 192.0 ** -0.5
    with ExitStack() as st:
        cos_sb = k.sb("cos", [128, 16, 128], F32, st); sin_sb = k.sb("sin", [128, 16, 128], F32, st)
        k.dma("sp", cos_sb[:], cos_d.t.rearrange("(tt p) c -> p tt c", p=128), writes=[cos_sb])
        k.dma("sp", sin_sb[:], sin_d.t.rearrange("(tt p) c -> p tt c", p=128), writes=[sin_sb])
        p_all = k.sb("pall", [128, NTT, 448], F32, st)
        k.dma("sp", p_all[:], p_tm_d.t.rearrange("(tt p) c -> p tt c", p=128)[:, :, 0:448], reads=[p_tm_d], writes=[p_all])
        qlnT = k.sb("qlnT", [128, 2, T], BF16, st); kvnT = k.sb("kvnT", [128, T], BF16, st)
        qTn = k.sb("qTn", [128, 4, T], BF16, st); qTr = k.sb("qTr", [64, 4, T], BF16, st)
        knT = k.sb("knT", [128, 4, T], BF16, st); krT = k.sb("krT", [64, T], BF16, st)
        v_sb = k.sb("vsb", [128, NTT, 512], BF16, st)
        rq = k.sb("rq", [128, NTT], F32, st); rkv = k.sb("rkv", [128, NTT], F32, st)
        with ExitStack() as st1:
            sq = k.sb("sq", [128, NTT, 384], F32, st1)
            k.op("act", lambda e: e.activation(sq[:], p_all[:, :, 0:384], AF.Square), reads=[p_all], writes=[sq])
            k.op("dve", lambda e: e.reduce_sum(rq[:], sq[:, :, 0:256], axis=AX.X), reads=[sq], writes=[rq])
            k.op("dve", lambda e: e.reduce_sum(rkv[:], sq[:, :, 256:384], axis=AX.X), reads=[sq], writes=[rkv])
            k.barrier()
        k.op("dve", lambda e: e.tensor_scalar(rq[:], rq[:], 1.0 / 256, 1e-6, op0=ALU.mult, op1=ALU.add), reads=[rq], writes=[rq])
        k.op("dve", lambda e: e.tensor_scalar(rkv[:], rkv[:], 1.0 / 128, 1e-6, op0=ALU.mult, op1=ALU.add), reads=[rkv], writes=[rkv])
        for r_ in (rq, rkv):
            k.op("act", lambda e: e.sqrt(r_[:], r_[:]), reads=[r_], writes=[r_])
            k.op("dve", lambda e: e.reciprocal(r_[:], r_[:]), reads=[r_], writes=[r_])
        st2 = ExitStack()
        nrm = [k.sb("nrm", [128, 384], BF16, st2) for _ in range(2)]
        pTs = k.ps("mlapT", [128, 3, 128], BF16, st2)
        pqs = [k.ps("mlapq", [128, 4, 256], F32, st2) for _ in range(2)]
        pv = k.ps("mlapv", [128, 512], F32, st2)
        q_sbs = [k.sb("qsb", [128, 4, 192], BF16, st2) for _ in range(2)]; kr_sbs = [k.sb("krsb", [128, 64], BF16, st2) for _ in range(2)]
        qr_fs = [k.sb("qrf", [128, 4, 64], F32, st2) for _ in range(2)]
        ra = k.sb("ra", [128, 4, 32], F32, st2); rb = k.sb("rb", [128, 4, 32], F32, st2)
        pqT = k.ps("mlapqT", [128, 4, 128], BF16, st2); pqTr = k.ps("mlapqTr", [64, 5, 128], BF16, st2)

        def stage_x(tt):
            isctx = tt < 2
            nt = nrm[tt % 2]; pT = pTs; pq = pqs[tt % 2]; q_sb = q_sbs[tt % 2]; kr_sb = kr_sbs[tt % 2]; qr_f = qr_fs[tt % 2]
            k.op("dve", lambda e: e.tensor_scalar(nt[:, 0:256], p_all[:, tt, 0:256], rq[:, tt:tt + 1], None, op0=ALU.mult), reads=[p_all, rq], writes=[nt])
            k.op("dve", lambda e: e.tensor_scalar(nt[:, 256:384], p_all[:, tt, 256:384], rkv[:, tt:tt + 1], None, op0=ALU.mult), reads=[p_all, rkv], writes=[nt])
            for c in range(3):
                k.op("pe", lambda e: e.transpose(pT[:, c, :], nt[:, c * 128:(c + 1) * 128], ident_bf[:]), reads=[nt, ident_bf], writes=[pT])
            sl = slice(tt * 128, (tt + 1) * 128)
            k.op("act", lambda e: e.copy(qlnT[:, :, sl], pT[:, 0:2, :]), reads=[pT], writes=[qlnT])
            k.op("act", lambda e: e.copy(kvnT[:, sl], pT[:, 2, :]), reads=[pT], writes=[kvnT])
            k.op("pe", lambda e: e.matmul(pv[:].rearrange("p (h c) -> p h c", h=4), lhsT=kvnT[:, sl],
                                          rhs=wukv[:].rearrange("p (h c) -> p h c", h=4)[:, :, 128:256], start=True, stop=True),
                 reads=[kvnT, wukv], writes=[pv])
            doq = (not isctx) or need_ctx
            if doq:
                for h in range(4):
                    for kc in range(2):
                        k.op("pe", lambda e: e.matmul(pq[:, h, 0:192], lhsT=qlnT[:, kc, sl], rhs=wuq[:, kc, h * 192:(h + 1) * 192], start=(kc == 0), stop=(kc == 1)),
                             reads=[qlnT, wuq], writes=[pq])
            k.op("act", lambda e: e.copy(v_sb[:, tt, :], pv[:]), reads=[pv], writes=[v_sb])
            kre = p_all[:, tt, 384:448:2]; kro = p_all[:, tt, 385:448:2]
            if isctx:
                k.op("dve", lambda e: e.tensor_copy(kr_sb[:, 0:32], kre), reads=[p_all], writes=[kr_sb])
                k.op("dve", lambda e: e.tensor_copy(kr_sb[:, 32:64], kro), reads=[p_all], writes=[kr_sb])
            else:
                cs = cos_sb[:, tt - 2, 0:32]; sn = sin_sb[:, tt - 2, 0:32]
                k.op("dve", lambda e: e.tensor_tensor(ra[:, 0, :], kre, cs, ALU.mult), reads=[p_all, cos_sb], writes=[ra])
                k.op("dve", lambda e: e.tensor_tensor(rb[:, 0, :], kro, sn, ALU.mult), reads=[p_all, sin_sb], writes=[rb])
                k.op("dve", lambda e: e.tensor_tensor(kr_sb[:, 0:32], ra[:, 0, :], rb[:, 0, :], ALU.subtract), reads=[ra, rb], writes=[kr_sb])
                k.op("dve", lambda e: e.tensor_tensor(ra[:, 0, :], kre, sn, ALU.mult), reads=[p_all, sin_sb], writes=[ra])
                k.op("dve", lambda e: e.tensor_tensor(rb[:, 0, :], kro, cs, ALU.mult), reads=[p_all, cos_sb], writes=[rb])
                k.op("dve", lambda e: e.tensor_tensor(kr_sb[:, 32:64], ra[:, 0, :], rb[:, 0, :], ALU.add), reads=[ra, rb], writes=[kr_sb])
            if doq:
                k.op("act", lambda e: e.copy(q_sb[:, :, 0:128], pq[:, :, 0:128]), reads=[pq], writes=[q_sb])
                k.op("act", lambda e: e.copy(qr_f[:], pq[:, :, 128:192]), reads=[pq], writes=[qr_f])
                qe = qr_f[:, :, 0:64:2]; qo = qr_f[:, :, 1:64:2]
                if isctx:
                    k.op("dve", lambda e: e.tensor_copy(q_sb[:, :, 128:160], qe), reads=[qr_f], writes=[q_sb])
                    k.op("dve", lambda e: e.tensor_copy(q_sb[:, :, 160:192], qo), reads=[qr_f], writes=[q_sb])
                else:
                    cs = cos_sb[:, tt - 2, :].rearrange("p (h c) -> p h c", h=4); sn = sin_sb[:, tt - 2, :].rearrange("p (h c) -> p h c", h=4)
                    k.op("dve", lambda e: e.tensor_tensor(ra[:], qe, cs, ALU.mult), reads=[qr_f, cos_sb], writes=[ra])
                    k.op("dve", lambda e: e.tensor_tensor(rb[:], qo, sn, ALU.mult), reads=[qr_f, sin_sb], writes=[rb])
                    k.op("dve", lambda e: e.tensor_tensor(q_sb[:, :, 128:160], ra[:], rb[:], ALU.subtract), reads=[ra, rb], writes=[q_sb])
                    k.op("dve", lambda e: e.tensor_tensor(ra[:], qe, sn, ALU.mult), reads=[qr_f, sin_sb], writes=[ra])
                    k.op("dve", lambda e: e.tensor_tensor(rb[:], qo, cs, ALU.mult), reads=[qr_f, cos_sb], writes=[rb])
                    k.op("dve", lambda e: e.tensor_tensor(q_sb[:, :, 160:192], ra[:], rb[:], ALU.add), reads=[ra, rb], writes=[q_sb])

        def stage_y(tt):
            isctx = tt < 2
            q_sb = q_sbs[tt % 2]; kr_sb = kr_sbs[tt % 2]
            sl = slice(tt * 128, (tt + 1) * 128)
            doq = (not isctx) or need_ctx
            if doq:
                for h in range(4):
                    k.op("pe", lambda e: e.transpose(pqT[:, h, :], q_sb[:, h, 0:128], ident_bf[:]), reads=[q_sb, ident_bf], writes=[pqT])
                    k.op("pe", lambda e: e.transpose(pqTr[:, h, :], q_sb[:, h, 128:192], ident_bf[:]), reads=[q_sb, ident_bf], writes=[pqTr])
            k.op("pe", lambda e: e.transpose(pqTr[:, 4, :], kr_sb[:], ident_bf[:]), reads=[kr_sb, ident_bf], writes=[pqTr])
            if doq:
                k.op("act", lambda e: e.copy(qTn[:, :, sl], pqT[:]), reads=[pqT], writes=[qTn])
                k.op("dve", lambda e: e.tensor_copy(qTr[:, :, sl], pqTr[:, 0:4, :]), reads=[pqTr], writes=[qTr])
            k.op("dve", lambda e: e.tensor_copy(krT[:, sl], pqTr[:, 4, :]), reads=[pqTr], writes=[krT])

        STOP = 99
        for tt in range(NTT):
            stage_x(tt)
            if tt > 0:
                stage_y(tt - 1)
        stage_y(NTT - 1)
        k.barrier(); st2.close()
        groups = [(0, 512), (512, 512), (1024, 512), (1536, 512), (2048, 256)]
        pk = [k.ps("mlapk", [128, 512], F32, st) for _ in range(2)]
        cnt = 0
        for h in range(4 if STOP > 2 else 0):
            for (o, n) in groups:
                ps = pk[cnt % 2]
                k.op("pe", lambda e: e.matmul(ps[:, 0:n], lhsT=wukv[:, h * 256:h * 256 + 128], rhs=kvnT[:, o:o + n], start=True, stop=True), reads=[wukv, kvnT], writes=[ps])
                k.op("act" if cnt % 2 else "dve", (lambda e: e.copy(knT[:, h, o:o + n], ps[:, 0:n])) if cnt % 2 else (lambda e: e.tensor_copy(knT[:, h, o:o + n], ps[:, 0:n])),
                     reads=[ps], writes=[knT])
                cnt += 1
        pos = [k.ps("mlapo", [128, 512], F32, st) for _ in range(2)]
        pss = [k.ps("mlapss", [128, 512], F32, st) for _ in range(2)]
        pts = [k.sb("mlapt", [128, 512], BF16, st) for _ in range(3)]
        rs = k.sb("mlars", [128, 512], F32, st); ob = [k.sb("mlaob", [128, 512], BF16, st) for _ in range(2)]
        qblocks = [(256 + i * 512, 512, list(range(NTT))) for i in range(4)]
        if need_ctx:
            qblocks.append((0, 256, [0, 1]))
        it = 0; bi = 0
        for h in range(4 if STOP > 3 else 0):
            for (qo_, nq, ktiles) in qblocks:
                po = pos[bi % 2]; psum = pss[bi % 2]
                for ji, j in enumerate(ktiles):
                    ps = pk[it % 2]; pt = pts[it % 3]
                    ks = slice(j * 128, (j + 1) * 128)
                    k.op("pe", lambda e: e.matmul(ps[:, 0:nq], lhsT=knT[:, h, ks], rhs=qTn[:, h, qo_:qo_ + nq], start=True, stop=False), reads=[knT, qTn], writes=[ps])
                    k.op("pe", lambda e: e.matmul(ps[:, 0:nq], lhsT=krT[:, ks], rhs=qTr[:, h, qo_:qo_ + nq], start=False, stop=True), reads=[krT, qTr], writes=[ps])
                    k.op("act", lambda e: e.activation(pt[:, 0:nq], ps[:, 0:nq], AF.Exp, scale=scale), reads=[ps], writes=[pt])
                    first = ji == 0; last = ji == len(ktiles) - 1
                    k.op("pe", lambda e: e.matmul(po[:, 0:nq], lhsT=v_sb[:, j, h * 128:(h + 1) * 128], rhs=pt[:, 0:nq], start=first, stop=last), reads=[v_sb, pt], writes=[po])
                    k.op("pe", lambda e: e.matmul(psum[:, 0:nq], lhsT=ones_bf[:], rhs=pt[:, 0:nq], start=first, stop=last), reads=[ones_bf, pt], writes=[psum])
                    it += 1
                o_ = ob[bi % 2]
                k.op("dve", lambda e: e.reciprocal(rs[:, 0:nq], psum[:, 0:nq]), reads=[psum], writes=[rs])
                k.op("dve", lambda e: e.tensor_tensor(o_[:, 0:nq], po[:, 0:nq], rs[:, 0:nq], ALU.mult), reads=[po, rs], writes=[o_])
                k.dma("sp", ymla_d[h][:, qo_:qo_ + nq], o_[:, 0:nq], reads=[o_], writes=[ymla_d])
                bi += 1
    k.barrier()


def rope_tables():
    t = np.arange(SEQ); row = (t // 64).astype(np.float32); col = (t % 64).astype(np.float32)
    inv = (10000.0 ** (-np.arange(16, dtype=np.float32) / 16)).astype(np.float32)
    ang = np.concatenate([row[:, None] * inv, col[:, None] * inv], -1).astype(np.float32)
    return np.tile(np.cos(ang).astype(np.float32), (1, 4)), np.tile(np.sin(ang).astype(np.float32), (1, 4))


def na_pattern(i):
    if i < 4:
        return i, 0
    if i <= 28:
        return 4, i - 4
    return i - 24, 24


def na_bias_host(rpb):
    L = rpb.shape[0]
    col = np.arange(64)
    dc = np.clip(col[None, :] - col[:, None] + 15, 0, 30)
    out = np.zeros((L, 128, 4, 8, 4, 64), np.float32)
    lk = np.arange(512); r = lk // 64; kc = lk % 64
    for i in list(range(5)) + [29, 30, 31]:
        pat, rs = na_pattern(i)
        dr = rs + r - i + 7
        g = rpb[:, :, dr[:, None], dc.T[kc, :]]
        out[:, :, :, pat] = g.reshape(L, 4, 4, 128, 64).transpose(0, 3, 1, 2, 4)
    return out


def na_mask_host():
    col = np.arange(64)
    cs = np.clip(col - 8, 0, 48)
    inw = (col[None, :] >= cs[:, None]) & (col[None, :] < cs[:, None] + 16)
    lk = np.arange(512); kc = lk % 64
    m = np.where(inw.T[kc, :], 0.0, -30000.0).astype(np.float32)
    return np.ascontiguousarray(m.reshape(4, 128, 64).transpose(1, 0, 2))


def load_na_bias(k, l, st, bias_d, mask_d):
    bm = k.sb("nabm", [128, 4, 8, 384], F32, st); mk = k.sb("namk", [128, 256], F32, st)
    k.op("pool", lambda e: e.memset(bm[:], 0.0), writes=[bm])
    k.dma("sp", mk[:], mask_d.t.rearrange("p j q -> p (j q)"), writes=[mk])
    for h in range(4):
        k.dma("sp", bm[:, h, :, 0:256], bias_d.t[l][:, h].rearrange("p a j q -> p a (j q)"), writes=[bm])
    for h in range(4):
        for pat in range(8):
            k.op("pool", lambda e: e.tensor_tensor(bm[:, h, pat, 0:256], bm[:, h, pat, 0:256], mk[:], ALU.add), reads=[bm, mk], writes=[bm])
    return bm


def phase_na(k, need_ctx, pT_na_d, p_tm_d, bm, ones_bf, yna_d):
    scale = 64.0 ** -0.5
    with ExitStack() as st:
        qT = k.sb("naqT", [64, 4, T], BF16, st); kT = k.sb("nakT", [64, 4, T], BF16, st)
        vA = k.sb("navA", [128, NTT, 256], BF16, st); vB = k.sb("navB", [128, NTT - 1, 256], BF16, st)
        yo = k.sb("nayo", [64, 4, T], BF16, st)
        k.dma("sp", qT[:], pT_na_d.t[0:256, :].rearrange("(h d) t -> d h t", h=4), reads=[pT_na_d], writes=[qT])
        k.dma("sp", kT[:], pT_na_d.t[256:512, :].rearrange("(h d) t -> d h t", h=4), reads=[pT_na_d], writes=[kT])
        k.dma("pool", vA[:], p_tm_d.t.rearrange("(tt p) c -> p tt c", p=128)[:, :, 448:704], reads=[p_tm_d], writes=[vA])
        k.dma("pool", vB[:], p_tm_d.t[64:64 + (NTT - 1) * 128, :].rearrange("(tt p) c -> p tt c", p=128)[:, :, 448:704], reads=[p_tm_d], writes=[vB])
        NSL = 4
        pSs = [k.ps("napS", [128, 6, 64], F32, st) for _ in range(NSL)]
        poms = [k.ps("napom", [64, 512], F32, st) for _ in range(NSL)]
        ssb = [k.sb("nassb", [128, 6, 64], F32, st) for _ in range(NSL)]
        pts = [k.sb("napt", [128, 6, 64], BF16, st) for _ in range(NSL)]
        ptc = [k.sb("naptc", [128, 2, 256], BF16, st) for _ in range(NSL)]
        rss = [k.sb("nars", [64, 256], F32, st) for _ in range(NSL)]
        it = 0
        for h in range(4):
            for i in range(32):
                pat, rs_ = na_pattern(i)
                pS = pSs[it % NSL]; pom = poms[it % NSL]; s_sb = ssb[it % NSL]; pt = pts[it % NSL]; rs = rss[it % NSL]
                tok0 = 256 + 64 * rs_
                qs = slice(256 + 64 * i, 256 + 64 * i + 64)
                for j in range(6):
                    ko = tok0 + 128 * j if j < 4 else (j - 4) * 128
                    k.op("pe", lambda e: e.matmul(pS[:, j, :], lhsT=kT[:, h, ko:ko + 128], rhs=qT[:, h, qs], start=True, stop=True), reads=[kT, qT], writes=[pS])
                k.op("dve", lambda e: e.scalar_tensor_tensor(s_sb[:].rearrange("p j q -> p (j q)"), pS[:].rearrange("p j q -> p (j q)"), scale, bm[:, h, pat, :], op0=ALU.mult, op1=ALU.add),
                     reads=[pS, bm], writes=[s_sb])
                k.op("act", lambda e: e.activation(pt[:], s_sb[:], AF.Exp), reads=[s_sb], writes=[pt])
                for j in range(6):
                    if j < 4:
                        vt = (vA, 2 + rs_ // 2 + j) if rs_ % 2 == 0 else (vB, (3 + rs_) // 2 + j)
                    else:
                        vt = (vA, j - 4)
                    vtk, vi = vt
                    k.op("pe", lambda e: e.matmul(pom[:, 0:64], lhsT=vtk[:, vi, h * 64:(h + 1) * 64], rhs=pt[:, j, :], start=(j == 0), stop=(j == 5)), reads=[vtk, pt], writes=[pom])
                for j in range(6):
                    k.op("pe", lambda e: e.matmul(pom[:, 256:320], lhsT=ones_bf[:, 0:64], rhs=pt[:, j, :], start=(j == 0), stop=(j == 5)), reads=[ones_bf, pt], writes=[pom])
                k.op("dve", lambda e: e.reciprocal(rs[:, 0:64], pom[:, 256:320]), reads=[pom], writes=[rs])
                k.op("dve", lambda e: e.tensor_tensor(yo[:, h, qs], pom[:, 0:64], rs[:, 0:64], ALU.mult), reads=[pom, rs], writes=[yo])
                it += 1
            if need_ctx:
                pS = pSs[it % NSL]; pom = poms[it % NSL]; pt = ptc[it % NSL]; rs = rss[it % NSL]
                pSv = pS[:].rearrange("p j q -> p (j q)")
                for j in range(2):
                    k.op("pe", lambda e: e.matmul(pSv[:, 0:256], lhsT=kT[:, h, j * 128:(j + 1) * 128], rhs=qT[:, h, 0:256], start=True, stop=True), reads=[kT, qT], writes=[pS])
                    k.op("act", lambda e: e.activation(pt[:, j, :], pSv[:, 0:256], AF.Exp, scale=scale), reads=[pS], writes=[pt])
                for j in range(2):
                    k.op("pe", lambda e: e.matmul(pom[:, 0:256], lhsT=vA[:, j, h * 64:(h + 1) * 64], rhs=pt[:, j, :], start=(j == 0), stop=(j == 1)), reads=[vA, pt], writes=[pom])
                for j in range(2):
                    k.op("pe", lambda e: e.matmul(pom[:, 256:512], lhsT=ones_bf[:, 0:64], rhs=pt[:, j, :], start=(j == 0), stop=(j == 1)), reads=[ones_bf, pt], writes=[pom])
                k.op("dve", lambda e: e.reciprocal(rs[:], pom[:, 256:512]), reads=[pom], writes=[rs])
                k.op("dve", lambda e: e.tensor_tensor(yo[:, h, 0:256], pom[:, 0:256], rs[:], ALU.mult), reads=[pom, rs], writes=[yo])
                it += 1
        k.dma("sp", yna_d[:], yo[:], reads=[yo], writes=[yna_d])
    k.barrier()


NCH = T // 64
RW_STOP = 99
class _Stop(Exception):
    pass
def chk(level):
    if RW_STOP == level:
        raise _Stop()


def rw_consts_host():
    i = np.arange(128) % 64
    lt = (i[None, :] < i[:, None]).astype(np.float32); le = (i[None, :] <= i[:, None]).astype(np.float32)
    gt = (i[None, :] > i[:, None]).astype(np.float32); ge = (i[None, :] >= i[:, None]).astype(np.float32)
    blk = ((np.arange(128)[:, None] // 64) == (np.arange(128)[None, :] // 64)).astype(np.float32)
    m2 = np.stack([np.concatenate([gt, ge], 1), np.concatenate([lt, le], 1)], 0)
    m1 = np.stack([lt, gt], 0)
    isel = np.concatenate([np.eye(64, dtype=np.float32)] * 2, 0)
    return m2, m1, blk, isel


def load_rw_w(k, l, st, W, ident_f):
    o = {}
    o["w2"] = k.sb("rww2", [128, 256], BF16, st); o["a2"] = k.sb("rwa2", [128, 256], BF16, st); o["g2"] = k.sb("rwg2", [128, 256], BF16, st)
    k.dma("pool", o["w2"][:], W["rw_w2"].t[l].rearrange("d k n -> (d k) n"), writes=[o["w2"]])
    k.dma("pool", o["a2"][:], W["rw_a2"].t[l].rearrange("d k n -> (d k) n"), writes=[o["a2"]])
    k.dma("pool", o["g2"][:], W["rw_g2"].t[l], writes=[o["g2"]])
    stage = k.sb("rwstage", [32, 128], F32, st)
    k.dma("sp", stage[0:18, :], W["rw_mu"].t[l].rearrange("d (c p) -> (d c) p", p=128), writes=[stage])
    k.dma("sp", stage[18:22, :], W["rw_w0"].t[l].rearrange("d (c p) -> (d c) p", p=128), writes=[stage])
    k.dma("sp", stage[22:26, :], W["rw_a0"].t[l].rearrange("d (c p) -> (d c) p", p=128), writes=[stage])
    k.dma("sp", stage[26:28, :], W["rw_kk"].t[l].rearrange("(c p) -> c p", p=128), writes=[stage])
    k.dma("sp", stage[28:30, :], W["rw_ka"].t[l].rearrange("(c p) -> c p", p=128), writes=[stage])
    k.dma("sp", stage[30:32, :], W["rw_rk"].t[l].rearrange("(c h) n -> c (h n)", c=2), writes=[stage])
    cols = k.sb("rwcols", [128, 48], F32, st)
    with ExitStack() as s2:
        pc = k.ps("rwpc", [128, 32], F32, s2)
        k.op("pe", lambda e: e.transpose(pc[:], stage[:], ident_f[0:32, 0:32]), reads=[stage, ident_f], writes=[pc])
        k.op("dve", lambda e: e.tensor_copy(cols[:, 0:32], pc[:]), reads=[pc], writes=[cols])
        k.barrier()
    k.op("dve", lambda e: e.tensor_tensor(cols[:, 32:41], cols[:, 0:9], cols[:, 9:18], ALU.add), reads=[cols], writes=[cols])
    k.op("dve", lambda e: e.tensor_scalar(cols[:, 32:41], cols[:, 32:41], -1.0, 1.0, op0=ALU.mult, op1=ALU.add), reads=[cols], writes=[cols])
    k.op("dve", lambda e: e.tensor_scalar(cols[:, 41:43], cols[:, 28:30], -1.0, 1.0, op0=ALU.mult, op1=ALU.add), reads=[cols], writes=[cols])
    o["cols"] = cols
    gn = k.sb("rwgn", [128, 2, 2, 64], F32, st)
    for P in range(2):
        for h in range(2):
            hd = 2 * P + h
            k.dma("sp", gn[h * 64:(h + 1) * 64, P, 0, :], W["rw_gn_w"].t[l:l + 1, hd * 64:(hd + 1) * 64].partition_broadcast(64), writes=[gn])
            k.dma("sp", gn[h * 64:(h + 1) * 64, P, 1, :], W["rw_gn_b"].t[l:l + 1, hd * 64:(hd + 1) * 64].partition_broadcast(64), writes=[gn])
    o["gn"] = gn
    return o


def phase_rwkv(k, pT_rw_d, RW, C, yrw_d):
    cols = RW["cols"]
    EM05 = float(np.exp(-0.5))
    with ExitStack() as st:
        tmpA = k.sb("rwtmpA", [128, T], F32, st); tmpB = k.sb("rwtmpB", [128, T], F32, st)

        def shifted(c, out):
            k.dma("sp", tmpA[:], pT_rw_d[c * 128:(c + 1) * 128, :], reads=[pT_rw_d], writes=[tmpA])
            k.op("act", lambda e: e.activation(out[:], tmpA[:], AF.Identity, scale=cols[:, 32 + c:33 + c]), reads=[tmpA, cols], writes=[out])
            for (dst, src, mc) in (((1, 256), (0, 255), c), ((257, T), (256, T - 1), c), ((0, 255), (1, 256), 9 + c), ((256, T - 1), (257, T), 9 + c)):
                k.op("dve", lambda e: e.scalar_tensor_tensor(out[:, dst[0]:dst[1]], tmpA[:, src[0]:src[1]], cols[:, mc:mc + 1], out[:, dst[0]:dst[1]], op0=ALU.mult, op1=ALU.add),
                     reads=[tmpA, cols, out], writes=[out])

        tw = k.sb("rwtw", [128, T], BF16, st); al = k.sb("rwal", [128, T], BF16, st); sg = k.sb("rwsg", [128, T], BF16, st)
        shifted(6, tmpB); k.op("act", lambda e: e.activation(tw[:], tmpB[:], AF.Tanh), reads=[tmpB], writes=[tw])
        shifted(7, tmpB); k.op("act", lambda e: e.copy(al[:], tmpB[:]), reads=[tmpB], writes=[al])
        shifted(8, tmpB); k.op("act", lambda e: e.activation(sg[:], tmpB[:], AF.Sigmoid), reads=[tmpB], writes=[sg])
        groups = [(0, 512), (512, 512), (1024, 512), (1536, 512), (2048, 256)]
        chk(1)
        for P in range(2 if RW_STOP > 1 else 0):
            with ExitStack() as sp:
                r_f = k.sb("rwr", [128, T], F32, sp); k_f = k.sb("rwk", [128, T], F32, sp); kk_f = k.sb("rwkk", [128, T], F32, sp)
                vst = k.sb("rwvst", [128, NCH, 64], BF16, sp)
                ccol = k.sb("rwccol", [128, NCH], F32, sp)
                o_all = k.sb("rwoall", [128, NCH, 64], F32, sp)
                shifted(0 + P, r_f); shifted(2 + P, k_f)
                sv = ExitStack()
                vexp = k.sb("rwvexp", [128, NCH, 128], BF16, sv)
                shifted(4 + P, tmpB)
                k.op("pool", lambda e: e.memset(vexp[:], 0.0), writes=[vexp])
                for h in range(2):
                    hs = slice(h * 64, (h + 1) * 64)
                    k.op("dve", lambda e: e.tensor_copy(vexp[hs, :, hs], tmpB[hs, :].rearrange("p (c s) -> p c s", s=64)), reads=[tmpB], writes=[vexp])
                k.op("dve", lambda e: e.tensor_scalar(kk_f[:], k_f[:], cols[:, 26 + P:27 + P], None, op0=ALU.mult), reads=[k_f, cols], writes=[kk_f])
                k.op("act", lambda e: e.activation(tmpB[:], kk_f[:], AF.Square), reads=[kk_f], writes=[tmpB])
                with ExitStack() as s2:
                    pg = [k.ps("rwpn", [128, 512], F32, s2) for _ in range(2)]
                    for gi, (o, n) in enumerate(groups):
                        ps = pg[gi % 2]
                        k.op("pe", lambda e: e.matmul(ps[:, 0:n], lhsT=C["blk"][:], rhs=tmpB[:, o:o + n], start=True, stop=True), reads=[C["blk"], tmpB], writes=[ps])
                        k.op("dve", lambda e: e.tensor_scalar(tmpA[:, o:o + n], ps[:, 0:n], 1e-12, None, op0=ALU.add), reads=[ps], writes=[tmpA])
                    k.op("act", lambda e: e.sqrt(tmpA[:], tmpA[:]), reads=[tmpA], writes=[tmpA])
                    k.op("dve", lambda e: e.reciprocal(tmpA[:], tmpA[:]), reads=[tmpA], writes=[tmpA])
                    k.op("dve", lambda e: e.tensor_tensor(kk_f[:], kk_f[:], tmpA[:], ALU.mult), reads=[kk_f, tmpA], writes=[kk_f])
                    rkexp = k.sb("rwrkexp", [128, NCH, 128], BF16, s2)
                    k.op("pool", lambda e: e.memset(rkexp[:], 0.0), writes=[rkexp])
                    k.op("dve", lambda e: e.scalar_tensor_tensor(tmpB[:], r_f[:], cols[:, 30 + P:31 + P], k_f[:], op0=ALU.mult, op1=ALU.mult), reads=[r_f, k_f, cols], writes=[tmpB])
                    for h in range(2):
                        hs = slice(h * 64, (h + 1) * 64)
                        k.op("dve", lambda e: e.tensor_copy(rkexp[hs, :, hs], tmpB[hs, :].rearrange("p (c s) -> p c s", s=64)), reads=[tmpB], writes=[rkexp])
                    pv = [k.ps("rwpv", [128, 8, 64], F32, s2) for _ in range(2)]
                    pcc = k.ps("rwpcc", [128, NCH], F32, s2)
                    for c0 in range(0, NCH, 8):
                        nn = min(8, NCH - c0); ps = pv[(c0 // 8) % 2]
                        for j in range(nn):
                            k.op("pe", lambda e: e.matmul(ps[:, j, :], lhsT=vexp[:, c0 + j, :], rhs=C["isel"][:], start=True, stop=True), reads=[vexp, C["isel"]], writes=[ps])
                        k.op("act", lambda e: e.copy(vst[:, c0:c0 + nn, :], ps[:, 0:nn, :]), reads=[ps], writes=[vst])
                    for c in range(NCH):
                        k.op("pe", lambda e: e.matmul(pcc[:, c:c + 1], lhsT=rkexp[:, c, :], rhs=C["ones_bf"][:, 0:1], start=True, stop=True), reads=[rkexp, C["ones_bf"]], writes=[pcc])
                    k.op("dve", lambda e: e.tensor_copy(ccol[:], pcc[:]), reads=[pcc], writes=[ccol])
                    k.barrier()
                sv.close()
                if RW_STOP == 2: continue

                for d in range(2):
                    with ExitStack() as sd:
                        if RW_STOP < 7 and (P, d) != (0, 0): continue
                        rw_dir(k, sd, P, d, RW, C, cols, tw, al, r_f, k_f, kk_f, vst, o_all, tmpA, tmpB, groups, EM05)
                        k.barrier()
                if RW_STOP < 7: continue
                with ExitStack() as s2:
                    s1 = k.sb("rws1", [128, NCH], F32, s2); s2_ = k.sb("rws2", [128, NCH], F32, s2); rstd = k.sb("rwrstd", [128, NCH], F32, s2)
                    sq = k.sb("rwsq", [128, NCH, 64], F32, s2); fin = k.sb("rwfin", [128, NCH, 64], F32, s2)
                    k.op("dve", lambda e: e.reduce_sum(s1[:], o_all[:], axis=AX.X), reads=[o_all], writes=[s1])
                    k.op("act", lambda e: e.activation(sq[:], o_all[:], AF.Square), reads=[o_all], writes=[sq])
                    k.op("dve", lambda e: e.reduce_sum(s2_[:], sq[:], axis=AX.X), reads=[sq], writes=[s2_])
                    k.op("dve", lambda e: e.tensor_scalar(s1[:], s1[:], 1.0 / 64, None, op0=ALU.mult), reads=[s1], writes=[s1])
                    k.op("dve", lambda e: e.tensor_tensor(rstd[:], s1[:], s1[:], ALU.mult), reads=[s1], writes=[rstd])
                    k.op("dve", lambda e: e.scalar_tensor_tensor(rstd[:], s2_[:], 1.0 / 64, rstd[:], op0=ALU.mult, op1=ALU.subtract), reads=[s2_, rstd], writes=[rstd])
                    k.op("dve", lambda e: e.tensor_scalar(rstd[:], rstd[:], 64e-5, None, op0=ALU.add), reads=[rstd], writes=[rstd])
                    k.op("act", lambda e: e.sqrt(rstd[:], rstd[:]), reads=[rstd], writes=[rstd])
                    k.op("dve", lambda e: e.reciprocal(rstd[:], rstd[:]), reads=[rstd], writes=[rstd])
                    yfm = k.sb("rwyfm", [64, 2, T], BF16, s2)
                    gfm = k.sb("rwgfm", [64, 2, T], F32, s2); vstf = k.sb("rwvstf", [128, NCH, 64], F32, s2)
                    k.op("pool", lambda e: e.tensor_copy(vstf[:], vst[:]), reads=[vst], writes=[vstf])
                    pgg = [k.ps("rwpg", [128, 512], F32, s2) for _ in range(2)]
                    cnt = 0
                    for h in range(2):
                        hd = 2 * P + h
                        for (o, n) in groups:
                            ps = pgg[cnt % 2]
                            k.op("pe", lambda e: e.matmul(ps[0:64, 0:n], lhsT=RW["g2"][:, hd * 64:(hd + 1) * 64], rhs=sg[:, o:o + n], start=True, stop=True), reads=[RW["g2"], sg], writes=[ps])
                            k.op("act", lambda e: e.copy(gfm[:, h, o:o + n], ps[0:64, 0:n]), reads=[ps], writes=[gfm])
                            cnt += 1
                    gnw = RW["gn"][:, P, 0, :]; gnb = RW["gn"][:, P, 1, :]
                    for c in range(NCH):
                        k.op("dve", lambda e: e.scalar_tensor_tensor(sq[:, c, :], o_all[:, c, :], s1[:, c:c + 1], gnw, op0=ALU.subtract, op1=ALU.mult), reads=[o_all, s1, RW["gn"]], writes=[sq])
                        k.op("dve", lambda e: e.scalar_tensor_tensor(sq[:, c, :], sq[:, c, :], rstd[:, c:c + 1], gnb, op0=ALU.mult, op1=ALU.add), reads=[sq, rstd, RW["gn"]], writes=[sq])
                        k.op("dve", lambda e: e.scalar_tensor_tensor(fin[:, c, :], vstf[:, c, :], ccol[:, c:c + 1], sq[:, c, :], op0=ALU.mult, op1=ALU.add), reads=[vstf, ccol, sq], writes=[fin])
                    pts = [k.ps("rwpt", [64, 4, 128], F32, s2) for _ in range(2)]
                    for c0 in range(0, NCH, 4):
                        ps = pts[(c0 // 4) % 2]
                        for j in range(4):
                            k.op("pe", lambda e: e.transpose(ps[:, j, :], fin[:, c0 + j, :], C["ident_f"][:]), reads=[fin, C["ident_f"]], writes=[ps])
                        for h in range(2):
                            hd = 2 * P + h
                            k.op("dve", lambda e: e.tensor_tensor(yfm[:, h, c0 * 64:(c0 + 4) * 64].rearrange("p (c s) -> p c s", s=64), ps[:, :, h * 64:(h + 1) * 64],
                                                                  gfm[:, h, c0 * 64:(c0 + 4) * 64].rearrange("p (c s) -> p c s", s=64), ALU.mult), reads=[ps, gfm], writes=[yfm])
                    k.dma("sp", yrw_d[:, 2 * P:2 * P + 2, :], yfm[:], reads=[yfm], writes=[yrw_d])
                    k.barrier()
    k.barrier()


def rw_dir(k, sd, P, d, RW, C, cols, tw, al, r_f, k_f, kk_f, vst, o_all, tmpA, tmpB, groups, EM05):
    dsl = slice(d * 64, (d + 1) * 64)
    AR = k.sb("rwAR", [128, NCH, 2, 128], BF16, sd); Bx = k.sb("rwBx", [128, NCH, 128], BF16, sd); Kx = k.sb("rwKx", [128, NCH, 128], BF16, sd)
    gc = k.sb("rwgc", [128, NCH], F32, sd); eoff = k.sb("rweoff", [128, NCH, 2], F32, sd)
    stmp = ExitStack()
    Gp = k.sb("rwGp", [128, T + 1], F32, stmp)
    b_f = k.sb("rwb", [128, T], F32, stmp); kd_f = tmpB
    eB = k.sb("rweB", [128, T], F32, stmp); eA = k.sb("rweA", [128, T], F32, stmp); eR = tmpA
    with ExitStack() as s2:
        pg = [k.ps("rwpl", [128, 512], F32, s2) for _ in range(2)]
        cnt = 0
        for (o, n) in groups:
            ps = pg[cnt % 2]; cnt += 1
            k.op("pe", lambda e: e.matmul(ps[:, 0:n], lhsT=RW["w2"][dsl, P * 128:(P + 1) * 128], rhs=tw[dsl, o:o + n], start=True, stop=True), reads=[RW["w2"], tw], writes=[ps])
            k.op("act", lambda e: e.activation(tmpA[:, o:o + n], ps[:, 0:n], AF.Sigmoid, bias=cols[:, 18 + 2 * d + P:19 + 2 * d + P]), reads=[ps, cols], writes=[tmpA])
            ps = pg[cnt % 2]; cnt += 1
            k.op("pe", lambda e: e.matmul(ps[:, 0:n], lhsT=RW["a2"][dsl, P * 128:(P + 1) * 128], rhs=al[dsl, o:o + n], start=True, stop=True), reads=[RW["a2"], al], writes=[ps])
            k.op("act", lambda e: e.activation(tmpB[:, o:o + n], ps[:, 0:n], AF.Sigmoid, bias=cols[:, 22 + 2 * d + P:23 + 2 * d + P]), reads=[ps, cols], writes=[tmpB])
        k.barrier()
    k.op("dve", lambda e: e.tensor_tensor(b_f[:], tmpB[:], kk_f[:], ALU.mult), reads=[tmpB, kk_f], writes=[b_f])
    k.op("dve", lambda e: e.tensor_scalar(tmpB[:], tmpB[:], cols[:, 28 + P:29 + P], cols[:, 41 + P:42 + P], op0=ALU.mult, op1=ALU.add), reads=[tmpB, cols], writes=[tmpB])
    k.op("dve", lambda e: e.tensor_tensor(kd_f[:], k_f[:], tmpB[:], ALU.mult), reads=[k_f, tmpB], writes=[kd_f])
    k.op("dve", lambda e: e.tensor_scalar(tmpA[:], tmpA[:], -EM05, None, op0=ALU.mult), reads=[tmpA], writes=[tmpA])
    k.op("dve", lambda e: e.memset(Gp[:, 0:1], 0.0), writes=[Gp])
    k.op("pool", lambda e: e.memset(eA[:], 1.0), writes=[eA])
    k.op("dve", lambda e: e.tensor_tensor_scan(Gp[:, 1:T + 1], eA[:], tmpA[:], 0.0, op0=ALU.mult, op1=ALU.add), reads=[tmpA, eA, Gp], writes=[Gp])
    Gc0 = Gp[:, 0:T].rearrange("p (c s) -> p c s", s=64)[:, :, 0]; Gc1 = Gp[:, 1:T + 1].rearrange("p (c s) -> p c s", s=64)[:, :, 63]
    k.op("dve", lambda e: e.tensor_tensor(gc[:], Gc1, Gc0, ALU.subtract), reads=[Gp], writes=[gc])
    k.op("act", lambda e: e.activation(gc[:], gc[:], AF.Exp), reads=[gc], writes=[gc])
    Eref = Gc1 if d == 0 else Gc0
    k.op("dve", lambda e: e.tensor_copy(eoff[:, :, 0], Eref), reads=[Gp], writes=[eoff])
    k.op("dve", lambda e: e.tensor_scalar(eoff[:, :, 1], Eref, -1.0, None, op0=ALU.mult), reads=[Gp], writes=[eoff])
    k.barrier()
    if RW_STOP == 3:
        stmp.close(); return
    for c in range(NCH):
        cs = slice(c * 64, (c + 1) * 64); g1 = Gp[:, 1 + c * 64:1 + (c + 1) * 64]; g0 = Gp[:, c * 64:(c + 1) * 64]
        pE = eoff[:, c, 0:1]; nE = eoff[:, c, 1:2]
        if d == 0:
            k.op("act", lambda e: e.activation(eB[:, cs], g1, AF.Exp, scale=-1.0, bias=pE), reads=[Gp, eoff], writes=[eB])
            k.op("act", lambda e: e.activation(eA[:, cs], g0, AF.Exp, scale=1.0, bias=nE), reads=[Gp, eoff], writes=[eA])
            k.op("act", lambda e: e.activation(eR[:, cs], g1, AF.Exp, scale=1.0, bias=nE), reads=[Gp, eoff], writes=[eR])
        else:
            k.op("act", lambda e: e.activation(eB[:, cs], g0, AF.Exp, scale=1.0, bias=nE), reads=[Gp, eoff], writes=[eB])
            k.op("act", lambda e: e.activation(eA[:, cs], g1, AF.Exp, scale=-1.0, bias=pE), reads=[Gp, eoff], writes=[eA])
            k.op("act", lambda e: e.activation(eR[:, cs], g0, AF.Exp, scale=-1.0, bias=pE), reads=[Gp, eoff], writes=[eR])
    k.op("pool", lambda e: e.memset(AR[:], 0.0), writes=[AR]); k.op("pool", lambda e: e.memset(Bx[:], 0.0), writes=[Bx]); k.op("pool", lambda e: e.memset(Kx[:], 0.0), writes=[Kx])
    v3 = lambda tle, hs: tle[hs, :].rearrange("p (c s) -> p c s", s=64)
    for h in range(2):
        hs = slice(h * 64, (h + 1) * 64)
        k.op("dve", lambda e: e.tensor_tensor(AR[hs, :, 0, hs], v3(kk_f, hs), v3(eA, hs), ALU.mult), reads=[kk_f, eA], writes=[AR])
        k.op("dve", lambda e: e.tensor_tensor(AR[hs, :, 1, hs], v3(r_f, hs), v3(eR, hs), ALU.mult), reads=[r_f, eR], writes=[AR])
        k.op("dve", lambda e: e.tensor_tensor(Bx[hs, :, hs], v3(b_f, hs), v3(eB, hs), ALU.mult), reads=[b_f, eB], writes=[Bx])
        k.op("dve", lambda e: e.tensor_tensor(Kx[hs, :, hs], v3(kd_f, hs), v3(eB, hs), ALU.mult), reads=[kd_f, eB], writes=[Kx])
    k.barrier(); stmp.close()
    if RW_STOP == 4: return
    Rhat = k.sb("rwRhat", [128, NCH, 128], BF16, sd); Tt = k.sb("rwTt", [128, NCH, 128], BF16, sd)
    OV = k.sb("rwOV", [128, NCH, 64], F32, sd); HV = k.sb("rwHV", [128, NCH, 64], F32, sd)
    m2 = C["m2"][d]; m1 = C["m1"][d]
    with ExitStack() as s2:
        NS = 2
        banks = [[k.ps("rwb" + n, [128, 512], F32, s2) for n in "ABCD"] for _ in range(NS)]
        sbs = []
        for _ in range(NS):
            sbs.append(dict(MbS=k.sb("rwMbS", [128, 256], BF16, s2), MkS=k.sb("rwMkS", [128, 256], BF16, s2),
                            Ls=[k.sb("rwLs", [128, 2, 128], F32, s2) for _ in range(2)],
                            tm=k.sb("rwtm", [128, 3, 128], BF16, s2),
                            Ys=[k.sb("rwY", [128, 192], F32, s2) for _ in range(2)], nZ=k.sb("rwnZ", [128, 192], BF16, s2)))
        idb = C["ident_bf"]; m2t = C["m2t"]; m1t = C["m1t"]

        def chunk_gen(c, sl):
            bA, bB, bC, bD = banks[sl]; S = sbs[sl]
            MbS = S["MbS"]; MkS = S["MkS"]; Ls = S["Ls"]; tm = S["tm"]; Ys = S["Ys"]; nZ = S["nZ"]
            pMb = bA[:, 0:256]; pMk = bA[:, 256:512]; pTr = bB[:, 0:384].rearrange("p (a b) -> p a b", a=3); pMa = bB[:, 384:512]
            pL = bC[:, 0:256]; pOH = bC[:, 256:384]; pY = bD[:, 0:192]; pRT = bD[:, 192:448]
            arc = AR[:, c].rearrange("p a b -> p (a b)")
            k.op("pe", lambda e: e.matmul(pMb, lhsT=Bx[:, c, :], rhs=arc, start=True, stop=True), reads=[Bx, AR], writes=[bA])
            k.op("pe", lambda e: e.matmul(pMk, lhsT=Kx[:, c, :], rhs=arc, start=True, stop=True), reads=[Kx, AR], writes=[bA])
            k.op("pe", lambda e: e.matmul(pMa, lhsT=AR[:, c, 0, :], rhs=Bx[:, c, :], start=True, stop=True), reads=[AR, Bx], writes=[bB])
            k.op("pe", lambda e: e.matmul(pTr[:, 0, :], lhsT=Bx[:, c, :], rhs=idb[:], start=True, stop=True), reads=[Bx, idb], writes=[bB])
            k.op("pe", lambda e: e.matmul(pTr[:, 1, :], lhsT=Kx[:, c, :], rhs=idb[:], start=True, stop=True), reads=[Kx, idb], writes=[bB])
            k.op("pe", lambda e: e.matmul(pTr[:, 2, :], lhsT=AR[:, c, 0, :], rhs=idb[:], start=True, stop=True), reads=[AR, idb], writes=[bB])
            L0 = Ls[0]
            k.op("dve", lambda e: e.tensor_tensor(L0[:, 1, :], pMb[:, 0:128], m2[:, 0:128], ALU.mult), reads=[bA, m2t], writes=[L0])
            k.op("dve", lambda e: e.tensor_tensor(MkS[:], pMk, m2, ALU.mult), reads=[bA, m2t], writes=[MkS])
            k.op("dve", lambda e: e.tensor_tensor(MbS[:], pMb, m2, ALU.mult), reads=[bA, m2t], writes=[MbS])
            k.op("act", lambda e: e.copy(tm[:], pTr), reads=[bB], writes=[tm])
            k.op("dve", lambda e: e.tensor_tensor(L0[:, 0, :], pMa, m1, ALU.mult), reads=[bB, m1t], writes=[L0])
            yield
            Y = Ys[0]
            k.op("pe", lambda e: e.matmul(pY[:, 0:64], lhsT=MkS[:, 0:128], rhs=vst[:, c, :], start=True, stop=True), reads=[MkS, vst], writes=[bD])
            k.op("act", lambda e: e.copy(Y[:, 0:128], tm[:, 2, :]), reads=[tm], writes=[Y])
            k.op("act", lambda e: e.copy(Y[:, 128:192], pY[:, 0:64]), reads=[bD], writes=[Y])
            yield
            cur = 0
            for lvl in range(6):
                Lc = Ls[cur % 2]; Yc = Ys[lvl % 2]; Yn = Ys[(lvl + 1) % 2]
                k.op("pe", lambda e: e.matmul(pY, lhsT=Lc[:, 1, :], rhs=Yc[:], start=True, stop=True), reads=[Lc, Yc], writes=[bD])
                if lvl < 5:
                    k.op("pe", lambda e: e.matmul(pL[:, 128:256], lhsT=Lc[:, 0, :], rhs=Lc[:, 1, :], start=True, stop=True), reads=[Lc], writes=[bC])
                    if lvl < 4:
                        k.op("pe", lambda e: e.matmul(pL[:, 0:128], lhsT=Lc[:, 1, :], rhs=Lc[:, 0, :], start=True, stop=True), reads=[Lc], writes=[bC])
                k.op("dve", lambda e: e.tensor_tensor(Yn[:], Yc[:], pY, ALU.subtract if lvl == 0 else ALU.add), reads=[Yc, bD], writes=[Yn])
                if lvl < 5:
                    Ln = Ls[(cur + 1) % 2]
                    if lvl < 4:
                        k.op("act", lambda e: e.copy(Ln[:].rearrange("p a b -> p (a b)"), pL), reads=[bC], writes=[Ln])
                    else:
                        k.op("act", lambda e: e.copy(Ln[:, 1, :], pL[:, 128:256]), reads=[bC], writes=[Ln])
                    cur += 1
                yield
            k.op("dve", lambda e: e.tensor_scalar(nZ[:], Ys[0][:], -1.0, None, op0=ALU.mult), reads=[Ys[0]], writes=[nZ])
            yield
            k.op("pe", lambda e: e.matmul(pRT[:, 0:128], lhsT=idb[:], rhs=AR[:, c, 1, :], start=True, stop=False), reads=[idb, AR], writes=[bD])
            k.op("pe", lambda e: e.matmul(pRT[:, 0:128], lhsT=nZ[:, 0:128], rhs=MbS[:, 128:256], start=False, stop=True), reads=[nZ, MbS], writes=[bD])
            k.op("pe", lambda e: e.matmul(pRT[:, 128:256], lhsT=idb[:], rhs=idb[:], start=True, stop=False), reads=[idb], writes=[bD])
            k.op("pe", lambda e: e.matmul(pRT[:, 128:256], lhsT=nZ[:, 0:128], rhs=tm[:, 0, :], start=False, stop=True), reads=[nZ, tm], writes=[bD])
            k.op("pe", lambda e: e.matmul(pOH[:, 0:64], lhsT=MkS[:, 128:256], rhs=vst[:, c, :], start=True, stop=False), reads=[MkS, vst], writes=[bC])
            k.op("pe", lambda e: e.matmul(pOH[:, 0:64], lhsT=MbS[:, 128:256], rhs=nZ[:, 128:192], start=False, stop=True), reads=[MbS, nZ], writes=[bC])
            k.op("pe", lambda e: e.matmul(pOH[:, 64:128], lhsT=tm[:, 1, :], rhs=vst[:, c, :], start=True, stop=False), reads=[tm, vst], writes=[bC])
            k.op("pe", lambda e: e.matmul(pOH[:, 64:128], lhsT=tm[:, 0, :], rhs=nZ[:, 128:192], start=False, stop=True), reads=[tm, nZ], writes=[bC])
            k.op("dve", lambda e: e.tensor_copy(Rhat[:, c, :], pRT[:, 0:128]), reads=[bD], writes=[Rhat])
            k.op("dve", lambda e: e.tensor_copy(Tt[:, c, :], pRT[:, 128:256]), reads=[bD], writes=[Tt])
            k.op("act", lambda e: e.copy(OV[:, c, :], pOH[:, 0:64]), reads=[bC], writes=[OV])
            k.op("act", lambda e: e.copy(HV[:, c, :], pOH[:, 64:128]), reads=[bC], writes=[HV])
            yield

        for c0 in range(0, NCH, NS):
            alive = [chunk_gen(c0 + i, i) for i in range(NS) if c0 + i < NCH]
            while alive:
                for g in list(alive):
                    try:
                        next(g)
                    except StopIteration:
                        alive.remove(g)
        k.barrier()
    with ExitStack() as s2:
        Hs = [k.sb("rwHs", [128, 64], BF16, s2) for _ in range(2)]; HVg = k.sb("rwHVg", [128, NCH, 64], F32, s2)
        pO = [k.ps("rwpO", [128, 64], F32, s2) for _ in range(2)]; pH = [k.ps("rwpH", [128, 64], F32, s2) for _ in range(2)]
        order = list(range(NCH)) if d == 0 else [3, 2, 1, 0] + list(range(NCH - 1, 3, -1))
        for i in range(NCH - 1):
            c = order[i]; cn = order[i + 1]
            k.op("act", lambda e: e.activation(HVg[:, c, :], HV[:, c, :], AF.Copy, scale=gc[:, cn:cn + 1]), reads=[HV, gc], writes=[HVg])
        if d == 1:
            k.op("pool", lambda e: e.tensor_tensor(OV[:], OV[:], o_all[:], ALU.add), reads=[OV, o_all], writes=[OV])
        c = order[0]
        k.op("dve", lambda e: e.tensor_copy(Hs[1][:], HVg[:, c, :]), reads=[HVg], writes=[Hs[1]])
        k.op("dve", lambda e: e.tensor_copy(o_all[:, c, :], OV[:, c, :]), reads=[OV], writes=[o_all])
        for i in range(1, NCH):
            c = order[i]; hs = Hs[i % 2]; hn = Hs[(i + 1) % 2]; po = pO[i % 2]; ph = pH[i % 2]
            k.op("pe", lambda e: e.matmul(ph[:], lhsT=Tt[:, c, :], rhs=hs[:], start=True, stop=True), reads=[Tt, hs], writes=[ph])
            k.op("pe", lambda e: e.matmul(po[:], lhsT=Rhat[:, c, :], rhs=hs[:], start=True, stop=True), reads=[Rhat, hs], writes=[po])
            if i < NCH - 1:
                cn = order[i + 1]
                k.op("dve", lambda e: e.scalar_tensor_tensor(hn[:], ph[:], gc[:, cn:cn + 1], HVg[:, c, :], op0=ALU.mult, op1=ALU.add), reads=[ph, gc, HVg], writes=[hn])
            k.op("dve", lambda e: e.tensor_tensor(o_all[:, c, :], po[:], OV[:, c, :], ALU.add), reads=[po, OV], writes=[o_all])
        k.barrier()


def load_consts(k, st, CD):
    C = {}
    C["ident_f"] = k.sb("identf", [128, 128], F32, st); C["ident_bf"] = k.sb("identbf", [128, 128], BF16, st)
    C["ones_bf"] = k.sb("onesbf", [128, 128], BF16, st)
    k.dma("sp", C["ident_f"][:], CD["ident"][:], writes=[C["ident_f"]])
    k.op("dve", lambda e: e.tensor_copy(C["ident_bf"][:], C["ident_f"][:]), reads=[C["ident_f"]], writes=[C["ident_bf"]])
    k.op("dve", lambda e: e.memset(C["ones_bf"][:], 1.0), writes=[C["ones_bf"]])
    C["cos_d"] = CD["cos"]; C["sin_d"] = CD["sin"]
    m2t = k.sb("rwm2", [128, 2, 256], F32, st); m1t = k.sb("rwm1", [128, 2, 128], F32, st)
    k.dma("sp", m2t[:], CD["rw_m2"].t.rearrange("d p c -> p d c"), writes=[m2t])
    k.dma("sp", m1t[:], CD["rw_m1"].t.rearrange("d p c -> p d c"), writes=[m1t])
    C["m2t"] = m2t; C["m1t"] = m1t; C["m2"] = [m2t[:, 0, :], m2t[:, 1, :]]; C["m1"] = [m1t[:, 0, :], m1t[:, 1, :]]
    C["blk"] = k.sb("rwblk", [128, 128], F32, st); C["isel"] = k.sb("rwisel", [128, 64], BF16, st)
    k.dma("sp", C["blk"][:], CD["rw_blk"][:], writes=[C["blk"]])
    k.dma("pool", C["isel"][:], CD["rw_isel"][:], writes=[C["isel"]])
    return C


def const_arrays():
    cos, sin = rope_tables()
    m2, m1, blk, isel = rw_consts_host()
    p = np.arange(128)
    return {"ident": np.eye(128, dtype=np.float32), "cos": cos, "sin": sin, "rw_m2": m2, "rw_m1": m1, "rw_blk": blk, "rw_isel": isel,
            "na_mask": na_mask_host(),
            "tri": (p[:, None] < p[None, :]).astype(np.float32), "iota64": (p % 32).astype(np.float32)[:, None],
            "rowbase": (np.arange(8)[None, :] * 128 + p[:, None]).astype(np.float32), "blk512": np.tile(512.0 * np.arange(NBLK_MAX, dtype=np.float32)[None, :], (128, 1))}


CONST_SHAPES = {"ident": [128, 128], "cos": [SEQ, 128], "sin": [SEQ, 128], "rw_m2": [2, 128, 256], "rw_m1": [2, 128, 128], "rw_blk": [128, 128],
                "rw_isel": [128, 64], "na_mask": [128, 4, 64], "tri": [128, 128], "iota64": [128, 1], "rowbase": [128, 8], "blk512": [128, NBLK_MAX]}


def phase_post(k, l, b, NB, need_ctx, x_src, x_src_tk, mod_d, yrw_d, ymla_d, yna_d, W, C, x1_d, hT_d, GT_d, col0, hTM_d=None, Gtm_d=None):
    with ExitStack() as st:
        wo_rw = k.sb("worw", [64, 4, D], BF16, st); wo_mla = k.sb("womla", [128, 4, D], BF16, st); wo_na = k.sb("wona", [64, 4, D], BF16, st)
        wv = W["w_out"].t[l]
        k.dma("pool", wo_rw[:], wv[0:256, :].rearrange("(h p) n -> p h n", p=64), writes=[wo_rw])
        k.dma("pool", wo_mla[:], wv[256:768, :].rearrange("(h p) n -> p h n", p=128), writes=[wo_mla])
        k.dma("pool", wo_na[:], wv[768:1024, :].rearrange("(h p) n -> p h n", p=64), writes=[wo_na])
        rwt = k.sb("routw", [128, 8, NE], BF16, st); rbt = k.sb("routb", [128, NE], F32, st)
        k.dma("pool", rwt[:], W["router_w"].t[l].rearrange("(kc p) n -> p kc n", p=128), writes=[rwt])
        k.dma("sp", rbt[:], W["router_b"].t[l:l + 1, :].partition_broadcast(128), writes=[rbt])
        bc = {}
        for nm, src in (("g1", W["ln1_g"].t[l:l + 1, :]), ("b1", W["ln1_b"].t[l:l + 1, :]),
                        ("ga_l", mod_d[l][b:b + 1, 2 * D:3 * D]), ("sf_l", mod_d[l][b:b + 1, 3 * D:4 * D]), ("cf_l", mod_d[l][b:b + 1, 4 * D:5 * D]),
                        ("ga_c", mod_d[l][NB:NB + 1, 2 * D:3 * D]), ("sf_c", mod_d[l][NB:NB + 1, 3 * D:4 * D]), ("cf_c", mod_d[l][NB:NB + 1, 4 * D:5 * D])):
            if nm.endswith("_c") and not need_ctx:
                continue
            tle = k.sb(nm, [128, D], F32, st)
            k.dma("sp", tle[:], src.partition_broadcast(128), reads=[mod_d], writes=[tle])
            bc[nm] = tle
        for nm in ("cf_l", "cf_c"):
            if nm in bc:
                tle = bc[nm]
                k.op("pool", lambda e: e.tensor_scalar(tle[:], tle[:], 1.0, None, op0=ALU.add), reads=[tle], writes=[tle])
        yr = k.sb("yrall", [64, 4, T], BF16, st); ym = k.sb("ymall", [128, 4, T], BF16, st); yn = k.sb("ynall", [64, 4, T], BF16, st)
        k.dma("sp", yr[:], yrw_d[:], reads=[yrw_d], writes=[yr])
        k.dma("sp", ym[:], ymla_d.t.rearrange("h p t -> p h t"), reads=[ymla_d], writes=[ym])
        k.dma("sp", yn[:], yna_d[:], reads=[yna_d], writes=[yn])
        xts = [k.sb("pxt", [128, D], F32, st) for _ in range(2)]
        t1 = k.sb("pt1", [128, D], F32, st); u = k.sb("pu", [128, D], F32, st); x1 = k.sb("px1", [128, D], F32, st); hb = k.sb("phb", [128, D], BF16, st)
        st6 = k.sb("pst6", [128, 2, 6], F32, st); mv = k.sb("pmv", [128, 2], F32, st); rstd = k.sb("prstd", [128, 1], F32, st)
        hT = k.sb("phT", [128, 8, 128], BF16, st)
        lg = k.sb("plg", [128, NE], F32, st); mx8 = k.sb("pmx8", [128, 8], F32, st); nm1 = k.sb("pnm1", [128, 1], F32, st)
        msk = k.sb("pmsk", [128, NE], F32, st); ex = k.sb("pex", [128, NE], F32, st); ssum = k.sb("pssum", [128, 1], F32, st); gts = k.sb("pgts", [32, 128], F32, st)
        py = [k.ps("ppy", [128, 512], F32, st) for _ in range(2)]
        pT = k.ps("ppT", [128, 8, 128], BF16, st); pl = k.ps("ppl", [128, NE], F32, st); pg = k.ps("ppg", [32, 128], F32, st)
        for tt in range(NTT):
            isctx = tt < 2
            if isctx and not need_ctx:
                continue
            sfx = "_c" if isctx else "_l"
            sl = slice(tt * 128, (tt + 1) * 128)
            xt = xts[tt % 2]
            k.dma("sp", xt[:], x_src(tt), reads=[x_src_tk], writes=[xt])
            for half in range(2):
                ps = py[half]; cs = slice(half * 512, (half + 1) * 512)
                ops = [(yr, wo_rw, h) for h in range(4)] + [(ym, wo_mla, h) for h in range(4)] + [(yn, wo_na, h) for h in range(4)]
                for i, (ya, wa, h) in enumerate(ops):
                    k.op("pe", lambda e: e.matmul(ps[:], lhsT=ya[:, h, sl], rhs=wa[:, h, cs], start=(i == 0), stop=(i == 11)), reads=[ya, wa], writes=[ps])
                k.op("dve", lambda e: e.tensor_tensor(t1[:, cs], ps[:], bc["ga" + sfx][:, cs], ALU.mult), reads=[ps, bc["ga" + sfx]], writes=[t1])
            k.op("dve", lambda e: e.scalar_tensor_tensor(u[:], xt[:], DN_ALPHA, t1[:], op0=ALU.mult, op1=ALU.add), reads=[xt, t1], writes=[u])
            ln_stats(k, u, st6, mv, rstd, 1e-5)
            k.op("dve", lambda e: e.scalar_tensor_tensor(t1[:], u[:], mv[:, 0:1], bc["g1"][:], op0=ALU.subtract, op1=ALU.mult), reads=[u, mv, bc["g1"]], writes=[t1])
            k.op("dve", lambda e: e.scalar_tensor_tensor(x1[:], t1[:], rstd[:], bc["b1"][:], op0=ALU.mult, op1=ALU.add), reads=[t1, rstd, bc["b1"]], writes=[x1])
            k.dma("sp", x1_d[sl, :], x1[:], reads=[x1], writes=[x1_d])
            ln_stats(k, x1, st6, mv, rstd, 1e-5)
            k.op("dve", lambda e: e.scalar_tensor_tensor(t1[:], x1[:], mv[:, 0:1], bc["cf" + sfx][:], op0=ALU.subtract, op1=ALU.mult), reads=[x1, mv, bc["cf" + sfx]], writes=[t1])
            k.op("dve", lambda e: e.scalar_tensor_tensor(hb[:], t1[:], rstd[:], bc["sf" + sfx][:], op0=ALU.mult, op1=ALU.add), reads=[t1, rstd, bc["sf" + sfx]], writes=[hb])
            for c in range(8):
                k.op("pe", lambda e: e.transpose(pT[:, c, :], hb[:, c * 128:(c + 1) * 128], C["ident_bf"][:]), reads=[hb, C["ident_bf"]], writes=[pT])
            k.op("act", lambda e: e.copy(hT[:], pT[:]), reads=[pT], writes=[hT])
            if hTM_d is not None:
                k.dma("sp", hTM_d.t[col0 + tt * 128:col0 + (tt + 1) * 128, :], hb[:], reads=[hb], writes=[hTM_d])
            else:
                k.dma("sp", hT_d.t[:, col0 + tt * 128:col0 + (tt + 1) * 128].rearrange("(kc p) t -> p kc t", p=128), hT[:], reads=[hT], writes=[hT_d])
            for kc in range(8):
                k.op("pe", lambda e: e.matmul(pl[:], lhsT=hT[:, kc, :], rhs=rwt[:, kc, :], start=(kc == 0), stop=(kc == 7)), reads=[hT, rwt], writes=[pl])
            k.op("dve", lambda e: e.tensor_tensor(lg[:], pl[:], rbt[:], ALU.add), reads=[pl, rbt], writes=[lg])
            k.op("dve", lambda e: e.max(mx8[:], lg[:]), reads=[lg], writes=[mx8])
            k.op("dve", lambda e: e.tensor_scalar(msk[:], lg[:], mx8[:, 3:4], None, op0=ALU.is_ge), reads=[lg, mx8], writes=[msk])
            k.op("dve", lambda e: e.tensor_scalar(nm1[:], mx8[:, 0:1], -1.0, None, op0=ALU.mult), reads=[mx8], writes=[nm1])
            k.op("act", lambda e: e.activation(ex[:], lg[:], AF.Exp, bias=nm1[:]), reads=[lg, nm1], writes=[ex])
            k.op("dve", lambda e: e.tensor_tensor(ex[:], ex[:], msk[:], ALU.mult), reads=[ex, msk], writes=[ex])
            k.op("dve", lambda e: e.reduce_sum(ssum[:], ex[:], axis=AX.X), reads=[ex], writes=[ssum])
            k.op("dve", lambda e: e.reciprocal(ssum[:], ssum[:]), reads=[ssum], writes=[ssum])
            k.op("dve", lambda e: e.tensor_scalar(ex[:], ex[:], ssum[:, 0:1], None, op0=ALU.mult), reads=[ex, ssum], writes=[ex])
            if Gtm_d is not None:
                k.dma("sp", Gtm_d.t[col0 + tt * 128:col0 + (tt + 1) * 128, :], ex[:], reads=[ex], writes=[Gtm_d])
            else:
                k.op("pe", lambda e: e.transpose(pg[:], ex[:], C["ident_f"][:]), reads=[ex, C["ident_f"]], writes=[pg])
                k.op("act", lambda e: e.copy(gts[:], pg[:]), reads=[pg], writes=[gts])
                k.dma("sp", GT_d[:, col0 + tt * 128:col0 + (tt + 1) * 128], gts[:], reads=[gts], writes=[GT_d])
    k.barrier()


def phase_moe(k, l, NB, groups, tile_io, mod_d, W, C, hT_d, GT_d):
    GP = 2
    with ExitStack() as st:
        bgl = k.sb("bgl", [128, NE, 8], F32, st); bln = k.sb("bln", [128, NE, 8], F32, st)
        bdn = k.sb("bdn", [NE, D], F32, st)
        k.dma("sp", bdn[:], W["b_dn"].t[l], writes=[bdn])
        with ExitStack() as s2:
            bgu = k.sb("bgu", [NE, 2 * D], F32, s2)
            pb = [k.ps("pbg", [128, NE], F32, s2) for _ in range(2)]
            k.dma("sp", bgu[:], W["b_gu"].t[l], writes=[bgu])
            for j in range(8):
                for par, dst in ((0, bgl), (1, bln)):
                    ps = pb[par]
                    k.op("pe", lambda e: e.transpose(ps[:], bgu[:, 256 * j + par:256 * (j + 1):2], C["ident_f"][0:NE, 0:NE]), reads=[bgu, C["ident_f"]], writes=[ps])
                    k.op("dve", lambda e: e.tensor_scalar(dst[:, :, j], ps[:], float(par), None, op0=ALU.add), reads=[ps], writes=[dst])
            k.barrier()
        gfs = [k.sb("gf%d" % i, [128, D], F32, st) for i in range(GP)]; gf_key = [None] * GP
        ln2g = k.sb("ln2g", [128, D], F32, st); ln2b = k.sb("ln2b", [128, D], F32, st)
        k.dma("sp", ln2g[:], W["ln2_g"].t[l:l + 1, :].partition_broadcast(128), writes=[ln2g])
        k.dma("sp", ln2b[:], W["ln2_b"].t[l:l + 1, :].partition_broadcast(128), writes=[ln2b])
        NU = 4
        units = [k.sb("wu%d" % i, [128, 8, 1024], BF16, st) for i in range(NU)]
        hTs = [k.sb("mhT%d" % i, [128, 8, 512], BF16, st) for i in range(GP)]
        accs = [k.sb("macc%d" % i, [128, 8, 512], F32, st) for i in range(GP)]
        gTs = [k.sb("mgT%d" % i, [NE, 512], F32, st) for i in range(GP)]
        acts = [k.sb("mact%d" % i, [128, 8, 512], BF16, st) for i in range(2)]
        gbcs = [k.sb("mgbc%d" % i, [128, 512], F32, st) for i in range(2)]
        tt_ = [k.sb("mt%d" % i, [128, 512], F32, st) for i in range(2)]
        ss_ = [k.sb("ms%d" % i, [128, 512], F32, st) for i in range(2)]
        uu_ = [k.sb("mu%d" % i, [128, 512], F32, st) for i in range(2)]
        psg = [k.ps("mpsg", [128, 512], F32, st) for _ in range(2)]
        psl = [k.ps("mpsl", [128, 512], F32, st) for _ in range(2)]
        psd = [k.ps("mpsd", [128, 512], F32, st) for _ in range(2)]
        st6 = k.sb("mst6", [128, 2, 6], F32, st); mv = k.sb("mmv", [128, 2], F32, st); rstd = k.sb("mrstd", [128, 1], F32, st)
        ucount = 0; it = 0; dcount = 0
        wgu = W["w_gu"].t[l]; wdn = W["w_dn"].t[l]
        for p0 in range(0, len(groups), GP):
            pg = groups[p0:p0 + GP]
            for gi, (col, n, bkey) in enumerate(pg):
                k.dma("sp", hTs[gi][:, :, 0:n], hT_d.t[:, col:col + n].rearrange("(kc p) t -> p kc t", p=128), reads=[hT_d], writes=[hTs[gi]])
                k.dma("sp", gTs[gi][:, 0:n], GT_d[:, col:col + n], reads=[GT_d], writes=[gTs[gi]])
                if gf_key[gi] != bkey:
                    row = NB if bkey == "c" else bkey
                    k.dma("sp", gfs[gi][:], mod_d[l][row:row + 1, 5 * D:6 * D].partition_broadcast(128), reads=[mod_d], writes=[gfs[gi]])
                    gf_key[gi] = bkey
            for e in range(NE):
                ua = units[ucount % NU]; ub = units[(ucount + 1) % NU]; ud = units[(ucount + 2) % NU]; ucount += 3
                k.dma("pool", ua[:], wgu[e][:, 0:1024].rearrange("(kc p) n -> p kc n", p=128), writes=[ua])
                k.dma("pool", ub[:], wgu[e][:, 1024:2048].rearrange("(kc p) n -> p kc n", p=128), writes=[ub])
                k.dma("pool", ud[:], wdn[e].rearrange("(kc p) n -> p kc n", p=128), writes=[ud])
                for gi, (col, n, bkey) in enumerate(pg):
                    hTg = hTs[gi]; acc = accs[gi]
                    act = acts[it % 2]; gbc = gbcs[it % 2]; it += 1
                    k.dma("act", gbc[:, 0:n], GT_d[e:e + 1, col:col + n].partition_broadcast(128), reads=[GT_d], writes=[gbc])
                    for j in range(8):
                        un = ua if j < 4 else ub; c0 = (j % 4) * 256
                        pg_ = psg[j % 2]; pl_ = psl[j % 2]; t_ = tt_[j % 2]; s_ = ss_[j % 2]; u_ = uu_[j % 2]
                        for kc in range(8):
                            k.op("pe", lambda e_: e_.matmul(pg_[:, 0:n], lhsT=un[:, kc, c0:c0 + 256:2], rhs=hTg[:, kc, 0:n], start=(kc == 0), stop=(kc == 7)), reads=[un, hTg], writes=[pg_])
                        for kc in range(8):
                            k.op("pe", lambda e_: e_.matmul(pl_[:, 0:n], lhsT=un[:, kc, c0 + 1:c0 + 256:2], rhs=hTg[:, kc, 0:n], start=(kc == 0), stop=(kc == 7)), reads=[un, hTg], writes=[pl_])
                        k.op("dve", lambda e_: e_.tensor_scalar(t_[:, 0:n], pg_[:, 0:n], bgl[:, e, j:j + 1], 7.0, op0=ALU.add, op1=ALU.min), reads=[pg_, bgl], writes=[t_])
                        k.op("act", lambda e_: e_.activation(s_[:, 0:n], t_[:, 0:n], AF.Sigmoid, scale=1.702), reads=[t_], writes=[s_])
                        k.op("dve", lambda e_: e_.tensor_scalar(u_[:, 0:n], pl_[:, 0:n], bln[:, e, j:j + 1], 8.0, op0=ALU.add, op1=ALU.min), reads=[pl_, bln], writes=[u_])
                        k.op("pool", lambda e_: e_.tensor_tensor(t_[:, 0:n], t_[:, 0:n], s_[:, 0:n], ALU.mult), reads=[t_, s_], writes=[t_])
                        k.op("pool", lambda e_: e_.tensor_scalar(u_[:, 0:n], u_[:, 0:n], -6.0, None, op0=ALU.max), reads=[u_], writes=[u_])
                        k.op("pool", lambda e_: e_.tensor_tensor(u_[:, 0:n], u_[:, 0:n], t_[:, 0:n], ALU.mult), reads=[u_, t_], writes=[u_])
                        k.op("dve", lambda e_: e_.tensor_tensor(act[:, j, 0:n], u_[:, 0:n], gbc[:, 0:n], ALU.mult), reads=[u_, gbc], writes=[act])
                    for oc in range(8):
                        pd = psd[dcount % 2]; dcount += 1
                        for j in range(8):
                            k.op("pe", lambda e_: e_.matmul(pd[:, 0:n], lhsT=ud[:, j, oc * 128:(oc + 1) * 128], rhs=act[:, j, 0:n], start=(j == 0), stop=(j == 7)), reads=[ud, act], writes=[pd])
                        if e == 0:
                            k.op("act", lambda e_: e_.copy(acc[:, oc, 0:n], pd[:, 0:n]), reads=[pd], writes=[acc])
                        else:
                            k.op("dve", lambda e_: e_.tensor_tensor(acc[:, oc, 0:n], acc[:, oc, 0:n], pd[:, 0:n], ALU.add), reads=[acc, pd], writes=[acc])
            for gi, (col, n, bkey) in enumerate(pg):
                acc = accs[gi]; gT = gTs[gi]; gf = gfs[gi]
                for oc in range(8):
                    pd = psd[dcount % 2]; dcount += 1
                    k.op("pe", lambda e_: e_.matmul(pd[:, 0:n], lhsT=bdn[:, oc * 128:(oc + 1) * 128], rhs=gT[:, 0:n], start=True, stop=True), reads=[bdn, gT], writes=[pd])
                    k.op("dve", lambda e_: e_.tensor_tensor(acc[:, oc, 0:n], acc[:, oc, 0:n], pd[:, 0:n], ALU.add), reads=[acc, pd], writes=[acc])
                for ti in range(n // 128):
                    x1_ap, x1_tk, out_ap, out_tk = tile_io(col + ti * 128)
                    for half in range(2):
                        ps = psg[half]; cs = slice(half * 512, (half + 1) * 512)
                        k.dma("sp", ss_[half][:], x1_ap[:, cs], reads=[x1_tk], writes=[ss_[half]])
                        for c in range(4):
                            oc = half * 4 + c
                            k.op("pe", lambda e_: e_.transpose(ps[:, c * 128:(c + 1) * 128], acc[:, oc, ti * 128:(ti + 1) * 128], C["ident_f"][:]), reads=[acc, C["ident_f"]], writes=[ps])
                        k.op("dve", lambda e_: e_.tensor_tensor(tt_[half][:], ps[:], gf[:, cs], ALU.mult), reads=[ps, gf], writes=[tt_[half]])
                        k.op("dve", lambda e_: e_.scalar_tensor_tensor(uu_[half][:], ss_[half][:], DN_ALPHA, tt_[half][:], op0=ALU.mult, op1=ALU.add), reads=[ss_[half], tt_[half]], writes=[uu_[half]])
                        k.op("dve", lambda e_: e_.bn_stats(st6[:, half, :], uu_[half][:]), reads=[uu_[half]], writes=[st6])
                    k.op("dve", lambda e_: e_.bn_aggr(mv[:], st6[:].rearrange("p a b -> p (a b)")), reads=[st6], writes=[mv])
                    k.op("dve", lambda e_: e_.tensor_scalar(rstd[:], mv[:, 1:2], 1e-5, None, op0=ALU.add), reads=[mv], writes=[rstd])
                    k.op("act", lambda e_: e_.sqrt(rstd[:], rstd[:]), reads=[rstd], writes=[rstd])
                    k.op("dve", lambda e_: e_.reciprocal(rstd[:], rstd[:]), reads=[rstd], writes=[rstd])
                    for half in range(2):
                        cs = slice(half * 512, (half + 1) * 512)
                        k.op("dve", lambda e_: e_.scalar_tensor_tensor(tt_[half][:], uu_[half][:], mv[:, 0:1], ln2g[:, cs], op0=ALU.subtract, op1=ALU.mult), reads=[uu_[half], mv, ln2g], writes=[tt_[half]])
                        k.op("dve", lambda e_: e_.scalar_tensor_tensor(uu_[half][:], tt_[half][:], rstd[:], ln2b[:, cs], op0=ALU.mult, op1=ALU.add), reads=[tt_[half], rstd, ln2b], writes=[uu_[half]])
                        k.dma("sp", out_ap[:, cs], uu_[half][:], reads=[uu_[half]], writes=[out_tk])
    k.barrier()


def phase_moe_sparse(k, l, NB, need_ctx, tile_io, mod_d, W, C, CD, hTM_d, Gtm_d, Hs_d, Ys_d):
    IOA = bass.IndirectOffsetOnAxis
    ntb = NTT if need_ctx else NTT - 2
    NT = NB * ntb; NTOK = NT * 128
    NBLK = (4 * NTOK + NE * 511) // 512
    assert NBLK <= NBLK_MAX
    BIG = 65536.0
    rows = [b * T + (0 if need_ctx else NCTX) for b in range(NB)]
    tile_row = [rows[b] + j * 128 for b in range(NB) for j in range(ntb)]
    tile_key = [("c" if (need_ctx and j < 2) else b) for b in range(NB) for j in range(ntb)]
    with ExitStack() as st:
        dest_i = k.sb("mdesti", [128, NT, 4], I32, st); gk = k.sb("mgk", [128, NT, 4], F32, st)
        widx = k.sb("mwidx", [128, NBLK, 8], I32, st); be = k.sb("mbe", [128, NBLK], F32, st)
        iota64 = k.sb("miota", [128, 1], F32, st)
        k.dma("sp", iota64[:], CD["iota64"][:], writes=[iota64])
        with ExitStack() as s2:
            rowb = k.sb("mrowb", [128, 8], F32, s2); blk512 = k.sb("mblk512", [128, NBLK], F32, s2)
            trif = k.sb("mtrif", [128, 128], F32, s2); tri = k.sb("mtri", [128, 128], BF16, s2)
            k.dma("sp", rowb[:], CD["rowbase"][:], writes=[rowb]); k.dma("sp", blk512[:], CD["blk512"][:, 0:NBLK], writes=[blk512])
            k.dma("sp", trif[:], CD["tri"][:], writes=[trif])
            k.op("dve", lambda e: e.tensor_scalar(rowb[:], rowb[:], float(l * NE * D), None, op0=ALU.add), reads=[rowb], writes=[rowb])
            k.op("dve", lambda e: e.tensor_copy(tri[:], trif[:]), reads=[trif], writes=[tri])
            G_all = k.sb("mGall", [128, NT, NE], F32, s2); maskf = k.sb("mmaskf", [128, NT, NE], F32, s2); mask = k.sb("mmask", [128, NT, NE], BF16, s2)
            rank = k.sb("mrank", [128, NT, NE], F32, s2); tot = k.sb("mtot", [128, NT, NE], F32, s2); off = k.sb("moff", [128, NT, NE], F32, s2)
            val = k.sb("mval", [128, NT, NE], F32, s2)
            cnt = k.sb("mcnt", [128, NE], F32, s2); nbe = k.sb("mnbe", [128, NE], F32, s2); start = k.sb("mstart", [128, NE + 1], F32, s2)
            mx8 = k.sb("mmx8", [128, 8], F32, s2); tmp = k.sb("mtmp", [128, NE], F32, s2); destf = k.sb("mdestf", [128, NT, 4], F32, s2)
            widxf = k.sb("mwidxf", [128, NBLK, 8], F32, s2)
            z = k.sb("mz", [128, 4 * D], BF16, s2)
            k.op("pool", lambda e: e.memset(z[:], 0.0), writes=[z])
            for blk in range(NBLK):
                k.dma("sp" if blk % 2 else "act", Hs_d.t[blk * 512:(blk + 1) * 512, :].rearrange("(p r) d -> p (r d)", p=128), z[:], reads=[z])
            for b in range(NB):
                k.dma("sp", G_all[:, b * ntb:(b + 1) * ntb, :], Gtm_d.t[rows[b]:rows[b] + ntb * 128, :].rearrange("(i p) e -> p i e", p=128), reads=[Gtm_d], writes=[G_all])
            k.op("dve", lambda e: e.tensor_scalar(maskf[:], G_all[:], 0.0, None, op0=ALU.is_gt), reads=[G_all], writes=[maskf])
            k.op("dve", lambda e: e.tensor_copy(mask[:], maskf[:]), reads=[maskf], writes=[mask])
            pP = [k.ps("mpP", [128, 512], F32, s2) for _ in range(2)]; pTt = [k.ps("mpTt", [128, 512], F32, s2) for _ in range(2)]
            mflat = mask[:].rearrange("p i e -> p (i e)"); rflat = rank[:].rearrange("p i e -> p (i e)"); tflat = tot[:].rearrange("p i e -> p (i e)")
            ncols = NT * NE
            for c in range((ncols + 511) // 512):
                c0 = c * 512; n = min(512, ncols - c0); pa = pP[c % 2]; pb = pTt[c % 2]
                k.op("pe", lambda e: e.matmul(pa[:, 0:n], lhsT=tri[:], rhs=mflat[:, c0:c0 + n], start=True, stop=True), reads=[tri, mask], writes=[pa])
                k.op("pe", lambda e: e.matmul(pb[:, 0:n], lhsT=C["ones_bf"][:], rhs=mflat[:, c0:c0 + n], start=True, stop=True), reads=[C["ones_bf"], mask], writes=[pb])
                k.op("dve", lambda e: e.tensor_copy(rflat[:, c0:c0 + n], pa[:, 0:n]), reads=[pa], writes=[rank])
                k.op("act", lambda e: e.copy(tflat[:, c0:c0 + n], pb[:, 0:n]), reads=[pb], writes=[tot])
            k.op("dve", lambda e: e.reduce_sum(cnt[:], tot[:].rearrange("p i e -> p e i"), axis=AX.X), reads=[tot], writes=[cnt])
            k.op("dve", lambda e: e.memset(nbe[:], 0.0), writes=[nbe])
            for j in range(NTOK // 512):
                k.op("dve", lambda e: e.scalar_tensor_tensor(nbe[:], cnt[:], 512.0 * j, nbe[:], op0=ALU.is_gt, op1=ALU.add), reads=[cnt, nbe], writes=[nbe])
            k.op("dve", lambda e: e.tensor_scalar(nbe[:], nbe[:], 512.0, None, op0=ALU.mult), reads=[nbe], writes=[nbe])
            k.op("dve", lambda e: e.memset(start[:, 0:1], 0.0), writes=[start])
            for ee in range(NE):
                k.op("dve", lambda e: e.tensor_tensor(start[:, ee + 1:ee + 2], start[:, ee:ee + 1], nbe[:, ee:ee + 1], ALU.add), reads=[start, nbe], writes=[start])
            k.op("dve", lambda e: e.tensor_copy(off[:, 0, :], start[:, 0:NE]), reads=[start], writes=[off])
            for i in range(1, NT):
                k.op("dve", lambda e: e.tensor_tensor(off[:, i, :], off[:, i - 1, :], tot[:, i - 1, :], ALU.add), reads=[off, tot], writes=[off])
            k.op("dve", lambda e: e.tensor_tensor(rank[:], rank[:], off[:], ALU.add), reads=[rank, off], writes=[rank])
            k.op("dve", lambda e: e.tensor_scalar(rank[:], rank[:], -1.0, BIG, op0=ALU.mult, op1=ALU.add), reads=[rank], writes=[rank])
            k.op("dve", lambda e: e.tensor_tensor(val[:], rank[:], maskf[:], ALU.mult), reads=[rank, maskf], writes=[val])
            for i in range(NT):
                k.op("dve", lambda e: e.max(mx8[:], val[:, i, :]), reads=[val], writes=[mx8])
                k.op("dve", lambda e: e.tensor_scalar(destf[:, i, :], mx8[:, 0:4], -1.0, BIG, op0=ALU.mult, op1=ALU.add), reads=[mx8], writes=[destf])
                for kk in range(4):
                    k.op("dve", lambda e: e.scalar_tensor_tensor(tmp[:], val[:, i, :], mx8[:, kk:kk + 1], G_all[:, i, :], op0=ALU.is_equal, op1=ALU.mult), reads=[val, mx8, G_all], writes=[tmp])
                    k.op("dve", lambda e: e.reduce_sum(gk[:, i, kk:kk + 1], tmp[:], axis=AX.X), reads=[tmp], writes=[gk])
            k.op("dve", lambda e: e.tensor_copy(dest_i[:], destf[:]), reads=[destf], writes=[dest_i])
            k.op("dve", lambda e: e.memset(be[:], 0.0), writes=[be])
            for ee in range(NE):
                k.op("dve", lambda e: e.scalar_tensor_tensor(be[:], blk512[:], start[:, ee + 1:ee + 2], be[:], op0=ALU.is_ge, op1=ALU.add), reads=[blk512, start, be], writes=[be])
            k.op("dve", lambda e: e.tensor_scalar(be[:], be[:], float(NE - 1), None, op0=ALU.min), reads=[be], writes=[be])
            for kc in range(8):
                k.op("dve", lambda e: e.tensor_scalar(widxf[:, :, kc], be[:], 1024.0, rowb[:, kc:kc + 1], op0=ALU.mult, op1=ALU.add), reads=[be, rowb], writes=[widxf])
            k.op("dve", lambda e: e.tensor_copy(widx[:], widxf[:]), reads=[widxf], writes=[widx])
            k.barrier()
            hts = [k.sb("mht", [128, D], BF16, s2) for _ in range(2)]
            for i in range(NT):
                ht = hts[i % 2]
                k.dma("sp", ht[:], hTM_d.t[tile_row[i]:tile_row[i] + 128, :], reads=[hTM_d], writes=[ht])
                for kk in range(4):
                    k.idma(Hs_d.t[:, :], ht[:], out_offset=IOA(ap=dest_i[:, i, kk:kk + 1], axis=0), reads=[dest_i, ht])
            k.barrier()
        with ExitStack() as s3:
            bguHL = k.sb("mbguhl", [64, 2 * D], BF16, s3); bdnHL = k.sb("mbdnhl", [64, D], BF16, s3); iotaE = k.sb("miotaE", [64, 512], F32, s3)
            with ExitStack() as s4:
                for (src, dst, n) in ((W["b_gu"].t[l], bguHL, 2 * D), (W["b_dn"].t[l], bdnHL, D)):
                    bf_ = k.sb("mbf", [64, n], F32, s4); hif = k.sb("mhif", [64, n], F32, s4)
                    k.dma("sp", bf_[0:32, :], src, writes=[bf_]); k.dma("sp", bf_[32:64, :], src, writes=[bf_])
                    k.op("dve", lambda e: e.tensor_copy(dst[:], bf_[:]), reads=[bf_], writes=[dst])
                    k.op("dve", lambda e: e.tensor_copy(hif[:], dst[:]), reads=[dst], writes=[hif])
                    k.op("dve", lambda e: e.tensor_tensor(hif[32:64, :], bf_[32:64, :], hif[32:64, :], ALU.subtract), reads=[bf_, hif], writes=[hif])
                    k.op("dve", lambda e: e.tensor_copy(dst[32:64, :], hif[32:64, :]), reads=[hif], writes=[dst])
                k.op("dve", lambda e: e.memset(iotaE[:], 0.0), writes=[iotaE])
                k.op("dve", lambda e: e.tensor_scalar(iotaE[:], iotaE[:], iota64[0:64, 0:1], None, op0=ALU.add), reads=[iotaE, iota64], writes=[iotaE])
                k.barrier()
            wgs = [[k.sb("mwg", [128, 2 * D], BF16, s3) for _ in range(8)] for _ in range(2)]
            wds = [[k.sb("mwd", [128, D], BF16, s3) for _ in range(8)] for _ in range(2)]
            hss = [k.sb("mhs", [128, 4, D], BF16, s3) for _ in range(2)]; hTs = [k.sb("mhT", [128, 8, 512], BF16, s3) for _ in range(1)]
            acts = [k.sb("mact", [128, 8, 512], BF16, s3) for _ in range(1)]; ohs = [k.sb("moh", [64, 512], BF16, s3) for _ in range(2)]
            tt_ = [k.sb("mt", [128, 512], F32, s3) for _ in range(2)]; ss_ = [k.sb("ms", [128, 512], F32, s3) for _ in range(2)]; uu_ = [k.sb("mu", [128, 512], F32, s3) for _ in range(2)]
            ysb = [k.sb("mysb", [128, D], F32, s3) for _ in range(2)]
            pT = k.ps("mpT", [128, 8, 128], BF16, s3)
            psg = [k.ps("mpsg", [128, 512], F32, s3) for _ in range(2)]; psl = [k.ps("mpsl", [128, 512], F32, s3) for _ in range(2)]; psd = [k.ps("mpsd", [128, 512], F32, s3) for _ in range(2)]
            wgu2d = W["w_gu"].t.rearrange("l e r n -> (l e r) n"); wdn2d = W["w_dn"].t.rearrange("l e r n -> (l e r) n")

            def fetch(blk):
                wg = wgs[blk % 2]; wd = wds[blk % 2]
                for kc in range(8):
                    k.idma(wg[kc][:], wgu2d, in_offset=IOA(ap=widx[:, blk, kc:kc + 1], axis=0), reads=[widx], writes=[wg[kc]])
                for kc in range(8):
                    k.idma(wd[kc][:], wdn2d, in_offset=IOA(ap=widx[:, blk, kc:kc + 1], axis=0), reads=[widx], writes=[wd[kc]])
                k.dma("sp", hss[blk % 2][:], Hs_d.t[blk * 512:(blk + 1) * 512, :].rearrange("(r p) d -> p r d", p=128), writes=[hss[blk % 2]])
            hT = hTs[0]; act = acts[0]

            def to_fm(blk):
                hs = hss[blk % 2]
                for r in range(4):
                    for c in range(8):
                        k.op("pe", lambda e: e.transpose(pT[:, c, :], hs[:, r, c * 128:(c + 1) * 128], C["ident_bf"][:]), reads=[hs, C["ident_bf"]], writes=[pT])
                    if r % 2:
                        k.op("act", lambda e: e.copy(hT[:, :, r * 128:(r + 1) * 128], pT[:]), reads=[pT], writes=[hT])
                    else:
                        k.op("dve", lambda e: e.tensor_copy(hT[:, :, r * 128:(r + 1) * 128], pT[:]), reads=[pT], writes=[hT])
            fetch(0)
            to_fm(0)
            yc = 0; dc = 0
            for blk in range(NBLK):
                if blk + 1 < NBLK:
                    fetch(blk + 1)
                wg = wgs[blk % 2]; wd = wds[blk % 2]; oh = ohs[blk % 2]
                k.op("dve", lambda e: e.tensor_scalar(oh[:], iotaE[:], be[0:64, blk:blk + 1], None, op0=ALU.is_equal), reads=[iotaE, be], writes=[oh])
                for j in range(8):
                    pg_ = psg[j % 2]; pl_ = psl[j % 2]; t_ = tt_[j % 2]; s_ = ss_[j % 2]; u_ = uu_[j % 2]
                    for (ps_, o_) in ((pg_, 0), (pl_, 1)):
                        for kc in range(8):
                            k.op("pe", lambda e: e.matmul(ps_[:], lhsT=wg[kc][:, 256 * j + o_:256 * (j + 1):2], rhs=hT[:, kc, :], start=(kc == 0), stop=False), reads=[wg[kc], hT], writes=[ps_])
                        k.op("pe", lambda e: e.matmul(ps_[:], lhsT=bguHL[:, 256 * j + o_:256 * (j + 1):2], rhs=oh[:], start=False, stop=True), reads=[bguHL, oh], writes=[ps_])
                    k.op("dve", lambda e: e.tensor_scalar(t_[:], pg_[:], 7.0, None, op0=ALU.min), reads=[pg_], writes=[t_])
                    k.op("act", lambda e: e.activation(s_[:], t_[:], AF.Sigmoid, scale=1.702), reads=[t_], writes=[s_])
                    k.op("dve", lambda e: e.tensor_scalar(u_[:], pl_[:], 7.0, -7.0, op0=ALU.min, op1=ALU.max), reads=[pl_], writes=[u_])
                    k.op("dve", lambda e: e.tensor_tensor(t_[:], t_[:], s_[:], ALU.mult), reads=[t_, s_], writes=[t_])
                    k.op("dve", lambda e: e.scalar_tensor_tensor(act[:, j, :], u_[:], 1.0, t_[:], op0=ALU.add, op1=ALU.mult), reads=[u_, t_], writes=[act])
                if blk + 1 < NBLK:
                    to_fm(blk + 1)
                for r in range(4):
                    y = ysb[yc % 2]; yc += 1
                    for half in range(2):
                        pd = psd[dc % 2]; dc += 1; cs = slice(half * 512, (half + 1) * 512)
                        for j in range(8):
                            k.op("pe", lambda e: e.matmul(pd[:], lhsT=act[:, j, r * 128:(r + 1) * 128], rhs=wd[j][:, cs], start=(j == 0), stop=False), reads=[act, wd[j]], writes=[pd])
                        k.op("pe", lambda e: e.matmul(pd[:], lhsT=oh[:, r * 128:(r + 1) * 128], rhs=bdnHL[:, cs], start=False, stop=True), reads=[oh, bdnHL], writes=[pd])
                        if half:
                            k.op("act", lambda e: e.copy(y[:, cs], pd[:]), reads=[pd], writes=[y])
                        else:
                            k.op("dve", lambda e: e.tensor_copy(y[:, cs], pd[:]), reads=[pd], writes=[y])
                    k.dma("sp", Ys_d.t[blk * 512 + r * 128:blk * 512 + (r + 1) * 128, :], y[:], reads=[y])
            k.barrier()
        with ExitStack() as s5:
            gfs = {}
            for key in sorted(set(tile_key), key=str):
                row = NB if key == "c" else key
                gfs[key] = k.sb("mgf", [128, D], F32, s5)
                k.dma("sp", gfs[key][:], mod_d[l][row:row + 1, 5 * D:6 * D].partition_broadcast(128), reads=[mod_d], writes=[gfs[key]])
            ln2g = k.sb("ln2g", [128, D], F32, s5); ln2b = k.sb("ln2b", [128, D], F32, s5)
            k.dma("sp", ln2g[:], W["ln2_g"].t[l:l + 1, :].partition_broadcast(128), writes=[ln2g])
            k.dma("sp", ln2b[:], W["ln2_b"].t[l:l + 1, :].partition_broadcast(128), writes=[ln2b])
            yks = [[k.sb("myk", [128, D], F32, s5) for _ in range(4)] for _ in range(2)]
            x1s = [k.sb("mx1", [128, D], F32, s5) for _ in range(2)]
            acc = k.sb("macc", [128, D], F32, s5); uu = k.sb("muu", [128, D], F32, s5); oo = [k.sb("moo", [128, D], F32, s5) for _ in range(2)]
            st6 = k.sb("mst6", [128, 2, 6], F32, s5); mv = k.sb("mmv", [128, 2], F32, s5); rstd = k.sb("mrstd", [128, 1], F32, s5)
            for i in range(NT):
                yk = yks[i % 2]; x1t = x1s[i % 2]; o_ = oo[i % 2]; gf = gfs[tile_key[i]]
                x1_ap, x1_tk, out_ap, out_tk = tile_io(tile_row[i])
                for kk in range(4):
                    k.idma(yk[kk][:], Ys_d.t[:, :], in_offset=IOA(ap=dest_i[:, i, kk:kk + 1], axis=0), reads=[dest_i], writes=[yk[kk]])
                k.dma("sp", x1t[:], x1_ap, writes=[x1t])
                k.op("act", lambda e: e.activation(acc[:], yk[0][:], AF.Copy, scale=gk[:, i, 0:1]), reads=[yk[0], gk], writes=[acc])
                for kk in range(1, 4):
                    k.op("dve", lambda e: e.scalar_tensor_tensor(acc[:], yk[kk][:], gk[:, i, kk:kk + 1], acc[:], op0=ALU.mult, op1=ALU.add), reads=[yk[kk], gk, acc], writes=[acc])
                k.op("pool", lambda e: e.tensor_tensor(acc[:], acc[:], gf[:], ALU.mult), reads=[acc, gf], writes=[acc])
                k.op("dve", lambda e: e.scalar_tensor_tensor(uu[:], x1t[:], DN_ALPHA, acc[:], op0=ALU.mult, op1=ALU.add), reads=[x1t, acc], writes=[uu])
                ln_stats(k, uu, st6, mv, rstd, 1e-5)
                k.op("dve", lambda e: e.scalar_tensor_tensor(acc[:], uu[:], mv[:, 0:1], ln2g[:], op0=ALU.subtract, op1=ALU.mult), reads=[uu, mv, ln2g], writes=[acc])
                k.op("dve", lambda e: e.scalar_tensor_tensor(o_[:], acc[:], rstd[:], ln2b[:], op0=ALU.mult, op1=ALU.add), reads=[acc, rstd, ln2b], writes=[o_])
                k.dma("sp", out_ap, o_[:], reads=[o_])
            k.barrier()
    k.barrier()


WSHAPES = {"ada_w": [2, D, 6 * D], "ada_b": [2, 6 * D], "w_in": [2, D, IN_COLS],
           "rw_mu": [2, 2, 1152], "rw_w0": [2, 2, 256], "rw_w2": [2, 2, 64, 256], "rw_a0": [2, 2, 256], "rw_a2": [2, 2, 64, 256], "rw_g2": [2, 128, 256],
           "rw_kk": [2, 256], "rw_ka": [2, 256], "rw_rk": [2, 4, 64], "rw_gn_w": [2, 256], "rw_gn_b": [2, 256],
           "mla_q_norm": [2, 256], "mla_kv_norm": [2, 128], "mla_w_uq": [2, 256, 768], "mla_w_ukv": [2, 128, 1024],
           "w_out": [2, D, D], "ln1_g": [2, D], "ln1_b": [2, D], "router_w": [2, D, NE], "router_b": [2, NE],
           "w_gu": [2, NE, D, 2 * D], "b_gu": [2, NE, 2 * D], "w_dn": [2, NE, D, D], "b_dn": [2, NE, D], "ln2_g": [2, D], "ln2_b": [2, D]}


def build_nc(NB, debug=False, layers=(0, 1), moe=True, sparse=True, phases="BCDEF"):
    nc = bass.Bass("TRN2", target_bir_lowering=False)
    with ExitStack() as st:
        k = K(nc, st)
        kin = "ExternalInput"; sk = "ExternalOutput" if debug else "Internal"
        CD = {n: k.dram("c_" + n, sh, F32, kin) for n, sh in CONST_SHAPES.items()}
        W = {n: k.dram(n, sh, F32, kin) for n, sh in WSHAPES.items()}
        xin = k.dram("xin", [NB, T, D], F32, kin)
        condT = k.dram("condT", [128, 8, NB + 1], F32, kin)
        na_bias = k.dram("na_bias", [2, 128, 4, 8, 4, 64], F32, kin)
        out_d = k.dram("out", [NB, SEQ, D], F32, "ExternalOutput")
        mod_d = k.dram("mod_d", [2, NB + 1, 6 * D], F32, sk)
        pT_rw_d = k.dram("pT_rw_d", [RW_COLS, T], F32, sk); pT_na_d = k.dram("pT_na_d", [512, T], BF16, sk); p_tm_d = k.dram("p_tm_d", [T, 704], F32, sk)
        yrw_d = k.dram("yrw_d", [64, 4, T], BF16, sk); ymla_d = k.dram("ymla_d", [4, 128, T], BF16, sk); yna_d = k.dram("yna_d", [64, 4, T], BF16, sk)
        x1_d = k.dram("x1_d", [NB * T, D], F32, sk); x2_d = k.dram("x2_d", [NB, T, D], F32, sk)
        hT_d = k.dram("hT_d", [D, NB * T], BF16, sk); GT_d = k.dram("GT_d", [NE, NB * T], F32, sk)
        hTM_d = k.dram("hTM_d", [NB * T, D], BF16, sk); Gtm_d = k.dram("Gtm_d", [NB * T, NE], F32, sk)
        nblk0 = (4 * NB * T + NE * 511) // 512
        Hs_d = k.dram("Hs_d", [nblk0 * 512, D], BF16, "Internal"); Ys_d = k.dram("Ys_d", [nblk0 * 512, D], F32, "Internal")
        C = load_consts(k, st, CD)
        k.barrier()
        for l in layers:
            need_ctx = l < DEPTH - 1
            phase_mod(k, l, NB, condT, W["ada_w"], W["ada_b"], mod_d)
            for b in range(NB):
                if l == 0:
                    x_src = (lambda tt, b=b: xin.t[b][tt * 128:(tt + 1) * 128, :]); x_tk = xin
                else:
                    x_src = (lambda tt, b=b: x2_d.t[b][tt * 128:(tt + 1) * 128, :]); x_tk = x2_d
                with ExitStack() as ps:
                    w_in_bf = k.sb("winbf", [128, 8, IN_COLS], BF16, ps)
                    load_w_in(k, l, W["w_in"], w_in_bf)
                    phase_inproj(k, l, b, NB, x_src, x_tk, mod_d, w_in_bf, C["ident_bf"], pT_rw_d, pT_na_d, p_tm_d)
                with ExitStack() as ps:
                    if "C" in phases:
                        RW = load_rw_w(k, l, ps, W, C["ident_f"])
                        phase_rwkv(k, pT_rw_d, RW, C, yrw_d)
                with ExitStack() as ps:
                    if "D" in phases:
                        wuq, wukv = load_mla_w(k, l, ps, W["mla_w_uq"], W["mla_w_ukv"], W["mla_q_norm"], W["mla_kv_norm"])
                        phase_mla(k, need_ctx, p_tm_d, wuq, wukv, C["cos_d"], C["sin_d"], C["ident_bf"], C["ones_bf"], ymla_d)
                with ExitStack() as ps:
                    if "E" in phases:
                        bm = load_na_bias(k, l, ps, na_bias, CD["na_mask"])
                        phase_na(k, need_ctx, pT_na_d, p_tm_d, bm, C["ones_bf"], yna_d)
                x1_b = Tk(x1_d.t[b * T:(b + 1) * T, :], "x1b")
                if "F" not in phases:
                    continue
                if sparse:
                    phase_post(k, l, b, NB, need_ctx, x_src, x_tk, mod_d, yrw_d, ymla_d, yna_d, W, C, x1_b, hT_d, GT_d, b * T, hTM_d, Gtm_d)
                else:
                    phase_post(k, l, b, NB, need_ctx, x_src, x_tk, mod_d, yrw_d, ymla_d, yna_d, W, C, x1_b, hT_d, GT_d, b * T)
            groups = []
            for b in range(NB):
                if need_ctx:
                    groups.append((b * T, 256, "c"))
                for i in range(4):
                    groups.append((b * T + 256 + i * 512, 512, b))
            groups.sort(key=lambda g: -g[1])

            def tile_io(col, l=l):
                b = col // T; t = col % T
                x1_ap = x1_d.t[col:col + 128, :]
                if l == DEPTH - 1:
                    return x1_ap, x1_d, out_d.t[b][t - NCTX:t - NCTX + 128, :], out_d
                return x1_ap, x1_d, x2_d.t[b][t:t + 128, :], x2_d
            if moe and sparse:
                phase_moe_sparse(k, l, NB, need_ctx, tile_io, mod_d, W, C, CD, hTM_d, Gtm_d, Hs_d, Ys_d)
            elif moe:
                phase_moe(k, l, NB, groups, tile_io, mod_d, W, C, hT_d, GT_d)
        k.barrier()
        print("ninst", k.ninst, k.cnt, flush=True)
    return nc


def core_inputs(inputs, b0, NB, shared):
    cc = np.concatenate([inputs["c"][b0:b0 + NB], inputs["c_ctx"][None]], 0).astype(np.float32)
    im = dict(shared)
    im["condT"] = np.ascontiguousarray(cc.T.reshape(8, 128, NB + 1).transpose(1, 0, 2))
    im["xin"] = np.ascontiguousarray(np.concatenate([inputs["ctx"][b0:b0 + NB], inputs["x"][b0:b0 + NB]], 1))
    return im


def shared_inputs(inputs):
    sh = {n: np.ascontiguousarray(np.asarray(inputs[n], np.float32)) for n in WSHAPES}
    for n, v in const_arrays().items():
        sh["c_" + n] = np.ascontiguousarray(v.astype(np.float32))
    sh["na_bias"] = na_bias_host(np.asarray(inputs["na_rpb"], np.float32))
    return sh


def kernel(**inputs):
    NCORES = 8; B = inputs["x"].shape[0]; NB = B // NCORES
    nc = build_nc(NB)
    sh = shared_inputs(inputs)
    in_maps = [core_inputs(inputs, c * NB, NB, sh) for c in range(NCORES)]
    res = run_bass_kernel_spmd(nc, in_maps, core_ids=list(range(NCORES)))
    return np.concatenate([r["out"] for r in res.results], 0).astype(np.float32)
```

```python
import numpy as np
from contextlib import ExitStack
import concourse.bass as bass
import concourse.mybir as mybir
from concourse.bass_utils import run_bass_kernel_spmd

F32 = mybir.dt.float32; BF16 = mybir.dt.bfloat16; I32 = mybir.dt.int32
AF = mybir.ActivationFunctionType; ALU = mybir.AluOpType; AX = mybir.AxisListType

D = 1024; SEQ = 2048; NCTX = 256; T = SEQ + NCTX; DEPTH = 2; NTT = T // 128
RW_COLS = 1152; MLA_COLS = 448; NA_COLS = 768; IN_COLS = 2368
NE = 32
NBLK_MAX = 104
DN_ALPHA = (2 * DEPTH) ** 0.25


class Tk:
    __slots__ = ("t", "w", "r", "name", "psum")
    def __init__(s, t, name="", psum=False):
        s.t = t; s.w = None; s.r = []; s.name = name; s.psum = psum
    def __getitem__(s, idx):
        return s.t[idx]


class K:
    NDMA = 6
    def __init__(s, nc, stack):
        s.nc = nc; s.stack = stack
        s.eng = {"pe": nc.tensor, "act": nc.scalar, "dve": nc.vector, "pool": nc.gpsimd, "sp": nc.sync}
        s.sem = {}; s.cnt = {}
        for e in s.eng:
            s.sem[e] = stack.enter_context(nc.semaphore("s_" + e)); s.cnt[e] = 0
        s.seen = {e: {} for e in s.eng}
        s.dsem = [stack.enter_context(nc.semaphore("d%d" % i)) for i in range(3 * s.NDMA)]
        s.dcnt = [0] * (3 * s.NDMA); s.dnext = {"sp": 0, "act": 0, "pool": 0}; s.dq = {"sp": 0, "act": 1, "pool": 2}
        s.ninst = 0; s.uid = 0
    def sb(s, name, shape, dt, stack=None):
        s.uid += 1
        t = Tk((stack or s.stack).enter_context(s.nc.sbuf_tensor("%s_%d" % (name, s.uid), list(shape), dt)), name)
        assert s.nc.sbuf_bytes_remaining >= 28 * 1024, "SBUF budget exceeded at %s: remaining %d" % (name, s.nc.sbuf_bytes_remaining)
        return t
    def ps(s, name, shape, dt=F32, stack=None):
        s.uid += 1
        isz = 4 if dt == F32 else 2
        free = int(np.prod(shape[1:])); nb = (free * isz + 2047) // 2048
        raw = (stack or s.stack).enter_context(s.nc.psum_tensor("%s_%d" % (name, s.uid), [128, nb * 2048 // isz], dt))
        ap = raw[0:shape[0], 0:free]
        if len(shape) == 3:
            ap = ap.rearrange("p (a b) -> p a b", a=shape[1])
        return Tk(ap, name, psum=True)
    def dram(s, name, shape, dt, kind="Internal"):
        return Tk(s.nc.dram_tensor(name, list(shape), dt, kind=kind).ap(), name)
    def _wait(s, e, toks):
        seen = s.seen[e]
        for (sem, val, key) in toks:
            if seen.get(key, 0) >= val:
                continue
            s.eng[e].wait_ge(sem, val); seen[key] = val; s.ninst += 1
    def _deps(s, e, reads, writes):
        toks = []
        for t in reads:
            if t.w is not None:
                toks.append(t.w)
            if t.psum:
                toks.extend(k for k in t.r if k[2] != e)
        for t in writes:
            if t.w is not None:
                toks.append(t.w)
            toks.extend(t.r)
        if e == "pe":
            toks = [k for k in toks if k[2] != "pe"]
        return toks
    def op(s, e, fn, reads=(), writes=()):
        s._wait(e, s._deps(e, reads, writes))
        ins = fn(s.eng[e]); s.cnt[e] += 1; ins.then_inc(s.sem[e], 1); s.ninst += 1
        tok = (s.sem[e], s.cnt[e], e)
        for t in reads:
            t.r = [k for k in t.r if k[2] != e]; t.r.append(tok)
        for t in writes:
            t.w = tok; t.r = []
        return ins
    def dma(s, q, out, in_, reads=(), writes=(), **kw):
        base = s.dq[q] * s.NDMA; i = base + s.dnext[q]; s.dnext[q] = (s.dnext[q] + 1) % s.NDMA
        key = "d%d" % i
        toks = s._deps(q, reads, writes)
        if s.dcnt[i] > 0:
            toks.append((s.dsem[i], s.dcnt[i], key))
        s._wait(q, toks)
        ins = s.eng[q].dma_start(out=out, in_=in_, **kw); s.dcnt[i] += 16; ins.then_inc(s.dsem[i], 16); s.ninst += 1
        tok = (s.dsem[i], s.dcnt[i], key)
        for t in reads:
            t.r.append(tok)
        for t in writes:
            t.w = tok; t.r = []
        return ins
    def idma(s, out, in_, out_offset=None, in_offset=None, reads=(), writes=(), **kw):
        q = "pool"
        base = s.dq[q] * s.NDMA; i = base + s.dnext[q]; s.dnext[q] = (s.dnext[q] + 1) % s.NDMA
        key = "d%d" % i
        toks = s._deps(q, reads, writes)
        if s.dcnt[i] > 0:
            toks.append((s.dsem[i], s.dcnt[i], key))
        s._wait(q, toks)
        ins = s.eng[q].indirect_dma_start(out=out, out_offset=out_offset, in_=in_, in_offset=in_offset, **kw)
        s.dcnt[i] += 16; ins.then_inc(s.dsem[i], 16); s.ninst += 1
        tok = (s.dsem[i], s.dcnt[i], key)
        for t in reads:
            t.r.append(tok)
        for t in writes:
            t.w = tok; t.r = []
        return ins
    def barrier(s):
        toks = [(s.sem[e], s.cnt[e], e) for e in s.eng if s.cnt[e] > 0]
        toks += [(s.dsem[i], s.dcnt[i], "d%d" % i) for i in range(len(s.dsem)) if s.dcnt[i] > 0]
        for e in s.eng:
            s._wait(e, [k for k in toks if k[2] != e])
    def finish(s, outs):
        s._wait("sp", [t.w for t in outs if t.w is not None])


def bcast_rows(ap_row, n):
    return ap_row.partition_broadcast(n)


def phase_mod(k, l, NB, condT_d, ada_w_d, ada_b_d, mod_d):
    with ExitStack() as st:
        R = NB + 1
        cond = k.sb("cond", [128, 8, R], F32, st); cond_bf = k.sb("condbf", [128, 8, R], BF16, st)
        adab = k.sb("adab", [R, 6 * D], F32, st); mod_sb = k.sb("modsb", [R, 6 * D], F32, st)
        wts = [k.sb("adaw", [128, 8, 512], BF16, st) for _ in range(2)]
        pss = [k.ps("modps", [R, 512], F32, st) for _ in range(2)]
        k.dma("sp", cond[:], condT_d[:], writes=[cond])
        k.dma("sp", adab[:], ada_b_d[l:l + 1, :].partition_broadcast(R), writes=[adab])
        k.op("act", lambda e: e.activation(cond_bf[:], cond[:], AF.Silu), reads=[cond], writes=[cond_bf])
        wv = ada_w_d.t[l].rearrange("(kc p) n -> p kc n", p=128)
        for g in range(12):
            wt = wts[g % 2]; ps = pss[g % 2]
            k.dma("pool", wt[:], wv[:, :, g * 512:(g + 1) * 512], writes=[wt])
            for kc in range(8):
                k.op("pe", lambda e: e.matmul(ps[:], lhsT=cond_bf[:, kc, :], rhs=wt[:, kc, :], start=(kc == 0), stop=(kc == 7)),
                     reads=[cond_bf, wt], writes=[ps])
            k.op("dve", lambda e: e.tensor_tensor(mod_sb[:, g * 512:(g + 1) * 512], ps[:], adab[:, g * 512:(g + 1) * 512], ALU.add),
                 reads=[ps, adab], writes=[mod_sb])
        k.dma("sp", mod_d[l], mod_sb[:], reads=[mod_sb], writes=[mod_d])
    k.barrier()


def ln_stats(k, xt, st6, mv, rstd, eps):
    for hh in range(2):
        k.op("dve", lambda e: e.bn_stats(st6[:, hh, :], xt[:, hh * 512:(hh + 1) * 512]), reads=[xt], writes=[st6])
    k.op("dve", lambda e: e.bn_aggr(mv[:], st6[:].rearrange("p a b -> p (a b)")), reads=[st6], writes=[mv])
    k.op("dve", lambda e: e.tensor_scalar(rstd[:], mv[:, 1:2], eps, None, op0=ALU.add), reads=[mv], writes=[rstd])
    k.op("act", lambda e: e.sqrt(rstd[:], rstd[:]), reads=[rstd], writes=[rstd])
    k.op("dve", lambda e: e.reciprocal(rstd[:], rstd[:]), reads=[rstd], writes=[rstd])


def phase_inproj(k, l, b, NB, x_src, x_src_tk, mod_d, w_in_bf, ident_bf, pT_rw_d, pT_na_d, p_tm_d):
    with ExitStack() as st:
        bc = {}
        for nm, row, off in (("sa_l", b, 0), ("ca_l", b, D), ("sa_c", NB, 0), ("ca_c", NB, D)):
            tle = k.sb(nm, [128, D], F32, st)
            k.dma("sp", tle[:], mod_d[l][row:row + 1, off:off + D].partition_broadcast(128), reads=[mod_d], writes=[tle])
            bc[nm] = tle
        for nm in ("ca_l", "ca_c"):
            tle = bc[nm]
            k.op("pool", lambda e: e.tensor_scalar(tle[:], tle[:], 1.0, None, op0=ALU.add), reads=[tle], writes=[tle])
        groups = [(0, 512), (512, 512), (1024, 512), (1536, 512), (2048, 256)]
        xmT = [k.sb("xmT%d" % g, [128, 8, n], BF16, st) for g, (o, n) in enumerate(groups)]
        xts = [k.sb("xt", [128, D], F32, st) for _ in range(2)]
        t1 = k.sb("t1", [128, D], F32, st); xm = k.sb("xm", [128, D], BF16, st)
        st6 = k.sb("st6", [128, 2, 6], F32, st); mv = k.sb("mv", [128, 2], F32, st); rstd = k.sb("rstd", [128, 1], F32, st)
        pTs = [k.ps("pT", [128, 8, 128], BF16, st) for _ in range(2)]
        ptm = [k.ps("ptm", [128, 512], F32, st) for _ in range(2)]
        pfm = [k.ps("pfm", [128, 512], F32, st) for _ in range(2)]
        tm_sb = [k.sb("tmsb", [128, 704], F32, st) for _ in range(2)]
        fm_sb = [k.sb("fmsb", [128, 512], F32, st) for _ in range(2)]
        fm_sbb = [k.sb("fmsbb", [128, 512], BF16, st) for _ in range(2)]
        for tt in range(NTT):
            isctx = tt < 2
            sa = bc["sa_c" if isctx else "sa_l"]; ca = bc["ca_c" if isctx else "ca_l"]
            xt = xts[tt % 2]
            k.dma("sp", xt[:], x_src(tt), reads=[x_src_tk], writes=[xt])
            ln_stats(k, xt, st6, mv, rstd, 1e-5)
            k.op("dve", lambda e: e.scalar_tensor_tensor(t1[:], xt[:], mv[:, 0:1], ca[:], op0=ALU.subtract, op1=ALU.mult),
                 reads=[xt, mv, ca], writes=[t1])
            k.op("dve", lambda e: e.scalar_tensor_tensor(xm[:], t1[:], rstd[:], sa[:], op0=ALU.mult, op1=ALU.add),
                 reads=[t1, rstd, sa], writes=[xm])
            pT = pTs[tt % 2]
            for c in range(8):
                k.op("pe", lambda e: e.transpose(pT[:, c, :], xm[:, c * 128:(c + 1) * 128], ident_bf[:]), reads=[xm, ident_bf], writes=[pT])
            g = tt // 4; o = (tt % 4) * 128
            xg = xmT[g]
            k.op("act", lambda e: e.copy(xg[:, :, o:o + 128], pT[:]), reads=[pT], writes=[xg])
            pa = ptm[0]; pb = ptm[1]
            for kc in range(8):
                k.op("pe", lambda e: e.matmul(pa[:, 0:448], lhsT=xg[:, kc, o:o + 128], rhs=w_in_bf[:, kc, 1152:1600], start=(kc == 0), stop=(kc == 7)),
                     reads=[xg, w_in_bf], writes=[pa])
            for kc in range(8):
                k.op("pe", lambda e: e.matmul(pb[:, 0:256], lhsT=xg[:, kc, o:o + 128], rhs=w_in_bf[:, kc, 2112:2368], start=(kc == 0), stop=(kc == 7)),
                     reads=[xg, w_in_bf], writes=[pb])
            ts = tm_sb[tt % 2]
            k.op("act", lambda e: e.copy(ts[:, 0:448], pa[:, 0:448]), reads=[pa], writes=[ts])
            k.op("dve", lambda e: e.tensor_copy(ts[:, 448:704], pb[:, 0:256]), reads=[pb], writes=[ts])
            k.dma("sp", p_tm_d[tt * 128:(tt + 1) * 128, :], ts[:], reads=[ts], writes=[p_tm_d])
        cnt = 0
        for g, (o, n) in enumerate(groups):
            xg = xmT[g]
            for c in range(13):
                col = c * 128 if c < 9 else 1600 + (c - 9) * 128
                ps = pfm[cnt % 2]
                for kc in range(8):
                    k.op("pe", lambda e: e.matmul(ps[:, 0:n], lhsT=w_in_bf[:, kc, col:col + 128], rhs=xg[:, kc, :], start=(kc == 0), stop=(kc == 7)),
                         reads=[w_in_bf, xg], writes=[ps])
                if c < 9:
                    fs = fm_sb[cnt % 2]
                    k.op("act" if cnt % 2 else "dve", (lambda e: e.copy(fs[:, 0:n], ps[:, 0:n])) if cnt % 2 else (lambda e: e.tensor_copy(fs[:, 0:n], ps[:, 0:n])),
                         reads=[ps], writes=[fs])
                    k.dma("sp", pT_rw_d[c * 128:(c + 1) * 128, o:o + n], fs[:, 0:n], reads=[fs], writes=[pT_rw_d])
                else:
                    fs = fm_sbb[cnt % 2]
                    k.op("act" if cnt % 2 else "dve", (lambda e: e.copy(fs[:, 0:n], ps[:, 0:n])) if cnt % 2 else (lambda e: e.tensor_copy(fs[:, 0:n], ps[:, 0:n])),
                         reads=[ps], writes=[fs])
                    k.dma("sp", pT_na_d[(c - 9) * 128:(c - 8) * 128, o:o + n], fs[:, 0:n], reads=[fs], writes=[pT_na_d])
                cnt += 1
    k.barrier()


def load_w_in(k, l, w_in_d, w_in_bf):
    wv = w_in_d.t[l].rearrange("(kc p) n -> p kc n", p=128)
    for (a, bb) in ((0, 1024), (1024, 2048), (2048, IN_COLS)):
        k.dma("pool", w_in_bf[:, :, a:bb], wv[:, :, a:bb], writes=[w_in_bf])


def load_mla_w(k, l, st, w_uq_d, w_ukv_d, qn_d, kvn_d):
    wuq = k.sb("wuq", [128, 2, 768], BF16, st); wukv = k.sb("wukv", [128, 1024], BF16, st)
    with ExitStack() as s2:
        wuq_f = k.sb("wuqf", [128, 2, 768], F32, s2); wukv_f = k.sb("wukvf", [128, 1024], F32, s2)
        qn = k.sb("qn", [128, 2], F32, s2); kvn = k.sb("kvn", [128, 1], F32, s2)
        k.dma("sp", wuq_f[:], w_uq_d.t[l].rearrange("(kc p) n -> p kc n", p=128), writes=[wuq_f])
        k.dma("sp", wukv_f[:], w_ukv_d.t[l], writes=[wukv_f])
        k.dma("sp", qn[:], qn_d.t[l].rearrange("(kc p) -> p kc", p=128), writes=[qn], allow_slow_non_contiguous=True)
        k.dma("sp", kvn[:], kvn_d.t[l].rearrange("(p o) -> p o", o=1), writes=[kvn])
        for kc in range(2):
            k.op("dve", lambda e: e.tensor_scalar(wuq[:, kc, :], wuq_f[:, kc, :], qn[:, kc:kc + 1], None, op0=ALU.mult), reads=[wuq_f, qn], writes=[wuq])
        k.op("dve", lambda e: e.tensor_scalar(wukv[:], wukv_f[:], kvn[:, 0:1], None, op0=ALU.mult), reads=[wukv_f, kvn], writes=[wukv])
        k.barrier()
    return wuq, wukv


def phase_mla(k, need_ctx, p_tm_d, wuq, wukv, cos_d, sin_d, ident_bf, ones_bf, ymla_d):
    scale = 192.0 ** -0.5
    with ExitStack() as st:
        cos_sb = k.sb("cos", [128, 16, 128], F32, st); sin_sb = k.sb("sin", [128, 16, 128], F32, st)
        k.dma("sp", cos_sb[:], cos_d.t.rearrange("(tt p) c -> p tt c", p=128), writes=[cos_sb])
        k.dma("sp", sin_sb[:], sin_d.t.rearrange("(tt p) c -> p tt c", p=128), writes=[sin_sb])
        p_all = k.sb("pall", [128, NTT, 448], F32, st)
        k.dma("sp", p_all[:], p_tm_d.t.rearrange("(tt p) c -> p tt c", p=128)[:, :, 0:448], reads=[p_tm_d], writes=[p_all])
        qlnT = k.sb("qlnT", [128, 2, T], BF16, st); kvnT = k.sb("kvnT", [128, T], BF16, st)
        qTn = k.sb("qTn", [128, 4, T], BF16, st); qTr = k.sb("qTr", [64, 4, T], BF16, st)
        knT = k.sb("knT", [128, 4, T], BF16, st); krT = k.sb("krT", [64, T], BF16, st)
        v_sb = k.sb("vsb", [128, NTT, 512], BF16, st)
        rq = k.sb("rq", [128, NTT], F32, st); rkv = k.sb("rkv", [128, NTT], F32, st)
        with ExitStack() as st1:
            sq = k.sb("sq", [128, NTT, 384], F32, st1)
            k.op("act", lambda e: e.activation(sq[:], p_all[:, :, 0:384], AF.Square), reads=[p_all], writes=[sq])
            k.op("dve", lambda e: e.reduce_sum(rq[:], sq[:, :, 0:256], axis=AX.X), reads=[sq], writes=[rq])
            k.op("dve", lambda e: e.reduce_sum(rkv[:], sq[:, :, 256:384], axis=AX.X), reads=[sq], writes=[rkv])
            k.barrier()
        k.op("dve", lambda e: e.tensor_scalar(rq[:], rq[:], 1.0 / 256, 1e-6, op0=ALU.mult, op1=ALU.add), reads=[rq], writes=[rq])
        k.op("dve", lambda e: e.tensor_scalar(rkv[:], rkv[:], 1.0 / 128, 1e-6, op0=ALU.mult, op1=ALU.add), reads=[rkv], writes=[rkv])
        for r_ in (rq, rkv):
            k.op("act", lambda e: e.sqrt(r_[:], r_[:]), reads=[r_], writes=[r_])
            k.op("dve", lambda e: e.reciprocal(r_[:], r_[:]), reads=[r_], writes=[r_])
        st2 = ExitStack()
        nrm = [k.sb("nrm", [128, 384], BF16, st2) for _ in range(2)]
        pTs = k.ps("mlapT", [128, 3, 128], BF16, st2)
        pqs = [k.ps("mlapq", [128, 4, 256], F32, st2) for _ in range(2)]
        pv = k.ps("mlapv", [128, 512], F32, st2)
        q_sbs = [k.sb("qsb", [128, 4, 192], BF16, st2) for _ in range(2)]; kr_sbs = [k.sb("krsb", [128, 64], BF16, st2) for _ in range(2)]
        qr_fs = [k.sb("qrf", [128, 4, 64], F32, st2) for _ in range(2)]
        ra = k.sb("ra", [128, 4, 32], F32, st2); rb = k.sb("rb", [128, 4, 32], F32, st2)
        pqT = k.ps("mlapqT", [128, 4, 128], BF16, st2); pqTr = k.ps("mlapqTr", [64, 5, 128], BF16, st2)

        def stage_x(tt):
            isctx = tt < 2
            nt = nrm[tt % 2]; pT = pTs; pq = pqs[tt % 2]; q_sb = q_sbs[tt % 2]; kr_sb = kr_sbs[tt % 2]; qr_f = qr_fs[tt % 2]
            k.op("dve", lambda e: e.tensor_scalar(nt[:, 0:256], p_all[:, tt, 0:256], rq[:, tt:tt + 1], None, op0=ALU.mult), reads=[p_all, rq], writes=[nt])
            k.op("dve", lambda e: e.tensor_scalar(nt[:, 256:384], p_all[:, tt, 256:384], rkv[:, tt:tt + 1], None, op0=ALU.mult), reads=[p_all, rkv], writes=[nt])
            for c in range(3):
                k.op("pe", lambda e: e.transpose(pT[:, c, :], nt[:, c * 128:(c + 1) * 128], ident_bf[:]), reads=[nt, ident_bf], writes=[pT])
            sl = slice(tt * 128, (tt + 1) * 128)
            k.op("act", lambda e: e.copy(qlnT[:, :, sl], pT[:, 0:2, :]), reads=[pT], writes=[qlnT])
            k.op("act", lambda e: e.copy(kvnT[:, sl], pT[:, 2, :]), reads=[pT], writes=[kvnT])
            k.op("pe", lambda e: e.matmul(pv[:].rearrange("p (h c) -> p h c", h=4), lhsT=kvnT[:, sl],
                                          rhs=wukv[:].rearrange("p (h c) -> p h c", h=4)[:, :, 128:256], start=True, stop=True),
                 reads=[kvnT, wukv], writes=[pv])
            doq = (not isctx) or need_ctx
            if doq:
                for h in range(4):
                    for kc in range(2):
                        k.op("pe", lambda e: e.matmul(pq[:, h, 0:192], lhsT=qlnT[:, kc, sl], rhs=wuq[:, kc, h * 192:(h + 1) * 192], start=(kc == 0), stop=(kc == 1)),
                             reads=[qlnT, wuq], writes=[pq])
            k.op("act", lambda e: e.copy(v_sb[:, tt, :], pv[:]), reads=[pv], writes=[v_sb])
            kre = p_all[:, tt, 384:448:2]; kro = p_all[:, tt, 385:448:2]
            if isctx:
                k.op("dve", lambda e: e.tensor_copy(kr_sb[:, 0:32], kre), reads=[p_all], writes=[kr_sb])
                k.op("dve", lambda e: e.tensor_copy(kr_sb[:, 32:64], kro), reads=[p_all], writes=[kr_sb])
            else:
                cs = cos_sb[:, tt - 2, 0:32]; sn = sin_sb[:, tt - 2, 0:32]
                k.op("dve", lambda e: e.tensor_tensor(ra[:, 0, :], kre, cs, ALU.mult), reads=[p_all, cos_sb], writes=[ra])
                k.op("dve", lambda e: e.tensor_tensor(rb[:, 0, :], kro, sn, ALU.mult), reads=[p_all, sin_sb], writes=[rb])
                k.op("dve", lambda e: e.tensor_tensor(kr_sb[:, 0:32], ra[:, 0, :], rb[:, 0, :], ALU.subtract), reads=[ra, rb], writes=[kr_sb])
                k.op("dve", lambda e: e.tensor_tensor(ra[:, 0, :], kre, sn, ALU.mult), reads=[p_all, sin_sb], writes=[ra])
                k.op("dve", lambda e: e.tensor_tensor(rb[:, 0, :], kro, cs, ALU.mult), reads=[p_all, cos_sb], writes=[rb])
                k.op("dve", lambda e: e.tensor_tensor(kr_sb[:, 32:64], ra[:, 0, :], rb[:, 0, :], ALU.add), reads=[ra, rb], writes=[kr_sb])
            if doq:
                k.op("act", lambda e: e.copy(q_sb[:, :, 0:128], pq[:, :, 0:128]), reads=[pq], writes=[q_sb])
                k.op("act", lambda e: e.copy(qr_f[:], pq[:, :, 128:192]), reads=[pq], writes=[qr_f])
                qe = qr_f[:, :, 0:64:2]; qo = qr_f[:, :, 1:64:2]
                if isctx:
                    k.op("dve", lambda e: e.tensor_copy(q_sb[:, :, 128:160], qe), reads=[qr_f], writes=[q_sb])
                    k.op("dve", lambda e: e.tensor_copy(q_sb[:, :, 160:192], qo), reads=[qr_f], writes=[q_sb])
                else:
                    cs = cos_sb[:, tt - 2, :].rearrange("p (h c) -> p h c", h=4); sn = sin_sb[:, tt - 2, :].rearrange("p (h c) -> p h c", h=4)
                    k.op("dve", lambda e: e.tensor_tensor(ra[:], qe, cs, ALU.mult), reads=[qr_f, cos_sb], writes=[ra])
                    k.op("dve", lambda e: e.tensor_tensor(rb[:], qo, sn, ALU.mult), reads=[qr_f, sin_sb], writes=[rb])
                    k.op("dve", lambda e: e.tensor_tensor(q_sb[:, :, 128:160], ra[:], rb[:], ALU.subtract), reads=[ra, rb], writes=[q_sb])
                    k.op("dve", lambda e: e.tensor_tensor(ra[:], qe, sn, ALU.mult), reads=[qr_f, sin_sb], writes=[ra])
                    k.op("dve", lambda e: e.tensor_tensor(rb[:], qo, cs, ALU.mult), reads=[qr_f, cos_sb], writes=[rb])
                    k.op("dve", lambda e: e.tensor_tensor(q_sb[:, :, 160:192], ra[:], rb[:], ALU.add), reads=[ra, rb], writes=[q_sb])

        def stage_y(tt):
            isctx = tt < 2
            q_sb = q_sbs[tt % 2]; kr_sb = kr_sbs[tt % 2]
            sl = slice(tt * 128, (tt + 1) * 128)
            doq = (not isctx) or need_ctx
            if doq:
                for h in range(4):
                    k.op("pe", lambda e: e.transpose(pqT[:, h, :], q_sb[:, h, 0:128], ident_bf[:]), reads=[q_sb, ident_bf], writes=[pqT])
                    k.op("pe", lambda e: e.transpose(pqTr[:, h, :], q_sb[:, h, 128:192], ident_bf[:]), reads=[q_sb, ident_bf], writes=[pqTr])
            k.op("pe", lambda e: e.transpose(pqTr[:, 4, :], kr_sb[:], ident_bf[:]), reads=[kr_sb, ident_bf], writes=[pqTr])
            if doq:
                k.op("act", lambda e: e.copy(qTn[:, :, sl], pqT[:]), reads=[pqT], writes=[qTn])
                k.op("dve", lambda e: e.tensor_copy(qTr[:, :, sl], pqTr[:, 0:4, :]), reads=[pqTr], writes=[qTr])
            k.op("dve", lambda e: e.tensor_copy(krT[:, sl], pqTr[:, 4, :]), reads=[pqTr], writes=[krT])

        STOP = 99
        for tt in range(NTT):
            stage_x(tt)
            if tt > 0:
                stage_y(tt - 1)
        stage_y(NTT - 1)
        k.barrier(); st2.close()
        groups = [(0, 512), (512, 512), (1024, 512), (1536, 512), (2048, 256)]
        pk = [k.ps("mlapk", [128, 512], F32, st) for _ in range(2)]
        cnt = 0
        for h in range(4 if STOP > 2 else 0):
            for (o, n) in groups:
                ps = pk[cnt % 2]
                k.op("pe", lambda e: e.matmul(ps[:, 0:n], lhsT=wukv[:, h * 256:h * 256 + 128], rhs=kvnT[:, o:o + n], start=True, stop=True), reads=[wukv, kvnT], writes=[ps])
                k.op("act" if cnt % 2 else "dve", (lambda e: e.copy(knT[:, h, o:o + n], ps[:, 0:n])) if cnt % 2 else (lambda e: e.tensor_copy(knT[:, h, o:o + n], ps[:, 0:n])),
                     reads=[ps], writes=[knT])
                cnt += 1
        pos = [k.ps("mlapo", [128, 512], F32, st) for _ in range(2)]
        pss = [k.ps("mlapss", [128, 512], F32, st) for _ in range(2)]
        pts = [k.sb("mlapt", [128, 512], BF16, st) for _ in range(3)]
        rs = k.sb("mlars", [128, 512], F32, st); ob = [k.sb("mlaob", [128, 512], BF16, st) for _ in range(2)]
        qblocks = [(256 + i * 512, 512, list(range(NTT))) for i in range(4)]
        if need_ctx:
            qblocks.append((0, 256, [0, 1]))
        it = 0; bi = 0
        for h in range(4 if STOP > 3 else 0):
            for (qo_, nq, ktiles) in qblocks:
                po = pos[bi % 2]; psum = pss[bi % 2]
                for ji, j in enumerate(ktiles):
                    ps = pk[it % 2]; pt = pts[it % 3]
                    ks = slice(j * 128, (j + 1) * 128)
                    k.op("pe", lambda e: e.matmul(ps[:, 0:nq], lhsT=knT[:, h, ks], rhs=qTn[:, h, qo_:qo_ + nq], start=True, stop=False), reads=[knT, qTn], writes=[ps])
                    k.op("pe", lambda e: e.matmul(ps[:, 0:nq], lhsT=krT[:, ks], rhs=qTr[:, h, qo_:qo_ + nq], start=False, stop=True), reads=[krT, qTr], writes=[ps])
                    k.op("act", lambda e: e.activation(pt[:, 0:nq], ps[:, 0:nq], AF.Exp, scale=scale), reads=[ps], writes=[pt])
                    first = ji == 0; last = ji == len(ktiles) - 1
                    k.op("pe", lambda e: e.matmul(po[:, 0:nq], lhsT=v_sb[:, j, h * 128:(h + 1) * 128], rhs=pt[:, 0:nq], start=first, stop=last), reads=[v_sb, pt], writes=[po])
                    k.op("pe", lambda e: e.matmul(psum[:, 0:nq], lhsT=ones_bf[:], rhs=pt[:, 0:nq], start=first, stop=last), reads=[ones_bf, pt], writes=[psum])
                    it += 1
                o_ = ob[bi % 2]
                k.op("dve", lambda e: e.reciprocal(rs[:, 0:nq], psum[:, 0:nq]), reads=[psum], writes=[rs])
                k.op("dve", lambda e: e.tensor_tensor(o_[:, 0:nq], po[:, 0:nq], rs[:, 0:nq], ALU.mult), reads=[po, rs], writes=[o_])
                k.dma("sp", ymla_d[h][:, qo_:qo_ + nq], o_[:, 0:nq], reads=[o_], writes=[ymla_d])
                bi += 1
    k.barrier()


def rope_tables():
    t = np.arange(SEQ); row = (t // 64).astype(np.float32); col = (t % 64).astype(np.float32)
    inv = (10000.0 ** (-np.arange(16, dtype=np.float32) / 16)).astype(np.float32)
    ang = np.concatenate([row[:, None] * inv, col[:, None] * inv], -1).astype(np.float32)
    return np.tile(np.cos(ang).astype(np.float32), (1, 4)), np.tile(np.sin(ang).astype(np.float32), (1, 4))


def na_pattern(i):
    if i < 4:
        return i, 0
    if i <= 28:
        return 4, i - 4
    return i - 24, 24


def na_bias_host(rpb):
    L = rpb.shape[0]
    col = np.arange(64)
    dc = np.clip(col[None, :] - col[:, None] + 15, 0, 30)
    out = np.zeros((L, 128, 4, 8, 4, 64), np.float32)
    lk = np.arange(512); r = lk // 64; kc = lk % 64
    for i in list(range(5)) + [29, 30, 31]:
        pat, rs = na_pattern(i)
        dr = rs + r - i + 7
        g = rpb[:, :, dr[:, None], dc.T[kc, :]]
        out[:, :, :, pat] = g.reshape(L, 4, 4, 128, 64).transpose(0, 3, 1, 2, 4)
    return out


def na_mask_host():
    col = np.arange(64)
    cs = np.clip(col - 8, 0, 48)
    inw = (col[None, :] >= cs[:, None]) & (col[None, :] < cs[:, None] + 16)
    lk = np.arange(512); kc = lk % 64
    m = np.where(inw.T[kc, :], 0.0, -30000.0).astype(np.float32)
    return np.ascontiguousarray(m.reshape(4, 128, 64).transpose(1, 0, 2))


def load_na_bias(k, l, st, bias_d, mask_d):
    bm = k.sb("nabm", [128, 4, 8, 384], F32, st); mk = k.sb("namk", [128, 256], F32, st)
    k.op("pool", lambda e: e.memset(bm[:], 0.0), writes=[bm])
    k.dma("sp", mk[:], mask_d.t.rearrange("p j q -> p (j q)"), writes=[mk])
    for h in range(4):
        k.dma("sp", bm[:, h, :, 0:256], bias_d.t[l][:, h].rearrange("p a j q -> p a (j q)"), writes=[bm])
    for h in range(4):
        for pat in range(8):
            k.op("pool", lambda e: e.tensor_tensor(bm[:, h, pat, 0:256], bm[:, h, pat, 0:256], mk[:], ALU.add), reads=[bm, mk], writes=[bm])
    return bm


def phase_na(k, need_ctx, pT_na_d, p_tm_d, bm, ones_bf, yna_d):
    scale = 64.0 ** -0.5
    with ExitStack() as st:
        qT = k.sb("naqT", [64, 4, T], BF16, st); kT = k.sb("nakT", [64, 4, T], BF16, st)
        vA = k.sb("navA", [128, NTT, 256], BF16, st); vB = k.sb("navB", [128, NTT - 1, 256], BF16, st)
        yo = k.sb("nayo", [64, 4, T], BF16, st)
        k.dma("sp", qT[:], pT_na_d.t[0:256, :].rearrange("(h d) t -> d h t", h=4), reads=[pT_na_d], writes=[qT])
        k.dma("sp", kT[:], pT_na_d.t[256:512, :].rearrange("(h d) t -> d h t", h=4), reads=[pT_na_d], writes=[kT])
        k.dma("pool", vA[:], p_tm_d.t.rearrange("(tt p) c -> p tt c", p=128)[:, :, 448:704], reads=[p_tm_d], writes=[vA])
        k.dma("pool", vB[:], p_tm_d.t[64:64 + (NTT - 1) * 128, :].rearrange("(tt p) c -> p tt c", p=128)[:, :, 448:704], reads=[p_tm_d], writes=[vB])
        NSL = 4
        pSs = [k.ps("napS", [128, 6, 64], F32, st) for _ in range(NSL)]
        poms = [k.ps("napom", [64, 512], F32, st) for _ in range(NSL)]
        ssb = [k.sb("nassb", [128, 6, 64], F32, st) for _ in range(NSL)]
        pts = [k.sb("napt", [128, 6, 64], BF16, st) for _ in range(NSL)]
        ptc = [k.sb("naptc", [128, 2, 256], BF16, st) for _ in range(NSL)]
        rss = [k.sb("nars", [64, 256], F32, st) for _ in range(NSL)]
        it = 0
        for h in range(4):
            for i in range(32):
                pat, rs_ = na_pattern(i)
                pS = pSs[it % NSL]; pom = poms[it % NSL]; s_sb = ssb[it % NSL]; pt = pts[it % NSL]; rs = rss[it % NSL]
                tok0 = 256 + 64 * rs_
                qs = slice(256 + 64 * i, 256 + 64 * i + 64)
                for j in range(6):
                    ko = tok0 + 128 * j if j < 4 else (j - 4) * 128
                    k.op("pe", lambda e: e.matmul(pS[:, j, :], lhsT=kT[:, h, ko:ko + 128], rhs=qT[:, h, qs], start=True, stop=True), reads=[kT, qT], writes=[pS])
                k.op("dve", lambda e: e.scalar_tensor_tensor(s_sb[:].rearrange("p j q -> p (j q)"), pS[:].rearrange("p j q -> p (j q)"), scale, bm[:, h, pat, :], op0=ALU.mult, op1=ALU.add),
                     reads=[pS, bm], writes=[s_sb])
                k.op("act", lambda e: e.activation(pt[:], s_sb[:], AF.Exp), reads=[s_sb], writes=[pt])
                for j in range(6):
                    if j < 4:
                        vt = (vA, 2 + rs_ // 2 + j) if rs_ % 2 == 0 else (vB, (3 + rs_) // 2 + j)
                    else:
                        vt = (vA, j - 4)
                    vtk, vi = vt
                    k.op("pe", lambda e: e.matmul(pom[:, 0:64], lhsT=vtk[:, vi, h * 64:(h + 1) * 64], rhs=pt[:, j, :], start=(j == 0), stop=(j == 5)), reads=[vtk, pt], writes=[pom])
                for j in range(6):
                    k.op("pe", lambda e: e.matmul(pom[:, 256:320], lhsT=ones_bf[:, 0:64], rhs=pt[:, j, :], start=(j == 0), stop=(j == 5)), reads=[ones_bf, pt], writes=[pom])
                k.op("dve", lambda e: e.reciprocal(rs[:, 0:64], pom[:, 256:320]), reads=[pom], writes=[rs])
                k.op("dve", lambda e: e.tensor_tensor(yo[:, h, qs], pom[:, 0:64], rs[:, 0:64], ALU.mult), reads=[pom, rs], writes=[yo])
                it += 1
            if need_ctx:
                pS = pSs[it % NSL]; pom = poms[it % NSL]; pt = ptc[it % NSL]; rs = rss[it % NSL]
                pSv = pS[:].rearrange("p j q -> p (j q)")
                for j in range(2):
                    k.op("pe", lambda e: e.matmul(pSv[:, 0:256], lhsT=kT[:, h, j * 128:(j + 1) * 128], rhs=qT[:, h, 0:256], start=True, stop=True), reads=[kT, qT], writes=[pS])
                    k.op("act", lambda e: e.activation(pt[:, j, :], pSv[:, 0:256], AF.Exp, scale=scale), reads=[pS], writes=[pt])
                for j in range(2):
                    k.op("pe", lambda e: e.matmul(pom[:, 0:256], lhsT=vA[:, j, h * 64:(h + 1) * 64], rhs=pt[:, j, :], start=(j == 0), stop=(j == 1)), reads=[vA, pt], writes=[pom])
                for j in range(2):
                    k.op("pe", lambda e: e.matmul(pom[:, 256:512], lhsT=ones_bf[:, 0:64], rhs=pt[:, j, :], start=(j == 0), stop=(j == 1)), reads=[ones_bf, pt], writes=[pom])
                k.op("dve", lambda e: e.reciprocal(rs[:], pom[:, 256:512]), reads=[pom], writes=[rs])
                k.op("dve", lambda e: e.tensor_tensor(yo[:, h, 0:256], pom[:, 0:256], rs[:], ALU.mult), reads=[pom, rs], writes=[yo])
                it += 1
        k.dma("sp", yna_d[:], yo[:], reads=[yo], writes=[yna_d])
    k.barrier()


NCH = T // 64
RW_STOP = 99
class _Stop(Exception):
    pass
def chk(level):
    if RW_STOP == level:
        raise _Stop()


def rw_consts_host():
    i = np.arange(128) % 64
    lt = (i[None, :] < i[:, None]).astype(np.float32); le = (i[None, :] <= i[:, None]).astype(np.float32)
    gt = (i[None, :] > i[:, None]).astype(np.float32); ge = (i[None, :] >= i[:, None]).astype(np.float32)
    blk = ((np.arange(128)[:, None] // 64) == (np.arange(128)[None, :] // 64)).astype(np.float32)
    m2 = np.stack([np.concatenate([gt, ge], 1), np.concatenate([lt, le], 1)], 0)
    m1 = np.stack([lt, gt], 0)
    isel = np.concatenate([np.eye(64, dtype=np.float32)] * 2, 0)
    return m2, m1, blk, isel


def load_rw_w(k, l, st, W, ident_f):
    o = {}
    o["w2"] = k.sb("rww2", [128, 256], BF16, st); o["a2"] = k.sb("rwa2", [128, 256], BF16, st); o["g2"] = k.sb("rwg2", [128, 256], BF16, st)
    k.dma("pool", o["w2"][:], W["rw_w2"].t[l].rearrange("d k n -> (d k) n"), writes=[o["w2"]])
    k.dma("pool", o["a2"][:], W["rw_a2"].t[l].rearrange("d k n -> (d k) n"), writes=[o["a2"]])
    k.dma("pool", o["g2"][:], W["rw_g2"].t[l], writes=[o["g2"]])
    stage = k.sb("rwstage", [32, 128], F32, st)
    k.dma("sp", stage[0:18, :], W["rw_mu"].t[l].rearrange("d (c p) -> (d c) p", p=128), writes=[stage])
    k.dma("sp", stage[18:22, :], W["rw_w0"].t[l].rearrange("d (c p) -> (d c) p", p=128), writes=[stage])
    k.dma("sp", stage[22:26, :], W["rw_a0"].t[l].rearrange("d (c p) -> (d c) p", p=128), writes=[stage])
    k.dma("sp", stage[26:28, :], W["rw_kk"].t[l].rearrange("(c p) -> c p", p=128), writes=[stage])
    k.dma("sp", stage[28:30, :], W["rw_ka"].t[l].rearrange("(c p) -> c p", p=128), writes=[stage])
    k.dma("sp", stage[30:32, :], W["rw_rk"].t[l].rearrange("(c h) n -> c (h n)", c=2), writes=[stage])
    cols = k.sb("rwcols", [128, 48], F32, st)
    with ExitStack() as s2:
        pc = k.ps("rwpc", [128, 32], F32, s2)
        k.op("pe", lambda e: e.transpose(pc[:], stage[:], ident_f[0:32, 0:32]), reads=[stage, ident_f], writes=[pc])
        k.op("dve", lambda e: e.tensor_copy(cols[:, 0:32], pc[:]), reads=[pc], writes=[cols])
        k.barrier()
    k.op("dve", lambda e: e.tensor_tensor(cols[:, 32:41], cols[:, 0:9], cols[:, 9:18], ALU.add), reads=[cols], writes=[cols])
    k.op("dve", lambda e: e.tensor_scalar(cols[:, 32:41], cols[:, 32:41], -1.0, 1.0, op0=ALU.mult, op1=ALU.add), reads=[cols], writes=[cols])
    k.op("dve", lambda e: e.tensor_scalar(cols[:, 41:43], cols[:, 28:30], -1.0, 1.0, op0=ALU.mult, op1=ALU.add), reads=[cols], writes=[cols])
    o["cols"] = cols
    gn = k.sb("rwgn", [128, 2, 2, 64], F32, st)
    for P in range(2):
        for h in range(2):
            hd = 2 * P + h
            k.dma("sp", gn[h * 64:(h + 1) * 64, P, 0, :], W["rw_gn_w"].t[l:l + 1, hd * 64:(hd + 1) * 64].partition_broadcast(64), writes=[gn])
            k.dma("sp", gn[h * 64:(h + 1) * 64, P, 1, :], W["rw_gn_b"].t[l:l + 1, hd * 64:(hd + 1) * 64].partition_broadcast(64), writes=[gn])
    o["gn"] = gn
    return o


def phase_rwkv(k, pT_rw_d, RW, C, yrw_d):
    cols = RW["cols"]
    EM05 = float(np.exp(-0.5))
    with ExitStack() as st:
        tmpA = k.sb("rwtmpA", [128, T], F32, st); tmpB = k.sb("rwtmpB", [128, T], F32, st)

        def shifted(c, out):
            k.dma("sp", tmpA[:], pT_rw_d[c * 128:(c + 1) * 128, :], reads=[pT_rw_d], writes=[tmpA])
            k.op("act", lambda e: e.activation(out[:], tmpA[:], AF.Identity, scale=cols[:, 32 + c:33 + c]), reads=[tmpA, cols], writes=[out])
            for (dst, src, mc) in (((1, 256), (0, 255), c), ((257, T), (256, T - 1), c), ((0, 255), (1, 256), 9 + c), ((256, T - 1), (257, T), 9 + c)):
                k.op("dve", lambda e: e.scalar_tensor_tensor(out[:, dst[0]:dst[1]], tmpA[:, src[0]:src[1]], cols[:, mc:mc + 1], out[:, dst[0]:dst[1]], op0=ALU.mult, op1=ALU.add),
                     reads=[tmpA, cols, out], writes=[out])

        tw = k.sb("rwtw", [128, T], BF16, st); al = k.sb("rwal", [128, T], BF16, st); sg = k.sb("rwsg", [128, T], BF16, st)
        shifted(6, tmpB); k.op("act", lambda e: e.activation(tw[:], tmpB[:], AF.Tanh), reads=[tmpB], writes=[tw])
        shifted(7, tmpB); k.op("act", lambda e: e.copy(al[:], tmpB[:]), reads=[tmpB], writes=[al])
        shifted(8, tmpB); k.op("act", lambda e: e.activation(sg[:], tmpB[:], AF.Sigmoid), reads=[tmpB], writes=[sg])
        groups = [(0, 512), (512, 512), (1024, 512), (1536, 512), (2048, 256)]
        chk(1)
        for P in range(2 if RW_STOP > 1 else 0):
            with ExitStack() as sp:
                r_f = k.sb("rwr", [128, T], F32, sp); k_f = k.sb("rwk", [128, T], F32, sp); kk_f = k.sb("rwkk", [128, T], F32, sp)
                vst = k.sb("rwvst", [128, NCH, 64], BF16, sp)
                ccol = k.sb("rwccol", [128, NCH], F32, sp)
                o_all = k.sb("rwoall", [128, NCH, 64], F32, sp)
                shifted(0 + P, r_f); shifted(2 + P, k_f)
                sv = ExitStack()
                vexp = k.sb("rwvexp", [128, NCH, 128], BF16, sv)
                shifted(4 + P, tmpB)
                k.op("pool", lambda e: e.memset(vexp[:], 0.0), writes=[vexp])
                for h in range(2):
                    hs = slice(h * 64, (h + 1) * 64)
                    k.op("dve", lambda e: e.tensor_copy(vexp[hs, :, hs], tmpB[hs, :].rearrange("p (c s) -> p c s", s=64)), reads=[tmpB], writes=[vexp])
                k.op("dve", lambda e: e.tensor_scalar(kk_f[:], k_f[:], cols[:, 26 + P:27 + P], None, op0=ALU.mult), reads=[k_f, cols], writes=[kk_f])
                k.op("act", lambda e: e.activation(tmpB[:], kk_f[:], AF.Square), reads=[kk_f], writes=[tmpB])
                with ExitStack() as s2:
                    pg = [k.ps("rwpn", [128, 512], F32, s2) for _ in range(2)]
                    for gi, (o, n) in enumerate(groups):
                        ps = pg[gi % 2]
                        k.op("pe", lambda e: e.matmul(ps[:, 0:n], lhsT=C["blk"][:], rhs=tmpB[:, o:o + n], start=True, stop=True), reads=[C["blk"], tmpB], writes=[ps])
                        k.op("dve", lambda e: e.tensor_scalar(tmpA[:, o:o + n], ps[:, 0:n], 1e-12, None, op0=ALU.add), reads=[ps], writes=[tmpA])
                    k.op("act", lambda e: e.sqrt(tmpA[:], tmpA[:]), reads=[tmpA], writes=[tmpA])
                    k.op("dve", lambda e: e.reciprocal(tmpA[:], tmpA[:]), reads=[tmpA], writes=[tmpA])
                    k.op("dve", lambda e: e.tensor_tensor(kk_f[:], kk_f[:], tmpA[:], ALU.mult), reads=[kk_f, tmpA], writes=[kk_f])
                    rkexp = k.sb("rwrkexp", [128, NCH, 128], BF16, s2)
                    k.op("pool", lambda e: e.memset(rkexp[:], 0.0), writes=[rkexp])
                    k.op("dve", lambda e: e.scalar_tensor_tensor(tmpB[:], r_f[:], cols[:, 30 + P:31 + P], k_f[:], op0=ALU.mult, op1=ALU.mult), reads=[r_f, k_f, cols], writes=[tmpB])
                    for h in range(2):
                        hs = slice(h * 64, (h + 1) * 64)
                        k.op("dve", lambda e: e.tensor_copy(rkexp[hs, :, hs], tmpB[hs, :].rearrange("p (c s) -> p c s", s=64)), reads=[tmpB], writes=[rkexp])
                    pv = [k.ps("rwpv", [128, 8, 64], F32, s2) for _ in range(2)]
                    pcc = k.ps("rwpcc", [128, NCH], F32, s2)
                    for c0 in range(0, NCH, 8):
                        nn = min(8, NCH - c0); ps = pv[(c0 // 8) % 2]
                        for j in range(nn):
                            k.op("pe", lambda e: e.matmul(ps[:, j, :], lhsT=vexp[:, c0 + j, :], rhs=C["isel"][:], start=True, stop=True), reads=[vexp, C["isel"]], writes=[ps])
                        k.op("act", lambda e: e.copy(vst[:, c0:c0 + nn, :], ps[:, 0:nn, :]), reads=[ps], writes=[vst])
                    for c in range(NCH):
                        k.op("pe", lambda e: e.matmul(pcc[:, c:c + 1], lhsT=rkexp[:, c, :], rhs=C["ones_bf"][:, 0:1], start=True, stop=True), reads=[rkexp, C["ones_bf"]], writes=[pcc])
                    k.op("dve", lambda e: e.tensor_copy(ccol[:], pcc[:]), reads=[pcc], writes=[ccol])
                    k.barrier()
                sv.close()
                if RW_STOP == 2: continue

                for d in range(2):
                    with ExitStack() as sd:
                        if RW_STOP < 7 and (P, d) != (0, 0): continue
                        rw_dir(k, sd, P, d, RW, C, cols, tw, al, r_f, k_f, kk_f, vst, o_all, tmpA, tmpB, groups, EM05)
                        k.barrier()
                if RW_STOP < 7: continue
                with ExitStack() as s2:
                    s1 = k.sb("rws1", [128, NCH], F32, s2); s2_ = k.sb("rws2", [128, NCH], F32, s2); rstd = k.sb("rwrstd", [128, NCH], F32, s2)
                    sq = k.sb("rwsq", [128, NCH, 64], F32, s2); fin = k.sb("rwfin", [128, NCH, 64], F32, s2)
                    k.op("dve", lambda e: e.reduce_sum(s1[:], o_all[:], axis=AX.X), reads=[o_all], writes=[s1])
                    k.op("act", lambda e: e.activation(sq[:], o_all[:], AF.Square), reads=[o_all], writes=[sq])
                    k.op("dve", lambda e: e.reduce_sum(s2_[:], sq[:], axis=AX.X), reads=[sq], writes=[s2_])
                    k.op("dve", lambda e: e.tensor_scalar(s1[:], s1[:], 1.0 / 64, None, op0=ALU.mult), reads=[s1], writes=[s1])
                    k.op("dve", lambda e: e.tensor_tensor(rstd[:], s1[:], s1[:], ALU.mult), reads=[s1], writes=[rstd])
                    k.op("dve", lambda e: e.scalar_tensor_tensor(rstd[:], s2_[:], 1.0 / 64, rstd[:], op0=ALU.mult, op1=ALU.subtract), reads=[s2_, rstd], writes=[rstd])
                    k.op("dve", lambda e: e.tensor_scalar(rstd[:], rstd[:], 64e-5, None, op0=ALU.add), reads=[rstd], writes=[rstd])
                    k.op("act", lambda e: e.sqrt(rstd[:], rstd[:]), reads=[rstd], writes=[rstd])
                    k.op("dve", lambda e: e.reciprocal(rstd[:], rstd[:]), reads=[rstd], writes=[rstd])
                    yfm = k.sb("rwyfm", [64, 2, T], BF16, s2)
                    gfm = k.sb("rwgfm", [64, 2, T], F32, s2); vstf = k.sb("rwvstf", [128, NCH, 64], F32, s2)
                    k.op("pool", lambda e: e.tensor_copy(vstf[:], vst[:]), reads=[vst], writes=[vstf])
                    pgg = [k.ps("rwpg", [128, 512], F32, s2) for _ in range(2)]
                    cnt = 0
                    for h in range(2):
                        hd = 2 * P + h
                        for (o, n) in groups:
                            ps = pgg[cnt % 2]
                            k.op("pe", lambda e: e.matmul(ps[0:64, 0:n], lhsT=RW["g2"][:, hd * 64:(hd + 1) * 64], rhs=sg[:, o:o + n], start=True, stop=True), reads=[RW["g2"], sg], writes=[ps])
                            k.op("act", lambda e: e.copy(gfm[:, h, o:o + n], ps[0:64, 0:n]), reads=[ps], writes=[gfm])
                            cnt += 1
                    gnw = RW["gn"][:, P, 0, :]; gnb = RW["gn"][:, P, 1, :]
                    for c in range(NCH):
                        k.op("dve", lambda e: e.scalar_tensor_tensor(sq[:, c, :], o_all[:, c, :], s1[:, c:c + 1], gnw, op0=ALU.subtract, op1=ALU.mult), reads=[o_all, s1, RW["gn"]], writes=[sq])
                        k.op("dve", lambda e: e.scalar_tensor_tensor(sq[:, c, :], sq[:, c, :], rstd[:, c:c + 1], gnb, op0=ALU.mult, op1=ALU.add), reads=[sq, rstd, RW["gn"]], writes=[sq])
                        k.op("dve", lambda e: e.scalar_tensor_tensor(fin[:, c, :], vstf[:, c, :], ccol[:, c:c + 1], sq[:, c, :], op0=ALU.mult, op1=ALU.add), reads=[vstf, ccol, sq], writes=[fin])
                    pts = [k.ps("rwpt", [64, 4, 128], F32, s2) for _ in range(2)]
                    for c0 in range(0, NCH, 4):
                        ps = pts[(c0 // 4) % 2]
                        for j in range(4):
                            k.op("pe", lambda e: e.transpose(ps[:, j, :], fin[:, c0 + j, :], C["ident_f"][:]), reads=[fin, C["ident_f"]], writes=[ps])
                        for h in range(2):
                            hd = 2 * P + h
                            k.op("dve", lambda e: e.tensor_tensor(yfm[:, h, c0 * 64:(c0 + 4) * 64].rearrange("p (c s) -> p c s", s=64), ps[:, :, h * 64:(h + 1) * 64],
                                                                  gfm[:, h, c0 * 64:(c0 + 4) * 64].rearrange("p (c s) -> p c s", s=64), ALU.mult), reads=[ps, gfm], writes=[yfm])
                    k.dma("sp", yrw_d[:, 2 * P:2 * P + 2, :], yfm[:], reads=[yfm], writes=[yrw_d])
                    k.barrier()
    k.barrier()


def rw_dir(k, sd, P, d, RW, C, cols, tw, al, r_f, k_f, kk_f, vst, o_all, tmpA, tmpB, groups, EM05):
    dsl = slice(d * 64, (d + 1) * 64)
    AR = k.sb("rwAR", [128, NCH, 2, 128], BF16, sd); Bx = k.sb("rwBx", [128, NCH, 128], BF16, sd); Kx = k.sb("rwKx", [128, NCH, 128], BF16, sd)
    gc = k.sb("rwgc", [128, NCH], F32, sd); eoff = k.sb("rweoff", [128, NCH, 2], F32, sd)
    stmp = ExitStack()
    Gp = k.sb("rwGp", [128, T + 1], F32, stmp)
    b_f = k.sb("rwb", [128, T], F32, stmp); kd_f = tmpB
    eB = k.sb("rweB", [128, T], F32, stmp); eA = k.sb("rweA", [128, T], F32, stmp); eR = tmpA
    with ExitStack() as s2:
        pg = [k.ps("rwpl", [128, 512], F32, s2) for _ in range(2)]
        cnt = 0
        for (o, n) in groups:
            ps = pg[cnt % 2]; cnt += 1
            k.op("pe", lambda e: e.matmul(ps[:, 0:n], lhsT=RW["w2"][dsl, P * 128:(P + 1) * 128], rhs=tw[dsl, o:o + n], start=True, stop=True), reads=[RW["w2"], tw], writes=[ps])
            k.op("act", lambda e: e.activation(tmpA[:, o:o + n], ps[:, 0:n], AF.Sigmoid, bias=cols[:, 18 + 2 * d + P:19 + 2 * d + P]), reads=[ps, cols], writes=[tmpA])
            ps = pg[cnt % 2]; cnt += 1
            k.op("pe", lambda e: e.matmul(ps[:, 0:n], lhsT=RW["a2"][dsl, P * 128:(P + 1) * 128], rhs=al[dsl, o:o + n], start=True, stop=True), reads=[RW["a2"], al], writes=[ps])
            k.op("act", lambda e: e.activation(tmpB[:, o:o + n], ps[:, 0:n], AF.Sigmoid, bias=cols[:, 22 + 2 * d + P:23 + 2 * d + P]), reads=[ps, cols], writes=[tmpB])
        k.barrier()
    k.op("dve", lambda e: e.tensor_tensor(b_f[:], tmpB[:], kk_f[:], ALU.mult), reads=[tmpB, kk_f], writes=[b_f])
    k.op("dve", lambda e: e.tensor_scalar(tmpB[:], tmpB[:], cols[:, 28 + P:29 + P], cols[:, 41 + P:42 + P], op0=ALU.mult, op1=ALU.add), reads=[tmpB, cols], writes=[tmpB])
    k.op("dve", lambda e: e.tensor_tensor(kd_f[:], k_f[:], tmpB[:], ALU.mult), reads=[k_f, tmpB], writes=[kd_f])
    k.op("dve", lambda e: e.tensor_scalar(tmpA[:], tmpA[:], -EM05, None, op0=ALU.mult), reads=[tmpA], writes=[tmpA])
    k.op("dve", lambda e: e.memset(Gp[:, 0:1], 0.0), writes=[Gp])
    k.op("pool", lambda e: e.memset(eA[:], 1.0), writes=[eA])
    k.op("dve", lambda e: e.tensor_tensor_scan(Gp[:, 1:T + 1], eA[:], tmpA[:], 0.0, op0=ALU.mult, op1=ALU.add), reads=[tmpA, eA, Gp], writes=[Gp])
    Gc0 = Gp[:, 0:T].rearrange("p (c s) -> p c s", s=64)[:, :, 0]; Gc1 = Gp[:, 1:T + 1].rearrange("p (c s) -> p c s", s=64)[:, :, 63]
    k.op("dve", lambda e: e.tensor_tensor(gc[:], Gc1, Gc0, ALU.subtract), reads=[Gp], writes=[gc])
    k.op("act", lambda e: e.activation(gc[:], gc[:], AF.Exp), reads=[gc], writes=[gc])
    Eref = Gc1 if d == 0 else Gc0
    k.op("dve", lambda e: e.tensor_copy(eoff[:, :, 0], Eref), reads=[Gp], writes=[eoff])
    k.op("dve", lambda e: e.tensor_scalar(eoff[:, :, 1], Eref, -1.0, None, op0=ALU.mult), reads=[Gp], writes=[eoff])
    k.barrier()
    if RW_STOP == 3:
        stmp.close(); return
    for c in range(NCH):
        cs = slice(c * 64, (c + 1) * 64); g1 = Gp[:, 1 + c * 64:1 + (c + 1) * 64]; g0 = Gp[:, c * 64:(c + 1) * 64]
        pE = eoff[:, c, 0:1]; nE = eoff[:, c, 1:2]
        if d == 0:
            k.op("act", lambda e: e.activation(eB[:, cs], g1, AF.Exp, scale=-1.0, bias=pE), reads=[Gp, eoff], writes=[eB])
            k.op("act", lambda e: e.activation(eA[:, cs], g0, AF.Exp, scale=1.0, bias=nE), reads=[Gp, eoff], writes=[eA])
            k.op("act", lambda e: e.activation(eR[:, cs], g1, AF.Exp, scale=1.0, bias=nE), reads=[Gp, eoff], writes=[eR])
        else:
            k.op("act", lambda e: e.activation(eB[:, cs], g0, AF.Exp, scale=1.0, bias=nE), reads=[Gp, eoff], writes=[eB])
            k.op("act", lambda e: e.activation(eA[:, cs], g1, AF.Exp, scale=-1.0, bias=pE), reads=[Gp, eoff], writes=[eA])
            k.op("act", lambda e: e.activation(eR[:, cs], g0, AF.Exp, scale=-1.0, bias=pE), reads=[Gp, eoff], writes=[eR])
    k.op("pool", lambda e: e.memset(AR[:], 0.0), writes=[AR]); k.op("pool", lambda e: e.memset(Bx[:], 0.0), writes=[Bx]); k.op("pool", lambda e: e.memset(Kx[:], 0.0), writes=[Kx])
    v3 = lambda tle, hs: tle[hs, :].rearrange("p (c s) -> p c s", s=64)
    for h in range(2):
        hs = slice(h * 64, (h + 1) * 64)
        k.op("dve", lambda e: e.tensor_tensor(AR[hs, :, 0, hs], v3(kk_f, hs), v3(eA, hs), ALU.mult), reads=[kk_f, eA], writes=[AR])
        k.op("dve", lambda e: e.tensor_tensor(AR[hs, :, 1, hs], v3(r_f, hs), v3(eR, hs), ALU.mult), reads=[r_f, eR], writes=[AR])
        k.op("dve", lambda e: e.tensor_tensor(Bx[hs, :, hs], v3(b_f, hs), v3(eB, hs), ALU.mult), reads=[b_f, eB], writes=[Bx])
        k.op("dve", lambda e: e.tensor_tensor(Kx[hs, :, hs], v3(kd_f, hs), v3(eB, hs), ALU.mult), reads=[kd_f, eB], writes=[Kx])
    k.barrier(); stmp.close()
    if RW_STOP == 4: return
    Rhat = k.sb("rwRhat", [128, NCH, 128], BF16, sd); Tt = k.sb("rwTt", [128, NCH, 128], BF16, sd)
    OV = k.sb("rwOV", [128, NCH, 64], F32, sd); HV = k.sb("rwHV", [128, NCH, 64], F32, sd)
    m2 = C["m2"][d]; m1 = C["m1"][d]
    with ExitStack() as s2:
        NS = 2
        banks = [[k.ps("rwb" + n, [128, 512], F32, s2) for n in "ABCD"] for _ in range(NS)]
        sbs = []
        for _ in range(NS):
            sbs.append(dict(MbS=k.sb("rwMbS", [128, 256], BF16, s2), MkS=k.sb("rwMkS", [128, 256], BF16, s2),
                            Ls=[k.sb("rwLs", [128, 2, 128], F32, s2) for _ in range(2)],
                            tm=k.sb("rwtm", [128, 3, 128], BF16, s2),
                            Ys=[k.sb("rwY", [128, 192], F32, s2) for _ in range(2)], nZ=k.sb("rwnZ", [128, 192], BF16, s2)))
        idb = C["ident_bf"]; m2t = C["m2t"]; m1t = C["m1t"]

        def chunk_gen(c, sl):
            bA, bB, bC, bD = banks[sl]; S = sbs[sl]
            MbS = S["MbS"]; MkS = S["MkS"]; Ls = S["Ls"]; tm = S["tm"]; Ys = S["Ys"]; nZ = S["nZ"]
            pMb = bA[:, 0:256]; pMk = bA[:, 256:512]; pTr = bB[:, 0:384].rearrange("p (a b) -> p a b", a=3); pMa = bB[:, 384:512]
            pL = bC[:, 0:256]; pOH = bC[:, 256:384]; pY = bD[:, 0:192]; pRT = bD[:, 192:448]
            arc = AR[:, c].rearrange("p a b -> p (a b)")
            k.op("pe", lambda e: e.matmul(pMb, lhsT=Bx[:, c, :], rhs=arc, start=True, stop=True), reads=[Bx, AR], writes=[bA])
            k.op("pe", lambda e: e.matmul(pMk, lhsT=Kx[:, c, :], rhs=arc, start=True, stop=True), reads=[Kx, AR], writes=[bA])
            k.op("pe", lambda e: e.matmul(pMa, lhsT=AR[:, c, 0, :], rhs=Bx[:, c, :], start=True, stop=True), reads=[AR, Bx], writes=[bB])
            k.op("pe", lambda e: e.matmul(pTr[:, 0, :], lhsT=Bx[:, c, :], rhs=idb[:], start=True, stop=True), reads=[Bx, idb], writes=[bB])
            k.op("pe", lambda e: e.matmul(pTr[:, 1, :], lhsT=Kx[:, c, :], rhs=idb[:], start=True, stop=True), reads=[Kx, idb], writes=[bB])
            k.op("pe", lambda e: e.matmul(pTr[:, 2, :], lhsT=AR[:, c, 0, :], rhs=idb[:], start=True, stop=True), reads=[AR, idb], writes=[bB])
            L0 = Ls[0]
            k.op("dve", lambda e: e.tensor_tensor(L0[:, 1, :], pMb[:, 0:128], m2[:, 0:128], ALU.mult), reads=[bA, m2t], writes=[L0])
            k.op("dve", lambda e: e.tensor_tensor(MkS[:], pMk, m2, ALU.mult), reads=[bA, m2t], writes=[MkS])
            k.op("dve", lambda e: e.tensor_tensor(MbS[:], pMb, m2, ALU.mult), reads=[bA, m2t], writes=[MbS])
            k.op("act", lambda e: e.copy(tm[:], pTr), reads=[bB], writes=[tm])
            k.op("dve", lambda e: e.tensor_tensor(L0[:, 0, :], pMa, m1, ALU.mult), reads=[bB, m1t], writes=[L0])
            yield
            Y = Ys[0]
            k.op("pe", lambda e: e.matmul(pY[:, 0:64], lhsT=MkS[:, 0:128], rhs=vst[:, c, :], start=True, stop=True), reads=[MkS, vst], writes=[bD])
            k.op("act", lambda e: e.copy(Y[:, 0:128], tm[:, 2, :]), reads=[tm], writes=[Y])
            k.op("act", lambda e: e.copy(Y[:, 128:192], pY[:, 0:64]), reads=[bD], writes=[Y])
            yield
            cur = 0
            for lvl in range(6):
                Lc = Ls[cur % 2]; Yc = Ys[lvl % 2]; Yn = Ys[(lvl + 1) % 2]
                k.op("pe", lambda e: e.matmul(pY, lhsT=Lc[:, 1, :], rhs=Yc[:], start=True, stop=True), reads=[Lc, Yc], writes=[bD])
                if lvl < 5:
                    k.op("pe", lambda e: e.matmul(pL[:, 128:256], lhsT=Lc[:, 0, :], rhs=Lc[:, 1, :], start=True, stop=True), reads=[Lc], writes=[bC])
                    if lvl < 4:
                        k.op("pe", lambda e: e.matmul(pL[:, 0:128], lhsT=Lc[:, 1, :], rhs=Lc[:, 0, :], start=True, stop=True), reads=[Lc], writes=[bC])
                k.op("dve", lambda e: e.tensor_tensor(Yn[:], Yc[:], pY, ALU.subtract if lvl == 0 else ALU.add), reads=[Yc, bD], writes=[Yn])
                if lvl < 5:
                    Ln = Ls[(cur + 1) % 2]
                    if lvl < 4:
                        k.op("act", lambda e: e.copy(Ln[:].rearrange("p a b -> p (a b)"), pL), reads=[bC], writes=[Ln])
                    else:
                        k.op("act", lambda e: e.copy(Ln[:, 1, :], pL[:, 128:256]), reads=[bC], writes=[Ln])
                    cur += 1
                yield
            k.op("dve", lambda e: e.tensor_scalar(nZ[:], Ys[0][:], -1.0, None, op0=ALU.mult), reads=[Ys[0]], writes=[nZ])
            yield
            k.op("pe", lambda e: e.matmul(pRT[:, 0:128], lhsT=idb[:], rhs=AR[:, c, 1, :], start=True, stop=False), reads=[idb, AR], writes=[bD])
            k.op("pe", lambda e: e.matmul(pRT[:, 0:128], lhsT=nZ[:, 0:128], rhs=MbS[:, 128:256], start=False, stop=True), reads=[nZ, MbS], writes=[bD])
            k.op("pe", lambda e: e.matmul(pRT[:, 128:256], lhsT=idb[:], rhs=idb[:], start=True, stop=False), reads=[idb], writes=[bD])
            k.op("pe", lambda e: e.matmul(pRT[:, 128:256], lhsT=nZ[:, 0:128], rhs=tm[:, 0, :], start=False, stop=True), reads=[nZ, tm], writes=[bD])
            k.op("pe", lambda e: e.matmul(pOH[:, 0:64], lhsT=MkS[:, 128:256], rhs=vst[:, c, :], start=True, stop=False), reads=[MkS, vst], writes=[bC])
            k.op("pe", lambda e: e.matmul(pOH[:, 0:64], lhsT=MbS[:, 128:256], rhs=nZ[:, 128:192], start=False, stop=True), reads=[MbS, nZ], writes=[bC])
            k.op("pe", lambda e: e.matmul(pOH[:, 64:128], lhsT=tm[:, 1, :], rhs=vst[:, c, :], start=True, stop=False), reads=[tm, vst], writes=[bC])
            k.op("pe", lambda e: e.matmul(pOH[:, 64:128], lhsT=tm[:, 0, :], rhs=nZ[:, 128:192], start=False, stop=True), reads=[tm, nZ], writes=[bC])
            k.op("dve", lambda e: e.tensor_copy(Rhat[:, c, :], pRT[:, 0:128]), reads=[bD], writes=[Rhat])
            k.op("dve", lambda e: e.tensor_copy(Tt[:, c, :], pRT[:, 128:256]), reads=[bD], writes=[Tt])
            k.op("act", lambda e: e.copy(OV[:, c, :], pOH[:, 0:64]), reads=[bC], writes=[OV])
            k.op("act", lambda e: e.copy(HV[:, c, :], pOH[:, 64:128]), reads=[bC], writes=[HV])
            yield

        for c0 in range(0, NCH, NS):
            alive = [chunk_gen(c0 + i, i) for i in range(NS) if c0 + i < NCH]
            while alive:
                for g in list(alive):
                    try:
                        next(g)
                    except StopIteration:
                        alive.remove(g)
        k.barrier()
    with ExitStack() as s2:
        Hs = [k.sb("rwHs", [128, 64], BF16, s2) for _ in range(2)]; HVg = k.sb("rwHVg", [128, NCH, 64], F32, s2)
        pO = [k.ps("rwpO", [128, 64], F32, s2) for _ in range(2)]; pH = [k.ps("rwpH", [128, 64], F32, s2) for _ in range(2)]
        order = list(range(NCH)) if d == 0 else [3, 2, 1, 0] + list(range(NCH - 1, 3, -1))
        for i in range(NCH - 1):
            c = order[i]; cn = order[i + 1]
            k.op("act", lambda e: e.activation(HVg[:, c, :], HV[:, c, :], AF.Copy, scale=gc[:, cn:cn + 1]), reads=[HV, gc], writes=[HVg])
        if d == 1:
            k.op("pool", lambda e: e.tensor_tensor(OV[:], OV[:], o_all[:], ALU.add), reads=[OV, o_all], writes=[OV])
        c = order[0]
        k.op("dve", lambda e: e.tensor_copy(Hs[1][:], HVg[:, c, :]), reads=[HVg], writes=[Hs[1]])
        k.op("dve", lambda e: e.tensor_copy(o_all[:, c, :], OV[:, c, :]), reads=[OV], writes=[o_all])
        for i in range(1, NCH):
            c = order[i]; hs = Hs[i % 2]; hn = Hs[(i + 1) % 2]; po = pO[i % 2]; ph = pH[i % 2]
            k.op("pe", lambda e: e.matmul(ph[:], lhsT=Tt[:, c, :], rhs=hs[:], start=True, stop=True), reads=[Tt, hs], writes=[ph])
            k.op("pe", lambda e: e.matmul(po[:], lhsT=Rhat[:, c, :], rhs=hs[:], start=True, stop=True), reads=[Rhat, hs], writes=[po])
            if i < NCH - 1:
                cn = order[i + 1]
                k.op("dve", lambda e: e.scalar_tensor_tensor(hn[:], ph[:], gc[:, cn:cn + 1], HVg[:, c, :], op0=ALU.mult, op1=ALU.add), reads=[ph, gc, HVg], writes=[hn])
            k.op("dve", lambda e: e.tensor_tensor(o_all[:, c, :], po[:], OV[:, c, :], ALU.add), reads=[po, OV], writes=[o_all])
        k.barrier()


def load_consts(k, st, CD):
    C = {}
    C["ident_f"] = k.sb("identf", [128, 128], F32, st); C["ident_bf"] = k.sb("identbf", [128, 128], BF16, st)
    C["ones_bf"] = k.sb("onesbf", [128, 128], BF16, st)
    k.dma("sp", C["ident_f"][:], CD["ident"][:], writes=[C["ident_f"]])
    k.op("dve", lambda e: e.tensor_copy(C["ident_bf"][:], C["ident_f"][:]), reads=[C["ident_f"]], writes=[C["ident_bf"]])
    k.op("dve", lambda e: e.memset(C["ones_bf"][:], 1.0), writes=[C["ones_bf"]])
    C["cos_d"] = CD["cos"]; C["sin_d"] = CD["sin"]
    m2t = k.sb("rwm2", [128, 2, 256], F32, st); m1t = k.sb("rwm1", [128, 2, 128], F32, st)
    k.dma("sp", m2t[:], CD["rw_m2"].t.rearrange("d p c -> p d c"), writes=[m2t])
    k.dma("sp", m1t[:], CD["rw_m1"].t.rearrange("d p c -> p d c"), writes=[m1t])
    C["m2t"] = m2t; C["m1t"] = m1t; C["m2"] = [m2t[:, 0, :], m2t[:, 1, :]]; C["m1"] = [m1t[:, 0, :], m1t[:, 1, :]]
    C["blk"] = k.sb("rwblk", [128, 128], F32, st); C["isel"] = k.sb("rwisel", [128, 64], BF16, st)
    k.dma("sp", C["blk"][:], CD["rw_blk"][:], writes=[C["blk"]])
    k.dma("pool", C["isel"][:], CD["rw_isel"][:], writes=[C["isel"]])
    return C


def const_arrays():
    cos, sin = rope_tables()
    m2, m1, blk, isel = rw_consts_host()
    p = np.arange(128)
    return {"ident": np.eye(128, dtype=np.float32), "cos": cos, "sin": sin, "rw_m2": m2, "rw_m1": m1, "rw_blk": blk, "rw_isel": isel,
            "na_mask": na_mask_host(),
            "tri": (p[:, None] < p[None, :]).astype(np.float32), "iota64": (p % 32).astype(np.float32)[:, None],
            "rowbase": (np.arange(8)[None, :] * 128 + p[:, None]).astype(np.float32), "blk512": np.tile(512.0 * np.arange(NBLK_MAX, dtype=np.float32)[None, :], (128, 1))}


CONST_SHAPES = {"ident": [128, 128], "cos": [SEQ, 128], "sin": [SEQ, 128], "rw_m2": [2, 128, 256], "rw_m1": [2, 128, 128], "rw_blk": [128, 128],
                "rw_isel": [128, 64], "na_mask": [128, 4, 64], "tri": [128, 128], "iota64": [128, 1], "rowbase": [128, 8], "blk512": [128, NBLK_MAX]}


def phase_post(k, l, b, NB, need_ctx, x_src, x_src_tk, mod_d, yrw_d, ymla_d, yna_d, W, C, x1_d, hT_d, GT_d, col0, hTM_d=None, Gtm_d=None):
    with ExitStack() as st:
        wo_rw = k.sb("worw", [64, 4, D], BF16, st); wo_mla = k.sb("womla", [128, 4, D], BF16, st); wo_na = k.sb("wona", [64, 4, D], BF16, st)
        wv = W["w_out"].t[l]
        k.dma("pool", wo_rw[:], wv[0:256, :].rearrange("(h p) n -> p h n", p=64), writes=[wo_rw])
        k.dma("pool", wo_mla[:], wv[256:768, :].rearrange("(h p) n -> p h n", p=128), writes=[wo_mla])
        k.dma("pool", wo_na[:], wv[768:1024, :].rearrange("(h p) n -> p h n", p=64), writes=[wo_na])
        rwt = k.sb("routw", [128, 8, NE], BF16, st); rbt = k.sb("routb", [128, NE], F32, st)
        k.dma("pool", rwt[:], W["router_w"].t[l].rearrange("(kc p) n -> p kc n", p=128), writes=[rwt])
        k.dma("sp", rbt[:], W["router_b"].t[l:l + 1, :].partition_broadcast(128), writes=[rbt])
        bc = {}
        for nm, src in (("g1", W["ln1_g"].t[l:l + 1, :]), ("b1", W["ln1_b"].t[l:l + 1, :]),
                        ("ga_l", mod_d[l][b:b + 1, 2 * D:3 * D]), ("sf_l", mod_d[l][b:b + 1, 3 * D:4 * D]), ("cf_l", mod_d[l][b:b + 1, 4 * D:5 * D]),
                        ("ga_c", mod_d[l][NB:NB + 1, 2 * D:3 * D]), ("sf_c", mod_d[l][NB:NB + 1, 3 * D:4 * D]), ("cf_c", mod_d[l][NB:NB + 1, 4 * D:5 * D])):
            if nm.endswith("_c") and not need_ctx:
                continue
            tle = k.sb(nm, [128, D], F32, st)
            k.dma("sp", tle[:], src.partition_broadcast(128), reads=[mod_d], writes=[tle])
            bc[nm] = tle
        for nm in ("cf_l", "cf_c"):
            if nm in bc:
                tle = bc[nm]
                k.op("pool", lambda e: e.tensor_scalar(tle[:], tle[:], 1.0, None, op0=ALU.add), reads=[tle], writes=[tle])
        yr = k.sb("yrall", [64, 4, T], BF16, st); ym = k.sb("ymall", [128, 4, T], BF16, st); yn = k.sb("ynall", [64, 4, T], BF16, st)
        k.dma("sp", yr[:], yrw_d[:], reads=[yrw_d], writes=[yr])
        k.dma("sp", ym[:], ymla_d.t.rearrange("h p t -> p h t"), reads=[ymla_d], writes=[ym])
        k.dma("sp", yn[:], yna_d[:], reads=[yna_d], writes=[yn])
        xts = [k.sb("pxt", [128, D], F32, st) for _ in range(2)]
        t1 = k.sb("pt1", [128, D], F32, st); u = k.sb("pu", [128, D], F32, st); x1 = k.sb("px1", [128, D], F32, st); hb = k.sb("phb", [128, D], BF16, st)
        st6 = k.sb("pst6", [128, 2, 6], F32, st); mv = k.sb("pmv", [128, 2], F32, st); rstd = k.sb("prstd", [128, 1], F32, st)
        hT = k.sb("phT", [128, 8, 128], BF16, st)
        lg = k.sb("plg", [128, NE], F32, st); mx8 = k.sb("pmx8", [128, 8], F32, st); nm1 = k.sb("pnm1", [128, 1], F32, st)
        msk = k.sb("pmsk", [128, NE], F32, st); ex = k.sb("pex", [128, NE], F32, st); ssum = k.sb("pssum", [128, 1], F32, st); gts = k.sb("pgts", [32, 128], F32, st)
        py = [k.ps("ppy", [128, 512], F32, st) for _ in range(2)]
        pT = k.ps("ppT", [128, 8, 128], BF16, st); pl = k.ps("ppl", [128, NE], F32, st); pg = k.ps("ppg", [32, 128], F32, st)
        for tt in range(NTT):
            isctx = tt < 2
            if isctx and not need_ctx:
                continue
            sfx = "_c" if isctx else "_l"
            sl = slice(tt * 128, (tt + 1) * 128)
            xt = xts[tt % 2]
            k.dma("sp", xt[:], x_src(tt), reads=[x_src_tk], writes=[xt])
            for half in range(2):
                ps = py[half]; cs = slice(half * 512, (half + 1) * 512)
                ops = [(yr, wo_rw, h) for h in range(4)] + [(ym, wo_mla, h) for h in range(4)] + [(yn, wo_na, h) for h in range(4)]
                for i, (ya, wa, h) in enumerate(ops):
                    k.op("pe", lambda e: e.matmul(ps[:], lhsT=ya[:, h, sl], rhs=wa[:, h, cs], start=(i == 0), stop=(i == 11)), reads=[ya, wa], writes=[ps])
                k.op("dve", lambda e: e.tensor_tensor(t1[:, cs], ps[:], bc["ga" + sfx][:, cs], ALU.mult), reads=[ps, bc["ga" + sfx]], writes=[t1])
            k.op("dve", lambda e: e.scalar_tensor_tensor(u[:], xt[:], DN_ALPHA, t1[:], op0=ALU.mult, op1=ALU.add), reads=[xt, t1], writes=[u])
            ln_stats(k, u, st6, mv, rstd, 1e-5)
            k.op("dve", lambda e: e.scalar_tensor_tensor(t1[:], u[:], mv[:, 0:1], bc["g1"][:], op0=ALU.subtract, op1=ALU.mult), reads=[u, mv, bc["g1"]], writes=[t1])
            k.op("dve", lambda e: e.scalar_tensor_tensor(x1[:], t1[:], rstd[:], bc["b1"][:], op0=ALU.mult, op1=ALU.add), reads=[t1, rstd, bc["b1"]], writes=[x1])
            k.dma("sp", x1_d[sl, :], x1[:], reads=[x1], writes=[x1_d])
            ln_stats(k, x1, st6, mv, rstd, 1e-5)
            k.op("dve", lambda e: e.scalar_tensor_tensor(t1[:], x1[:], mv[:, 0:1], bc["cf" + sfx][:], op0=ALU.subtract, op1=ALU.mult), reads=[x1, mv, bc["cf" + sfx]], writes=[t1])
            k.op("dve", lambda e: e.scalar_tensor_tensor(hb[:], t1[:], rstd[:], bc["sf" + sfx][:], op0=ALU.mult, op1=ALU.add), reads=[t1, rstd, bc["sf" + sfx]], writes=[hb])
            for c in range(8):
                k.op("pe", lambda e: e.transpose(pT[:, c, :], hb[:, c * 128:(c + 1) * 128], C["ident_bf"][:]), reads=[hb, C["ident_bf"]], writes=[pT])
            k.op("act", lambda e: e.copy(hT[:], pT[:]), reads=[pT], writes=[hT])
            if hTM_d is not None:
                k.dma("sp", hTM_d.t[col0 + tt * 128:col0 + (tt + 1) * 128, :], hb[:], reads=[hb], writes=[hTM_d])
            else:
                k.dma("sp", hT_d.t[:, col0 + tt * 128:col0 + (tt + 1) * 128].rearrange("(kc p) t -> p kc t", p=128), hT[:], reads=[hT], writes=[hT_d])
            for kc in range(8):
                k.op("pe", lambda e: e.matmul(pl[:], lhsT=hT[:, kc, :], rhs=rwt[:, kc, :], start=(kc == 0), stop=(kc == 7)), reads=[hT, rwt], writes=[pl])
            k.op("dve", lambda e: e.tensor_tensor(lg[:], pl[:], rbt[:], ALU.add), reads=[pl, rbt], writes=[lg])
            k.op("dve", lambda e: e.max(mx8[:], lg[:]), reads=[lg], writes=[mx8])
            k.op("dve", lambda e: e.tensor_scalar(msk[:], lg[:], mx8[:, 3:4], None, op0=ALU.is_ge), reads=[lg, mx8], writes=[msk])
            k.op("dve", lambda e: e.tensor_scalar(nm1[:], mx8[:, 0:1], -1.0, None, op0=ALU.mult), reads=[mx8], writes=[nm1])
            k.op("act", lambda e: e.activation(ex[:], lg[:], AF.Exp, bias=nm1[:]), reads=[lg, nm1], writes=[ex])
            k.op("dve", lambda e: e.tensor_tensor(ex[:], ex[:], msk[:], ALU.mult), reads=[ex, msk], writes=[ex])
            k.op("dve", lambda e: e.reduce_sum(ssum[:], ex[:], axis=AX.X), reads=[ex], writes=[ssum])
            k.op("dve", lambda e: e.reciprocal(ssum[:], ssum[:]), reads=[ssum], writes=[ssum])
            k.op("dve", lambda e: e.tensor_scalar(ex[:], ex[:], ssum[:, 0:1], None, op0=ALU.mult), reads=[ex, ssum], writes=[ex])
            if Gtm_d is not None:
                k.dma("sp", Gtm_d.t[col0 + tt * 128:col0 + (tt + 1) * 128, :], ex[:], reads=[ex], writes=[Gtm_d])
            else:
                k.op("pe", lambda e: e.transpose(pg[:], ex[:], C["ident_f"][:]), reads=[ex, C["ident_f"]], writes=[pg])
                k.op("act", lambda e: e.copy(gts[:], pg[:]), reads=[pg], writes=[gts])
                k.dma("sp", GT_d[:, col0 + tt * 128:col0 + (tt + 1) * 128], gts[:], reads=[gts], writes=[GT_d])
    k.barrier()


def phase_moe(k, l, NB, groups, tile_io, mod_d, W, C, hT_d, GT_d):
    GP = 2
    with ExitStack() as st:
        bgl = k.sb("bgl", [128, NE, 8], F32, st); bln = k.sb("bln", [128, NE, 8], F32, st)
        bdn = k.sb("bdn", [NE, D], F32, st)
        k.dma("sp", bdn[:], W["b_dn"].t[l], writes=[bdn])
        with ExitStack() as s2:
            bgu = k.sb("bgu", [NE, 2 * D], F32, s2)
            pb = [k.ps("pbg", [128, NE], F32, s2) for _ in range(2)]
            k.dma("sp", bgu[:], W["b_gu"].t[l], writes=[bgu])
            for j in range(8):
                for par, dst in ((0, bgl), (1, bln)):
                    ps = pb[par]
                    k.op("pe", lambda e: e.transpose(ps[:], bgu[:, 256 * j + par:256 * (j + 1):2], C["ident_f"][0:NE, 0:NE]), reads=[bgu, C["ident_f"]], writes=[ps])
                    k.op("dve", lambda e: e.tensor_scalar(dst[:, :, j], ps[:], float(par), None, op0=ALU.add), reads=[ps], writes=[dst])
            k.barrier()
        gfs = [k.sb("gf%d" % i, [128, D], F32, st) for i in range(GP)]; gf_key = [None] * GP
        ln2g = k.sb("ln2g", [128, D], F32, st); ln2b = k.sb("ln2b", [128, D], F32, st)
        k.dma("sp", ln2g[:], W["ln2_g"].t[l:l + 1, :].partition_broadcast(128), writes=[ln2g])
        k.dma("sp", ln2b[:], W["ln2_b"].t[l:l + 1, :].partition_broadcast(128), writes=[ln2b])
        NU = 4
        units = [k.sb("wu%d" % i, [128, 8, 1024], BF16, st) for i in range(NU)]
        hTs = [k.sb("mhT%d" % i, [128, 8, 512], BF16, st) for i in range(GP)]
        accs = [k.sb("macc%d" % i, [128, 8, 512], F32, st) for i in range(GP)]
        gTs = [k.sb("mgT%d" % i, [NE, 512], F32, st) for i in range(GP)]
        acts = [k.sb("mact%d" % i, [128, 8, 512], BF16, st) for i in range(2)]
        gbcs = [k.sb("mgbc%d" % i, [128, 512], F32, st) for i in range(2)]
        tt_ = [k.sb("mt%d" % i, [128, 512], F32, st) for i in range(2)]
        ss_ = [k.sb("ms%d" % i, [128, 512], F32, st) for i in range(2)]
        uu_ = [k.sb("mu%d" % i, [128, 512], F32, st) for i in range(2)]
        psg = [k.ps("mpsg", [128, 512], F32, st) for _ in range(2)]
        psl = [k.ps("mpsl", [128, 512], F32, st) for _ in range(2)]
        psd = [k.ps("mpsd", [128, 512], F32, st) for _ in range(2)]
        st6 = k.sb("mst6", [128, 2, 6], F32, st); mv = k.sb("mmv", [128, 2], F32, st); rstd = k.sb("mrstd", [128, 1], F32, st)
        ucount = 0; it = 0; dcount = 0
        wgu = W["w_gu"].t[l]; wdn = W["w_dn"].t[l]
        for p0 in range(0, len(groups), GP):
            pg = groups[p0:p0 + GP]
            for gi, (col, n, bkey) in enumerate(pg):
                k.dma("sp", hTs[gi][:, :, 0:n], hT_d.t[:, col:col + n].rearrange("(kc p) t -> p kc t", p=128), reads=[hT_d], writes=[hTs[gi]])
                k.dma("sp", gTs[gi][:, 0:n], GT_d[:, col:col + n], reads=[GT_d], writes=[gTs[gi]])
                if gf_key[gi] != bkey:
                    row = NB if bkey == "c" else bkey
                    k.dma("sp", gfs[gi][:], mod_d[l][row:row + 1, 5 * D:6 * D].partition_broadcast(128), reads=[mod_d], writes=[gfs[gi]])
                    gf_key[gi] = bkey
            for e in range(NE):
                ua = units[ucount % NU]; ub = units[(ucount + 1) % NU]; ud = units[(ucount + 2) % NU]; ucount += 3
                k.dma("pool", ua[:], wgu[e][:, 0:1024].rearrange("(kc p) n -> p kc n", p=128), writes=[ua])
                k.dma("pool", ub[:], wgu[e][:, 1024:2048].rearrange("(kc p) n -> p kc n", p=128), writes=[ub])
                k.dma("pool", ud[:], wdn[e].rearrange("(kc p) n -> p kc n", p=128), writes=[ud])
                for gi, (col, n, bkey) in enumerate(pg):
                    hTg = hTs[gi]; acc = accs[gi]
                    act = acts[it % 2]; gbc = gbcs[it % 2]; it += 1
                    k.dma("act", gbc[:, 0:n], GT_d[e:e + 1, col:col + n].partition_broadcast(128), reads=[GT_d], writes=[gbc])
                    for j in range(8):
                        un = ua if j < 4 else ub; c0 = (j % 4) * 256
                        pg_ = psg[j % 2]; pl_ = psl[j % 2]; t_ = tt_[j % 2]; s_ = ss_[j % 2]; u_ = uu_[j % 2]
                        for kc in range(8):
                            k.op("pe", lambda e_: e_.matmul(pg_[:, 0:n], lhsT=un[:, kc, c0:c0 + 256:2], rhs=hTg[:, kc, 0:n], start=(kc == 0), stop=(kc == 7)), reads=[un, hTg], writes=[pg_])
                        for kc in range(8):
                            k.op("pe", lambda e_: e_.matmul(pl_[:, 0:n], lhsT=un[:, kc, c0 + 1:c0 + 256:2], rhs=hTg[:, kc, 0:n], start=(kc == 0), stop=(kc == 7)), reads=[un, hTg], writes=[pl_])
                        k.op("dve", lambda e_: e_.tensor_scalar(t_[:, 0:n], pg_[:, 0:n], bgl[:, e, j:j + 1], 7.0, op0=ALU.add, op1=ALU.min), reads=[pg_, bgl], writes=[t_])
                        k.op("act", lambda e_: e_.activation(s_[:, 0:n], t_[:, 0:n], AF.Sigmoid, scale=1.702), reads=[t_], writes=[s_])
                        k.op("dve", lambda e_: e_.tensor_scalar(u_[:, 0:n], pl_[:, 0:n], bln[:, e, j:j + 1], 8.0, op0=ALU.add, op1=ALU.min), reads=[pl_, bln], writes=[u_])
                        k.op("pool", lambda e_: e_.tensor_tensor(t_[:, 0:n], t_[:, 0:n], s_[:, 0:n], ALU.mult), reads=[t_, s_], writes=[t_])
                        k.op("pool", lambda e_: e_.tensor_scalar(u_[:, 0:n], u_[:, 0:n], -6.0, None, op0=ALU.max), reads=[u_], writes=[u_])
                        k.op("pool", lambda e_: e_.tensor_tensor(u_[:, 0:n], u_[:, 0:n], t_[:, 0:n], ALU.mult), reads=[u_, t_], writes=[u_])
                        k.op("dve", lambda e_: e_.tensor_tensor(act[:, j, 0:n], u_[:, 0:n], gbc[:, 0:n], ALU.mult), reads=[u_, gbc], writes=[act])
                    for oc in range(8):
                        pd = psd[dcount % 2]; dcount += 1
                        for j in range(8):
                            k.op("pe", lambda e_: e_.matmul(pd[:, 0:n], lhsT=ud[:, j, oc * 128:(oc + 1) * 128], rhs=act[:, j, 0:n], start=(j == 0), stop=(j == 7)), reads=[ud, act], writes=[pd])
                        if e == 0:
                            k.op("act", lambda e_: e_.copy(acc[:, oc, 0:n], pd[:, 0:n]), reads=[pd], writes=[acc])
                        else:
                            k.op("dve", lambda e_: e_.tensor_tensor(acc[:, oc, 0:n], acc[:, oc, 0:n], pd[:, 0:n], ALU.add), reads=[acc, pd], writes=[acc])
            for gi, (col, n, bkey) in enumerate(pg):
                acc = accs[gi]; gT = gTs[gi]; gf = gfs[gi]
                for oc in range(8):
                    pd = psd[dcount % 2]; dcount += 1
                    k.op("pe", lambda e_: e_.matmul(pd[:, 0:n], lhsT=bdn[:, oc * 128:(oc + 1) * 128], rhs=gT[:, 0:n], start=True, stop=True), reads=[bdn, gT], writes=[pd])
                    k.op("dve", lambda e_: e_.tensor_tensor(acc[:, oc, 0:n], acc[:, oc, 0:n], pd[:, 0:n], ALU.add), reads=[acc, pd], writes=[acc])
                for ti in range(n // 128):
                    x1_ap, x1_tk, out_ap, out_tk = tile_io(col + ti * 128)
                    for half in range(2):
                        ps = psg[half]; cs = slice(half * 512, (half + 1) * 512)
                        k.dma("sp", ss_[half][:], x1_ap[:, cs], reads=[x1_tk], writes=[ss_[half]])
                        for c in range(4):
                            oc = half * 4 + c
                            k.op("pe", lambda e_: e_.transpose(ps[:, c * 128:(c + 1) * 128], acc[:, oc, ti * 128:(ti + 1) * 128], C["ident_f"][:]), reads=[acc, C["ident_f"]], writes=[ps])
                        k.op("dve", lambda e_: e_.tensor_tensor(tt_[half][:], ps[:], gf[:, cs], ALU.mult), reads=[ps, gf], writes=[tt_[half]])
                        k.op("dve", lambda e_: e_.scalar_tensor_tensor(uu_[half][:], ss_[half][:], DN_ALPHA, tt_[half][:], op0=ALU.mult, op1=ALU.add), reads=[ss_[half], tt_[half]], writes=[uu_[half]])
                        k.op("dve", lambda e_: e_.bn_stats(st6[:, half, :], uu_[half][:]), reads=[uu_[half]], writes=[st6])
                    k.op("dve", lambda e_: e_.bn_aggr(mv[:], st6[:].rearrange("p a b -> p (a b)")), reads=[st6], writes=[mv])
                    k.op("dve", lambda e_: e_.tensor_scalar(rstd[:], mv[:, 1:2], 1e-5, None, op0=ALU.add), reads=[mv], writes=[rstd])
                    k.op("act", lambda e_: e_.sqrt(rstd[:], rstd[:]), reads=[rstd], writes=[rstd])
                    k.op("dve", lambda e_: e_.reciprocal(rstd[:], rstd[:]), reads=[rstd], writes=[rstd])
                    for half in range(2):
                        cs = slice(half * 512, (half + 1) * 512)
                        k.op("dve", lambda e_: e_.scalar_tensor_tensor(tt_[half][:], uu_[half][:], mv[:, 0:1], ln2g[:, cs], op0=ALU.subtract, op1=ALU.mult), reads=[uu_[half], mv, ln2g], writes=[tt_[half]])
                        k.op("dve", lambda e_: e_.scalar_tensor_tensor(uu_[half][:], tt_[half][:], rstd[:], ln2b[:, cs], op0=ALU.mult, op1=ALU.add), reads=[tt_[half], rstd, ln2b], writes=[uu_[half]])
                        k.dma("sp", out_ap[:, cs], uu_[half][:], reads=[uu_[half]], writes=[out_tk])
    k.barrier()


def phase_moe_sparse(k, l, NB, need_ctx, tile_io, mod_d, W, C, CD, hTM_d, Gtm_d, Hs_d, Ys_d):
    IOA = bass.IndirectOffsetOnAxis
    ntb = NTT if need_ctx else NTT - 2
    NT = NB * ntb; NTOK = NT * 128
    NBLK = (4 * NTOK + NE * 511) // 512
    assert NBLK <= NBLK_MAX
    BIG = 65536.0
    rows = [b * T + (0 if need_ctx else NCTX) for b in range(NB)]
    tile_row = [rows[b] + j * 128 for b in range(NB) for j in range(ntb)]
    tile_key = [("c" if (need_ctx and j < 2) else b) for b in range(NB) for j in range(ntb)]
    with ExitStack() as st:
        dest_i = k.sb("mdesti", [128, NT, 4], I32, st); gk = k.sb("mgk", [128, NT, 4], F32, st)
        widx = k.sb("mwidx", [128, NBLK, 8], I32, st); be = k.sb("mbe", [128, NBLK], F32, st)
        iota64 = k.sb("miota", [128, 1], F32, st)
        k.dma("sp", iota64[:], CD["iota64"][:], writes=[iota64])
        with ExitStack() as s2:
            rowb = k.sb("mrowb", [128, 8], F32, s2); blk512 = k.sb("mblk512", [128, NBLK], F32, s2)
            trif = k.sb("mtrif", [128, 128], F32, s2); tri = k.sb("mtri", [128, 128], BF16, s2)
            k.dma("sp", rowb[:], CD["rowbase"][:], writes=[rowb]); k.dma("sp", blk512[:], CD["blk512"][:, 0:NBLK], writes=[blk512])
            k.dma("sp", trif[:], CD["tri"][:], writes=[trif])
            k.op("dve", lambda e: e.tensor_scalar(rowb[:], rowb[:], float(l * NE * D), None, op0=ALU.add), reads=[rowb], writes=[rowb])
            k.op("dve", lambda e: e.tensor_copy(tri[:], trif[:]), reads=[trif], writes=[tri])
            G_all = k.sb("mGall", [128, NT, NE], F32, s2); maskf = k.sb("mmaskf", [128, NT, NE], F32, s2); mask = k.sb("mmask", [128, NT, NE], BF16, s2)
            rank = k.sb("mrank", [128, NT, NE], F32, s2); tot = k.sb("mtot", [128, NT, NE], F32, s2); off = k.sb("moff", [128, NT, NE], F32, s2)
            val = k.sb("mval", [128, NT, NE], F32, s2)
            cnt = k.sb("mcnt", [128, NE], F32, s2); nbe = k.sb("mnbe", [128, NE], F32, s2); start = k.sb("mstart", [128, NE + 1], F32, s2)
            mx8 = k.sb("mmx8", [128, 8], F32, s2); tmp = k.sb("mtmp", [128, NE], F32, s2); destf = k.sb("mdestf", [128, NT, 4], F32, s2)
            widxf = k.sb("mwidxf", [128, NBLK, 8], F32, s2)
            z = k.sb("mz", [128, 4 * D], BF16, s2)
            k.op("pool", lambda e: e.memset(z[:], 0.0), writes=[z])
            for blk in range(NBLK):
                k.dma("sp" if blk % 2 else "act", Hs_d.t[blk * 512:(blk + 1) * 512, :].rearrange("(p r) d -> p (r d)", p=128), z[:], reads=[z])
            for b in range(NB):
                k.dma("sp", G_all[:, b * ntb:(b + 1) * ntb, :], Gtm_d.t[rows[b]:rows[b] + ntb * 128, :].rearrange("(i p) e -> p i e", p=128), reads=[Gtm_d], writes=[G_all])
            k.op("dve", lambda e: e.tensor_scalar(maskf[:], G_all[:], 0.0, None, op0=ALU.is_gt), reads=[G_all], writes=[maskf])
            k.op("dve", lambda e: e.tensor_copy(mask[:], maskf[:]), reads=[maskf], writes=[mask])
            pP = [k.ps("mpP", [128, 512], F32, s2) for _ in range(2)]; pTt = [k.ps("mpTt", [128, 512], F32, s2) for _ in range(2)]
            mflat = mask[:].rearrange("p i e -> p (i e)"); rflat = rank[:].rearrange("p i e -> p (i e)"); tflat = tot[:].rearrange("p i e -> p (i e)")
            ncols = NT * NE
            for c in range((ncols + 511) // 512):
                c0 = c * 512; n = min(512, ncols - c0); pa = pP[c % 2]; pb = pTt[c % 2]
                k.op("pe", lambda e: e.matmul(pa[:, 0:n], lhsT=tri[:], rhs=mflat[:, c0:c0 + n], start=True, stop=True), reads=[tri, mask], writes=[pa])
                k.op("pe", lambda e: e.matmul(pb[:, 0:n], lhsT=C["ones_bf"][:], rhs=mflat[:, c0:c0 + n], start=True, stop=True), reads=[C["ones_bf"], mask], writes=[pb])
                k.op("dve", lambda e: e.tensor_copy(rflat[:, c0:c0 + n], pa[:, 0:n]), reads=[pa], writes=[rank])
                k.op("act", lambda e: e.copy(tflat[:, c0:c0 + n], pb[:, 0:n]), reads=[pb], writes=[tot])
            k.op("dve", lambda e: e.reduce_sum(cnt[:], tot[:].rearrange("p i e -> p e i"), axis=AX.X), reads=[tot], writes=[cnt])
            k.op("dve", lambda e: e.memset(nbe[:], 0.0), writes=[nbe])
            for j in range(NTOK // 512):
                k.op("dve", lambda e: e.scalar_tensor_tensor(nbe[:], cnt[:], 512.0 * j, nbe[:], op0=ALU.is_gt, op1=ALU.add), reads=[cnt, nbe], writes=[nbe])
            k.op("dve", lambda e: e.tensor_scalar(nbe[:], nbe[:], 512.0, None, op0=ALU.mult), reads=[nbe], writes=[nbe])
            k.op("dve", lambda e: e.memset(start[:, 0:1], 0.0), writes=[start])
            for ee in range(NE):
                k.op("dve", lambda e: e.tensor_tensor(start[:, ee + 1:ee + 2], start[:, ee:ee + 1], nbe[:, ee:ee + 1], ALU.add), reads=[start, nbe], writes=[start])
            k.op("dve", lambda e: e.tensor_copy(off[:, 0, :], start[:, 0:NE]), reads=[start], writes=[off])
            for i in range(1, NT):
                k.op("dve", lambda e: e.tensor_tensor(off[:, i, :], off[:, i - 1, :], tot[:, i - 1, :], ALU.add), reads=[off, tot], writes=[off])
            k.op("dve", lambda e: e.tensor_tensor(rank[:], rank[:], off[:], ALU.add), reads=[rank, off], writes=[rank])
            k.op("dve", lambda e: e.tensor_scalar(rank[:], rank[:], -1.0, BIG, op0=ALU.mult, op1=ALU.add), reads=[rank], writes=[rank])
            k.op("dve", lambda e: e.tensor_tensor(val[:], rank[:], maskf[:], ALU.mult), reads=[rank, maskf], writes=[val])
            for i in range(NT):
                k.op("dve", lambda e: e.max(mx8[:], val[:, i, :]), reads=[val], writes=[mx8])
                k.op("dve", lambda e: e.tensor_scalar(destf[:, i, :], mx8[:, 0:4], -1.0, BIG, op0=ALU.mult, op1=ALU.add), reads=[mx8], writes=[destf])
                for kk in range(4):
                    k.op("dve", lambda e: e.scalar_tensor_tensor(tmp[:], val[:, i, :], mx8[:, kk:kk + 1], G_all[:, i, :], op0=ALU.is_equal, op1=ALU.mult), reads=[val, mx8, G_all], writes=[tmp])
                    k.op("dve", lambda e: e.reduce_sum(gk[:, i, kk:kk + 1], tmp[:], axis=AX.X), reads=[tmp], writes=[gk])
            k.op("dve", lambda e: e.tensor_copy(dest_i[:], destf[:]), reads=[destf], writes=[dest_i])
            k.op("dve", lambda e: e.memset(be[:], 0.0), writes=[be])
            for ee in range(NE):
                k.op("dve", lambda e: e.scalar_tensor_tensor(be[:], blk512[:], start[:, ee + 1:ee + 2], be[:], op0=ALU.is_ge, op1=ALU.add), reads=[blk512, start, be], writes=[be])
            k.op("dve", lambda e: e.tensor_scalar(be[:], be[:], float(NE - 1), None, op0=ALU.min), reads=[be], writes=[be])
            for kc in range(8):
                k.op("dve", lambda e: e.tensor_scalar(widxf[:, :, kc], be[:], 1024.0, rowb[:, kc:kc + 1], op0=ALU.mult, op1=ALU.add), reads=[be, rowb], writes=[widxf])
            k.op("dve", lambda e: e.tensor_copy(widx[:], widxf[:]), reads=[widxf], writes=[widx])
            k.barrier()
            hts = [k.sb("mht", [128, D], BF16, s2) for _ in range(2)]
            for i in range(NT):
                ht = hts[i % 2]
                k.dma("sp", ht[:], hTM_d.t[tile_row[i]:tile_row[i] + 128, :], reads=[hTM_d], writes=[ht])
                for kk in range(4):
                    k.idma(Hs_d.t[:, :], ht[:], out_offset=IOA(ap=dest_i[:, i, kk:kk + 1], axis=0), reads=[dest_i, ht])
            k.barrier()
        with ExitStack() as s3:
            bguHL = k.sb("mbguhl", [64, 2 * D], BF16, s3); bdnHL = k.sb("mbdnhl", [64, D], BF16, s3); iotaE = k.sb("miotaE", [64, 512], F32, s3)
            with ExitStack() as s4:
                for (src, dst, n) in ((W["b_gu"].t[l], bguHL, 2 * D), (W["b_dn"].t[l], bdnHL, D)):
                    bf_ = k.sb("mbf", [64, n], F32, s4); hif = k.sb("mhif", [64, n], F32, s4)
                    k.dma("sp", bf_[0:32, :], src, writes=[bf_]); k.dma("sp", bf_[32:64, :], src, writes=[bf_])
                    if n == 2 * D:
                        k.op("dve", lambda e: e.tensor_scalar(bf_[:, 1:n:2], bf_[:, 1:n:2], 1.0, None, op0=ALU.add), reads=[bf_], writes=[bf_])
                    k.op("dve", lambda e: e.tensor_copy(dst[:], bf_[:]), reads=[bf_], writes=[dst])
                    k.op("dve", lambda e: e.tensor_copy(hif[:], dst[:]), reads=[dst], writes=[hif])
                    k.op("dve", lambda e: e.tensor_tensor(hif[32:64, :], bf_[32:64, :], hif[32:64, :], ALU.subtract), reads=[bf_, hif], writes=[hif])
                    k.op("dve", lambda e: e.tensor_copy(dst[32:64, :], hif[32:64, :]), reads=[hif], writes=[dst])
                k.op("dve", lambda e: e.memset(iotaE[:], 0.0), writes=[iotaE])
                k.op("dve", lambda e: e.tensor_scalar(iotaE[:], iotaE[:], iota64[0:64, 0:1], None, op0=ALU.add), reads=[iotaE, iota64], writes=[iotaE])
                k.barrier()
            wgs = [[k.sb("mwg", [128, 2 * D], BF16, s3) for _ in range(8)] for _ in range(2)]
            wds = [[k.sb("mwd", [128, D], BF16, s3) for _ in range(8)] for _ in range(2)]
            hss = [k.sb("mhs", [128, 4, D], BF16, s3) for _ in range(2)]; hTs = [k.sb("mhT", [128, 8, 512], BF16, s3) for _ in range(1)]
            acts = [k.sb("mact", [128, 8, 512], BF16, s3) for _ in range(1)]; ohs = [k.sb("moh", [64, 512], BF16, s3) for _ in range(2)]
            tt_ = [k.sb("mt", [128, 512], F32, s3) for _ in range(2)]; ss_ = [k.sb("ms", [128, 512], F32, s3) for _ in range(2)]; uu_ = [k.sb("mu", [128, 512], F32, s3) for _ in range(2)]
            ysb = [k.sb("mysb", [128, D], F32, s3) for _ in range(2)]
            pT = k.ps("mpT", [128, 8, 128], BF16, s3)
            psg = [k.ps("mpsg", [128, 512], F32, s3) for _ in range(2)]; psl = [k.ps("mpsl", [128, 512], F32, s3) for _ in range(2)]; psd = [k.ps("mpsd", [128, 512], F32, s3) for _ in range(2)]
            pbias = k.ps("mpbias", [128, 16], F32, s3); bsels = [k.sb("mbsel", [128, 16], F32, s3) for _ in range(2)]
            wgu2d = W["w_gu"].t.rearrange("l e r n -> (l e r) n"); wdn2d = W["w_dn"].t.rearrange("l e r n -> (l e r) n")

            def fetch(blk):
                wg = wgs[blk % 2]; wd = wds[blk % 2]
                for kc in range(8):
                    k.idma(wg[kc][:], wgu2d, in_offset=IOA(ap=widx[:, blk, kc:kc + 1], axis=0), reads=[widx], writes=[wg[kc]])
                for kc in range(8):
                    k.idma(wd[kc][:], wdn2d, in_offset=IOA(ap=widx[:, blk, kc:kc + 1], axis=0), reads=[widx], writes=[wd[kc]])
                k.dma("sp", hss[blk % 2][:], Hs_d.t[blk * 512:(blk + 1) * 512, :].rearrange("(r p) d -> p r d", p=128), writes=[hss[blk % 2]])
            hT = hTs[0]; act = acts[0]

            def to_fm(blk):
                hs = hss[blk % 2]
                for r in range(4):
                    for c in range(8):
                        k.op("pe", lambda e: e.transpose(pT[:, c, :], hs[:, r, c * 128:(c + 1) * 128], C["ident_bf"][:]), reads=[hs, C["ident_bf"]], writes=[pT])
                    if r % 2:
                        k.op("act", lambda e: e.copy(hT[:, :, r * 128:(r + 1) * 128], pT[:]), reads=[pT], writes=[hT])
                    else:
                        k.op("dve", lambda e: e.tensor_copy(hT[:, :, r * 128:(r + 1) * 128], pT[:]), reads=[pT], writes=[hT])
            fetch(0)
            to_fm(0)
            yc = 0; dc = 0
            for blk in range(NBLK):
                if blk + 1 < NBLK:
                    fetch(blk + 1)
                wg = wgs[blk % 2]; wd = wds[blk % 2]; oh = ohs[blk % 2]
                k.op("dve", lambda e: e.tensor_scalar(oh[:], iotaE[:], be[0:64, blk:blk + 1], None, op0=ALU.is_equal), reads=[iotaE, be], writes=[oh])
                bs = bsels[blk % 2]
                for jo in range(16):
                    k.op("pe", lambda e: e.matmul(pbias[:, jo:jo + 1], lhsT=bguHL[:, 256 * (jo // 2) + (jo % 2):256 * (jo // 2 + 1):2], rhs=oh[:, 0:1], start=True, stop=True),
                         reads=[bguHL, oh], writes=[pbias])
                k.op("act", lambda e: e.copy(bs[:], pbias[:]), reads=[pbias], writes=[bs])
                for j in range(8):
                    pg_ = psg[j % 2]; pl_ = psl[j % 2]; t_ = tt_[j % 2]; s_ = ss_[j % 2]; u_ = uu_[j % 2]
                    for (ps_, o_) in ((pg_, 0), (pl_, 1)):
                        for kc in range(8):
                            k.op("pe", lambda e: e.matmul(ps_[:], lhsT=wg[kc][:, 256 * j + o_:256 * (j + 1):2], rhs=hT[:, kc, :], start=(kc == 0), stop=(kc == 7)), reads=[wg[kc], hT], writes=[ps_])
                    k.op("dve", lambda e: e.tensor_scalar(t_[:], pg_[:], bs[:, 2 * j:2 * j + 1], 7.0, op0=ALU.add, op1=ALU.min), reads=[pg_, bs], writes=[t_])
                    k.op("act", lambda e: e.activation(s_[:], t_[:], AF.Sigmoid, scale=1.702), reads=[t_], writes=[s_])
                    k.op("dve", lambda e: e.tensor_scalar(u_[:], pl_[:], bs[:, 2 * j + 1:2 * j + 2], 8.0, op0=ALU.add, op1=ALU.min), reads=[pl_, bs], writes=[u_])
                    k.op("dve", lambda e: e.tensor_tensor(t_[:], t_[:], s_[:], ALU.mult), reads=[t_, s_], writes=[t_])
                    k.op("dve", lambda e: e.scalar_tensor_tensor(act[:, j, :], u_[:], -6.0, t_[:], op0=ALU.max, op1=ALU.mult), reads=[u_, t_], writes=[act])
                if blk + 1 < NBLK:
                    to_fm(blk + 1)
                for r in range(4):
                    y = ysb[yc % 2]; yc += 1
                    for half in range(2):
                        pd = psd[dc % 2]; dc += 1; cs = slice(half * 512, (half + 1) * 512)
                        for j in range(8):
                            k.op("pe", lambda e: e.matmul(pd[:], lhsT=act[:, j, r * 128:(r + 1) * 128], rhs=wd[j][:, cs], start=(j == 0), stop=False), reads=[act, wd[j]], writes=[pd])
                        k.op("pe", lambda e: e.matmul(pd[:], lhsT=oh[:, r * 128:(r + 1) * 128], rhs=bdnHL[:, cs], start=False, stop=True), reads=[oh, bdnHL], writes=[pd])
                        if half:
                            k.op("act", lambda e: e.copy(y[:, cs], pd[:]), reads=[pd], writes=[y])
                        else:
                            k.op("dve", lambda e: e.tensor_copy(y[:, cs], pd[:]), reads=[pd], writes=[y])
                    k.dma("sp", Ys_d.t[blk * 512 + r * 128:blk * 512 + (r + 1) * 128, :], y[:], reads=[y])
            k.barrier()
        with ExitStack() as s5:
            gfs = {}
            for key in sorted(set(tile_key), key=str):
                row = NB if key == "c" else key
                gfs[key] = k.sb("mgf", [128, D], F32, s5)
                k.dma("sp", gfs[key][:], mod_d[l][row:row + 1, 5 * D:6 * D].partition_broadcast(128), reads=[mod_d], writes=[gfs[key]])
            ln2g = k.sb("ln2g", [128, D], F32, s5); ln2b = k.sb("ln2b", [128, D], F32, s5)
            k.dma("sp", ln2g[:], W["ln2_g"].t[l:l + 1, :].partition_broadcast(128), writes=[ln2g])
            k.dma("sp", ln2b[:], W["ln2_b"].t[l:l + 1, :].partition_broadcast(128), writes=[ln2b])
            yks = [[k.sb("myk", [128, D], F32, s5) for _ in range(4)] for _ in range(2)]
            x1s = [k.sb("mx1", [128, D], F32, s5) for _ in range(2)]
            acc = k.sb("macc", [128, D], F32, s5); uu = k.sb("muu", [128, D], F32, s5); oo = [k.sb("moo", [128, D], F32, s5) for _ in range(2)]
            st6 = k.sb("mst6", [128, 2, 6], F32, s5); mv = k.sb("mmv", [128, 2], F32, s5); rstd = k.sb("mrstd", [128, 1], F32, s5)
            for i in range(NT):
                yk = yks[i % 2]; x1t = x1s[i % 2]; o_ = oo[i % 2]; gf = gfs[tile_key[i]]
                x1_ap, x1_tk, out_ap, out_tk = tile_io(tile_row[i])
                for kk in range(4):
                    k.idma(yk[kk][:], Ys_d.t[:, :], in_offset=IOA(ap=dest_i[:, i, kk:kk + 1], axis=0), reads=[dest_i], writes=[yk[kk]])
                k.dma("sp", x1t[:], x1_ap, writes=[x1t])
                k.op("act", lambda e: e.activation(acc[:], yk[0][:], AF.Copy, scale=gk[:, i, 0:1]), reads=[yk[0], gk], writes=[acc])
                for kk in range(1, 4):
                    k.op("dve", lambda e: e.scalar_tensor_tensor(acc[:], yk[kk][:], gk[:, i, kk:kk + 1], acc[:], op0=ALU.mult, op1=ALU.add), reads=[yk[kk], gk, acc], writes=[acc])
                k.op("pool", lambda e: e.tensor_tensor(acc[:], acc[:], gf[:], ALU.mult), reads=[acc, gf], writes=[acc])
                k.op("dve", lambda e: e.scalar_tensor_tensor(uu[:], x1t[:], DN_ALPHA, acc[:], op0=ALU.mult, op1=ALU.add), reads=[x1t, acc], writes=[uu])
                ln_stats(k, uu, st6, mv, rstd, 1e-5)
                k.op("dve", lambda e: e.scalar_tensor_tensor(acc[:], uu[:], mv[:, 0:1], ln2g[:], op0=ALU.subtract, op1=ALU.mult), reads=[uu, mv, ln2g], writes=[acc])
                k.op("dve", lambda e: e.scalar_tensor_tensor(o_[:], acc[:], rstd[:], ln2b[:], op0=ALU.mult, op1=ALU.add), reads=[acc, rstd, ln2b], writes=[o_])
                k.dma("sp", out_ap, o_[:], reads=[o_])
            k.barrier()
    k.barrier()


WSHAPES = {"ada_w": [2, D, 6 * D], "ada_b": [2, 6 * D], "w_in": [2, D, IN_COLS],
           "rw_mu": [2, 2, 1152], "rw_w0": [2, 2, 256], "rw_w2": [2, 2, 64, 256], "rw_a0": [2, 2, 256], "rw_a2": [2, 2, 64, 256], "rw_g2": [2, 128, 256],
           "rw_kk": [2, 256], "rw_ka": [2, 256], "rw_rk": [2, 4, 64], "rw_gn_w": [2, 256], "rw_gn_b": [2, 256],
           "mla_q_norm": [2, 256], "mla_kv_norm": [2, 128], "mla_w_uq": [2, 256, 768], "mla_w_ukv": [2, 128, 1024],
           "w_out": [2, D, D], "ln1_g": [2, D], "ln1_b": [2, D], "router_w": [2, D, NE], "router_b": [2, NE],
           "w_gu": [2, NE, D, 2 * D], "b_gu": [2, NE, 2 * D], "w_dn": [2, NE, D, D], "b_dn": [2, NE, D], "ln2_g": [2, D], "ln2_b": [2, D]}


def build_nc(NB, debug=False, layers=(0, 1), moe=True, sparse=True, phases="BCDEF"):
    nc = bass.Bass("TRN2", target_bir_lowering=False)
    with ExitStack() as st:
        k = K(nc, st)
        kin = "ExternalInput"; sk = "ExternalOutput" if debug else "Internal"
        CD = {n: k.dram("c_" + n, sh, F32, kin) for n, sh in CONST_SHAPES.items()}
        W = {n: k.dram(n, sh, F32, kin) for n, sh in WSHAPES.items()}
        xin = k.dram("xin", [NB, T, D], F32, kin)
        condT = k.dram("condT", [128, 8, NB + 1], F32, kin)
        na_bias = k.dram("na_bias", [2, 128, 4, 8, 4, 64], F32, kin)
        out_d = k.dram("out", [NB, SEQ, D], F32, "ExternalOutput")
        mod_d = k.dram("mod_d", [2, NB + 1, 6 * D], F32, sk)
        pT_rw_d = k.dram("pT_rw_d", [RW_COLS, T], F32, sk); pT_na_d = k.dram("pT_na_d", [512, T], BF16, sk); p_tm_d = k.dram("p_tm_d", [T, 704], F32, sk)
        yrw_d = k.dram("yrw_d", [64, 4, T], BF16, sk); ymla_d = k.dram("ymla_d", [4, 128, T], BF16, sk); yna_d = k.dram("yna_d", [64, 4, T], BF16, sk)
        x1_d = k.dram("x1_d", [NB * T, D], F32, sk); x2_d = k.dram("x2_d", [NB, T, D], F32, sk)
        hT_d = k.dram("hT_d", [D, NB * T], BF16, sk); GT_d = k.dram("GT_d", [NE, NB * T], F32, sk)
        hTM_d = k.dram("hTM_d", [NB * T, D], BF16, sk); Gtm_d = k.dram("Gtm_d", [NB * T, NE], F32, sk)
        nblk0 = (4 * NB * T + NE * 511) // 512
        Hs_d = k.dram("Hs_d", [nblk0 * 512, D], BF16, "Internal"); Ys_d = k.dram("Ys_d", [nblk0 * 512, D], F32, "Internal")
        C = load_consts(k, st, CD)
        k.barrier()
        for l in layers:
            need_ctx = l < DEPTH - 1
            phase_mod(k, l, NB, condT, W["ada_w"], W["ada_b"], mod_d)
            for b in range(NB):
                if l == 0:
                    x_src = (lambda tt, b=b: xin.t[b][tt * 128:(tt + 1) * 128, :]); x_tk = xin
                else:
                    x_src = (lambda tt, b=b: x2_d.t[b][tt * 128:(tt + 1) * 128, :]); x_tk = x2_d
                with ExitStack() as ps:
                    w_in_bf = k.sb("winbf", [128, 8, IN_COLS], BF16, ps)
                    load_w_in(k, l, W["w_in"], w_in_bf)
                    phase_inproj(k, l, b, NB, x_src, x_tk, mod_d, w_in_bf, C["ident_bf"], pT_rw_d, pT_na_d, p_tm_d)
                with ExitStack() as ps:
                    if "C" in phases:
                        RW = load_rw_w(k, l, ps, W, C["ident_f"])
                        phase_rwkv(k, pT_rw_d, RW, C, yrw_d)
                with ExitStack() as ps:
                    if "D" in phases:
                        wuq, wukv = load_mla_w(k, l, ps, W["mla_w_uq"], W["mla_w_ukv"], W["mla_q_norm"], W["mla_kv_norm"])
                        phase_mla(k, need_ctx, p_tm_d, wuq, wukv, C["cos_d"], C["sin_d"], C["ident_bf"], C["ones_bf"], ymla_d)
                with ExitStack() as ps:
                    if "E" in phases:
                        bm = load_na_bias(k, l, ps, na_bias, CD["na_mask"])
                        phase_na(k, need_ctx, pT_na_d, p_tm_d, bm, C["ones_bf"], yna_d)
                x1_b = Tk(x1_d.t[b * T:(b + 1) * T, :], "x1b")
                if "F" not in phases:
                    continue
                if sparse:
                    phase_post(k, l, b, NB, need_ctx, x_src, x_tk, mod_d, yrw_d, ymla_d, yna_d, W, C, x1_b, hT_d, GT_d, b * T, hTM_d, Gtm_d)
                else:
                    phase_post(k, l, b, NB, need_ctx, x_src, x_tk, mod_d, yrw_d, ymla_d, yna_d, W, C, x1_b, hT_d, GT_d, b * T)
            groups = []
            for b in range(NB):
                if need_ctx:
                    groups.append((b * T, 256, "c"))
                for i in range(4):
                    groups.append((b * T + 256 + i * 512, 512, b))
            groups.sort(key=lambda g: -g[1])

            def tile_io(col, l=l):
                b = col // T; t = col % T
                x1_ap = x1_d.t[col:col + 128, :]
                if l == DEPTH - 1:
                    return x1_ap, x1_d, out_d.t[b][t - NCTX:t - NCTX + 128, :], out_d
                return x1_ap, x1_d, x2_d.t[b][t:t + 128, :], x2_d
            if moe and sparse:
                phase_moe_sparse(k, l, NB, need_ctx, tile_io, mod_d, W, C, CD, hTM_d, Gtm_d, Hs_d, Ys_d)
            elif moe:
                phase_moe(k, l, NB, groups, tile_io, mod_d, W, C, hT_d, GT_d)
        k.barrier()
        print("ninst", k.ninst, k.cnt, flush=True)
    return nc


def core_inputs(inputs, b0, NB, shared):
    cc = np.concatenate([inputs["c"][b0:b0 + NB], inputs["c_ctx"][None]], 0).astype(np.float32)
    im = dict(shared)
    im["condT"] = np.ascontiguousarray(cc.T.reshape(8, 128, NB + 1).transpose(1, 0, 2))
    im["xin"] = np.ascontiguousarray(np.concatenate([inputs["ctx"][b0:b0 + NB], inputs["x"][b0:b0 + NB]], 1))
    return im


def shared_inputs(inputs):
    sh = {n: np.ascontiguousarray(np.asarray(inputs[n], np.float32)) for n in WSHAPES}
    for n, v in const_arrays().items():
        sh["c_" + n] = np.ascontiguousarray(v.astype(np.float32))
    sh["na_bias"] = na_bias_host(np.asarray(inputs["na_rpb"], np.float32))
    return sh


def kernel(**inputs):
    NCORES = 8; B = inputs["x"].shape[0]; NB = B // NCORES
    nc = build_nc(NB)
    sh = shared_inputs(inputs)
    in_maps = [core_inputs(inputs, c * NB, NB, sh) for c in range(NCORES)]
    res = run_bass_kernel_spmd(nc, in_maps, core_ids=list(range(NCORES)))
    return np.concatenate([r["out"] for r in res.results], 0).astype(np.float32)
```
